# Optimizing a Trainium2 kernel written in Bass

```python
import jax, jax.numpy as jnp
from jax import lax
import numpy as np

D_MODEL = 2048
BATCH = 4
SEQ = 2048
DEPTH = 2

A_WIDTH = D_MODEL // 2
A_GROUPS = 8
A_GROUP_DIM = A_WIDTH // A_GROUPS
A_CHUNK = 128
B_WIDTH = D_MODEL // 2
B_HEAD_DIM = 64
B_HEADS = B_WIDTH // B_HEAD_DIM
B_DECAY_LORA = 64
B_AAA_LORA = 64
B_GATE_LORA = 160
B_GN_EPS = 64e-5
B_PROJ = 3 * B_WIDTH + B_DECAY_LORA + B_AAA_LORA + B_GATE_LORA
AB_PROJ = 2 * A_WIDTH + B_PROJ
C_HEADS = 16
C_HEAD_DIM = D_MODEL // C_HEADS
MOBA_BLOCK = 256
MOBA_TOPK = 3
MOBA_QCHUNK = 16
FFN_HIDDEN = -(-8 * D_MODEL // (3 * 256)) * 256
N_EVEN = (DEPTH + 1) // 2
N_ODD = DEPTH // 2
NORM_EPS = 1e-6

kernel_name = "hybrid_gmlp_rwkv7_moba_trunk"


def rmsnorm(x, gain):
    xf = x.astype(jnp.float32)
    xf = xf * lax.rsqrt(jnp.mean(xf * xf, axis=-1, keepdims=True) + NORM_EPS)
    return (xf * gain.astype(jnp.float32)).astype(x.dtype)


def layernorm(x, gain, bias, eps=1e-5):
    xf = x.astype(jnp.float32)
    mu = jnp.mean(xf, axis=-1, keepdims=True)
    var = jnp.mean(jnp.square(xf - mu), axis=-1, keepdims=True)
    y = (xf - mu) * lax.rsqrt(var + eps)
    return (y * gain.astype(jnp.float32) + bias.astype(jnp.float32)).astype(x.dtype)


def token_shift(z, mu):
    z_prev = jnp.pad(z, ((0, 0), (1, 0), (0, 0)))[:, :-1]
    return z + mu * (z_prev - z)


def chunked_spatial_gating(z, v_gain, v_bias, w_s, b_s):
    bsz, s, _ = z.shape
    z = jax.nn.gelu(z)
    u, v = jnp.split(z, 2, axis=-1)
    v = layernorm(v, v_gain, v_bias)
    v = v.reshape(bsz, s // A_CHUNK, A_CHUNK, A_GROUPS, A_GROUP_DIM)
    causal = jnp.tril(jnp.ones((A_CHUNK, A_CHUNK), dtype=bool))
    w = jnp.where(causal[None], w_s, 0.0)
    sv = jnp.einsum('gij,bcjgd->bcigd', w, v) + b_s.T[None, None, :, :, None]
    return u * sv.reshape(bsz, s, A_WIDTH)


def rwkv7_time_mix(z, mu, w0, w2, a0, a2, g2, k_k, k_a, r_k, lnx_gain, lnx_bias):
    bsz, s, _ = z.shape
    f32 = jnp.float32
    z = token_shift(z, mu)
    cuts = [B_WIDTH, 2 * B_WIDTH, 3 * B_WIDTH, 3 * B_WIDTH + B_DECAY_LORA,
            3 * B_WIDTH + B_DECAY_LORA + B_AAA_LORA]
    r, k, v, zw, za, zg = jnp.split(z, cuts, axis=-1)
    w_log = -jax.nn.softplus(-(w0 + jnp.tanh(zw) @ w2)) - 0.5
    decay = jnp.exp(-jnp.exp(w_log.astype(f32)))
    a = jax.nn.sigmoid(a0 + za @ a2)
    g = jax.nn.sigmoid(zg) @ g2

    def heads(t):
        return t.reshape(bsz, s, B_HEADS, B_HEAD_DIM)

    kk = heads(k * k_k).astype(f32)
    kk = kk / jnp.maximum(jnp.sqrt(jnp.sum(kk * kk, axis=-1, keepdims=True)), 1e-12)
    k = k * (1 + (a - 1) * k_a)
    r_h, k_h, v_h, a_h = heads(r), heads(k), heads(v), heads(a)
    w_h = heads(decay)
    vec_a = -kk
    vec_b = kk * a_h.astype(f32)

    def step(state, inp):
        r_t, w_t, k_t, v_t, a_t, b_t = inp
        sa = jnp.einsum('bhij,bhj->bhi', state, a_t)
        state = (state * w_t[:, :, None, :] + sa[..., None] * b_t[:, :, None, :]
                 + v_t[..., None] * k_t[:, :, None, :])
        y_t = jnp.einsum('bhij,bhj->bhi', state, r_t)
        return state, y_t

    xs = tuple(jnp.moveaxis(t.astype(f32), 1, 0) for t in (r_h, w_h, k_h, v_h, vec_a, vec_b))
    state0 = jnp.zeros((bsz, B_HEADS, B_HEAD_DIM, B_HEAD_DIM), f32)
    _, y = lax.scan(step, state0, xs)
    y = jnp.moveaxis(y, 0, 1)
    mean = jnp.mean(y, axis=-1, keepdims=True)
    var = jnp.mean(jnp.square(y - mean), axis=-1, keepdims=True)
    y = ((y - mean) * lax.rsqrt(var + B_GN_EPS)).reshape(bsz, s, B_WIDTH)
    y = y * lnx_gain.astype(f32) + lnx_bias.astype(f32)
    bonus = jnp.sum((r_h * k_h * r_k).astype(f32), axis=-1, keepdims=True) * v_h.astype(f32)
    y = (y + bonus.reshape(bsz, s, B_WIDTH)) * g.astype(f32)
    return y.astype(z.dtype)


def alibi_slopes(n_heads):
    return 2.0 ** (-8.0 * jnp.arange(1, n_heads + 1, dtype=jnp.float32) / n_heads)


def moba_attention(q, k, v):
    bsz, nh, s, dh = q.shape
    f32 = jnp.float32
    s_pad = -(-s // MOBA_BLOCK) * MOBA_BLOCK
    pad = ((0, 0), (0, 0), (0, s_pad - s), (0, 0))
    qf, kf, vf = (jnp.pad(t.astype(f32), pad) for t in (q, k, v))
    nb = s_pad // MOBA_BLOCK
    topk = min(MOBA_TOPK, nb)
    kb = kf.reshape(bsz, nh, nb, MOBA_BLOCK, dh)
    vb = vf.reshape(bsz, nh, nb, MOBA_BLOCK, dh)
    k_mean = jnp.mean(kb, axis=3)
    q_block = jnp.arange(s_pad) // MOBA_BLOCK
    gate = jnp.einsum('bhtd,bhnd->bhtn', qf, k_mean)
    fully_past = jnp.arange(nb)[None, :] < q_block[:, None]
    gate = jnp.where(fully_past, gate, -jnp.inf)
    _, sel = lax.top_k(gate, topk)
    sel_valid = jnp.arange(topk)[None, :] < q_block[:, None]

    slopes = alibi_slopes(nh)[None, :, None, None]
    scale = dh ** -0.5
    b_ix = jnp.arange(bsz)[:, None, None, None]
    h_ix = jnp.arange(nh)[None, :, None, None]
    blk_off = jnp.arange(MOBA_BLOCK)

    def chunk(ci):
        t0 = ci * MOBA_QCHUNK
        q_c = lax.dynamic_slice_in_dim(qf, t0, MOBA_QCHUNK, axis=2)
        t_c = t0 + jnp.arange(MOBA_QCHUNK)
        own0 = (t0 // MOBA_BLOCK) * MOBA_BLOCK
        k_own = lax.dynamic_slice_in_dim(kf, own0, MOBA_BLOCK, axis=2)
        v_own = lax.dynamic_slice_in_dim(vf, own0, MOBA_BLOCK, axis=2)
        dist_own = (t_c[:, None] - (own0 + blk_off)[None, :]).astype(f32)
        s_own = jnp.einsum('bhqd,bhsd->bhqs', q_c, k_own) * scale - slopes * jnp.abs(dist_own)
        s_own = jnp.where(dist_own >= 0, s_own, -jnp.inf)
        sel_c = lax.dynamic_slice_in_dim(sel, t0, MOBA_QCHUNK, axis=2)
        val_c = lax.dynamic_slice_in_dim(sel_valid, t0, MOBA_QCHUNK, axis=0)
        k_sel = kb[b_ix, h_ix, sel_c]
        v_sel = vb[b_ix, h_ix, sel_c]
        sel_pos = sel_c[..., None] * MOBA_BLOCK + blk_off
        dist_sel = (t_c[None, None, :, None, None] - sel_pos).astype(f32)
        s_sel = (jnp.einsum('bhqd,bhqksd->bhqks', q_c, k_sel) * scale
                 - slopes[..., None] * jnp.abs(dist_sel))
        s_sel = jnp.where(val_c[None, None, :, :, None], s_sel, -jnp.inf)
        s_all = jnp.concatenate(
            [s_own, s_sel.reshape(bsz, nh, MOBA_QCHUNK, topk * MOBA_BLOCK)], axis=-1)
        p = jax.nn.softmax(s_all, axis=-1)
        p_own = p[..., :MOBA_BLOCK]
        p_sel = p[..., MOBA_BLOCK:].reshape(bsz, nh, MOBA_QCHUNK, topk, MOBA_BLOCK)
        return (jnp.einsum('bhqs,bhsd->bhqd', p_own, v_own)
                + jnp.einsum('bhqks,bhqksd->bhqd', p_sel, v_sel))

    out = lax.map(chunk, jnp.arange(s_pad // MOBA_QCHUNK))
    out = jnp.moveaxis(out, 0, 2).reshape(bsz, nh, s_pad, dh)[:, :, :s]
    return out.astype(q.dtype)


def setup_inputs(seed: int = 0) -> dict:
    key = jax.random.key(seed)
    ks = jax.random.split(key, 32)
    f32 = jnp.float32
    D, F = D_MODEL, FFN_HIDDEN

    def nrm(k, shape, std):
        return jax.random.normal(k, shape, f32) * std

    def gain(k, shape):
        return 1.0 + 0.05 * jax.random.normal(k, shape, f32)

    return {
        "x": nrm(ks[0], (BATCH, SEQ, D), 1.0),
        "c": nrm(ks[1], (BATCH, D), 1.0),
        "w_ada": nrm(ks[2], (DEPTH, D, 6 * D), 0.5 * D ** -0.5),
        "b_ada": nrm(ks[3], (DEPTH, 6 * D), 0.01),
        "g_pre_mix": gain(ks[4], (DEPTH, D)),
        "g_post_mix": gain(ks[5], (DEPTH, D)),
        "g_pre_ffn": gain(ks[6], (DEPTH, D)),
        "g_post_ffn": gain(ks[7], (DEPTH, D)),
        "w_ffn_in": nrm(ks[8], (DEPTH, D, 2 * F), D ** -0.5),
        "w_ffn_out": nrm(ks[9], (DEPTH, F, D), F ** -0.5),
        "w_in_ab": nrm(ks[10], (N_EVEN, D, AB_PROJ), D ** -0.5),
        "w_out_ab": nrm(ks[11], (N_EVEN, A_WIDTH + B_WIDTH, D), (A_WIDTH + B_WIDTH) ** -0.5),
        "a_v_gain": gain(ks[12], (N_EVEN, A_WIDTH)),
        "a_v_bias": nrm(ks[13], (N_EVEN, A_WIDTH), 0.02),
        "a_w_s": nrm(ks[14], (N_EVEN, A_GROUPS, A_CHUNK, A_CHUNK), A_CHUNK ** -0.5),
        "a_b_s": gain(ks[15], (N_EVEN, A_GROUPS, A_CHUNK)),
        "b_mu": jax.random.uniform(ks[16], (N_EVEN, B_PROJ), f32),
        "b_w0": jax.random.uniform(ks[17], (N_EVEN, B_WIDTH), f32, -5.0, 0.5),
        "b_w2": nrm(ks[18], (N_EVEN, B_DECAY_LORA, B_WIDTH), 0.5 * B_DECAY_LORA ** -0.5),
        "b_a0": nrm(ks[19], (N_EVEN, B_WIDTH), 0.1),
        "b_a2": nrm(ks[20], (N_EVEN, B_AAA_LORA, B_WIDTH), 0.5 * B_AAA_LORA ** -0.5),
        "b_g2": nrm(ks[21], (N_EVEN, B_GATE_LORA, B_WIDTH), B_GATE_LORA ** -0.5),
        "b_k_k": 0.85 + 0.05 * jax.random.normal(ks[22], (N_EVEN, B_WIDTH), f32),
        "b_k_a": gain(ks[23], (N_EVEN, B_WIDTH)),
        "b_r_k": nrm(ks[24], (N_EVEN, B_HEADS, B_HEAD_DIM), 0.1),
        "b_lnx_gain": gain(ks[25], (N_EVEN, B_WIDTH)),
        "b_lnx_bias": nrm(ks[26], (N_EVEN, B_WIDTH), 0.02),
        "w_qkv": nrm(ks[27], (N_ODD, D, 3 * D), D ** -0.5),
        "w_o": nrm(ks[28], (N_ODD, D, D), D ** -0.5),
    }


def reference(x, c, w_ada, b_ada, g_pre_mix, g_post_mix, g_pre_ffn, g_post_ffn,
              w_ffn_in, w_ffn_out, w_in_ab, w_out_ab, a_v_gain, a_v_bias, a_w_s, a_b_s,
              b_mu, b_w0, b_w2, b_a0, b_a2, b_g2, b_k_k, b_k_a, b_r_k, b_lnx_gain,
              b_lnx_bias, w_qkv, w_o):
    bsz, s, d = x.shape
    cond = jax.nn.silu(c)
    for layer in range(DEPTH):
        mod = cond @ w_ada[layer] + b_ada[layer]
        sh_m, sc_m, gt_m, sh_f, sc_f, gt_f = [m[:, None, :] for m in jnp.split(mod, 6, axis=-1)]
        i = layer // 2
        h = rmsnorm(x, g_pre_mix[layer]) * (1 + sc_m) + sh_m
        if layer % 2 == 0:
            z = h @ w_in_ab[i]
            y_a = chunked_spatial_gating(z[..., :2 * A_WIDTH], a_v_gain[i], a_v_bias[i],
                                         a_w_s[i], a_b_s[i])
            y_b = rwkv7_time_mix(z[..., 2 * A_WIDTH:], b_mu[i], b_w0[i], b_w2[i], b_a0[i],
                                 b_a2[i], b_g2[i], b_k_k[i], b_k_a[i], b_r_k[i],
                                 b_lnx_gain[i], b_lnx_bias[i])
            y = jnp.concatenate([y_a, y_b], axis=-1) @ w_out_ab[i]
        else:
            q, k, v = jnp.split(h @ w_qkv[i], 3, axis=-1)
            q, k, v = (t.reshape(bsz, s, C_HEADS, C_HEAD_DIM).transpose(0, 2, 1, 3)
                       for t in (q, k, v))
            o = moba_attention(q, k, v)
            y = o.transpose(0, 2, 1, 3).reshape(bsz, s, d) @ w_o[i]
        x = x + gt_m * rmsnorm(y, g_post_mix[layer])
        h = rmsnorm(x, g_pre_ffn[layer]) * (1 + sc_f) + sh_f
        gate, up = jnp.split(h @ w_ffn_in[layer], 2, axis=-1)
        y = (jax.nn.silu(gate) * up) @ w_ffn_out[layer]
        x = x + gt_f * rmsnorm(y, g_post_ffn[layer])
    return x
```

```python
import numpy as np
import concourse.bass as bass
import concourse.mybir as mybir

F32 = mybir.dt.float32
BF16 = mybir.dt.bfloat16
AF = mybir.ActivationFunctionType
ALU = mybir.AluOpType
AX = mybir.AxisListType


class Buf:
    __slots__ = ("name", "lw", "rd", "dsem", "demit")

    def __init__(self, name):
        self.name = name
        self.lw = None
        self.rd = []
        self.dsem = {}
        self.demit = {}


class Op:
    __slots__ = ("eng", "emit", "deps", "dbuf", "needed", "sig", "inc", "barrier")

    def __init__(self, eng, emit, deps, dbuf, inc=16):
        self.eng = eng
        self.emit = emit
        self.deps = deps
        self.dbuf = dbuf
        self.needed = False
        self.sig = 0
        self.inc = inc
        self.barrier = False


class Sch:
    def __init__(self, nc, same_engine_sync=True):
        self.nc = nc
        self.E = {"pe": nc.tensor, "act": nc.scalar, "dve": nc.vector, "pool": nc.gpsimd, "sp": nc.sync}
        self.ops = []
        self.same = same_engine_sync
        self.nbuf = 0

    def buf(self, name=None):
        self.nbuf += 1
        return Buf(name or "b%d" % self.nbuf)

    def barrier(self):
        op = Op("sp", None, [], None)
        op.barrier = True
        self.ops.append(op)

    def add(self, eng, emit, reads=(), writes=(), dma=None, inc=16):
        deps = []
        for b in reads:
            if b.lw is not None:
                deps.append(b.lw)
        for b in writes:
            if b.lw is not None:
                deps.append(b.lw)
            deps.extend(b.rd)
        op = Op(eng, emit, deps, dma, inc)
        for d in deps:
            if d.dbuf is None and d.eng == eng and (eng == "pe" or not self.same):
                continue
            d.needed = True
        for b in reads:
            b.rd.append(op)
        for b in writes:
            b.lw = op
            b.rd = []
        self.ops.append(op)
        return op

    def finish(self):
        nc = self.nc
        esem = {k: nc.alloc_semaphore("es_" + k) for k in self.E}
        cnt = {k: 0 for k in self.E}
        last = {}
        for op in self.ops:
            if op.barrier:
                for o in last.values():
                    o.needed = True
            elif op.dbuf is None:
                last[op.eng] = op
        for op in self.ops:
            if op.barrier:
                continue
            if op.dbuf is None and op.needed:
                cnt[op.eng] += 1
                op.sig = cnt[op.eng]
        waited = {k: {} for k in self.E}
        dbufs = []
        nsem = len(esem)
        lastsig = {k: 0 for k in self.E}
        for op in self.ops:
            if op.barrier:
                for en, e in self.E.items():
                    w = waited[en]
                    for x in self.E:
                        if x == en or lastsig[x] == 0:
                            continue
                        key = ("e", x)
                        if w.get(key, 0) < lastsig[x]:
                            e.wait_ge(esem[x], lastsig[x]); w[key] = lastsig[x]
                    for b, q in dbufs:
                        key = ("d", id(b), q)
                        if w.get(key, 0) < b.demit[q]:
                            e.wait_ge(b.dsem[q], b.demit[q]); w[key] = b.demit[q]
                continue
            eng = self.E[op.eng]
            need = {}
            for d in op.deps:
                if d.dbuf is not None:
                    key = ("d", id(d.dbuf), d.eng)
                    sem = d.dbuf.dsem[d.eng]
                    val = d.dbuf.demit[d.eng]
                else:
                    if d.eng == op.eng and (d.eng == "pe" or not self.same):
                        continue
                    key = ("e", d.eng)
                    sem = esem[d.eng]
                    val = d.sig
                if key not in need or need[key][1] < val:
                    need[key] = (sem, val)
            w = waited[op.eng]
            for key, (sem, val) in need.items():
                if w.get(key, 0) >= val:
                    continue
                eng.wait_ge(sem, val)
                w[key] = val
            inst = op.emit()
            if op.dbuf is not None:
                b = op.dbuf
                if op.eng not in b.dsem:
                    b.dsem[op.eng] = nc.alloc_semaphore("ds_%d" % nsem)
                    b.demit[op.eng] = 0
                    nsem += 1
                    dbufs.append((b, op.eng))
                inst.then_inc(b.dsem[op.eng], op.inc)
                b.demit[op.eng] += op.inc
            elif op.needed:
                inst.then_inc(esem[op.eng], 1)
                lastsig[op.eng] = op.sig
        for b, q in dbufs:
            nc.sync.wait_ge(b.dsem[q], b.demit[q])
        self.nsem = nsem
        return nsem


class KB:
    def __init__(self, name="k"):
        self.nc = bass.Bass("TRN2", target_bir_lowering=False)
        self.s = Sch(self.nc)
        self.nps = 0
        self.ps_banks = None
        self.prefix = ""
        self.alias = {}
        self.cms = []
        self.fused = False

    def dram(self, name, shape, dt=F32, kind="ExternalInput"):
        name = self.prefix + name
        if name in self.alias:
            return self.alias[name]
        return self.nc.dram_tensor(name, list(shape), dt, kind=kind).ap()

    def scratch(self, name, shape, dt=F32, shared=False):
        if shared:
            return self.nc.dram_tensor(name, list(shape), dt, addr_space="Shared").ap()
        return self.nc.dram_tensor(name, list(shape), dt).ap()

    def sb(self, name, shape, dt=F32):
        if not self.fused:
            return self.nc.alloc_sbuf_tensor(self.prefix + name, list(shape), dt)
        cm = self.nc.sbuf_tensor(self.prefix + name, list(shape), dt)
        t = cm.__enter__()
        self.cms.append(cm)
        return t

    def end_phase(self):
        self.s.barrier()
        for cm in reversed(self.cms):
            cm.__exit__(None, None, None)
        self.cms = []

    def finish_build(self):
        if not self.fused:
            self.s.finish()

    def allgather(self, out_ap, in_ap, groups, R, W, cbuf):
        nc = self.nc
        return self.s.add("pool", lambda: nc.gpsimd.collective_compute("AllGather", mybir.AluOpType.bypass, replica_groups=groups, ins=[in_ap], outs=[out_ap]), R, W, dma=cbuf, inc=1)

    def psum_banks(self):
        if self.ps_banks is not None:
            return
        self.ps_banks = []
        for i in range(8):
            t = self.nc.alloc_psum_tensor("psb%d" % i, [128, 512], F32)
            self.ps_banks.append((t, self.s.buf("ps%d" % i)))
        self.ps_i = 0

    def bank(self):
        n = len(self.ps_banks)
        b = self.ps_banks[self.ps_i % n]
        self.ps_i += 1
        return b

    def reserve(self):
        return self.ps_banks.pop()

    def unreserve(self, b):
        self.ps_banks.append(b)

    def mm(self, out, lhsT, rhs, start, stop, R, W):
        nc = self.nc
        return self.s.add("pe", lambda: nc.tensor.matmul(out, lhsT, rhs, start=start, stop=stop), R, W)

    def act(self, out, in_, func, R, W, bias=None, scale=None, accum=None):
        nc = self.nc
        kw = {}
        if bias is not None:
            kw["bias"] = bias
        if scale is not None:
            kw["scale"] = scale
        if accum is not None:
            kw["accum_out"] = accum
        return self.s.add("act", lambda: nc.scalar.activation(out=out, in_=in_, func=func, **kw), R, W)

    def _e(self, eng):
        return {"dve": self.nc.vector, "pool": self.nc.gpsimd}[eng]

    def tt(self, eng, out, in0, in1, op, R, W):
        e = self._e(eng)
        return self.s.add(eng, lambda: e.tensor_tensor(out=out, in0=in0, in1=in1, op=op), R, W)

    def ts(self, eng, out, in0, s1, s2, op0, op1, R, W, accum=None):
        e = self._e(eng)
        if op1 is None:
            return self.s.add(eng, lambda: e.tensor_scalar(out=out, in0=in0, scalar1=s1, scalar2=None, op0=op0), R, W)
        if accum is not None:
            return self.s.add(eng, lambda: e.tensor_scalar(out=out, in0=in0, scalar1=s1, scalar2=s2, op0=op0, op1=op1, accum_out=accum), R, W)
        return self.s.add(eng, lambda: e.tensor_scalar(out=out, in0=in0, scalar1=s1, scalar2=s2, op0=op0, op1=op1), R, W)

    def stt(self, eng, out, in0, scalar, in1, op0, op1, R, W):
        e = self._e(eng)
        return self.s.add(eng, lambda: e.scalar_tensor_tensor(out=out, in0=in0, scalar=scalar, in1=in1, op0=op0, op1=op1), R, W)

    def cp(self, eng, out, in_, R, W):
        if eng == "act":
            nc = self.nc
            return self.s.add("act", lambda: nc.scalar.copy(out=out, in_=in_), R, W)
        e = self._e(eng)
        return self.s.add(eng, lambda: e.tensor_copy(out=out, in_=in_), R, W)

    def red(self, eng, out, in_, op, axis, R, W):
        e = self._e(eng)
        return self.s.add(eng, lambda: e.tensor_reduce(out=out, in_=in_, axis=axis, op=op), R, W)

    def memset(self, eng, ap, val, W):
        e = self._e(eng)
        return self.s.add(eng, lambda: e.memset(ap, val), (), W)

    def dma(self, q, out, in_, R, W, dbuf):
        e = {"sp": self.nc.sync, "pool": self.nc.gpsimd, "act": self.nc.scalar}[q]
        return self.s.add(q, lambda: e.dma_start(out=out, in_=in_), R, W, dma=dbuf)


def _kb_recip(self, eng, out, in_, R, W):
    e = self._e(eng)
    return self.s.add(eng, lambda: e.reciprocal(out=out, in_=in_), R, W)


KB.recip = _kb_recip


def _kb_max8(self, out, in_, R, W):
    nc = self.nc
    return self.s.add("dve", lambda: nc.vector.max(out=out, in_=in_), R, W)


KB.max8 = _kb_max8


EPS = 1e-6


def row_to_col(k, row_t, row_b, ncols, ps_ap, ps_b, one_f, b_const):
    for i in range(ncols):
        k.mm(ps_ap[:, i:i + 1], row_t[0:1, i * 128:(i + 1) * 128], one_f[0:1, 0:1], True, True, [row_b, b_const], [ps_b])


class Pre:
    def __init__(self, k, D, wada2, bada2, cvec, gpre, ident, nwsl=3):
        self.k = k; s = k.s; B = s.buf
        self.D = D; DC = D // 128; self.DC = DC
        self.identf = k.sb("identf", [128, 128]); self.identb = k.sb("identb", [128, 128], BF16)
        self.ones_f = k.sb("ones_f", [1, 128]); self.ones_b = k.sb("ones_b", [1, 128], BF16)
        self.rowf = [k.sb("rowf%d" % i, [1, 512]) for i in range(2)]
        self.rowb = [k.sb("rowb%d" % i, [1, 512], BF16) for i in range(2)]
        self.condT_f = k.sb("condT_f", [128, DC]); self.condT_b = k.sb("condT_b", [128, DC], BF16)
        self.modc = k.sb("modc", [128, 2 * DC]); self.gpre_c = k.sb("gpre_c", [128, DC]); self.gs_c = k.sb("gs_c", [128, DC])
        self.wsl = [k.sb("wsl%d" % i, [128, DC, 512], BF16) for i in range(nwsl)]
        self.xt = k.sb("xt", [128, D]); self.xn = k.sb("xn", [128, D], BF16); self.st = k.sb("st", [128, 8])
        self.b_const = B(); self.b_rowf = [B(), B()]; self.b_rowb = [B(), B()]; self.b_cond = B(); self.b_modc = B()
        self.b_gpre = B(); self.b_gs = B(); self.b_wsl = [B() for _ in range(nwsl)]; self.b_xt = B(); self.b_xn = B(); self.b_st = B()
        self.nw = 0; self.nwsl = nwsl
        k.dma("sp", self.identf[:], ident[:, :], [], [self.b_const], self.b_const)
        k.cp("dve", self.identb[:], self.identf[:], [self.b_const], [self.b_const])
        k.memset("dve", self.ones_f[:], 1.0, [self.b_const]); k.memset("dve", self.ones_b[:], 1.0, [self.b_const])
        pt, pb = k.bank()
        self.col_from_dram(cvec, D, pt, pb)
        k.act(self.condT_f[:], pt[:, 0:DC], AF.Silu, [pb], [self.b_cond])
        k.cp("dve", self.condT_b[:], self.condT_f[:], [self.b_cond], [self.b_cond])
        pt, pb = k.bank()
        self.col_from_dram(gpre, D, pt, pb)
        k.cp("act", self.gpre_c[:], pt[:, 0:DC], [pb], [self.b_gpre])
        pc, pcb = k.bank()
        for j in range(2 * D // 512):
            w, wb = self.next_wsl()
            self.load_w(w, wb, wada2[:, j * 512:(j + 1) * 512], 512)
            r = j % 2
            k.dma("pool", self.rowb[r][0:1, :], bada2[0:1, j * 512:(j + 1) * 512], [], [self.b_rowb[r]], self.b_rowb[r])
            for q in range(4):
                col = j * 4 + q
                for kc in range(DC):
                    k.mm(pc[:, col:col + 1], w[:, kc, q * 128:(q + 1) * 128], self.condT_b[:, kc:kc + 1], kc == 0, False, [self.b_cond, wb], [pcb])
                k.mm(pc[:, col:col + 1], self.rowb[r][0:1, q * 128:(q + 1) * 128], self.ones_b[0:1, 0:1], False, True, [self.b_rowb[r], self.b_const], [pcb])
        k.cp("act", self.modc[:], pc[:, 0:2 * DC], [pcb], [self.b_modc])
        k.stt("dve", self.gs_c[:], self.modc[:, DC:2 * DC], 1.0, self.gpre_c[:], ALU.add, ALU.mult, [self.b_modc, self.b_gpre], [self.b_gs])

    def next_wsl(self):
        i = self.nw % self.nwsl; self.nw += 1
        return self.wsl[i], self.b_wsl[i]

    def load_w(self, dst, dbuf, src_cols, width, off=0):
        self.k.dma("pool", dst[:, :, off:off + width], src_cols.rearrange("(kc p) n -> p kc n", p=128), [], [dbuf], dbuf)

    def col_from_dram(self, vec, n, pt, pb, col0=0):
        k = self.k
        done = 0
        j = 0
        while done < n:
            w = min(512, n - done)
            r = j % 2
            k.dma("sp", self.rowf[r][0:1, 0:w], vec[0:1, done:done + w], [], [self.b_rowf[r]], self.b_rowf[r])
            row_to_col(k, self.rowf[r], self.b_rowf[r], w // 128, pt[:, col0 + done // 128: col0 + (done + w) // 128], pb, self.ones_f, self.b_const)
            done += w; j += 1

    def rstd_of(self, src_ap, src_bufs, col, n):
        k = self.k; st = self.st; b_st = self.b_st
        k.memset("dve", st[:, col:col + 1], 0.0, [b_st])
        k.act(self.xn[:, 0:n], src_ap, AF.Square, src_bufs + [b_st], [self.b_xn, b_st], accum=st[:, col:col + 1])
        k.ts("dve", st[:, col:col + 1], st[:, col:col + 1], 1.0 / n, EPS, ALU.mult, ALU.add, [b_st], [b_st])
        k.act(st[:, col:col + 1], st[:, col:col + 1], AF.Ln, [b_st], [b_st])
        k.act(st[:, col:col + 1], st[:, col:col + 1], AF.Exp, [b_st], [b_st], scale=-0.5)

    def norm_transpose(self, x_dram_tile, hT, hcol0, b_h):
        k = self.k; D = self.D; DC = self.DC
        k.dma("sp", self.xt[:], x_dram_tile, [], [self.b_xt], self.b_xt)
        self.rstd_of(self.xt[:], [self.b_xt], 1, D)
        k.act(self.xn[:], self.xt[:], AF.Copy, [self.b_xt, self.b_st], [self.b_xn], scale=self.st[:, 1:2])
        for g in range(max(1, DC // 4)):
            nq = min(4, DC)
            pt, pb = k.bank()
            for q in range(nq):
                kc = g * 4 + q
                k.mm(pt[:, q * 128:(q + 1) * 128], self.xn[:, kc * 128:(kc + 1) * 128], self.identb[:, :], True, True, [self.b_xn, self.b_const], [pb])
            for q in range(nq):
                kc = g * 4 + q
                o = hT[:, kc, hcol0:hcol0 + 128]
                i = pt[:, q * 128:(q + 1) * 128]
                if q % 2 == 0:
                    k.act(o, i, AF.Identity, [pb, self.b_gs, self.b_modc], [b_h], bias=self.modc[:, kc:kc + 1], scale=self.gs_c[:, kc:kc + 1])
                else:
                    k.ts("dve", o, i, self.gs_c[:, kc:kc + 1], self.modc[:, kc:kc + 1], ALU.mult, ALU.add, [pb, self.b_gs, self.b_modc], [b_h])


EPS = 1e-6


def row_to_col(k, row_t, row_b, ncols, ps_ap, ps_b, one_f, b_const):
    for i in range(ncols):
        k.mm(ps_ap[:, i:i + 1], row_t[0:1, i * 128:(i + 1) * 128], one_f[0:1, 0:1], True, True, [row_b, b_const], [ps_b])


def build_bd(D, F, NT, HC=256, TN=512, k=None, ysrc=None, out_kind="ExternalOutput"):
    k = k or KB()
    nc, s = k.nc, k.s
    DC = D // 128
    NTT = NT // 128
    NB = D // 512
    SUB = HC // 128
    TPB = TN // 128
    k.psum_banks()
    xin = k.dram("xin", [NT, D]); cvec = k.dram("cvec", [1, D])
    ycat = k.dram("ycat", [NT, D]) if ysrc is None else None
    seld = k.dram("sel", [128, 2]) if ysrc is not None else None
    wada = k.dram("wada", [D, 4 * D]); bada = k.dram("bada", [1, 4 * D])
    wout = k.dram("wout", [D, D]); gpost = k.dram("gpost", [1, D]); gpre = k.dram("gpre", [1, D])
    gpost2 = k.dram("gpost2", [1, D]); wfi = k.dram("wfi", [D, 2 * F]); wfo = k.dram("wfo", [F, D])
    ident = k.dram("ident", [128, 128])
    xmid = k.dram("xmid", [NT, D], kind="ExternalOutput")
    xout = k.dram("xout", [NT, D], kind=out_kind)
    identf = k.sb("identf", [128, 128]); identb = k.sb("identb", [128, 128], BF16)
    ones_f = k.sb("ones_f", [1, 128]); ones_b = k.sb("ones_b", [1, 128], BF16)
    zeros = k.sb("zeros", [128, 128])
    rowf = [k.sb("rowf%d" % i, [1, 512]) for i in range(2)]
    rowb = [k.sb("rowb%d" % i, [1, 512], BF16) for i in range(2)]
    condT_f = k.sb("condT_f", [128, DC]); condT_b = k.sb("condT_b", [128, DC], BF16)
    cond_rep = k.sb("cond_rep", [128, DC, 128], BF16)
    modc = k.sb("modc", [128, 2 * DC]); gpre_c = k.sb("gpre_c", [128, DC]); gsf_c = k.sb("gsf_c", [128, DC])
    gt_b = k.sb("gt_b", [128, D])
    wsl = [k.sb("wsl%d" % i, [128, DC, 512], BF16) for i in range(3)]
    wos = [k.sb("wos%d" % i, [128, SUB, D], BF16) for i in range(2)]
    acc = k.sb("acc", [128, NTT, D])
    hT = k.sb("hT", [128, DC, NT], BF16)
    xt = k.sb("xt", [128, D]); xn = k.sb("xn", [128, D], BF16)
    tmp = [k.sb("tmp%d" % i, [128, 512]) for i in range(2)]
    actT = [k.sb("actT%d" % i, [128, SUB, TN], BF16) for i in range(2)]
    sg = [k.sb("sg%d" % i, [128, TN]) for i in range(2)]
    st = k.sb("st", [128, 8])
    selt = k.sb("selt", [128, 2])
    B = s.buf
    b_const = B(); b_rowf = [B(), B()]; b_rowb = [B(), B()]; b_cond = B(); b_modc = B(); b_gpre = B(); b_gsf = B()
    b_gt = [B() for _ in range(NB)]
    b_wsl = [B() for _ in range(3)]; b_wos = [B() for _ in range(2)]
    b_acc = [[B() for _ in range(NB)] for _ in range(NTT)]
    b_hT = [B() for _ in range(NTT)]
    b_xt = B(); b_xn = B(); b_tmp = [B(), B()]; b_actT = [B(), B()]; b_sg = [B(), B()]; b_st = B()
    b_xmid = [B() for _ in range(NTT)]
    cnt = {"wsl": 0, "wos": 0, "row": 0, "tmp": 0, "ev": 0}

    def next_wsl():
        i = cnt["wsl"] % 3; cnt["wsl"] += 1
        return wsl[i], b_wsl[i]

    def load_w(dst, dbuf, src_cols, width, off=0):
        k.dma("pool", dst[:, :, off:off + width], src_cols.rearrange("(kc p) n -> p kc n", p=128), [], [dbuf], dbuf)

    k.dma("sp", identf[:], ident[:, :], [], [b_const], b_const)
    k.cp("dve", identb[:], identf[:], [b_const], [b_const])
    k.memset("dve", ones_f[:], 1.0, [b_const]); k.memset("dve", ones_b[:], 1.0, [b_const])
    k.memset("dve", zeros[:], 0.0, [b_const])
    one_f = ones_f
    if ysrc is not None:
        k.dma("sp", selt[:], seld[:, :], [], [b_const], b_const)

    pt, pb = k.bank()
    for j in range(D // 512):
        r = j % 2
        k.dma("sp", rowf[r][0:1, :], cvec[0:1, j * 512:(j + 1) * 512], [], [b_rowf[r]], b_rowf[r])
        row_to_col(k, rowf[r], b_rowf[r], 4, pt[:, j * 4:(j + 1) * 4], pb, one_f, b_const)
    k.act(condT_f[:], pt[:, 0:DC], AF.Silu, [pb], [b_cond])
    k.cp("dve", condT_b[:], condT_f[:], [b_cond], [b_cond])
    for kc in range(DC):
        k.ts("dve", cond_rep[:, kc, :], zeros[:], condT_f[:, kc:kc + 1], None, ALU.add, None, [b_cond, b_const], [b_cond])
    pt, pb = k.bank()
    for j in range(D // 512):
        r = j % 2
        k.dma("sp", rowf[r][0:1, :], gpre[0:1, j * 512:(j + 1) * 512], [], [b_rowf[r]], b_rowf[r])
        row_to_col(k, rowf[r], b_rowf[r], 4, pt[:, j * 4:(j + 1) * 4], pb, one_f, b_const)
    k.cp("act", gpre_c[:], pt[:, 0:DC], [pb], [b_gpre])

    def mod_bcast(col0, grow):
        for j in range(NB):
            w, wb = next_wsl()
            load_w(w, wb, wada[:, col0 + j * 512: col0 + (j + 1) * 512], 512)
            r = j % 2
            k.dma("pool", rowb[r][0:1, :], bada[0:1, col0 + j * 512: col0 + (j + 1) * 512], [], [b_rowb[r]], b_rowb[r])
            k.dma("sp", rowf[r][0:1, :], grow[0:1, j * 512:(j + 1) * 512], [], [b_rowf[r]], b_rowf[r])
            pa, pab = k.bank()
            for kc in range(DC):
                k.mm(pa[:, :], cond_rep[:, kc, :], w[:, kc, :], kc == 0, False, [b_cond, wb], [pab])
            k.mm(pa[:, :], ones_b[0:1, :], rowb[r][0:1, :], False, True, [b_const, b_rowb[r]], [pab])
            pg, pgb = k.bank()
            k.mm(pg[:, :], ones_f[0:1, :], rowf[r][0:1, :], True, True, [b_const, b_rowf[r]], [pgb])
            k.cp("act", tmp[r][:], pg[:, :], [pgb], [b_tmp[r]])
            k.tt("dve", gt_b[:, j * 512:(j + 1) * 512], pa[:, :], tmp[r][:], ALU.mult, [pab, b_tmp[r]], [b_gt[j]])

    mod_bcast(0, gpost)

    pc, pcb = k.bank()
    for j in range(2 * D // 512):
        w, wb = next_wsl()
        load_w(w, wb, wada[:, D + j * 512: D + (j + 1) * 512], 512)
        r = j % 2
        k.dma("pool", rowb[r][0:1, :], bada[0:1, D + j * 512: D + (j + 1) * 512], [], [b_rowb[r]], b_rowb[r])
        for q in range(4):
            col = j * 4 + q
            for kc in range(DC):
                k.mm(pc[:, col:col + 1], w[:, kc, q * 128:(q + 1) * 128], condT_b[:, kc:kc + 1], kc == 0, False, [b_cond, wb], [pcb])
            k.mm(pc[:, col:col + 1], rowb[r][0:1, q * 128:(q + 1) * 128], ones_b[0:1, 0:1], False, True, [b_rowb[r], b_const], [pcb])
    k.cp("act", modc[:], pc[:, 0:2 * DC], [pcb], [b_modc])
    k.stt("dve", gsf_c[:], modc[:, DC:2 * DC], 1.0, gpre_c[:], ALU.add, ALU.mult, [b_modc, b_gpre], [b_gsf])
    shf_c = modc

    def transpose_tile(t, modulate):
        for g in range(DC // 4 if DC >= 4 else 1):
            nq = min(4, DC)
            pt, pb = k.bank()
            for q in range(nq):
                kc = g * 4 + q
                k.mm(pt[:, q * 128:(q + 1) * 128], xn[:, kc * 128:(kc + 1) * 128], identb[:, :], True, True, [b_xn, b_const], [pb])
            if not modulate:
                src = pt[:, 0:nq * 128].rearrange("p (a b) -> p a b", a=nq)
                eng = "act" if g % 2 == 0 else "dve"
                k.cp(eng, hT[:, g * 4:g * 4 + nq, t * 128:(t + 1) * 128], src, [pb], [b_hT[t]])
            else:
                for q in range(nq):
                    kc = g * 4 + q
                    o = hT[:, kc, t * 128:(t + 1) * 128]
                    i = pt[:, q * 128:(q + 1) * 128]
                    if q % 2 == 0:
                        k.act(o, i, AF.Identity, [pb, b_gsf, b_modc], [b_hT[t]], bias=shf_c[:, kc:kc + 1], scale=gsf_c[:, kc:kc + 1])
                    else:
                        k.ts("dve", o, i, gsf_c[:, kc:kc + 1], shf_c[:, kc:kc + 1], ALU.mult, ALU.add, [pb, b_gsf, b_modc], [b_hT[t]])

    for t in range(NTT):
        if ysrc is None:
            k.dma("sp", xt[:], ycat[t * 128:(t + 1) * 128, :], [], [b_xt], b_xt)
        else:
            for (c0, wd, fa, fb) in ysrc:
                if fb is None:
                    k.dma("sp", xt[:, c0:c0 + wd], fa(t), [], [b_xt], b_xt)
                    continue
                for c1 in range(0, wd, 512):
                    w_ = min(512, wd - c1)
                    r = (c1 // 512) % 2
                    k.dma("sp", xt[:, c0 + c1:c0 + c1 + w_], fa(t)[:, c1:c1 + w_], [], [b_xt], b_xt)
                    k.dma("sp", tmp[r][:, 0:w_], fb(t)[:, c1:c1 + w_], [], [b_tmp[r]], b_tmp[r])
                    k.ts("dve", xt[:, c0 + c1:c0 + c1 + w_], xt[:, c0 + c1:c0 + c1 + w_], selt[:, 0:1], None, ALU.mult, None, [b_xt, b_const], [b_xt])
                    k.stt("dve", xt[:, c0 + c1:c0 + c1 + w_], tmp[r][:, 0:w_], selt[:, 1:2], xt[:, c0 + c1:c0 + c1 + w_], ALU.mult, ALU.add, [b_tmp[r], b_xt, b_const], [b_xt])
        k.cp("act", xn[:], xt[:], [b_xt], [b_xn])
        transpose_tile(t, False)

    for n in range(NB):
        w, wb = next_wsl()
        load_w(w, wb, wout[:, n * 512:(n + 1) * 512], 512)
        for t in range(NTT):
            pt, pb = k.bank()
            for kc in range(DC):
                k.mm(pt[:, :], hT[:, kc, t * 128:(t + 1) * 128], w[:, kc, :], kc == 0, kc == DC - 1, [b_hT[t], wb], [pb])
            eng = "act" if t % 2 == 0 else "dve"
            k.cp(eng, acc[:, t, n * 512:(n + 1) * 512], pt[:, :], [pb], [b_acc[t][n]])

    def rstd_of(src_ap, src_bufs, col):
        k.memset("dve", st[:, col:col + 1], 0.0, [b_st])
        k.act(xn[:], src_ap, AF.Square, src_bufs + [b_st], [b_xn, b_st], accum=st[:, col:col + 1])
        k.ts("dve", st[:, col:col + 1], st[:, col:col + 1], 1.0 / D, EPS, ALU.mult, ALU.add, [b_st], [b_st])
        k.act(st[:, col:col + 1], st[:, col:col + 1], AF.Ln, [b_st], [b_st])
        k.act(st[:, col:col + 1], st[:, col:col + 1], AF.Exp, [b_st], [b_st], scale=-0.5)

    for t in range(NTT):
        k.dma("sp", xt[:], xin[t * 128:(t + 1) * 128, :], [], [b_xt], b_xt)
        rstd_of(acc[:, t, :], b_acc[t], 0)
        k.stt("dve", acc[:, t, :], acc[:, t, :], st[:, 0:1], gt_b[:, :], ALU.mult, ALU.mult, b_acc[t] + b_gt + [b_st], b_acc[t])
        k.tt("dve", xt[:], xt[:], acc[:, t, :], ALU.add, [b_xt] + b_acc[t], [b_xt])
        k.dma("sp", xmid[t * 128:(t + 1) * 128, :], xt[:], [b_xt], [b_xmid[t]], b_xt)
        rstd_of(xt[:], [b_xt], 1)
        k.act(xn[:], xt[:], AF.Copy, [b_xt, b_st], [b_xn], scale=st[:, 1:2])
        transpose_tile(t, True)

    mod_bcast(3 * D, gpost2)

    blocks = [(hc, th) for hc in range(F // HC) for th in range(NT // TN)]
    wcur = {}

    def emit_gu(i):
        hc, th = blocks[i]
        if th == 0:
            w, wb = next_wsl()
            load_w(w, wb, wfi[:, hc * HC:(hc + 1) * HC], HC, 0)
            load_w(w, wb, wfi[:, F + hc * HC: F + (hc + 1) * HC], HC, HC)
            j = cnt["wos"] % 2; cnt["wos"] += 1
            k.dma("pool", wos[j][:, :, :], wfo[hc * HC:(hc + 1) * HC, :].rearrange("(s p) n -> p s n", p=128), [], [b_wos[j]], b_wos[j])
            wcur[hc] = (w, wb, wos[j], b_wos[j])
        w, wb, _, _ = wcur[hc]
        a = i % 2
        hbufs = [b_hT[th * TPB + q] for q in range(TPB)]
        for sub in range(SUB):
            pg, pgb = k.bank()
            for kc in range(DC):
                k.mm(pg[:, 0:TN], w[:, kc, sub * 128:(sub + 1) * 128], hT[:, kc, th * TN:(th + 1) * TN], kc == 0, kc == DC - 1, [wb] + hbufs, [pgb])
            pu, pub = k.bank()
            for kc in range(DC):
                k.mm(pu[:, 0:TN], w[:, kc, HC + sub * 128: HC + (sub + 1) * 128], hT[:, kc, th * TN:(th + 1) * TN], kc == 0, kc == DC - 1, [wb] + hbufs, [pub])
            r = sub % 2
            k.act(sg[r][:, :], pg[:, 0:TN], AF.Silu, [pgb], [b_sg[r]])
            k.tt("dve", actT[a][:, sub, :], sg[r][:, :], pu[:, 0:TN], ALU.mult, [b_sg[r], pub], [b_actT[a]])

    def emit_y(i):
        hc, th = blocks[i]
        _, _, wo, wob = wcur[hc]
        a = i % 2
        for tq in range(TPB):
            t = th * TPB + tq
            for n in range(NB):
                py, pyb = k.bank()
                for sub in range(SUB):
                    k.mm(py[:, :], actT[a][:, sub, tq * 128:(tq + 1) * 128], wo[:, sub, n * 512:(n + 1) * 512], sub == 0, sub == SUB - 1, [b_actT[a], wob], [pyb])
                dst = acc[:, t, n * 512:(n + 1) * 512]
                if hc == 0:
                    k.cp("dve", dst, py[:, :], [pyb], [b_acc[t][n]])
                else:
                    k.tt("dve", dst, dst, py[:, :], ALU.add, [pyb, b_acc[t][n]], [b_acc[t][n]])

    for i in range(len(blocks)):
        emit_gu(i)
        if i > 0:
            emit_y(i - 1)
    emit_y(len(blocks) - 1)

    for t in range(NTT):
        k.dma("sp", xt[:], xmid[t * 128:(t + 1) * 128, :], [b_xmid[t]], [b_xt], b_xt)
        rstd_of(acc[:, t, :], b_acc[t], 2)
        k.stt("dve", acc[:, t, :], acc[:, t, :], st[:, 2:3], gt_b[:, :], ALU.mult, ALU.mult, b_acc[t] + b_gt + [b_st], b_acc[t])
        k.tt("dve", xt[:], xt[:], acc[:, t, :], ALU.add, [b_xt] + b_acc[t], [b_xt])
        k.dma("sp", xout[t * 128:(t + 1) * 128, :], xt[:], [b_xt], [], b_xt)
    k.finish_build()
    return nc


def build_rw(D, T, HO, SEG=512, RDT=F32, stop=99, k=None):
    k = k or KB(); nc, s = k.nc, k.s; B = s.buf
    DC = D // 128; CH = 64 * HO; NP = HO // 2; NSEG = T // SEG; NCH = SEG // 64; NZ = 3 * NP + 3
    k.psum_banks()
    x = k.dram("x", [T, D]); cvec = k.dram("cvec", [1, D]); wada2 = k.dram("wada2", [D, 2 * D]); bada2 = k.dram("bada2", [1, 2 * D])
    gpre = k.dram("gpre", [1, D]); ident = k.dram("ident", [128, 128])
    wr = k.dram("wr", [D, CH]); wk = k.dram("wk", [D, CH]); wv = k.dram("wv", [D, CH]); wl = k.dram("wl", [D, 288])
    mu_cat = k.dram("mu_cat", [1, NZ * 128]); pvec = k.dram("pvec", [1, 5 * CH]); lnx = k.dram("lnx", [1, 2 * CH])
    w2 = k.dram("w2", [64, CH]); a2 = k.dram("a2", [64, CH]); g2 = k.dram("g2", [160, CH])
    mask5 = k.dram("mask5", [64, 320]); tri2 = k.dram("tri2", [128, 128]); bones = k.dram("bones", [128, 128]); hsel = k.dram("hsel", [128, 2])
    yb = k.dram("yb", [T, CH], kind="ExternalOutput")
    ybanks = [k.reserve(), k.reserve()]
    pre = Pre(k, D, wada2, bada2, cvec, gpre, ident, nwsl=2)
    identf = pre.identf; bc = pre.b_const
    hTs = k.sb("hTs", [128, DC, SEG], BF16); b_hT = B()
    Rt = k.sb("Rt", [128, NP, SEG]); Kt = k.sb("Kt", [128, NP, SEG]); Vt = k.sb("Vt", [128, NP, SEG])
    BTt = k.sb("BTt", [128, NP, SEG]); ATt = k.sb("ATt", [128, NP, SEG]); RKt = k.sb("RKt", [128, NP, SEG])
    b_R = [B() for _ in range(NP)]; b_K = [B() for _ in range(NP)]; b_V = [B() for _ in range(NP)]
    b_BT = [B() for _ in range(NP)]; b_AT = [B() for _ in range(NP)]; b_RK = [B() for _ in range(NP)]
    zwa = k.sb("zwa", [128, SEG]); zga = k.sb("zga", [128, SEG]); zgb = k.sb("zgb", [32, SEG]); b_zl = [B(), B(), B()]
    twb = k.sb("twb", [128, SEG], BF16); sga = k.sb("sga", [128, SEG], BF16); sgb = k.sb("sgb", [32, SEG], BF16); b_lo = B()
    zraw = [k.sb("zraw%d" % i, [128, SEG + 1]) for i in range(2)]; b_zraw = [B(), B()]
    carry = k.sb("carry", [128, NZ]); b_carry = B()
    S = [k.sb("S%d" % i, [128, SEG]) for i in range(7)]; b_S = [B() for _ in range(7)]
    mu_c = k.sb("mu_c", [128, NZ]); omm_c = k.sb("omm_c", [128, NZ]); pv_c = k.sb("pv_c", [128, 5 * NP]); b_par = B()
    pcs = k.sb("pcs", [128, NP, NCH]); b_pc = [B() for _ in range(NP)]
    w2a2 = k.sb("w2a2", [128, CH], BF16); g2a = k.sb("g2a", [128, CH], BF16); g2b = k.sb("g2b", [32, CH], BF16)
    m5 = k.sb("m5", [64, 320]); tri = k.sb("tri", [128, 128]); bon1 = k.sb("bon1", [128, 128]); hs = k.sb("hs", [128, 2])
    lg_b = k.sb("lg_b", [64, CH]); lb_b = k.sb("lb_b", [64, CH])
    RtO = k.sb("RtO", [64, NP, SEG]); KtO = k.sb("KtO", [64, NP, SEG]); BTO = k.sb("BTO", [64, NP, SEG]); ATO = k.sb("ATO", [64, NP, SEG])
    b_RO = [B() for _ in range(NP)]; b_KO = [B() for _ in range(NP)]; b_BO = [B() for _ in range(NP)]; b_AO = [B() for _ in range(NP)]
    pcsO = k.sb("pcsO", [64, NP, NCH]); b_pcO = [B() for _ in range(NP)]
    Tst = [[k.sb("Tst%d_%d" % (h, i), [64, 64]) for i in range(2)] for h in range(HO)]
    b_T = [[B(), B()] for _ in range(HO)]
    TM = [k.sb("TM%d" % p, [64, 3, 128]) for p in range(NP)]; b_TM = [B() for _ in range(NP)]
    NS = 2
    G = [k.sb("G%d" % i, [64, 320]) for i in range(NS)]; b_G = [B() for _ in range(NS)]
    NTt = [k.sb("NT%d" % i, [64, 64]) for i in range(NS)]; b_NT = [B() for _ in range(NS)]
    LP = [[k.sb("LP%d_%d" % (i, j), [64, 128]) for j in range(2)] for i in range(NS)]; b_LP = [[B(), B()] for _ in range(NS)]
    Wsb = [k.sb("Wsb%d" % i, [64, 64]) for i in range(NS)]; b_W = [B() for _ in range(NS)]
    Usb = [k.sb("Usb%d" % h, [64, 64]) for h in range(HO)]; b_U = [B() for _ in range(HO)]
    Y1 = k.sb("Y1", [64, CH]); b_Y1 = B(); bon = k.sb("bon", [64, HO]); b_bon = B()
    stt_ = k.sb("stt_", [64, 4 * HO]); b_stt = B(); Y2 = k.sb("Y2", [64, CH]); b_Y2 = B()

    for (dst, src) in ((m5, mask5), (tri, tri2), (bon1, bones), (hs, hsel)):
        k.dma("sp", dst[:], src[:, :], [], [bc], bc)
    k.dma("pool", w2a2[0:64, :], w2[:, :], [], [bc], bc)
    k.dma("pool", w2a2[64:128, :], a2[:, :], [], [bc], bc)
    k.dma("pool", g2a[:, :], g2[0:128, :], [], [bc], bc)
    k.dma("pool", g2b[:, :], g2[128:160, :], [], [bc], bc)
    pt, pb = k.bank()
    pre.col_from_dram(mu_cat, NZ * 128, pt, pb)
    k.cp("act", mu_c[:], pt[:, 0:NZ], [pb], [b_par])
    k.ts("dve", omm_c[:], mu_c[:], -1.0, 1.0, ALU.mult, ALU.add, [b_par], [b_par])
    pt, pb = k.bank()
    pre.col_from_dram(pvec, 5 * CH, pt, pb)
    k.cp("act", pv_c[:], pt[:, 0:5 * NP], [pb], [b_par])
    for (dst, off) in ((lg_b, 0), (lb_b, CH)):
        done = 0
        while done < CH:
            w = min(512, CH - done)
            k.dma("sp", pre.rowf[0][0:1, 0:w], lnx[0:1, off + done: off + done + w], [], [pre.b_rowf[0]], pre.b_rowf[0])
            pt, pb = k.bank()
            k.mm(pt[0:64, 0:w], pre.ones_f[0:1, 0:64], pre.rowf[0][0:1, 0:w], True, True, [bc, pre.b_rowf[0]], [pb])
            k.cp("act", dst[:, done:done + w], pt[0:64, 0:w], [pb], [b_par])
            done += w
    k.memset("dve", carry[:], 0.0, [b_carry])
    for h in range(HO):
        k.memset("dve", Tst[h][0][:], 0.0, [b_T[h][0]])
    W0, A0, KK, KA, RKc = [lambda p, i=i: pv_c[:, i * NP + p: i * NP + p + 1] for i in range(5)]

    if stop == 1:
        k.finish_build(); return nc
    zi = {"n": 0}

    def ztile(w, wb, c0, M, dst, dbufs, idx):
        ps, pb = k.bank()
        for kc in range(DC):
            k.mm(ps[0:M, 0:SEG], w[:, kc, c0:c0 + M], hTs[:, kc, :], kc == 0, kc == DC - 1, [wb, b_hT], [pb])
        r = zi["n"] % 2; zi["n"] += 1
        zr = zraw[r]; bz = b_zraw[r]
        k.cp("act", zr[0:M, 1:SEG + 1], ps[0:M, 0:SEG], [pb], [bz])
        k.cp("dve", zr[0:M, 0:1], carry[0:M, idx:idx + 1], [b_carry], [bz])
        k.ts("dve", S[0][0:M, :], zr[0:M, 0:SEG], mu_c[0:M, idx:idx + 1], None, ALU.mult, None, [bz, b_par], [b_S[0]])
        k.stt("dve", dst, zr[0:M, 1:SEG + 1], omm_c[0:M, idx:idx + 1], S[0][0:M, :], ALU.mult, ALU.add, [bz, b_par, b_S[0]], dbufs)
        k.cp("dve", carry[0:M, idx:idx + 1], zr[0:M, SEG:SEG + 1], [bz], [b_carry])

    for sg_ in range(NSEG):
        t0 = sg_ * SEG
        for tq in range(SEG // 128):
            pre.norm_transpose(x[t0 + tq * 128: t0 + (tq + 1) * 128, :], hTs, tq * 128, b_hT)
        for (wsrc, arr, bufs, zbase) in ((wr, Rt, b_R, 0), (wk, Kt, b_K, NP), (wv, Vt, b_V, 2 * NP)):
            w, wb = pre.next_wsl()
            pre.load_w(w, wb, wsrc[:, :], CH)
            for p in range(NP):
                ztile(w, wb, p * 128, 128, arr[:, p, :], [bufs[p]], zbase + p)
        w, wb = pre.next_wsl()
        pre.load_w(w, wb, wl[:, :], 288)
        ztile(w, wb, 0, 128, zwa[:, :], [b_zl[0]], 3 * NP)
        ztile(w, wb, 128, 128, zga[:, :], [b_zl[1]], 3 * NP + 1)
        ztile(w, wb, 256, 32, zgb[:, :], [b_zl[2]], 3 * NP + 2)
        k.act(twb[0:64, :], zwa[0:64, :], AF.Tanh, [b_zl[0]], [b_lo])
        k.cp("dve", twb[64:128, :], zwa[64:128, :], [b_zl[0]], [b_lo])
        k.act(sga[:, :], zga[:, :], AF.Sigmoid, [b_zl[1]], [b_lo])
        k.act(sgb[:, :], zgb[:, :], AF.Sigmoid, [b_zl[2]], [b_lo])
        if stop == 2:
            k.finish_build(); return nc
        for p in range(NP):
            cs = slice(p * 128, (p + 1) * 128)
            ps, pb = k.bank()
            k.mm(ps[:, 0:SEG], w2a2[0:64, cs], twb[0:64, :], True, True, [bc, b_lo], [pb])
            k.act(S[1][:, :], ps[:, 0:SEG], AF.Sigmoid, [pb, b_par], [b_S[1]], bias=W0(p))
            k.ts("dve", S[1][:, :], S[1][:, :], -0.6065306597126334, None, ALU.mult, None, [b_S[1]], [b_S[1]])
            ps, pb = k.bank()
            k.mm(ps[:, 0:SEG], w2a2[64:128, cs], twb[64:128, :], True, True, [bc, b_lo], [pb])
            k.act(S[2][:, :], ps[:, 0:SEG], AF.Sigmoid, [pb, b_par], [b_S[2]], bias=A0(p))
            k.ts("dve", S[3][:, :], Kt[:, p, :], KK(p), None, ALU.mult, None, [b_K[p], b_par], [b_S[3]])
            k.tt("dve", S[4][:, :], S[3][:, :], S[3][:, :], ALU.mult, [b_S[3]], [b_S[4]])
            ps, pb = k.bank()
            k.mm(ps[:, 0:SEG], bon1[:, :], S[4][:, :], True, True, [bc, b_S[4]], [pb])
            k.act(S[4][:, :], ps[:, 0:SEG], AF.Sqrt, [pb], [b_S[4]])
            k.ts("dve", S[4][:, :], S[4][:, :], 1e-12, None, ALU.max, None, [b_S[4]], [b_S[4]])
            k.recip("dve", S[4][:, :], S[4][:, :], [b_S[4]], [b_S[4]])
            k.tt("dve", S[3][:, :], S[3][:, :], S[4][:, :], ALU.mult, [b_S[3], b_S[4]], [b_S[3]])
            k.ts("dve", S[4][:, :], S[2][:, :], 1.0, KA(p), ALU.subtract, ALU.mult, [b_S[2], b_par], [b_S[4]])
            k.stt("dve", Kt[:, p, :], S[4][:, :], 1.0, Kt[:, p, :], ALU.add, ALU.mult, [b_S[4], b_K[p]], [b_K[p]])
            k.stt("dve", RKt[:, p, :], Rt[:, p, :], RKc(p), Kt[:, p, :], ALU.mult, ALU.mult, [b_R[p], b_K[p], b_par], [b_RK[p]])
            k.tt("dve", S[2][:, :], S[3][:, :], S[2][:, :], ALU.mult, [b_S[3], b_S[2]], [b_S[2]])
            pc_, pcb = k.bank()
            for q in range(SEG // 128):
                qs = slice(q * 128, (q + 1) * 128)
                pt, ptb = k.bank()
                k.mm(pt[:, 0:128], S[1][:, qs], identf[:, :], True, True, [b_S[1], bc], [ptb])
                k.cp("act", S[5][:, qs], pt[:, 0:128], [ptb], [b_S[5]])
                k.mm(pc_[:, qs], S[5][:, qs], tri[:, :], True, True, [b_S[5], bc], [pcb])
            k.cp("act", S[4][:, :], pc_[:, 0:SEG], [pcb], [b_S[4]])
            k.act(S[5][:, :], S[4][:, :], AF.Exp, [b_S[4]], [b_S[5]])
            k.tt("dve", Rt[:, p, :], Rt[:, p, :], S[5][:, :], ALU.mult, [b_R[p], b_S[5]], [b_R[p]])
            k.cp("dve", pcs[:, p, :], S[5][:, :].rearrange("p (c t) -> p c t", t=64)[:, :, 63], [b_S[5]], [b_pc[p]])
            k.act(S[6][:, :], S[4][:, :], AF.Exp, [b_S[4]], [b_S[6]], scale=-1.0)
            k.tt("dve", Kt[:, p, :], Kt[:, p, :], S[6][:, :], ALU.mult, [b_K[p], b_S[6]], [b_K[p]])
            k.tt("dve", BTt[:, p, :], S[2][:, :], S[6][:, :], ALU.mult, [b_S[2], b_S[6]], [b_BT[p]])
            k.tt("dve", S[4][:, :], S[4][:, :], S[1][:, :], ALU.subtract, [b_S[4], b_S[1]], [b_S[4]])
            k.act(S[4][:, :], S[4][:, :], AF.Exp, [b_S[4]], [b_S[4]])
            k.stt("dve", ATt[:, p, :], S[3][:, :], -1.0, S[4][:, :], ALU.mult, ALU.mult, [b_S[3], b_S[4]], [b_AT[p]])
            for (dst, src, bs_, bd_) in ((RtO, Rt, b_R, b_RO), (KtO, Kt, b_K, b_KO), (BTO, BTt, b_BT, b_BO), (ATO, ATt, b_AT, b_AO)):
                k.dma("sp", dst[:, p, :], src[64:128, p, :], [bs_[p]], [bd_[p]], bd_[p])
            k.dma("sp", pcsO[:, p, :], pcs[64:128, p, :], [b_pc[p]], [b_pcO[p]], b_pcO[p])
        if stop == 3:
            k.finish_build(); return nc
        for c in range(NCH):
            cg = sg_ * NCH + c
            cur = cg % 2
            cols = slice(c * 64, (c + 1) * 64)
            yps, ypb = ybanks[cg % 2]
            for p in range(NP):
                pt, ptb = k.bank()
                for i, (arr, bb) in enumerate(((Vt, b_V), (BTt, b_BT), (Kt, b_K))):
                    k.mm(pt[0:64, i * 128:(i + 1) * 128], arr[:, p, cols], identf[:, :], True, True, [bb[p], bc], [ptb])
                k.cp("act", TM[p][:, :, :], pt[0:64, 0:384].rearrange("p (a b) -> p a b", a=3), [ptb], [b_TM[p]])
                if stop == 41:
                    k.finish_build(); return nc
                for e in range(2):
                    h = 2 * p + e; si = h % NS
                    rows = slice(e * 64, (e + 1) * 64)
                    if e == 0:
                        bt = BTt[0:64, p, cols]; at = ATt[0:64, p, cols]; rt = Rt[0:64, p, cols]; kt = Kt[0:64, p, cols]
                        deps = [b_BT[p], b_AT[p], b_R[p], b_K[p]]
                    else:
                        bt = BTO[:, p, cols]; at = ATO[:, p, cols]; rt = RtO[:, p, cols]; kt = KtO[:, p, cols]
                        deps = [b_BO[p], b_AO[p], b_RO[p], b_KO[p]]
                    b_at, b_rt = deps[1], deps[2]
                    ps, pb = k.bank()
                    k.mm(ps[0:64, 0:64], bt, at, True, True, deps, [pb])
                    k.mm(ps[0:64, 64:128], bt, rt, True, True, deps, [pb])
                    k.mm(ps[0:64, 128:192], kt, at, True, True, deps, [pb])
                    k.mm(ps[0:64, 192:256], kt, rt, True, True, deps, [pb])
                    k.mm(ps[0:64, 256:320], at, bt, True, True, deps, [pb])
                    k.tt("dve", G[si][:, :], ps[0:64, 0:320], m5[:, :], ALU.mult, [pb, bc], [b_G[si]])
                    if stop == 42 + e * 10:
                        k.finish_build(); return nc
                    k.tt("dve", NTt[si][:, :], G[si][:, 0:64], identf[0:64, 0:64], ALU.add, [b_G[si], bc], [b_NT[si]])
                    Lk = G[si][:, 256:320]; Pk = G[si][:, 0:64]; lb_ = [b_G[si]]
                    for lev in range(5):
                        ps, pb = k.bank()
                        k.mm(ps[0:64, 0:64], Pk, Lk, True, True, lb_, [pb])
                        if lev < 4:
                            k.mm(ps[0:64, 64:128], Lk, Pk, True, True, lb_, [pb])
                        j = lev % 2
                        k.cp("act", LP[si][j][:, :], ps[0:64, 0:128], [pb], [b_LP[si][j]])
                        Lk = LP[si][j][:, 0:64]; Pk = LP[si][j][:, 64:128]; lb_ = [b_LP[si][j]]
                        ps2, pb2 = k.bank()
                        k.mm(ps2[0:64, 0:64], Lk, NTt[si][:, :], True, True, lb_ + [b_NT[si]], [pb2])
                        k.tt("dve", NTt[si][:, :], NTt[si][:, :], ps2[0:64, 0:64], ALU.add, [pb2, b_NT[si]], [b_NT[si]])
                    if stop == 43 + e * 10:
                        k.finish_build(); return nc
                    vte = TM[p][:, 0, rows]
                    tst = Tst[h][cur][:, :]
                    ps, pb = k.bank()
                    k.mm(ps[0:64, 0:64], G[si][:, 128:192], vte, True, False, [b_G[si], b_TM[p]], [pb])
                    k.mm(ps[0:64, 0:64], at, tst, False, True, [b_at, b_T[h][cur]], [pb])
                    k.cp("act", Wsb[si][:, :], ps[0:64, 0:64], [pb], [b_W[si]])
                    ps, pb = k.bank()
                    k.mm(ps[0:64, 0:64], NTt[si][:, :], Wsb[si][:, :], True, True, [b_NT[si], b_W[si]], [pb])
                    k.cp("act", Usb[h][:, :], ps[0:64, 0:64], [pb], [b_U[h]])
                    yo = yps[0:64, h * 64:(h + 1) * 64]
                    k.mm(yo, rt, tst, True, False, [b_rt, b_T[h][cur]], [ypb])
                    k.mm(yo, G[si][:, 64:128], Usb[h][:, :], False, False, [b_G[si], b_U[h]], [ypb])
                    k.mm(yo, G[si][:, 192:256], vte, False, True, [b_G[si], b_TM[p]], [ypb])
                    if stop == 44 + e * 10:
                        k.finish_build(); return nc
                for e in range(2):
                    h = 2 * p + e
                    rows = slice(e * 64, (e + 1) * 64)
                    ps, pb = k.bank()
                    k.mm(ps[0:64, 0:64], identf[0:64, 0:64], Tst[h][cur][:, :], True, False, [bc, b_T[h][cur]], [pb])
                    k.mm(ps[0:64, 0:64], TM[p][:, 1, rows], Usb[h][:, :], False, False, [b_TM[p], b_U[h]], [pb])
                    k.mm(ps[0:64, 0:64], TM[p][:, 2, rows], TM[p][:, 0, rows], False, True, [b_TM[p]], [pb])
                    if e == 0:
                        k.ts("dve", Tst[h][1 - cur][:, :], ps[0:64, 0:64], pcs[0:64, p, c:c + 1], None, ALU.mult, None, [pb, b_pc[p]], [b_T[h][1 - cur]])
                    else:
                        k.ts("dve", Tst[h][1 - cur][:, :], ps[0:64, 0:64], pcsO[:, p, c:c + 1], None, ALU.mult, None, [pb, b_pcO[p]], [b_T[h][1 - cur]])
            if stop == 4:
                k.finish_build(); return nc
            pbn, pbnb = k.bank()
            for p in range(NP):
                k.mm(pbn[0:64, 2 * p:2 * p + 2], RKt[:, p, cols], hs[:, :], True, True, [b_RK[p], bc], [pbnb])
            k.cp("act", bon[:, :], pbn[0:64, 0:HO], [pbnb], [b_bon])
            pg, pgb = k.bank()
            k.mm(pg[0:64, 0:CH], sga[:, cols], g2a[:, :], True, False, [b_lo, bc], [pgb])
            k.mm(pg[0:64, 0:CH], sgb[:, cols], g2b[:, :], False, True, [b_lo, bc], [pgb])
            if stop == 61:
                k.finish_build(); return nc
            yv = yps[0:64, 0:CH].rearrange("p (h d) -> p h d", d=64)
            k.cp("act", Y1[:, :], yps[0:64, 0:CH], [ypb], [b_Y1])
            k.red("dve", stt_[:, 0:HO], Y1[:, :].rearrange("p (h d) -> p h d", d=64), ALU.add, AX.X, [b_Y1], [b_stt])
            if stop == 615:
                k.finish_build(); return nc
            k.tt("dve", Y2[:, :], Y1[:, :], Y1[:, :], ALU.mult, [b_Y1], [b_Y2])
            k.red("dve", stt_[:, HO:2 * HO], Y2[:, :].rearrange("p (h d) -> p h d", d=64), ALU.add, AX.X, [b_Y2], [b_stt])
            if stop == 616:
                k.finish_build(); return nc
            k.ts("dve", stt_[:, 0:HO], stt_[:, 0:HO], 1.0 / 64, None, ALU.mult, None, [b_stt], [b_stt])
            k.tt("dve", stt_[:, 2 * HO:3 * HO], stt_[:, 0:HO], stt_[:, 0:HO], ALU.mult, [b_stt], [b_stt])
            k.stt("dve", stt_[:, HO:2 * HO], stt_[:, HO:2 * HO], 1.0 / 64, stt_[:, 2 * HO:3 * HO], ALU.mult, ALU.subtract, [b_stt], [b_stt])
            if stop == 617:
                k.finish_build(); return nc
            k.ts("dve", stt_[:, HO:2 * HO], stt_[:, HO:2 * HO], 64e-5, None, ALU.add, None, [b_stt], [b_stt])
            k.act(stt_[:, HO:2 * HO], stt_[:, HO:2 * HO], AF.Ln, [b_stt], [b_stt])
            k.act(stt_[:, HO:2 * HO], stt_[:, HO:2 * HO], AF.Exp, [b_stt], [b_stt], scale=-0.5)
            if stop == 62:
                k.finish_build(); return nc
            for h in range(HO):
                hsl = slice(h * 64, (h + 1) * 64)
                k.ts("dve", Y1[:, hsl], Y1[:, hsl], stt_[:, h:h + 1], stt_[:, HO + h:HO + h + 1], ALU.subtract, ALU.mult, [b_Y1, b_stt], [b_Y1])
            k.tt("dve", Y1[:, :], Y1[:, :], lg_b[:, :], ALU.mult, [b_Y1, b_par], [b_Y1])
            k.tt("dve", Y1[:, :], Y1[:, :], lb_b[:, :], ALU.add, [b_Y1, b_par], [b_Y1])
            for h in range(HO):
                p, e = h // 2, h % 2
                hsl = slice(h * 64, (h + 1) * 64)
                k.stt("dve", Y1[:, hsl], TM[p][:, 0, e * 64:(e + 1) * 64], bon[:, h:h + 1], Y1[:, hsl], ALU.mult, ALU.add, [b_TM[p], b_bon, b_Y1], [b_Y1])
            k.tt("dve", Y2[:, :], Y1[:, :], pg[0:64, 0:CH], ALU.mult, [b_Y1, pgb], [b_Y2])
            if stop == 63:
                k.finish_build(); return nc
            k.dma("sp", yb[t0 + c * 64: t0 + (c + 1) * 64, :], Y2[:, :], [b_Y2], [], b_Y2)
            if stop == 64 + cg:
                k.finish_build(); return nc
    for b_ in ybanks:
        k.unreserve(b_)
    k.finish_build()
    return nc


def build_gm(D, NT, AW, NG, k=None):
    k = k or KB(); nc, s = k.nc, k.s; B = s.buf
    DC = D // 128; NTT = NT // 128; NB = AW // 512 if AW >= 512 else 1; CW = min(512, AW)
    k.psum_banks()
    x = k.dram("x", [NT, D]); cvec = k.dram("cvec", [1, D]); wada2 = k.dram("wada2", [D, 2 * D]); bada2 = k.dram("bada2", [1, 2 * D])
    gpre = k.dram("gpre", [1, D]); ident = k.dram("ident", [128, 128])
    wu = k.dram("wu", [D, AW]); wv = k.dram("wv", [D, AW]); vgb = k.dram("vgb", [1, 2 * AW])
    ws = k.dram("ws", [NG, 128, 128]); bs = k.dram("bs", [1, NG * 128]); triu = k.dram("triu", [128, 128])
    ya = k.dram("ya", [NT, AW], kind="ExternalOutput")
    pre = Pre(k, D, wada2, bada2, cvec, gpre, ident, nwsl=3)
    identf = pre.identf; bc = pre.b_const
    hT = k.sb("hT", [128, DC, NT], BF16); b_hT = [B() for _ in range(NTT)]
    U = k.sb("U", [128, NTT, AW]); V = k.sb("V", [128, NTT, AW]); b_U = [B() for _ in range(NTT)]; b_V = [B() for _ in range(NTT)]
    wsT = k.sb("wsT", [128, NG, 128], BF16); tmpw = k.sb("tmpw", [128, 128]); b_tw = B(); tru = k.sb("tru", [128, 128])
    bs_c = k.sb("bs_c", [128, NG]); vg_b = k.sb("vg_b", [128, AW]); vb_b = k.sb("vb_b", [128, AW]); b_par = B()
    vnb = k.sb("vnb", [128, AW], BF16); b_vn = B(); st2 = k.sb("st2", [128, 4]); b_st2 = B(); yo = k.sb("yo", [128, AW]); b_yo = B()
    junk = k.sb("junk", [128, AW], BF16); b_junk = B()
    k.dma("sp", tru[:], triu[:, :], [], [bc], bc)
    for g in range(NG):
        k.dma("sp", tmpw[:], ws[g, :, :], [], [b_tw], b_tw)
        pt, pb = k.bank()
        k.mm(pt[:, 0:128], tmpw[:, :], identf[:, :], True, True, [b_tw, bc], [pb])
        k.tt("dve", wsT[:, g, :], pt[:, 0:128], tru[:, :], ALU.mult, [pb, bc], [b_par])
    pt, pb = k.bank()
    pre.col_from_dram(bs, NG * 128, pt, pb)
    k.cp("act", bs_c[:], pt[:, 0:NG], [pb], [b_par])
    for (dst, off) in ((vg_b, 0), (vb_b, AW)):
        done = 0
        while done < AW:
            w = min(512, AW - done)
            k.dma("sp", pre.rowf[0][0:1, 0:w], vgb[0:1, off + done: off + done + w], [], [pre.b_rowf[0]], pre.b_rowf[0])
            pt, pb = k.bank()
            k.mm(pt[:, 0:w], pre.ones_f[0:1, :], pre.rowf[0][0:1, 0:w], True, True, [bc, pre.b_rowf[0]], [pb])
            k.cp("act", dst[:, done:done + w], pt[:, 0:w], [pb], [b_par])
            done += w
    for t in range(NTT):
        pre.norm_transpose(x[t * 128:(t + 1) * 128, :], hT, t * 128, b_hT[t])
    for (wsrc, dst, bufs) in ((wu, U, b_U), (wv, V, b_V)):
        for n in range(AW // CW):
            w, wb = pre.next_wsl()
            pre.load_w(w, wb, wsrc[:, n * CW:(n + 1) * CW], CW)
            for t in range(NTT):
                pt, pb = k.bank()
                for kc in range(DC):
                    k.mm(pt[:, 0:CW], hT[:, kc, t * 128:(t + 1) * 128], w[:, kc, 0:CW], kc == 0, kc == DC - 1, [b_hT[t], wb], [pb])
                k.act(dst[:, t, n * CW:(n + 1) * CW], pt[:, 0:CW], AF.Gelu, [pb], [bufs[t]])
    for t in range(NTT):
        v = V[:, t, :]
        k.red("dve", st2[:, 0:1], v, ALU.add, AX.X, [b_V[t]], [b_st2])
        k.memset("dve", st2[:, 1:2], 0.0, [b_st2])
        k.act(junk[:, :], v, AF.Square, [b_V[t], b_st2], [b_junk, b_st2], accum=st2[:, 1:2])
        k.ts("dve", st2[:, 0:1], st2[:, 0:1], 1.0 / AW, None, ALU.mult, None, [b_st2], [b_st2])
        k.tt("dve", st2[:, 2:3], st2[:, 0:1], st2[:, 0:1], ALU.mult, [b_st2], [b_st2])
        k.stt("dve", st2[:, 1:2], st2[:, 1:2], 1.0 / AW, st2[:, 2:3], ALU.mult, ALU.subtract, [b_st2], [b_st2])
        k.ts("dve", st2[:, 1:2], st2[:, 1:2], 1e-5, None, ALU.add, None, [b_st2], [b_st2])
        k.act(st2[:, 1:2], st2[:, 1:2], AF.Ln, [b_st2], [b_st2])
        k.act(st2[:, 1:2], st2[:, 1:2], AF.Exp, [b_st2], [b_st2], scale=-0.5)
        k.ts("dve", v, v, st2[:, 0:1], st2[:, 1:2], ALU.subtract, ALU.mult, [b_V[t], b_st2], [b_V[t]])
        k.tt("dve", v, v, vg_b[:, :], ALU.mult, [b_V[t], b_par], [b_V[t]])
        k.tt("dve", vnb[:, :], v, vb_b[:, :], ALU.add, [b_V[t], b_par], [b_vn])
        for g4 in range(max(1, NG // 4)):
            pt, pb = k.bank()
            ng = min(4, NG)
            for q in range(ng):
                g = g4 * 4 + q
                k.mm(pt[:, q * 128:(q + 1) * 128], wsT[:, g, :], vnb[:, g * 128:(g + 1) * 128], True, True, [b_par, b_vn], [pb])
            for q in range(ng):
                g = g4 * 4 + q
                gs = slice(g * 128, (g + 1) * 128)
                k.stt("dve", yo[:, gs], pt[:, q * 128:(q + 1) * 128], bs_c[:, g:g + 1], U[:, t, gs], ALU.add, ALU.mult, [pb, b_par, b_U[t]], [b_yo])
        k.dma("sp", ya[t * 128:(t + 1) * 128, :], yo[:, :], [b_yo], [], b_yo)
    k.finish_build()
    return nc


NEG = -1.0e30


def build_mb(D, T, HO, SEG=512, k=None, xfn=None):
    k = k or KB(); nc, s = k.nc, k.s; B = s.buf
    DC = D // 128; DH = 128; BLK = 256; NBLK = T // BLK; NQT = T // 128; HC = HO * DH; NSEG = T // SEG; CW = min(512, HC)
    assert NBLK == 8
    k.psum_banks()
    x = k.dram("x", [T, D]); cvec = k.dram("cvec", [1, D]); wada2 = k.dram("wada2", [D, 2 * D]); bada2 = k.dram("bada2", [1, 2 * D])
    gpre = k.dram("gpre", [1, D]); ident = k.dram("ident", [128, 128])
    wq = k.dram("wq", [D, HC]); wk = k.dram("wk", [D, HC]); wv = k.dram("wv", [D, HC])
    slope = k.dram("slope", [1, 128]); kpos = k.dram("kpos", [128, T]); cmask = k.dram("cmask", [128, 128]); gmask = k.dram("gmask", [128, 64])
    o = k.dram("o", [T, HC], kind="ExternalOutput")
    pre = Pre(k, D, wada2, bada2, cvec, gpre, ident, nwsl=2)
    identb = pre.identb; bc = pre.b_const
    hTs = k.sb("hTs", [128, DC, SEG], BF16); b_hT = B()
    QF = k.sb("QF", [128, HO, T], BF16); KF = k.sb("KF", [128, HO, T], BF16); VT = k.sb("VT", [128, NQT, HC], BF16)
    b_Q = [B() for _ in range(HO)]; b_K = [B() for _ in range(HO)]; b_V = [B() for _ in range(NQT)]
    kp = k.sb("kp", [128, T]); cm = k.sb("cm", [128, 128]); gmc = k.sb("gmc", [128, 64]); slc = k.sb("slc", [128, 128])
    kmf = k.sb("kmf", [128, 8]); kmb = k.sb("kmb", [128, HO, 8], BF16); b_km = B()
    ali = k.sb("ali", [128, T]); b_ali = B()
    Ssb = k.sb("Ssb", [128, T]); b_S = B(); Pb = k.sb("Pb", [128, T], BF16); b_P = B()
    PT = k.sb("PT", [128, NQT, 128], BF16); b_PT = B()
    gsb = k.sb("gsb", [128, 8]); mx8 = k.sb("mx8", [128, 8]); selb = k.sb("selb", [128, 8]); b_g = B()
    sm = k.sb("sm", [128, 4]); b_sm = B(); osb = [k.sb("osb%d" % i, [128, 128]) for i in range(2)]; b_o = [B(), B()]
    for (dst, src) in ((kp, kpos), (cm, cmask), (gmc, gmask)):
        k.dma("sp", dst[:], src[:, :], [], [bc], bc)
    k.dma("sp", pre.rowf[0][0:1, 0:128], slope[0:1, :], [], [pre.b_rowf[0]], pre.b_rowf[0])
    pt, pb = k.bank()
    k.mm(pt[:, 0:128], pre.ones_f[0:1, :], pre.rowf[0][0:1, 0:128], True, True, [bc, pre.b_rowf[0]], [pb])
    k.cp("act", slc[:, :], pt[:, 0:128], [pb], [bc])
    for sg_ in range(NSEG):
        t0 = sg_ * SEG
        for tq in range(SEG // 128):
            xsrc_ = xfn(sg_ * (SEG // 128) + tq) if xfn is not None else x[t0 + tq * 128: t0 + (tq + 1) * 128, :]
            pre.norm_transpose(xsrc_, hTs, tq * 128, b_hT)
        for (wsrc, dst, bufs, sc_) in ((wq, QF, b_Q, DH ** -0.5), (wk, KF, b_K, None)):
            for n in range(HC // CW):
                w, wb = pre.next_wsl()
                pre.load_w(w, wb, wsrc[:, n * CW:(n + 1) * CW], CW)
                for hh in range(CW // 128):
                    h = n * (CW // 128) + hh
                    pt, pb = k.bank()
                    for kc in range(DC):
                        k.mm(pt[:, 0:SEG], w[:, kc, hh * 128:(hh + 1) * 128], hTs[:, kc, :], kc == 0, kc == DC - 1, [wb, b_hT], [pb])
                    if sc_ is not None:
                        k.act(dst[:, h, t0:t0 + SEG], pt[:, 0:SEG], AF.Copy, [pb], [bufs[h]], scale=sc_)
                    else:
                        k.cp("dve", dst[:, h, t0:t0 + SEG], pt[:, 0:SEG], [pb], [bufs[h]])
        for n in range(HC // CW):
            w, wb = pre.next_wsl()
            pre.load_w(w, wb, wv[:, n * CW:(n + 1) * CW], CW)
            for tq in range(SEG // 128):
                tt_ = sg_ * (SEG // 128) + tq
                pt, pb = k.bank()
                for kc in range(DC):
                    k.mm(pt[:, 0:CW], hTs[:, kc, tq * 128:(tq + 1) * 128], w[:, kc, 0:CW], kc == 0, kc == DC - 1, [wb, b_hT], [pb])
                if tq % 2 == 0:
                    k.cp("act", VT[:, tt_, n * CW:(n + 1) * CW], pt[:, 0:CW], [pb], [b_V[tt_]])
                else:
                    k.cp("dve", VT[:, tt_, n * CW:(n + 1) * CW], pt[:, 0:CW], [pb], [b_V[tt_]])
    for h in range(HO):
        k.red("dve", kmf[:, :], KF[:, h, :].rearrange("p (n s) -> p n s", s=BLK), ALU.add, AX.X, [b_K[h]], [b_km])
        k.ts("dve", kmb[:, h, :], kmf[:, :], 1.0 / BLK, None, ALU.mult, None, [b_km], [b_km])
    it = 0
    for h in range(HO):
        k.ts("dve", ali[:, :], kp[:, :], slc[:, h:h + 1], None, ALU.mult, None, [bc], [b_ali])
        for qt in range(NQT):
            qb = qt // 2; nk = (qt + 1) * 128
            ql = QF[:, h, qt * 128:(qt + 1) * 128]
            pg, pgb = k.bank()
            k.mm(pg[:, 0:8], ql, kmb[:, h, :], True, True, [b_Q[h], b_km], [pgb])
            k.tt("dve", gsb[:, :], pg[:, 0:8], gmc[:, qb * 8:(qb + 1) * 8], ALU.add, [pgb, bc], [b_g])
            k.max8(mx8[:, :], gsb[:, :], [b_g], [b_g])
            k.ts("dve", selb[:, :], gsb[:, :], mx8[:, 2:3], 1.0, ALU.is_ge, ALU.subtract, [b_g], [b_g])
            k.ts("dve", selb[:, :], selb[:, :], 1.0e30, None, ALU.mult, None, [b_g], [b_g])
            for kg in range((nk + 511) // 512):
                w_ = min(512, nk - kg * 512)
                ps, pb = k.bank()
                k.mm(ps[:, 0:w_], ql, KF[:, h, kg * 512: kg * 512 + w_], True, True, [b_Q[h], b_K[h]], [pb])
                for kt in range(kg * 4, kg * 4 + w_ // 128):
                    n = kt // 2
                    lo = (kt - kg * 4) * 128
                    ks = slice(kt * 128, (kt + 1) * 128)
                    if n < qb:
                        k.stt("dve", Ssb[:, ks], ps[:, lo:lo + 128], selb[:, n:n + 1], ali[:, ks], ALU.add, ALU.add, [pb, b_g, b_ali], [b_S])
                    else:
                        k.tt("dve", Ssb[:, ks], ps[:, lo:lo + 128], ali[:, ks], ALU.add, [pb, b_ali], [b_S])
                        if kt == qt:
                            k.tt("dve", Ssb[:, ks], Ssb[:, ks], cm[:, :], ALU.add, [b_S, bc], [b_S])
            k.red("dve", sm[:, 0:1], Ssb[:, 0:nk], ALU.max, AX.X, [b_S], [b_sm])
            k.ts("dve", sm[:, 0:1], sm[:, 0:1], -1.0, None, ALU.mult, None, [b_sm], [b_sm])
            k.memset("dve", sm[:, 1:2], 0.0, [b_sm])
            k.act(Pb[:, 0:nk], Ssb[:, 0:nk], AF.Exp, [b_S, b_sm], [b_P, b_sm], bias=sm[:, 0:1], accum=sm[:, 1:2])
            k.recip("dve", sm[:, 2:3], sm[:, 1:2], [b_sm], [b_sm])
            for g4 in range((qt + 4) // 4):
                nq = min(4, qt + 1 - g4 * 4)
                pt, pb = k.bank()
                for q in range(nq):
                    kt = g4 * 4 + q
                    k.mm(pt[:, q * 128:(q + 1) * 128], Pb[:, kt * 128:(kt + 1) * 128], identb[:, :], True, True, [b_P, bc], [pb])
                src = pt[:, 0:nq * 128].rearrange("p (a b) -> p a b", a=nq)
                if g4 % 2 == 0:
                    k.cp("act", PT[:, g4 * 4:g4 * 4 + nq, :], src, [pb], [b_PT])
                else:
                    k.cp("dve", PT[:, g4 * 4:g4 * 4 + nq, :], src, [pb], [b_PT])
            po, pob = k.bank()
            for kt in range(qt + 1):
                k.mm(po[:, 0:128], PT[:, kt, :], VT[:, kt, h * 128:(h + 1) * 128], kt == 0, kt == qt, [b_PT, b_V[kt]], [pob])
            j = it % 2; it += 1
            k.ts("dve", osb[j][:, :], po[:, 0:128], sm[:, 2:3], None, ALU.mult, None, [pob, b_sm], [b_o[j]])
            k.dma("sp", o[qt * 128:(qt + 1) * 128, h * 128:(h + 1) * 128], osb[j][:, :], [b_o[j]], [], b_o[j])
    k.finish_build()
    return nc


def _ag_chunks(k, loc, rows, cols, name, groups, cbuf):
    rpc = (1 << 20) // (cols * 4)
    n = rows // rpc
    gs = [k.scratch("%s_%d" % (name, i), [2 * rpc, cols]) for i in range(n)]
    for i in range(n):
        k.allgather(gs[i][:, :], loc[i * rpc:(i + 1) * rpc, :], groups, [], [], cbuf)
    return gs, rpc


def _grow(gs, rpc, r, tau):
    ci = tau // rpc; w = tau % rpc
    return gs[ci][r * rpc + w: r * rpc + w + 128, :]


def build_fused(D, F, T, HOA, HOM, AW, NG, NCORES=8, SEG=512, HCF=256, TN=512):
    k = KB(); k.fused = True
    NTo = T // 2; CH = 64 * HOA; HC = 128 * HOM
    groups = [[2 * i, 2 * i + 1] for i in range(NCORES // 2)]
    k.psum_banks()
    ya_loc = k.scratch("ya_loc", [NTo, AW]); yb_loc = k.scratch("yb_loc", [T, CH])
    x1loc = k.scratch("x1loc", [NTo, D]); o_loc = k.scratch("o_loc", [T, HC])
    xmid0 = k.scratch("xmid0", [NTo, D]); xmid1 = k.scratch("xmid1", [NTo, D])
    cb = k.s.buf()
    k.prefix = "gm_"; k.alias["gm_ya"] = ya_loc
    build_gm(D, NTo, AW, NG, k=k); k.end_phase()
    k.prefix = "rw_"; k.alias["rw_yb"] = yb_loc
    build_rw(D, T, HOA, SEG, k=k); k.end_phase()
    ybg, rp0 = _ag_chunks(k, yb_loc, T, CH, "ybg", groups, cb); k.s.barrier()
    k.prefix = "b0_"; k.alias["b0_xmid"] = xmid0; k.alias["b0_xout"] = x1loc
    ys0 = [(0, AW, lambda t: ya_loc[t * 128:(t + 1) * 128, :], None)]
    for r in range(2):
        ys0.append((AW + r * CH, CH, (lambda t, r=r: _grow(ybg, rp0, r, t * 128)), (lambda t, r=r: _grow(ybg, rp0, r, NTo + t * 128))))
    build_bd(D, F, NTo, HCF, TN, k=k, ysrc=ys0); k.end_phase()
    x1g, rp1 = _ag_chunks(k, x1loc, NTo, D, "x1g", groups, cb); k.s.barrier()
    k.prefix = "mb_"; k.alias["mb_o"] = o_loc; k.alias["mb_x"] = x1loc
    NTT = NTo // 128
    build_mb(D, T, HOM, SEG, k=k, xfn=lambda tt: _grow(x1g, rp1, tt // NTT, (tt % NTT) * 128)); k.end_phase()
    ogs, rp2 = _ag_chunks(k, o_loc, T, HC, "og", groups, cb); k.s.barrier()
    k.prefix = "b1_"; k.alias["b1_xmid"] = xmid1; k.alias["b1_xin"] = x1loc
    ys1 = []
    for r in range(2):
        ys1.append((r * HC, HC, (lambda t, r=r: _grow(ogs, rp2, r, t * 128)), (lambda t, r=r: _grow(ogs, rp2, r, NTo + t * 128))))
    build_bd(D, F, NTo, HCF, TN, k=k, ysrc=ys1); k.end_phase()
    k.s.finish()
    return k.nc


from concourse.bass_utils import run_bass_kernel_spmd

_D = 2048; _F = 5632; _S = 2048; _NB = 4; _NC = 8
_PROG = {}


def _c(a):
    return np.ascontiguousarray(a, dtype=np.float32)


def kernel(x, c, w_ada, b_ada, g_pre_mix, g_post_mix, g_pre_ffn, g_post_ffn, w_ffn_in, w_ffn_out, w_in_ab, w_out_ab,
           a_v_gain, a_v_bias, a_w_s, a_b_s, b_mu, b_w0, b_w2, b_a0, b_a2, b_g2, b_k_k, b_k_a, b_r_k, b_lnx_gain,
           b_lnx_bias, w_qkv, w_o):
    f = np.float32
    x = np.asarray(x, f); c = np.asarray(c, f); w_ada = np.asarray(w_ada, f); b_ada = np.asarray(b_ada, f)
    D = _D
    if "f" not in _PROG:
        _PROG["f"] = build_fused(_D, _F, _S, 8, 8, 1024, 8)
    nc = _PROG["f"]
    ident = np.eye(128, dtype=f)
    own = lambda hh: slice(hh * 1024, (hh + 1) * 1024)
    su = np.triu(np.ones((64, 64)), 1); iu = np.triu(np.ones((64, 64)), 0)
    mask5 = np.concatenate([su, iu, su, iu, su.T], axis=1).astype(f)
    blk = np.kron(np.eye(2), np.ones((64, 64))).astype(f)
    tri2 = (blk * np.triu(np.ones((128, 128)))).astype(f)
    hsel = np.kron(np.eye(2), np.ones((64, 1))).astype(f)
    triu = np.triu(np.ones((128, 128))).astype(f)
    kpos = np.tile(np.arange(_S, dtype=f)[None, :], (128, 1))
    cmask = np.where(np.arange(128)[None, :] <= np.arange(128)[:, None], 0.0, -1e30).astype(f)
    gmask = np.zeros((128, 64), f)
    for qb in range(8):
        for n in range(8):
            gmask[:, qb * 8 + n] = 0.0 if n < qb else -1e30
    w_in_ab0 = np.asarray(w_in_ab[0], f)
    wada0_a = _c(w_ada[0][:, 0:2 * D]); bada0_a = _c(b_ada[0][None, 0:2 * D])
    wada0_b = _c(w_ada[0][:, 2 * D:6 * D]); bada0_b = _c(b_ada[0][None, 2 * D:6 * D])
    wada1_a = _c(w_ada[1][:, 0:2 * D]); bada1_a = _c(b_ada[1][None, 0:2 * D])
    wada1_b = _c(w_ada[1][:, 2 * D:6 * D]); bada1_b = _c(b_ada[1][None, 2 * D:6 * D])
    wu = _c(w_in_ab0[:, 0:1024]); wvv = _c(w_in_ab0[:, 1024:2048])
    vgb = _c(np.concatenate([np.asarray(a_v_gain[0]), np.asarray(a_v_bias[0])])[None, :])
    ws = _c(a_w_s[0]); bs = _c(np.asarray(a_b_s[0]).reshape(1, 1024))
    gpre0 = _c(np.asarray(g_pre_mix[0])[None, :]); gpre1 = _c(np.asarray(g_pre_mix[1])[None, :])
    mu = np.asarray(b_mu[0], f); rk = np.asarray(b_r_k[0], f).reshape(1024)
    wqkv = np.asarray(w_qkv[0], f)
    shared = {}
    rwp = []; mbp = []
    for hh in range(2):
        cs = slice(hh * 512, (hh + 1) * 512)
        mu_cat = np.zeros((1, 15 * 128), f)
        mu_cat[0, 0:512] = mu[0:1024][cs]; mu_cat[0, 512:1024] = mu[1024:2048][cs]; mu_cat[0, 1024:1536] = mu[2048:3072][cs]
        mu_cat[0, 1536:1536 + 288] = mu[3072:3360]
        pvec = np.concatenate([np.asarray(b_w0[0], f)[cs], np.asarray(b_a0[0], f)[cs], np.asarray(b_k_k[0], f)[cs],
                               np.asarray(b_k_a[0], f)[cs], rk[cs]])[None, :]
        lnx = np.concatenate([np.asarray(b_lnx_gain[0], f)[cs], np.asarray(b_lnx_bias[0], f)[cs]])[None, :]
        rwp.append(dict(rw_wr=_c(w_in_ab0[:, 2048:3072][:, cs]), rw_wk=_c(w_in_ab0[:, 3072:4096][:, cs]), rw_wv=_c(w_in_ab0[:, 4096:5120][:, cs]),
                        rw_mu_cat=mu_cat, rw_pvec=_c(pvec), rw_lnx=_c(lnx), rw_w2=_c(np.asarray(b_w2[0], f)[:, cs]),
                        rw_a2=_c(np.asarray(b_a2[0], f)[:, cs]), rw_g2=_c(np.asarray(b_g2[0], f)[:, cs])))
        cs2 = slice(hh * 1024, (hh + 1) * 1024)
        sl = np.zeros((1, 128), f)
        sl[0, 0:8] = 2.0 ** (-8.0 * (np.arange(8) + hh * 8 + 1) / 16.0)
        mbp.append(dict(mb_wq=_c(wqkv[:, 0:2048][:, cs2]), mb_wk=_c(wqkv[:, 2048:4096][:, cs2]), mb_wv=_c(wqkv[:, 4096:6144][:, cs2]), mb_slope=sl))
    wl = _c(w_in_ab0[:, 5120:5408])
    com = dict(
        gm_wada2=wada0_a, gm_bada2=bada0_a, gm_gpre=gpre0, gm_ident=ident, gm_wu=wu, gm_wv=wvv, gm_vgb=vgb, gm_ws=ws, gm_bs=bs, gm_triu=triu,
        rw_wada2=wada0_a, rw_bada2=bada0_a, rw_gpre=gpre0, rw_ident=ident, rw_wl=wl, rw_mask5=mask5, rw_tri2=tri2, rw_bones=blk, rw_hsel=hsel,
        b0_wada=wada0_b, b0_bada=bada0_b, b0_wout=_c(w_out_ab[0]), b0_gpost=_c(np.asarray(g_post_mix[0])[None]), b0_gpre=_c(np.asarray(g_pre_ffn[0])[None]),
        b0_gpost2=_c(np.asarray(g_post_ffn[0])[None]), b0_wfi=_c(w_ffn_in[0]), b0_wfo=_c(w_ffn_out[0]), b0_ident=ident,
        mb_wada2=wada1_a, mb_bada2=bada1_a, mb_gpre=gpre1, mb_ident=ident, mb_kpos=kpos, mb_cmask=cmask, mb_gmask=gmask,
        b1_wada=wada1_b, b1_bada=bada1_b, b1_wout=_c(w_o[0]), b1_gpost=_c(np.asarray(g_post_mix[1])[None]), b1_gpre=_c(np.asarray(g_pre_ffn[1])[None]),
        b1_gpost2=_c(np.asarray(g_post_ffn[1])[None]), b1_wfi=_c(w_ffn_in[1]), b1_wfo=_c(w_ffn_out[1]), b1_ident=ident)
    maps = []
    for core in range(_NC):
        b, hh = core // 2, core % 2
        sel = np.zeros((128, 2), f); sel[:, hh] = 1.0
        cv = _c(c[b][None, :])
        m = dict(com)
        m.update(rwp[hh]); m.update(mbp[hh])
        m.update(gm_x=_c(x[b, own(hh)]), gm_cvec=cv, rw_x=_c(x[b]), rw_cvec=cv, b0_xin=_c(x[b, own(hh)]), b0_cvec=cv, b0_sel=sel,
                 mb_cvec=cv, b1_cvec=cv, b1_sel=sel)
        maps.append(m)
    res = run_bass_kernel_spmd(nc, maps, core_ids=list(range(_NC))).results
    out = np.zeros((_NB, _S, D), f)
    for core in range(_NC):
        b, hh = core // 2, core % 2
        out[b, own(hh)] = res[core]["b1_xout"]
    return out
```

```python
import numpy as np
import concourse.bass as bass
import concourse.mybir as mybir

F32 = mybir.dt.float32
BF16 = mybir.dt.bfloat16
AF = mybir.ActivationFunctionType
ALU = mybir.AluOpType
AX = mybir.AxisListType


class Buf:
    __slots__ = ("name", "lw", "rd", "dsem", "demit")

    def __init__(self, name):
        self.name = name
        self.lw = None
        self.rd = []
        self.dsem = {}
        self.demit = {}


class Op:
    __slots__ = ("eng", "emit", "deps", "dbuf", "needed", "sig", "inc", "barrier")

    def __init__(self, eng, emit, deps, dbuf, inc=16):
        self.eng = eng
        self.emit = emit
        self.deps = deps
        self.dbuf = dbuf
        self.needed = False
        self.sig = 0
        self.inc = inc
        self.barrier = False


class Sch:
    def __init__(self, nc, same_engine_sync=True):
        self.nc = nc
        self.E = {"pe": nc.tensor, "act": nc.scalar, "dve": nc.vector, "pool": nc.gpsimd, "sp": nc.sync}
        self.ops = []
        self.same = same_engine_sync
        self.nbuf = 0

    def buf(self, name=None):
        self.nbuf += 1
        return Buf(name or "b%d" % self.nbuf)

    def barrier(self):
        op = Op("sp", None, [], None)
        op.barrier = True
        self.ops.append(op)

    def add(self, eng, emit, reads=(), writes=(), dma=None, inc=16):
        deps = []
        for b in reads:
            if b.lw is not None:
                deps.append(b.lw)
        for b in writes:
            if b.lw is not None:
                deps.append(b.lw)
            deps.extend(b.rd)
        op = Op(eng, emit, deps, dma, inc)
        for d in deps:
            if d.dbuf is None and d.eng == eng and (eng == "pe" or not self.same):
                continue
            d.needed = True
        for b in reads:
            b.rd.append(op)
        for b in writes:
            b.lw = op
            b.rd = []
        self.ops.append(op)
        return op

    def finish(self):
        nc = self.nc
        esem = {k: nc.alloc_semaphore("es_" + k) for k in self.E}
        cnt = {k: 0 for k in self.E}
        last = {}
        for op in self.ops:
            if op.barrier:
                for o in last.values():
                    o.needed = True
            elif op.dbuf is None:
                last[op.eng] = op
        for op in self.ops:
            if op.barrier:
                continue
            if op.dbuf is None and op.needed:
                cnt[op.eng] += 1
                op.sig = cnt[op.eng]
        waited = {k: {} for k in self.E}
        dbufs = []
        nsem = len(esem)
        lastsig = {k: 0 for k in self.E}
        for op in self.ops:
            if op.barrier:
                for en, e in self.E.items():
                    w = waited[en]
                    for x in self.E:
                        if x == en or lastsig[x] == 0:
                            continue
                        key = ("e", x)
                        if w.get(key, 0) < lastsig[x]:
                            e.wait_ge(esem[x], lastsig[x]); w[key] = lastsig[x]
                    for b, q in dbufs:
                        key = ("d", id(b), q)
                        if w.get(key, 0) < b.demit[q]:
                            e.wait_ge(b.dsem[q], b.demit[q]); w[key] = b.demit[q]
                continue
            eng = self.E[op.eng]
            need = {}
            for d in op.deps:
                if d.dbuf is not None:
                    key = ("d", id(d.dbuf), d.eng)
                    sem = d.dbuf.dsem[d.eng]
                    val = d.dbuf.demit[d.eng]
                else:
                    if d.eng == op.eng and (d.eng == "pe" or not self.same):
                        continue
                    key = ("e", d.eng)
                    sem = esem[d.eng]
                    val = d.sig
                if key not in need or need[key][1] < val:
                    need[key] = (sem, val)
            w = waited[op.eng]
            for key, (sem, val) in need.items():
                if w.get(key, 0) >= val:
                    continue
                eng.wait_ge(sem, val)
                w[key] = val
            inst = op.emit()
            if op.dbuf is not None:
                b = op.dbuf
                if op.eng not in b.dsem:
                    b.dsem[op.eng] = nc.alloc_semaphore("ds_%d" % nsem)
                    b.demit[op.eng] = 0
                    nsem += 1
                    dbufs.append((b, op.eng))
                inst.then_inc(b.dsem[op.eng], op.inc)
                b.demit[op.eng] += op.inc
            elif op.needed:
                inst.then_inc(esem[op.eng], 1)
                lastsig[op.eng] = op.sig
        for b, q in dbufs:
            nc.sync.wait_ge(b.dsem[q], b.demit[q])
        self.nsem = nsem
        return nsem


class KB:
    def __init__(self, name="k"):
        self.nc = bass.Bass("TRN2", target_bir_lowering=False)
        self.s = Sch(self.nc)
        self.nps = 0
        self.ps_banks = None
        self.prefix = ""
        self.alias = {}
        self.cms = []
        self.fused = False

    def dram(self, name, shape, dt=F32, kind="ExternalInput"):
        name = self.prefix + name
        if name in self.alias:
            return self.alias[name]
        return self.nc.dram_tensor(name, list(shape), dt, kind=kind).ap()

    def scratch(self, name, shape, dt=F32, shared=False):
        if shared:
            return self.nc.dram_tensor(name, list(shape), dt, addr_space="Shared").ap()
        return self.nc.dram_tensor(name, list(shape), dt).ap()

    def sb(self, name, shape, dt=F32):
        if not self.fused:
            return self.nc.alloc_sbuf_tensor(self.prefix + name, list(shape), dt)
        cm = self.nc.sbuf_tensor(self.prefix + name, list(shape), dt)
        t = cm.__enter__()
        self.cms.append(cm)
        return t

    def end_phase(self):
        self.s.barrier()
        for cm in reversed(self.cms):
            cm.__exit__(None, None, None)
        self.cms = []

    def finish_build(self):
        if not self.fused:
            self.s.finish()

    def allgather(self, out_ap, in_ap, groups, R, W, cbuf):
        nc = self.nc
        return self.s.add("pool", lambda: nc.gpsimd.collective_compute("AllGather", mybir.AluOpType.bypass, replica_groups=groups, ins=[in_ap], outs=[out_ap]), R, W, dma=cbuf, inc=1)

    def psum_banks(self):
        if self.ps_banks is not None:
            return
        self.ps_banks = []
        for i in range(8):
            t = self.nc.alloc_psum_tensor("psb%d" % i, [128, 512], F32)
            self.ps_banks.append((t, self.s.buf("ps%d" % i)))
        self.ps_i = 0

    def bank(self):
        n = len(self.ps_banks)
        b = self.ps_banks[self.ps_i % n]
        self.ps_i += 1
        return b

    def reserve(self):
        return self.ps_banks.pop()

    def unreserve(self, b):
        self.ps_banks.append(b)

    def mm(self, out, lhsT, rhs, start, stop, R, W):
        nc = self.nc
        return self.s.add("pe", lambda: nc.tensor.matmul(out, lhsT, rhs, start=start, stop=stop), R, W)

    def act(self, out, in_, func, R, W, bias=None, scale=None, accum=None):
        nc = self.nc
        kw = {}
        if bias is not None:
            kw["bias"] = bias
        if scale is not None:
            kw["scale"] = scale
        if accum is not None:
            kw["accum_out"] = accum
        return self.s.add("act", lambda: nc.scalar.activation(out=out, in_=in_, func=func, **kw), R, W)

    def _e(self, eng):
        return {"dve": self.nc.vector, "pool": self.nc.gpsimd}[eng]

    def tt(self, eng, out, in0, in1, op, R, W):
        e = self._e(eng)
        return self.s.add(eng, lambda: e.tensor_tensor(out=out, in0=in0, in1=in1, op=op), R, W)

    def ts(self, eng, out, in0, s1, s2, op0, op1, R, W, accum=None):
        e = self._e(eng)
        if op1 is None:
            return self.s.add(eng, lambda: e.tensor_scalar(out=out, in0=in0, scalar1=s1, scalar2=None, op0=op0), R, W)
        if accum is not None:
            return self.s.add(eng, lambda: e.tensor_scalar(out=out, in0=in0, scalar1=s1, scalar2=s2, op0=op0, op1=op1, accum_out=accum), R, W)
        return self.s.add(eng, lambda: e.tensor_scalar(out=out, in0=in0, scalar1=s1, scalar2=s2, op0=op0, op1=op1), R, W)

    def stt(self, eng, out, in0, scalar, in1, op0, op1, R, W):
        e = self._e(eng)
        return self.s.add(eng, lambda: e.scalar_tensor_tensor(out=out, in0=in0, scalar=scalar, in1=in1, op0=op0, op1=op1), R, W)

    def cp(self, eng, out, in_, R, W):
        if eng == "act":
            nc = self.nc
            return self.s.add("act", lambda: nc.scalar.copy(out=out, in_=in_), R, W)
        e = self._e(eng)
        return self.s.add(eng, lambda: e.tensor_copy(out=out, in_=in_), R, W)

    def red(self, eng, out, in_, op, axis, R, W):
        e = self._e(eng)
        return self.s.add(eng, lambda: e.tensor_reduce(out=out, in_=in_, axis=axis, op=op), R, W)

    def memset(self, eng, ap, val, W):
        e = self._e(eng)
        return self.s.add(eng, lambda: e.memset(ap, val), (), W)

    def dma(self, q, out, in_, R, W, dbuf):
        e = {"sp": self.nc.sync, "pool": self.nc.gpsimd, "act": self.nc.scalar}[q]
        return self.s.add(q, lambda: e.dma_start(out=out, in_=in_), R, W, dma=dbuf)


def _kb_recip(self, eng, out, in_, R, W):
    e = self._e(eng)
    return self.s.add(eng, lambda: e.reciprocal(out=out, in_=in_), R, W)


KB.recip = _kb_recip


def _kb_max8(self, out, in_, R, W):
    nc = self.nc
    return self.s.add("dve", lambda: nc.vector.max(out=out, in_=in_), R, W)


KB.max8 = _kb_max8


EPS = 1e-6


def row_to_col(k, row_t, row_b, ncols, ps_ap, ps_b, one_f, b_const):
    for i in range(ncols):
        k.mm(ps_ap[:, i:i + 1], row_t[0:1, i * 128:(i + 1) * 128], one_f[0:1, 0:1], True, True, [row_b, b_const], [ps_b])


class Pre:
    def __init__(self, k, D, wada2, bada2, cvec, gpre, ident, nwsl=3):
        self.k = k; s = k.s; B = s.buf
        self.D = D; DC = D // 128; self.DC = DC
        self.identf = k.sb("identf", [128, 128]); self.identb = k.sb("identb", [128, 128], BF16)
        self.ones_f = k.sb("ones_f", [1, 128]); self.ones_b = k.sb("ones_b", [1, 128], BF16)
        self.rowf = [k.sb("rowf%d" % i, [1, 512]) for i in range(2)]
        self.rowb = [k.sb("rowb%d" % i, [1, 512], BF16) for i in range(2)]
        self.condT_f = k.sb("condT_f", [128, DC]); self.condT_b = k.sb("condT_b", [128, DC], BF16)
        self.modc = k.sb("modc", [128, 2 * DC]); self.gpre_c = k.sb("gpre_c", [128, DC]); self.gs_c = k.sb("gs_c", [128, DC])
        self.wsl = [k.sb("wsl%d" % i, [128, DC, 512], BF16) for i in range(nwsl)]
        self.xt = k.sb("xt", [128, D]); self.xn = k.sb("xn", [128, D], BF16); self.st = k.sb("st", [128, 8])
        self.b_const = B(); self.b_rowf = [B(), B()]; self.b_rowb = [B(), B()]; self.b_cond = B(); self.b_modc = B()
        self.b_gpre = B(); self.b_gs = B(); self.b_wsl = [B() for _ in range(nwsl)]; self.b_xt = B(); self.b_xn = B(); self.b_st = B()
        self.nw = 0; self.nwsl = nwsl
        k.dma("sp", self.identf[:], ident[:, :], [], [self.b_const], self.b_const)
        k.cp("dve", self.identb[:], self.identf[:], [self.b_const], [self.b_const])
        k.memset("dve", self.ones_f[:], 1.0, [self.b_const]); k.memset("dve", self.ones_b[:], 1.0, [self.b_const])
        pt, pb = k.bank()
        self.col_from_dram(cvec, D, pt, pb)
        k.act(self.condT_f[:], pt[:, 0:DC], AF.Silu, [pb], [self.b_cond])
        k.cp("dve", self.condT_b[:], self.condT_f[:], [self.b_cond], [self.b_cond])
        pt, pb = k.bank()
        self.col_from_dram(gpre, D, pt, pb)
        k.cp("act", self.gpre_c[:], pt[:, 0:DC], [pb], [self.b_gpre])
        pc, pcb = k.bank()
        for j in range(2 * D // 512):
            w, wb = self.next_wsl()
            self.load_w(w, wb, wada2[:, j * 512:(j + 1) * 512], 512)
            r = j % 2
            k.dma("pool", self.rowb[r][0:1, :], bada2[0:1, j * 512:(j + 1) * 512], [], [self.b_rowb[r]], self.b_rowb[r])
            for q in range(4):
                col = j * 4 + q
                for kc in range(DC):
                    k.mm(pc[:, col:col + 1], w[:, kc, q * 128:(q + 1) * 128], self.condT_b[:, kc:kc + 1], kc == 0, False, [self.b_cond, wb], [pcb])
                k.mm(pc[:, col:col + 1], self.rowb[r][0:1, q * 128:(q + 1) * 128], self.ones_b[0:1, 0:1], False, True, [self.b_rowb[r], self.b_const], [pcb])
        k.cp("act", self.modc[:], pc[:, 0:2 * DC], [pcb], [self.b_modc])
        k.stt("dve", self.gs_c[:], self.modc[:, DC:2 * DC], 1.0, self.gpre_c[:], ALU.add, ALU.mult, [self.b_modc, self.b_gpre], [self.b_gs])

    def next_wsl(self):
        i = self.nw % self.nwsl; self.nw += 1
        return self.wsl[i], self.b_wsl[i]

    def load_w(self, dst, dbuf, src_cols, width, off=0):
        self.k.dma("pool", dst[:, :, off:off + width], src_cols.rearrange("(kc p) n -> p kc n", p=128), [], [dbuf], dbuf)

    def col_from_dram(self, vec, n, pt, pb, col0=0):
        k = self.k
        done = 0
        j = 0
        while done < n:
            w = min(512, n - done)
            r = j % 2
            k.dma("sp", self.rowf[r][0:1, 0:w], vec[0:1, done:done + w], [], [self.b_rowf[r]], self.b_rowf[r])
            row_to_col(k, self.rowf[r], self.b_rowf[r], w // 128, pt[:, col0 + done // 128: col0 + (done + w) // 128], pb, self.ones_f, self.b_const)
            done += w; j += 1

    def rstd_of(self, src_ap, src_bufs, col, n):
        k = self.k; st = self.st; b_st = self.b_st
        k.memset("dve", st[:, col:col + 1], 0.0, [b_st])
        k.act(self.xn[:, 0:n], src_ap, AF.Square, src_bufs + [b_st], [self.b_xn, b_st], accum=st[:, col:col + 1])
        k.ts("dve", st[:, col:col + 1], st[:, col:col + 1], 1.0 / n, EPS, ALU.mult, ALU.add, [b_st], [b_st])
        k.act(st[:, col:col + 1], st[:, col:col + 1], AF.Ln, [b_st], [b_st])
        k.act(st[:, col:col + 1], st[:, col:col + 1], AF.Exp, [b_st], [b_st], scale=-0.5)

    def norm_transpose(self, x_dram_tile, hT, hcol0, b_h):
        k = self.k; D = self.D; DC = self.DC
        k.dma("sp", self.xt[:], x_dram_tile, [], [self.b_xt], self.b_xt)
        self.rstd_of(self.xt[:], [self.b_xt], 1, D)
        k.act(self.xn[:], self.xt[:], AF.Copy, [self.b_xt, self.b_st], [self.b_xn], scale=self.st[:, 1:2])
        for g in range(max(1, DC // 4)):
            nq = min(4, DC)
            pt, pb = k.bank()
            for q in range(nq):
                kc = g * 4 + q
                k.mm(pt[:, q * 128:(q + 1) * 128], self.xn[:, kc * 128:(kc + 1) * 128], self.identb[:, :], True, True, [self.b_xn, self.b_const], [pb])
            for q in range(nq):
                kc = g * 4 + q
                o = hT[:, kc, hcol0:hcol0 + 128]
                i = pt[:, q * 128:(q + 1) * 128]
                if q % 2 == 0:
                    k.act(o, i, AF.Identity, [pb, self.b_gs, self.b_modc], [b_h], bias=self.modc[:, kc:kc + 1], scale=self.gs_c[:, kc:kc + 1])
                else:
                    k.ts("dve", o, i, self.gs_c[:, kc:kc + 1], self.modc[:, kc:kc + 1], ALU.mult, ALU.add, [pb, self.b_gs, self.b_modc], [b_h])


EPS = 1e-6


def row_to_col(k, row_t, row_b, ncols, ps_ap, ps_b, one_f, b_const):
    for i in range(ncols):
        k.mm(ps_ap[:, i:i + 1], row_t[0:1, i * 128:(i + 1) * 128], one_f[0:1, 0:1], True, True, [row_b, b_const], [ps_b])


def build_bd(D, F, NT, HC=256, TN=512, k=None, ysrc=None, out_kind="ExternalOutput"):
    k = k or KB()
    nc, s = k.nc, k.s
    DC = D // 128
    NTT = NT // 128
    NB = D // 512
    SUB = HC // 128
    TPB = TN // 128
    k.psum_banks()
    xin = k.dram("xin", [NT, D]); cvec = k.dram("cvec", [1, D])
    ycat = k.dram("ycat", [NT, D]) if ysrc is None else None
    seld = k.dram("sel", [128, 2]) if ysrc is not None else None
    wada = k.dram("wada", [D, 4 * D]); bada = k.dram("bada", [1, 4 * D])
    wout = k.dram("wout", [D, D]); gpost = k.dram("gpost", [1, D]); gpre = k.dram("gpre", [1, D])
    gpost2 = k.dram("gpost2", [1, D]); wfi = k.dram("wfi", [D, 2 * F]); wfo = k.dram("wfo", [F, D])
    ident = k.dram("ident", [128, 128])
    xmid = k.dram("xmid", [NT, D], kind="ExternalOutput")
    xout = k.dram("xout", [NT, D], kind=out_kind)
    identf = k.sb("identf", [128, 128]); identb = k.sb("identb", [128, 128], BF16)
    ones_f = k.sb("ones_f", [1, 128]); ones_b = k.sb("ones_b", [1, 128], BF16)
    zeros = k.sb("zeros", [128, 128])
    rowf = [k.sb("rowf%d" % i, [1, 512]) for i in range(2)]
    rowb = [k.sb("rowb%d" % i, [1, 512], BF16) for i in range(2)]
    condT_f = k.sb("condT_f", [128, DC]); condT_b = k.sb("condT_b", [128, DC], BF16)
    cond_rep = k.sb("cond_rep", [128, DC, 128], BF16)
    modc = k.sb("modc", [128, 2 * DC]); gpre_c = k.sb("gpre_c", [128, DC]); gsf_c = k.sb("gsf_c", [128, DC])
    gt_b = k.sb("gt_b", [128, D])
    wsl = [k.sb("wsl%d" % i, [128, DC, 512], BF16) for i in range(3)]
    wos = [k.sb("wos%d" % i, [128, SUB, D], BF16) for i in range(2)]
    acc = k.sb("acc", [128, NTT, D])
    hT = k.sb("hT", [128, DC, NT], BF16)
    xt = k.sb("xt", [128, D]); xn = k.sb("xn", [128, D], BF16)
    tmp = [k.sb("tmp%d" % i, [128, 512]) for i in range(2)]
    actT = [k.sb("actT%d" % i, [128, SUB, TN], BF16) for i in range(2)]
    sg = [k.sb("sg%d" % i, [128, TN]) for i in range(2)]
    st = k.sb("st", [128, 8])
    selt = k.sb("selt", [128, 2])
    B = s.buf
    b_const = B(); b_rowf = [B(), B()]; b_rowb = [B(), B()]; b_cond = B(); b_modc = B(); b_gpre = B(); b_gsf = B()
    b_gt = [B() for _ in range(NB)]
    b_wsl = [B() for _ in range(3)]; b_wos = [B() for _ in range(2)]
    b_acc = [[B() for _ in range(NB)] for _ in range(NTT)]
    b_hT = [B() for _ in range(NTT)]
    b_xt = B(); b_xn = B(); b_tmp = [B(), B()]; b_actT = [B(), B()]; b_sg = [B(), B()]; b_st = B()
    b_xmid = [B() for _ in range(NTT)]
    cnt = {"wsl": 0, "wos": 0, "row": 0, "tmp": 0, "ev": 0}

    def next_wsl():
        i = cnt["wsl"] % 3; cnt["wsl"] += 1
        return wsl[i], b_wsl[i]

    def load_w(dst, dbuf, src_cols, width, off=0):
        k.dma("pool", dst[:, :, off:off + width], src_cols.rearrange("(kc p) n -> p kc n", p=128), [], [dbuf], dbuf)

    k.dma("sp", identf[:], ident[:, :], [], [b_const], b_const)
    k.cp("dve", identb[:], identf[:], [b_const], [b_const])
    k.memset("dve", ones_f[:], 1.0, [b_const]); k.memset("dve", ones_b[:], 1.0, [b_const])
    k.memset("dve", zeros[:], 0.0, [b_const])
    one_f = ones_f
    if ysrc is not None:
        k.dma("sp", selt[:], seld[:, :], [], [b_const], b_const)

    pt, pb = k.bank()
    for j in range(D // 512):
        r = j % 2
        k.dma("sp", rowf[r][0:1, :], cvec[0:1, j * 512:(j + 1) * 512], [], [b_rowf[r]], b_rowf[r])
        row_to_col(k, rowf[r], b_rowf[r], 4, pt[:, j * 4:(j + 1) * 4], pb, one_f, b_const)
    k.act(condT_f[:], pt[:, 0:DC], AF.Silu, [pb], [b_cond])
    k.cp("dve", condT_b[:], condT_f[:], [b_cond], [b_cond])
    for kc in range(DC):
        k.ts("dve", cond_rep[:, kc, :], zeros[:], condT_f[:, kc:kc + 1], None, ALU.add, None, [b_cond, b_const], [b_cond])
    pt, pb = k.bank()
    for j in range(D // 512):
        r = j % 2
        k.dma("sp", rowf[r][0:1, :], gpre[0:1, j * 512:(j + 1) * 512], [], [b_rowf[r]], b_rowf[r])
        row_to_col(k, rowf[r], b_rowf[r], 4, pt[:, j * 4:(j + 1) * 4], pb, one_f, b_const)
    k.cp("act", gpre_c[:], pt[:, 0:DC], [pb], [b_gpre])

    def mod_bcast(col0, grow):
        for j in range(NB):
            w, wb = next_wsl()
            load_w(w, wb, wada[:, col0 + j * 512: col0 + (j + 1) * 512], 512)
            r = j % 2
            k.dma("pool", rowb[r][0:1, :], bada[0:1, col0 + j * 512: col0 + (j + 1) * 512], [], [b_rowb[r]], b_rowb[r])
            k.dma("sp", rowf[r][0:1, :], grow[0:1, j * 512:(j + 1) * 512], [], [b_rowf[r]], b_rowf[r])
            pa, pab = k.bank()
            for kc in range(DC):
                k.mm(pa[:, :], cond_rep[:, kc, :], w[:, kc, :], kc == 0, False, [b_cond, wb], [pab])
            k.mm(pa[:, :], ones_b[0:1, :], rowb[r][0:1, :], False, True, [b_const, b_rowb[r]], [pab])
            pg, pgb = k.bank()
            k.mm(pg[:, :], ones_f[0:1, :], rowf[r][0:1, :], True, True, [b_const, b_rowf[r]], [pgb])
            k.cp("act", tmp[r][:], pg[:, :], [pgb], [b_tmp[r]])
            k.tt("dve", gt_b[:, j * 512:(j + 1) * 512], pa[:, :], tmp[r][:], ALU.mult, [pab, b_tmp[r]], [b_gt[j]])

    mod_bcast(0, gpost)

    pc, pcb = k.bank()
    for j in range(2 * D // 512):
        w, wb = next_wsl()
        load_w(w, wb, wada[:, D + j * 512: D + (j + 1) * 512], 512)
        r = j % 2
        k.dma("pool", rowb[r][0:1, :], bada[0:1, D + j * 512: D + (j + 1) * 512], [], [b_rowb[r]], b_rowb[r])
        for q in range(4):
            col = j * 4 + q
            for kc in range(DC):
                k.mm(pc[:, col:col + 1], w[:, kc, q * 128:(q + 1) * 128], condT_b[:, kc:kc + 1], kc == 0, False, [b_cond, wb], [pcb])
            k.mm(pc[:, col:col + 1], rowb[r][0:1, q * 128:(q + 1) * 128], ones_b[0:1, 0:1], False, True, [b_rowb[r], b_const], [pcb])
    k.cp("act", modc[:], pc[:, 0:2 * DC], [pcb], [b_modc])
    k.stt("dve", gsf_c[:], modc[:, DC:2 * DC], 1.0, gpre_c[:], ALU.add, ALU.mult, [b_modc, b_gpre], [b_gsf])
    shf_c = modc

    def transpose_tile(t, modulate):
        for g in range(DC // 4 if DC >= 4 else 1):
            nq = min(4, DC)
            pt, pb = k.bank()
            for q in range(nq):
                kc = g * 4 + q
                k.mm(pt[:, q * 128:(q + 1) * 128], xn[:, kc * 128:(kc + 1) * 128], identb[:, :], True, True, [b_xn, b_const], [pb])
            if not modulate:
                src = pt[:, 0:nq * 128].rearrange("p (a b) -> p a b", a=nq)
                eng = "act" if g % 2 == 0 else "dve"
                k.cp(eng, hT[:, g * 4:g * 4 + nq, t * 128:(t + 1) * 128], src, [pb], [b_hT[t]])
            else:
                for q in range(nq):
                    kc = g * 4 + q
                    o = hT[:, kc, t * 128:(t + 1) * 128]
                    i = pt[:, q * 128:(q + 1) * 128]
                    if q % 2 == 0:
                        k.act(o, i, AF.Identity, [pb, b_gsf, b_modc], [b_hT[t]], bias=shf_c[:, kc:kc + 1], scale=gsf_c[:, kc:kc + 1])
                    else:
                        k.ts("dve", o, i, gsf_c[:, kc:kc + 1], shf_c[:, kc:kc + 1], ALU.mult, ALU.add, [pb, b_gsf, b_modc], [b_hT[t]])

    for t in range(NTT):
        if ysrc is None:
            k.dma("sp", xt[:], ycat[t * 128:(t + 1) * 128, :], [], [b_xt], b_xt)
        else:
            for (c0, wd, fa, fb) in ysrc:
                if fb is None:
                    k.dma("sp", xt[:, c0:c0 + wd], fa(t), [], [b_xt], b_xt)
                    continue
                for c1 in range(0, wd, 512):
                    w_ = min(512, wd - c1)
                    r = (c1 // 512) % 2
                    k.dma("sp", xt[:, c0 + c1:c0 + c1 + w_], fa(t)[:, c1:c1 + w_], [], [b_xt], b_xt)
                    k.dma("sp", tmp[r][:, 0:w_], fb(t)[:, c1:c1 + w_], [], [b_tmp[r]], b_tmp[r])
                    k.ts("dve", xt[:, c0 + c1:c0 + c1 + w_], xt[:, c0 + c1:c0 + c1 + w_], selt[:, 0:1], None, ALU.mult, None, [b_xt, b_const], [b_xt])
                    k.stt("dve", xt[:, c0 + c1:c0 + c1 + w_], tmp[r][:, 0:w_], selt[:, 1:2], xt[:, c0 + c1:c0 + c1 + w_], ALU.mult, ALU.add, [b_tmp[r], b_xt, b_const], [b_xt])
        k.cp("act", xn[:], xt[:], [b_xt], [b_xn])
        transpose_tile(t, False)

    for n in range(NB):
        w, wb = next_wsl()
        load_w(w, wb, wout[:, n * 512:(n + 1) * 512], 512)
        for t in range(NTT):
            pt, pb = k.bank()
            for kc in range(DC):
                k.mm(pt[:, :], hT[:, kc, t * 128:(t + 1) * 128], w[:, kc, :], kc == 0, kc == DC - 1, [b_hT[t], wb], [pb])
            eng = "act" if t % 2 == 0 else "dve"
            k.cp(eng, acc[:, t, n * 512:(n + 1) * 512], pt[:, :], [pb], [b_acc[t][n]])

    def rstd_of(src_ap, src_bufs, col):
        k.memset("dve", st[:, col:col + 1], 0.0, [b_st])
        k.act(xn[:], src_ap, AF.Square, src_bufs + [b_st], [b_xn, b_st], accum=st[:, col:col + 1])
        k.ts("dve", st[:, col:col + 1], st[:, col:col + 1], 1.0 / D, EPS, ALU.mult, ALU.add, [b_st], [b_st])
        k.act(st[:, col:col + 1], st[:, col:col + 1], AF.Ln, [b_st], [b_st])
        k.act(st[:, col:col + 1], st[:, col:col + 1], AF.Exp, [b_st], [b_st], scale=-0.5)

    for t in range(NTT):
        k.dma("sp", xt[:], xin[t * 128:(t + 1) * 128, :], [], [b_xt], b_xt)
        rstd_of(acc[:, t, :], b_acc[t], 0)
        k.stt("dve", acc[:, t, :], acc[:, t, :], st[:, 0:1], gt_b[:, :], ALU.mult, ALU.mult, b_acc[t] + b_gt + [b_st], b_acc[t])
        k.tt("dve", xt[:], xt[:], acc[:, t, :], ALU.add, [b_xt] + b_acc[t], [b_xt])
        k.dma("sp", xmid[t * 128:(t + 1) * 128, :], xt[:], [b_xt], [b_xmid[t]], b_xt)
        rstd_of(xt[:], [b_xt], 1)
        k.act(xn[:], xt[:], AF.Copy, [b_xt, b_st], [b_xn], scale=st[:, 1:2])
        transpose_tile(t, True)

    mod_bcast(3 * D, gpost2)

    blocks = [(hc, th) for hc in range(F // HC) for th in range(NT // TN)]
    wcur = {}

    def emit_gu(i):
        hc, th = blocks[i]
        if th == 0:
            w, wb = next_wsl()
            load_w(w, wb, wfi[:, hc * HC:(hc + 1) * HC], HC, 0)
            load_w(w, wb, wfi[:, F + hc * HC: F + (hc + 1) * HC], HC, HC)
            j = cnt["wos"] % 2; cnt["wos"] += 1
            k.dma("pool", wos[j][:, :, :], wfo[hc * HC:(hc + 1) * HC, :].rearrange("(s p) n -> p s n", p=128), [], [b_wos[j]], b_wos[j])
            wcur[hc] = (w, wb, wos[j], b_wos[j])
        w, wb, _, _ = wcur[hc]
        a = i % 2
        hbufs = [b_hT[th * TPB + q] for q in range(TPB)]
        for sub in range(SUB):
            pg, pgb = k.bank()
            for kc in range(DC):
                k.mm(pg[:, 0:TN], w[:, kc, sub * 128:(sub + 1) * 128], hT[:, kc, th * TN:(th + 1) * TN], kc == 0, kc == DC - 1, [wb] + hbufs, [pgb])
            pu, pub = k.bank()
            for kc in range(DC):
                k.mm(pu[:, 0:TN], w[:, kc, HC + sub * 128: HC + (sub + 1) * 128], hT[:, kc, th * TN:(th + 1) * TN], kc == 0, kc == DC - 1, [wb] + hbufs, [pub])
            r = sub % 2
            k.act(sg[r][:, :], pg[:, 0:TN], AF.Silu, [pgb], [b_sg[r]])
            k.tt("dve", actT[a][:, sub, :], sg[r][:, :], pu[:, 0:TN], ALU.mult, [b_sg[r], pub], [b_actT[a]])

    def emit_y(i):
        hc, th = blocks[i]
        _, _, wo, wob = wcur[hc]
        a = i % 2
        for tq in range(TPB):
            t = th * TPB + tq
            for n in range(NB):
                py, pyb = k.bank()
                for sub in range(SUB):
                    k.mm(py[:, :], actT[a][:, sub, tq * 128:(tq + 1) * 128], wo[:, sub, n * 512:(n + 1) * 512], sub == 0, sub == SUB - 1, [b_actT[a], wob], [pyb])
                dst = acc[:, t, n * 512:(n + 1) * 512]
                if hc == 0:
                    k.cp("dve", dst, py[:, :], [pyb], [b_acc[t][n]])
                else:
                    k.tt("dve", dst, dst, py[:, :], ALU.add, [pyb, b_acc[t][n]], [b_acc[t][n]])

    for i in range(len(blocks)):
        emit_gu(i)
        if i > 0:
            emit_y(i - 1)
    emit_y(len(blocks) - 1)

    for t in range(NTT):
        k.dma("sp", xt[:], xmid[t * 128:(t + 1) * 128, :], [b_xmid[t]], [b_xt], b_xt)
        rstd_of(acc[:, t, :], b_acc[t], 2)
        k.stt("dve", acc[:, t, :], acc[:, t, :], st[:, 2:3], gt_b[:, :], ALU.mult, ALU.mult, b_acc[t] + b_gt + [b_st], b_acc[t])
        k.tt("dve", xt[:], xt[:], acc[:, t, :], ALU.add, [b_xt] + b_acc[t], [b_xt])
        k.dma("sp", xout[t * 128:(t + 1) * 128, :], xt[:], [b_xt], [], b_xt)
    k.finish_build()
    return nc


def build_rw(D, T, HO, SEG=512, RDT=F32, stop=99, k=None):
    k = k or KB(); nc, s = k.nc, k.s; B = s.buf
    DC = D // 128; CH = 64 * HO; NP = HO // 2; NSEG = T // SEG; NCH = SEG // 64; NZ = 3 * NP + 3
    k.psum_banks()
    x = k.dram("x", [T, D]); cvec = k.dram("cvec", [1, D]); wada2 = k.dram("wada2", [D, 2 * D]); bada2 = k.dram("bada2", [1, 2 * D])
    gpre = k.dram("gpre", [1, D]); ident = k.dram("ident", [128, 128])
    wr = k.dram("wr", [D, CH]); wk = k.dram("wk", [D, CH]); wv = k.dram("wv", [D, CH]); wl = k.dram("wl", [D, 288])
    mu_cat = k.dram("mu_cat", [1, NZ * 128]); pvec = k.dram("pvec", [1, 5 * CH]); lnx = k.dram("lnx", [1, 2 * CH])
    w2 = k.dram("w2", [64, CH]); a2 = k.dram("a2", [64, CH]); g2 = k.dram("g2", [160, CH])
    mask5 = k.dram("mask5", [64, 320]); tri2 = k.dram("tri2", [128, 128]); bones = k.dram("bones", [128, 128]); hsel = k.dram("hsel", [128, 2])
    yb = k.dram("yb", [T, CH], kind="ExternalOutput")
    ybanks = [k.reserve(), k.reserve()]
    pre = Pre(k, D, wada2, bada2, cvec, gpre, ident, nwsl=2)
    identf = pre.identf; bc = pre.b_const
    hTs = k.sb("hTs", [128, DC, SEG], BF16); b_hT = B()
    Rt = k.sb("Rt", [128, NP, SEG]); Kt = k.sb("Kt", [128, NP, SEG]); Vt = k.sb("Vt", [128, NP, SEG], BF16)
    Rb = k.sb("Rb", [128, NP, SEG], BF16); Kb = k.sb("Kb", [128, NP, SEG], BF16)
    BTt = k.sb("BTt", [128, NP, SEG], BF16); ATt = k.sb("ATt", [128, NP, SEG], BF16); RKt = k.sb("RKt", [128, NP, SEG])
    b_Rb = [B() for _ in range(NP)]; b_Kb = [B() for _ in range(NP)]
    b_R = [B() for _ in range(NP)]; b_K = [B() for _ in range(NP)]; b_V = [B() for _ in range(NP)]
    b_BT = [B() for _ in range(NP)]; b_AT = [B() for _ in range(NP)]; b_RK = [B() for _ in range(NP)]
    zwa = k.sb("zwa", [128, SEG]); zga = k.sb("zga", [128, SEG]); zgb = k.sb("zgb", [32, SEG]); b_zl = [B(), B(), B()]
    twb = k.sb("twb", [128, SEG], BF16); sga = k.sb("sga", [128, SEG], BF16); sgb = k.sb("sgb", [32, SEG], BF16); b_lo = B()
    zraw = [k.sb("zraw%d" % i, [128, SEG + 1]) for i in range(2)]; b_zraw = [B(), B()]
    carry = k.sb("carry", [128, NZ]); b_carry = B()
    S = [k.sb("S%d" % i, [128, SEG]) for i in range(7)]; b_S = [B() for _ in range(7)]
    mu_c = k.sb("mu_c", [128, NZ]); omm_c = k.sb("omm_c", [128, NZ]); pv_c = k.sb("pv_c", [128, 5 * NP]); b_par = B()
    pcs = k.sb("pcs", [128, NP, NCH]); b_pc = [B() for _ in range(NP)]
    w2a2 = k.sb("w2a2", [128, CH], BF16); g2a = k.sb("g2a", [128, CH], BF16); g2b = k.sb("g2b", [32, CH], BF16)
    m5 = k.sb("m5", [64, 320]); tri = k.sb("tri", [128, 128]); bon1 = k.sb("bon1", [128, 128]); hs = k.sb("hs", [128, 2])
    lg_b = k.sb("lg_b", [64, CH]); lb_b = k.sb("lb_b", [64, CH])
    RtO = k.sb("RtO", [64, NP, SEG], BF16); KtO = k.sb("KtO", [64, NP, SEG], BF16); BTO = k.sb("BTO", [64, NP, SEG], BF16); ATO = k.sb("ATO", [64, NP, SEG], BF16)
    b_RO = [B() for _ in range(NP)]; b_KO = [B() for _ in range(NP)]; b_BO = [B() for _ in range(NP)]; b_AO = [B() for _ in range(NP)]
    pcsO = k.sb("pcsO", [64, NP, NCH]); b_pcO = [B() for _ in range(NP)]
    Tst = [[k.sb("Tst%d_%d" % (h, i), [64, 64]) for i in range(2)] for h in range(HO)]
    Tb = [[k.sb("Tb%d_%d" % (h, i), [64, 64], BF16) for i in range(2)] for h in range(HO)]
    Ttmp = [k.sb("Ttmp%d" % i, [64, 64]) for i in range(2)]; b_Ttmp = [B(), B()]
    b_T = [[B(), B()] for _ in range(HO)]
    TM = [k.sb("TM%d" % p, [64, 3, 128], BF16) for p in range(NP)]; b_TM = [B() for _ in range(NP)]
    NS = 2
    G = [k.sb("G%d" % i, [64, 320], BF16) for i in range(NS)]; b_G = [B() for _ in range(NS)]
    NTt = [k.sb("NT%d" % i, [64, 64], BF16) for i in range(NS)]; b_NT = [B() for _ in range(NS)]
    LP = [[k.sb("LP%d_%d" % (i, j), [64, 128], BF16) for j in range(2)] for i in range(NS)]; b_LP = [[B(), B()] for _ in range(NS)]
    Wsb = [k.sb("Wsb%d" % i, [64, 64], BF16) for i in range(NS)]; b_W = [B() for _ in range(NS)]
    Usb = [k.sb("Usb%d" % h, [64, 64], BF16) for h in range(HO)]; b_U = [B() for _ in range(HO)]
    Y1 = k.sb("Y1", [64, CH]); b_Y1 = B(); bon = k.sb("bon", [64, HO]); b_bon = B()
    stt_ = k.sb("stt_", [64, 4 * HO]); b_stt = B(); Y2 = k.sb("Y2", [64, CH]); b_Y2 = B()

    for (dst, src) in ((m5, mask5), (tri, tri2), (bon1, bones), (hs, hsel)):
        k.dma("sp", dst[:], src[:, :], [], [bc], bc)
    k.dma("pool", w2a2[0:64, :], w2[:, :], [], [bc], bc)
    k.dma("pool", w2a2[64:128, :], a2[:, :], [], [bc], bc)
    k.dma("pool", g2a[:, :], g2[0:128, :], [], [bc], bc)
    k.dma("pool", g2b[:, :], g2[128:160, :], [], [bc], bc)
    pt, pb = k.bank()
    pre.col_from_dram(mu_cat, NZ * 128, pt, pb)
    k.cp("act", mu_c[:], pt[:, 0:NZ], [pb], [b_par])
    k.ts("dve", omm_c[:], mu_c[:], -1.0, 1.0, ALU.mult, ALU.add, [b_par], [b_par])
    pt, pb = k.bank()
    pre.col_from_dram(pvec, 5 * CH, pt, pb)
    k.cp("act", pv_c[:], pt[:, 0:5 * NP], [pb], [b_par])
    for (dst, off) in ((lg_b, 0), (lb_b, CH)):
        done = 0
        while done < CH:
            w = min(512, CH - done)
            k.dma("sp", pre.rowf[0][0:1, 0:w], lnx[0:1, off + done: off + done + w], [], [pre.b_rowf[0]], pre.b_rowf[0])
            pt, pb = k.bank()
            k.mm(pt[0:64, 0:w], pre.ones_f[0:1, 0:64], pre.rowf[0][0:1, 0:w], True, True, [bc, pre.b_rowf[0]], [pb])
            k.cp("act", dst[:, done:done + w], pt[0:64, 0:w], [pb], [b_par])
            done += w
    k.memset("dve", carry[:], 0.0, [b_carry])
    for h in range(HO):
        k.memset("dve", Tst[h][0][:], 0.0, [b_T[h][0]])
        k.memset("dve", Tb[h][0][:], 0.0, [b_T[h][0]])
    W0, A0, KK, KA, RKc = [lambda p, i=i: pv_c[:, i * NP + p: i * NP + p + 1] for i in range(5)]

    if stop == 1:
        k.finish_build(); return nc
    zi = {"n": 0}

    def ztile(w, wb, c0, M, dst, dbufs, idx):
        ps, pb = k.bank()
        for kc in range(DC):
            k.mm(ps[0:M, 0:SEG], w[:, kc, c0:c0 + M], hTs[:, kc, :], kc == 0, kc == DC - 1, [wb, b_hT], [pb])
        r = zi["n"] % 2; zi["n"] += 1
        zr = zraw[r]; bz = b_zraw[r]
        k.cp("act", zr[0:M, 1:SEG + 1], ps[0:M, 0:SEG], [pb], [bz])
        k.cp("dve", zr[0:M, 0:1], carry[0:M, idx:idx + 1], [b_carry], [bz])
        k.ts("dve", S[0][0:M, :], zr[0:M, 0:SEG], mu_c[0:M, idx:idx + 1], None, ALU.mult, None, [bz, b_par], [b_S[0]])
        k.stt("dve", dst, zr[0:M, 1:SEG + 1], omm_c[0:M, idx:idx + 1], S[0][0:M, :], ALU.mult, ALU.add, [bz, b_par, b_S[0]], dbufs)
        k.cp("dve", carry[0:M, idx:idx + 1], zr[0:M, SEG:SEG + 1], [bz], [b_carry])

    for sg_ in range(NSEG):
        t0 = sg_ * SEG
        for tq in range(SEG // 128):
            pre.norm_transpose(x[t0 + tq * 128: t0 + (tq + 1) * 128, :], hTs, tq * 128, b_hT)
        for (wsrc, arr, bufs, zbase) in ((wr, Rt, b_R, 0), (wk, Kt, b_K, NP), (wv, Vt, b_V, 2 * NP)):
            w, wb = pre.next_wsl()
            pre.load_w(w, wb, wsrc[:, :], CH)
            for p in range(NP):
                ztile(w, wb, p * 128, 128, arr[:, p, :], [bufs[p]], zbase + p)
        w, wb = pre.next_wsl()
        pre.load_w(w, wb, wl[:, :], 288)
        ztile(w, wb, 0, 128, zwa[:, :], [b_zl[0]], 3 * NP)
        ztile(w, wb, 128, 128, zga[:, :], [b_zl[1]], 3 * NP + 1)
        ztile(w, wb, 256, 32, zgb[:, :], [b_zl[2]], 3 * NP + 2)
        k.act(twb[0:64, :], zwa[0:64, :], AF.Tanh, [b_zl[0]], [b_lo])
        k.cp("dve", twb[64:128, :], zwa[64:128, :], [b_zl[0]], [b_lo])
        k.act(sga[:, :], zga[:, :], AF.Sigmoid, [b_zl[1]], [b_lo])
        k.act(sgb[:, :], zgb[:, :], AF.Sigmoid, [b_zl[2]], [b_lo])
        if stop == 2:
            k.finish_build(); return nc
        for p in range(NP):
            cs = slice(p * 128, (p + 1) * 128)
            ps, pb = k.bank()
            k.mm(ps[:, 0:SEG], w2a2[0:64, cs], twb[0:64, :], True, True, [bc, b_lo], [pb])
            k.act(S[1][:, :], ps[:, 0:SEG], AF.Sigmoid, [pb, b_par], [b_S[1]], bias=W0(p))
            k.ts("dve", S[1][:, :], S[1][:, :], -0.6065306597126334, None, ALU.mult, None, [b_S[1]], [b_S[1]])
            ps, pb = k.bank()
            k.mm(ps[:, 0:SEG], w2a2[64:128, cs], twb[64:128, :], True, True, [bc, b_lo], [pb])
            k.act(S[2][:, :], ps[:, 0:SEG], AF.Sigmoid, [pb, b_par], [b_S[2]], bias=A0(p))
            k.ts("dve", S[3][:, :], Kt[:, p, :], KK(p), None, ALU.mult, None, [b_K[p], b_par], [b_S[3]])
            k.tt("dve", S[4][:, :], S[3][:, :], S[3][:, :], ALU.mult, [b_S[3]], [b_S[4]])
            ps, pb = k.bank()
            k.mm(ps[:, 0:SEG], bon1[:, :], S[4][:, :], True, True, [bc, b_S[4]], [pb])
            k.act(S[4][:, :], ps[:, 0:SEG], AF.Sqrt, [pb], [b_S[4]])
            k.ts("dve", S[4][:, :], S[4][:, :], 1e-12, None, ALU.max, None, [b_S[4]], [b_S[4]])
            k.recip("dve", S[4][:, :], S[4][:, :], [b_S[4]], [b_S[4]])
            k.tt("dve", S[3][:, :], S[3][:, :], S[4][:, :], ALU.mult, [b_S[3], b_S[4]], [b_S[3]])
            k.ts("dve", S[4][:, :], S[2][:, :], 1.0, KA(p), ALU.subtract, ALU.mult, [b_S[2], b_par], [b_S[4]])
            k.stt("dve", Kt[:, p, :], S[4][:, :], 1.0, Kt[:, p, :], ALU.add, ALU.mult, [b_S[4], b_K[p]], [b_K[p]])
            k.stt("dve", RKt[:, p, :], Rt[:, p, :], RKc(p), Kt[:, p, :], ALU.mult, ALU.mult, [b_R[p], b_K[p], b_par], [b_RK[p]])
            k.tt("dve", S[2][:, :], S[3][:, :], S[2][:, :], ALU.mult, [b_S[3], b_S[2]], [b_S[2]])
            pc_, pcb = k.bank()
            for q in range(SEG // 128):
                qs = slice(q * 128, (q + 1) * 128)
                pt, ptb = k.bank()
                k.mm(pt[:, 0:128], S[1][:, qs], identf[:, :], True, True, [b_S[1], bc], [ptb])
                k.cp("act", S[5][:, qs], pt[:, 0:128], [ptb], [b_S[5]])
                k.mm(pc_[:, qs], S[5][:, qs], tri[:, :], True, True, [b_S[5], bc], [pcb])
            k.cp("act", S[4][:, :], pc_[:, 0:SEG], [pcb], [b_S[4]])
            k.act(S[5][:, :], S[4][:, :], AF.Exp, [b_S[4]], [b_S[5]])
            k.tt("dve", Rb[:, p, :], Rt[:, p, :], S[5][:, :], ALU.mult, [b_R[p], b_S[5]], [b_Rb[p]])
            k.cp("dve", pcs[:, p, :], S[5][:, :].rearrange("p (c t) -> p c t", t=64)[:, :, 63], [b_S[5]], [b_pc[p]])
            k.act(S[6][:, :], S[4][:, :], AF.Exp, [b_S[4]], [b_S[6]], scale=-1.0)
            k.tt("dve", Kb[:, p, :], Kt[:, p, :], S[6][:, :], ALU.mult, [b_K[p], b_S[6]], [b_Kb[p]])
            k.tt("dve", BTt[:, p, :], S[2][:, :], S[6][:, :], ALU.mult, [b_S[2], b_S[6]], [b_BT[p]])
            k.tt("dve", S[4][:, :], S[4][:, :], S[1][:, :], ALU.subtract, [b_S[4], b_S[1]], [b_S[4]])
            k.act(S[4][:, :], S[4][:, :], AF.Exp, [b_S[4]], [b_S[4]])
            k.stt("dve", ATt[:, p, :], S[3][:, :], -1.0, S[4][:, :], ALU.mult, ALU.mult, [b_S[3], b_S[4]], [b_AT[p]])
            for (dst, src, bs_, bd_) in ((RtO, Rb, b_Rb, b_RO), (KtO, Kb, b_Kb, b_KO), (BTO, BTt, b_BT, b_BO), (ATO, ATt, b_AT, b_AO)):
                k.dma("sp", dst[:, p, :], src[64:128, p, :], [bs_[p]], [bd_[p]], bd_[p])
            k.dma("sp", pcsO[:, p, :], pcs[64:128, p, :], [b_pc[p]], [b_pcO[p]], b_pcO[p])
        if stop == 3:
            k.finish_build(); return nc
        for c in range(NCH):
            cg = sg_ * NCH + c
            cur = cg % 2
            cols = slice(c * 64, (c + 1) * 64)
            yps, ypb = ybanks[cg % 2]
            for p in range(NP):
                pt, ptb = k.bank()
                for i, (arr, bb) in enumerate(((Vt, b_V), (BTt, b_BT), (Kb, b_Kb))):
                    k.mm(pt[0:64, i * 128:(i + 1) * 128], arr[:, p, cols], pre.identb[:, :], True, True, [bb[p], bc], [ptb])
                k.cp("act", TM[p][:, :, :], pt[0:64, 0:384].rearrange("p (a b) -> p a b", a=3), [ptb], [b_TM[p]])
                if stop == 41:
                    k.finish_build(); return nc
                for e in range(2):
                    h = 2 * p + e; si = h % NS
                    rows = slice(e * 64, (e + 1) * 64)
                    if e == 0:
                        bt = BTt[0:64, p, cols]; at = ATt[0:64, p, cols]; rt = Rb[0:64, p, cols]; kt = Kb[0:64, p, cols]
                        deps = [b_BT[p], b_AT[p], b_Rb[p], b_Kb[p]]
                    else:
                        bt = BTO[:, p, cols]; at = ATO[:, p, cols]; rt = RtO[:, p, cols]; kt = KtO[:, p, cols]
                        deps = [b_BO[p], b_AO[p], b_RO[p], b_KO[p]]
                    b_at, b_rt = deps[1], deps[2]
                    ps, pb = k.bank()
                    k.mm(ps[0:64, 0:64], bt, at, True, True, deps, [pb])
                    k.mm(ps[0:64, 64:128], bt, rt, True, True, deps, [pb])
                    k.mm(ps[0:64, 128:192], kt, at, True, True, deps, [pb])
                    k.mm(ps[0:64, 192:256], kt, rt, True, True, deps, [pb])
                    k.mm(ps[0:64, 256:320], at, bt, True, True, deps, [pb])
                    k.tt("dve", G[si][:, :], ps[0:64, 0:320], m5[:, :], ALU.mult, [pb, bc], [b_G[si]])
                    if stop == 42 + e * 10:
                        k.finish_build(); return nc
                    k.tt("dve", NTt[si][:, :], G[si][:, 0:64], pre.identb[0:64, 0:64], ALU.add, [b_G[si], bc], [b_NT[si]])
                    Lk = G[si][:, 256:320]; Pk = G[si][:, 0:64]; lb_ = [b_G[si]]
                    for lev in range(5):
                        ps, pb = k.bank()
                        k.mm(ps[0:64, 0:64], Pk, Lk, True, True, lb_, [pb])
                        if lev < 4:
                            k.mm(ps[0:64, 64:128], Lk, Pk, True, True, lb_, [pb])
                        j = lev % 2
                        k.cp("act", LP[si][j][:, :], ps[0:64, 0:128], [pb], [b_LP[si][j]])
                        Lk = LP[si][j][:, 0:64]; Pk = LP[si][j][:, 64:128]; lb_ = [b_LP[si][j]]
                        ps2, pb2 = k.bank()
                        k.mm(ps2[0:64, 0:64], Lk, NTt[si][:, :], True, True, lb_ + [b_NT[si]], [pb2])
                        k.tt("dve", NTt[si][:, :], NTt[si][:, :], ps2[0:64, 0:64], ALU.add, [pb2, b_NT[si]], [b_NT[si]])
                    if stop == 43 + e * 10:
                        k.finish_build(); return nc
                    vte = TM[p][:, 0, rows]
                    tst = Tb[h][cur][:, :]
                    ps, pb = k.bank()
                    k.mm(ps[0:64, 0:64], G[si][:, 128:192], vte, True, False, [b_G[si], b_TM[p]], [pb])
                    k.mm(ps[0:64, 0:64], at, tst, False, True, [b_at, b_T[h][cur]], [pb])
                    k.cp("act", Wsb[si][:, :], ps[0:64, 0:64], [pb], [b_W[si]])
                    ps, pb = k.bank()
                    k.mm(ps[0:64, 0:64], NTt[si][:, :], Wsb[si][:, :], True, True, [b_NT[si], b_W[si]], [pb])
                    k.cp("act", Usb[h][:, :], ps[0:64, 0:64], [pb], [b_U[h]])
                    yo = yps[0:64, h * 64:(h + 1) * 64]
                    k.mm(yo, rt, tst, True, False, [b_rt, b_T[h][cur]], [ypb])
                    k.mm(yo, G[si][:, 64:128], Usb[h][:, :], False, False, [b_G[si], b_U[h]], [ypb])
                    k.mm(yo, G[si][:, 192:256], vte, False, True, [b_G[si], b_TM[p]], [ypb])
                    if stop == 44 + e * 10:
                        k.finish_build(); return nc
                for e in range(2):
                    h = 2 * p + e
                    rows = slice(e * 64, (e + 1) * 64)
                    ps, pb = k.bank()
                    k.mm(ps[0:64, 0:64], TM[p][:, 1, rows], Usb[h][:, :], True, False, [b_TM[p], b_U[h]], [pb])
                    k.mm(ps[0:64, 0:64], TM[p][:, 2, rows], TM[p][:, 0, rows], False, True, [b_TM[p]], [pb])
                    pc_ap = pcs[0:64, p, c:c + 1] if e == 0 else pcsO[:, p, c:c + 1]
                    pcb_ = b_pc[p] if e == 0 else b_pcO[p]
                    tj = h % 2
                    k.ts("dve", Ttmp[tj][:, :], Tst[h][cur][:, :], pc_ap, None, ALU.mult, None, [b_T[h][cur], pcb_], [b_Ttmp[tj]])
                    k.stt("dve", Tst[h][1 - cur][:, :], ps[0:64, 0:64], pc_ap, Ttmp[tj][:, :], ALU.mult, ALU.add, [pb, pcb_, b_Ttmp[tj]], [b_T[h][1 - cur]])
                    k.cp("act", Tb[h][1 - cur][:, :], Tst[h][1 - cur][:, :], [b_T[h][1 - cur]], [b_T[h][1 - cur]])
            if stop == 4:
                k.finish_build(); return nc
            pbn, pbnb = k.bank()
            for p in range(NP):
                k.mm(pbn[0:64, 2 * p:2 * p + 2], RKt[:, p, cols], hs[:, :], True, True, [b_RK[p], bc], [pbnb])
            k.cp("act", bon[:, :], pbn[0:64, 0:HO], [pbnb], [b_bon])
            pg, pgb = k.bank()
            k.mm(pg[0:64, 0:CH], sga[:, cols], g2a[:, :], True, False, [b_lo, bc], [pgb])
            k.mm(pg[0:64, 0:CH], sgb[:, cols], g2b[:, :], False, True, [b_lo, bc], [pgb])
            if stop == 61:
                k.finish_build(); return nc
            yv = yps[0:64, 0:CH].rearrange("p (h d) -> p h d", d=64)
            k.cp("act", Y1[:, :], yps[0:64, 0:CH], [ypb], [b_Y1])
            k.red("dve", stt_[:, 0:HO], Y1[:, :].rearrange("p (h d) -> p h d", d=64), ALU.add, AX.X, [b_Y1], [b_stt])
            if stop == 615:
                k.finish_build(); return nc
            k.tt("dve", Y2[:, :], Y1[:, :], Y1[:, :], ALU.mult, [b_Y1], [b_Y2])
            k.red("dve", stt_[:, HO:2 * HO], Y2[:, :].rearrange("p (h d) -> p h d", d=64), ALU.add, AX.X, [b_Y2], [b_stt])
            if stop == 616:
                k.finish_build(); return nc
            k.ts("dve", stt_[:, 0:HO], stt_[:, 0:HO], 1.0 / 64, None, ALU.mult, None, [b_stt], [b_stt])
            k.tt("dve", stt_[:, 2 * HO:3 * HO], stt_[:, 0:HO], stt_[:, 0:HO], ALU.mult, [b_stt], [b_stt])
            k.stt("dve", stt_[:, HO:2 * HO], stt_[:, HO:2 * HO], 1.0 / 64, stt_[:, 2 * HO:3 * HO], ALU.mult, ALU.subtract, [b_stt], [b_stt])
            if stop == 617:
                k.finish_build(); return nc
            k.ts("dve", stt_[:, HO:2 * HO], stt_[:, HO:2 * HO], 64e-5, None, ALU.add, None, [b_stt], [b_stt])
            k.act(stt_[:, HO:2 * HO], stt_[:, HO:2 * HO], AF.Ln, [b_stt], [b_stt])
            k.act(stt_[:, HO:2 * HO], stt_[:, HO:2 * HO], AF.Exp, [b_stt], [b_stt], scale=-0.5)
            if stop == 62:
                k.finish_build(); return nc
            for h in range(HO):
                hsl = slice(h * 64, (h + 1) * 64)
                k.ts("dve", Y1[:, hsl], Y1[:, hsl], stt_[:, h:h + 1], stt_[:, HO + h:HO + h + 1], ALU.subtract, ALU.mult, [b_Y1, b_stt], [b_Y1])
            k.tt("dve", Y1[:, :], Y1[:, :], lg_b[:, :], ALU.mult, [b_Y1, b_par], [b_Y1])
            k.tt("dve", Y1[:, :], Y1[:, :], lb_b[:, :], ALU.add, [b_Y1, b_par], [b_Y1])
            for h in range(HO):
                p, e = h // 2, h % 2
                hsl = slice(h * 64, (h + 1) * 64)
                k.stt("dve", Y1[:, hsl], TM[p][:, 0, e * 64:(e + 1) * 64], bon[:, h:h + 1], Y1[:, hsl], ALU.mult, ALU.add, [b_TM[p], b_bon, b_Y1], [b_Y1])
            k.tt("dve", Y2[:, :], Y1[:, :], pg[0:64, 0:CH], ALU.mult, [b_Y1, pgb], [b_Y2])
            if stop == 63:
                k.finish_build(); return nc
            k.dma("sp", yb[t0 + c * 64: t0 + (c + 1) * 64, :], Y2[:, :], [b_Y2], [], b_Y2)
            if stop == 64 + cg:
                k.finish_build(); return nc
    for b_ in ybanks:
        k.unreserve(b_)
    k.finish_build()
    return nc


def build_gm(D, NT, AW, NG, k=None):
    k = k or KB(); nc, s = k.nc, k.s; B = s.buf
    DC = D // 128; NTT = NT // 128; NB = AW // 512 if AW >= 512 else 1; CW = min(512, AW)
    k.psum_banks()
    x = k.dram("x", [NT, D]); cvec = k.dram("cvec", [1, D]); wada2 = k.dram("wada2", [D, 2 * D]); bada2 = k.dram("bada2", [1, 2 * D])
    gpre = k.dram("gpre", [1, D]); ident = k.dram("ident", [128, 128])
    wu = k.dram("wu", [D, AW]); wv = k.dram("wv", [D, AW]); vgb = k.dram("vgb", [1, 2 * AW])
    ws = k.dram("ws", [NG, 128, 128]); bs = k.dram("bs", [1, NG * 128]); triu = k.dram("triu", [128, 128])
    ya = k.dram("ya", [NT, AW], kind="ExternalOutput")
    pre = Pre(k, D, wada2, bada2, cvec, gpre, ident, nwsl=3)
    identf = pre.identf; bc = pre.b_const
    hT = k.sb("hT", [128, DC, NT], BF16); b_hT = [B() for _ in range(NTT)]
    U = k.sb("U", [128, NTT, AW]); V = k.sb("V", [128, NTT, AW]); b_U = [B() for _ in range(NTT)]; b_V = [B() for _ in range(NTT)]
    wsT = k.sb("wsT", [128, NG, 128], BF16); tmpw = k.sb("tmpw", [128, 128]); b_tw = B(); tru = k.sb("tru", [128, 128])
    bs_c = k.sb("bs_c", [128, NG]); vg_b = k.sb("vg_b", [128, AW]); vb_b = k.sb("vb_b", [128, AW]); b_par = B()
    vnb = k.sb("vnb", [128, AW], BF16); b_vn = B(); st2 = k.sb("st2", [128, 4]); b_st2 = B(); yo = k.sb("yo", [128, AW]); b_yo = B()
    junk = k.sb("junk", [128, AW], BF16); b_junk = B()
    k.dma("sp", tru[:], triu[:, :], [], [bc], bc)
    for g in range(NG):
        k.dma("sp", tmpw[:], ws[g, :, :], [], [b_tw], b_tw)
        pt, pb = k.bank()
        k.mm(pt[:, 0:128], tmpw[:, :], identf[:, :], True, True, [b_tw, bc], [pb])
        k.tt("dve", wsT[:, g, :], pt[:, 0:128], tru[:, :], ALU.mult, [pb, bc], [b_par])
    pt, pb = k.bank()
    pre.col_from_dram(bs, NG * 128, pt, pb)
    k.cp("act", bs_c[:], pt[:, 0:NG], [pb], [b_par])
    for (dst, off) in ((vg_b, 0), (vb_b, AW)):
        done = 0
        while done < AW:
            w = min(512, AW - done)
            k.dma("sp", pre.rowf[0][0:1, 0:w], vgb[0:1, off + done: off + done + w], [], [pre.b_rowf[0]], pre.b_rowf[0])
            pt, pb = k.bank()
            k.mm(pt[:, 0:w], pre.ones_f[0:1, :], pre.rowf[0][0:1, 0:w], True, True, [bc, pre.b_rowf[0]], [pb])
            k.cp("act", dst[:, done:done + w], pt[:, 0:w], [pb], [b_par])
            done += w
    for t in range(NTT):
        pre.norm_transpose(x[t * 128:(t + 1) * 128, :], hT, t * 128, b_hT[t])
    for (wsrc, dst, bufs) in ((wu, U, b_U), (wv, V, b_V)):
        for n in range(AW // CW):
            w, wb = pre.next_wsl()
            pre.load_w(w, wb, wsrc[:, n * CW:(n + 1) * CW], CW)
            for t in range(NTT):
                pt, pb = k.bank()
                for kc in range(DC):
                    k.mm(pt[:, 0:CW], hT[:, kc, t * 128:(t + 1) * 128], w[:, kc, 0:CW], kc == 0, kc == DC - 1, [b_hT[t], wb], [pb])
                k.act(dst[:, t, n * CW:(n + 1) * CW], pt[:, 0:CW], AF.Gelu, [pb], [bufs[t]])
    for t in range(NTT):
        v = V[:, t, :]
        k.red("dve", st2[:, 0:1], v, ALU.add, AX.X, [b_V[t]], [b_st2])
        k.memset("dve", st2[:, 1:2], 0.0, [b_st2])
        k.act(junk[:, :], v, AF.Square, [b_V[t], b_st2], [b_junk, b_st2], accum=st2[:, 1:2])
        k.ts("dve", st2[:, 0:1], st2[:, 0:1], 1.0 / AW, None, ALU.mult, None, [b_st2], [b_st2])
        k.tt("dve", st2[:, 2:3], st2[:, 0:1], st2[:, 0:1], ALU.mult, [b_st2], [b_st2])
        k.stt("dve", st2[:, 1:2], st2[:, 1:2], 1.0 / AW, st2[:, 2:3], ALU.mult, ALU.subtract, [b_st2], [b_st2])
        k.ts("dve", st2[:, 1:2], st2[:, 1:2], 1e-5, None, ALU.add, None, [b_st2], [b_st2])
        k.act(st2[:, 1:2], st2[:, 1:2], AF.Ln, [b_st2], [b_st2])
        k.act(st2[:, 1:2], st2[:, 1:2], AF.Exp, [b_st2], [b_st2], scale=-0.5)
        k.ts("dve", v, v, st2[:, 0:1], st2[:, 1:2], ALU.subtract, ALU.mult, [b_V[t], b_st2], [b_V[t]])
        k.tt("dve", v, v, vg_b[:, :], ALU.mult, [b_V[t], b_par], [b_V[t]])
        k.tt("dve", vnb[:, :], v, vb_b[:, :], ALU.add, [b_V[t], b_par], [b_vn])
        for g4 in range(max(1, NG // 4)):
            pt, pb = k.bank()
            ng = min(4, NG)
            for q in range(ng):
                g = g4 * 4 + q
                k.mm(pt[:, q * 128:(q + 1) * 128], wsT[:, g, :], vnb[:, g * 128:(g + 1) * 128], True, True, [b_par, b_vn], [pb])
            for q in range(ng):
                g = g4 * 4 + q
                gs = slice(g * 128, (g + 1) * 128)
                k.stt("dve", yo[:, gs], pt[:, q * 128:(q + 1) * 128], bs_c[:, g:g + 1], U[:, t, gs], ALU.add, ALU.mult, [pb, b_par, b_U[t]], [b_yo])
        k.dma("sp", ya[t * 128:(t + 1) * 128, :], yo[:, :], [b_yo], [], b_yo)
    k.finish_build()
    return nc


NEG = -1.0e30


def build_mb(D, T, HO, SEG=512, k=None, xfn=None):
    k = k or KB(); nc, s = k.nc, k.s; B = s.buf
    DC = D // 128; DH = 128; BLK = 256; NBLK = T // BLK; NQT = T // 128; HC = HO * DH; NSEG = T // SEG; CW = min(512, HC)
    assert NBLK == 8
    k.psum_banks()
    x = k.dram("x", [T, D]); cvec = k.dram("cvec", [1, D]); wada2 = k.dram("wada2", [D, 2 * D]); bada2 = k.dram("bada2", [1, 2 * D])
    gpre = k.dram("gpre", [1, D]); ident = k.dram("ident", [128, 128])
    wq = k.dram("wq", [D, HC]); wk = k.dram("wk", [D, HC]); wv = k.dram("wv", [D, HC])
    slope = k.dram("slope", [1, 128]); kpos = k.dram("kpos", [128, T]); cmask = k.dram("cmask", [128, 128]); gmask = k.dram("gmask", [128, 64])
    o = k.dram("o", [T, HC], kind="ExternalOutput")
    pre = Pre(k, D, wada2, bada2, cvec, gpre, ident, nwsl=2)
    identb = pre.identb; bc = pre.b_const
    hTs = k.sb("hTs", [128, DC, SEG], BF16); b_hT = B()
    QF = k.sb("QF", [128, HO, T], BF16); KF = k.sb("KF", [128, HO, T], BF16); VT = k.sb("VT", [128, NQT, HC], BF16)
    b_Q = [B() for _ in range(HO)]; b_K = [B() for _ in range(HO)]; b_V = [B() for _ in range(NQT)]
    kp = k.sb("kp", [128, T]); cm = k.sb("cm", [128, 128]); gmc = k.sb("gmc", [128, 64]); slc = k.sb("slc", [128, 128])
    kmf = k.sb("kmf", [128, 8]); kmb = k.sb("kmb", [128, HO, 8], BF16); b_km = B()
    ali = k.sb("ali", [128, T]); b_ali = B()
    Ssb = k.sb("Ssb", [128, T]); b_S = B(); Pb = k.sb("Pb", [128, T], BF16); b_P = B()
    PT = k.sb("PT", [128, NQT, 128], BF16); b_PT = B()
    gsb = k.sb("gsb", [128, 8]); mx8 = k.sb("mx8", [128, 8]); selb = k.sb("selb", [128, 8]); b_g = B()
    sm = k.sb("sm", [128, 4]); b_sm = B(); osb = [k.sb("osb%d" % i, [128, 128]) for i in range(2)]; b_o = [B(), B()]
    for (dst, src) in ((kp, kpos), (cm, cmask), (gmc, gmask)):
        k.dma("sp", dst[:], src[:, :], [], [bc], bc)
    k.dma("sp", pre.rowf[0][0:1, 0:128], slope[0:1, :], [], [pre.b_rowf[0]], pre.b_rowf[0])
    pt, pb = k.bank()
    k.mm(pt[:, 0:128], pre.ones_f[0:1, :], pre.rowf[0][0:1, 0:128], True, True, [bc, pre.b_rowf[0]], [pb])
    k.cp("act", slc[:, :], pt[:, 0:128], [pb], [bc])
    for sg_ in range(NSEG):
        t0 = sg_ * SEG
        for tq in range(SEG // 128):
            xsrc_ = xfn(sg_ * (SEG // 128) + tq) if xfn is not None else x[t0 + tq * 128: t0 + (tq + 1) * 128, :]
            pre.norm_transpose(xsrc_, hTs, tq * 128, b_hT)
        for (wsrc, dst, bufs, sc_) in ((wq, QF, b_Q, DH ** -0.5), (wk, KF, b_K, None)):
            for n in range(HC // CW):
                w, wb = pre.next_wsl()
                pre.load_w(w, wb, wsrc[:, n * CW:(n + 1) * CW], CW)
                for hh in range(CW // 128):
                    h = n * (CW // 128) + hh
                    pt, pb = k.bank()
                    for kc in range(DC):
                        k.mm(pt[:, 0:SEG], w[:, kc, hh * 128:(hh + 1) * 128], hTs[:, kc, :], kc == 0, kc == DC - 1, [wb, b_hT], [pb])
                    if sc_ is not None:
                        k.act(dst[:, h, t0:t0 + SEG], pt[:, 0:SEG], AF.Copy, [pb], [bufs[h]], scale=sc_)
                    else:
                        k.cp("dve", dst[:, h, t0:t0 + SEG], pt[:, 0:SEG], [pb], [bufs[h]])
        for n in range(HC // CW):
            w, wb = pre.next_wsl()
            pre.load_w(w, wb, wv[:, n * CW:(n + 1) * CW], CW)
            for tq in range(SEG // 128):
                tt_ = sg_ * (SEG // 128) + tq
                pt, pb = k.bank()
                for kc in range(DC):
                    k.mm(pt[:, 0:CW], hTs[:, kc, tq * 128:(tq + 1) * 128], w[:, kc, 0:CW], kc == 0, kc == DC - 1, [wb, b_hT], [pb])
                if tq % 2 == 0:
                    k.cp("act", VT[:, tt_, n * CW:(n + 1) * CW], pt[:, 0:CW], [pb], [b_V[tt_]])
                else:
                    k.cp("dve", VT[:, tt_, n * CW:(n + 1) * CW], pt[:, 0:CW], [pb], [b_V[tt_]])
    for h in range(HO):
        k.red("dve", kmf[:, :], KF[:, h, :].rearrange("p (n s) -> p n s", s=BLK), ALU.add, AX.X, [b_K[h]], [b_km])
        k.ts("dve", kmb[:, h, :], kmf[:, :], 1.0 / BLK, None, ALU.mult, None, [b_km], [b_km])
    it = 0
    for h in range(HO):
        k.ts("dve", ali[:, :], kp[:, :], slc[:, h:h + 1], None, ALU.mult, None, [bc], [b_ali])
        for qt in range(NQT):
            qb = qt // 2; nk = (qt + 1) * 128
            ql = QF[:, h, qt * 128:(qt + 1) * 128]
            pg, pgb = k.bank()
            k.mm(pg[:, 0:8], ql, kmb[:, h, :], True, True, [b_Q[h], b_km], [pgb])
            k.tt("dve", gsb[:, :], pg[:, 0:8], gmc[:, qb * 8:(qb + 1) * 8], ALU.add, [pgb, bc], [b_g])
            k.max8(mx8[:, :], gsb[:, :], [b_g], [b_g])
            k.ts("dve", selb[:, :], gsb[:, :], mx8[:, 2:3], 1.0, ALU.is_ge, ALU.subtract, [b_g], [b_g])
            k.ts("dve", selb[:, :], selb[:, :], 1.0e30, None, ALU.mult, None, [b_g], [b_g])
            for kg in range((nk + 511) // 512):
                w_ = min(512, nk - kg * 512)
                ps, pb = k.bank()
                k.mm(ps[:, 0:w_], ql, KF[:, h, kg * 512: kg * 512 + w_], True, True, [b_Q[h], b_K[h]], [pb])
                for kt in range(kg * 4, kg * 4 + w_ // 128):
                    n = kt // 2
                    lo = (kt - kg * 4) * 128
                    ks = slice(kt * 128, (kt + 1) * 128)
                    if n < qb:
                        k.stt("dve", Ssb[:, ks], ps[:, lo:lo + 128], selb[:, n:n + 1], ali[:, ks], ALU.add, ALU.add, [pb, b_g, b_ali], [b_S])
                    else:
                        k.tt("dve", Ssb[:, ks], ps[:, lo:lo + 128], ali[:, ks], ALU.add, [pb, b_ali], [b_S])
                        if kt == qt:
                            k.tt("dve", Ssb[:, ks], Ssb[:, ks], cm[:, :], ALU.add, [b_S, bc], [b_S])
            k.red("dve", sm[:, 0:1], Ssb[:, 0:nk], ALU.max, AX.X, [b_S], [b_sm])
            k.ts("dve", sm[:, 0:1], sm[:, 0:1], -1.0, None, ALU.mult, None, [b_sm], [b_sm])
            k.memset("dve", sm[:, 1:2], 0.0, [b_sm])
            k.act(Pb[:, 0:nk], Ssb[:, 0:nk], AF.Exp, [b_S, b_sm], [b_P, b_sm], bias=sm[:, 0:1], accum=sm[:, 1:2])
            k.recip("dve", sm[:, 2:3], sm[:, 1:2], [b_sm], [b_sm])
            for g4 in range((qt + 4) // 4):
                nq = min(4, qt + 1 - g4 * 4)
                pt, pb = k.bank()
                for q in range(nq):
                    kt = g4 * 4 + q
                    k.mm(pt[:, q * 128:(q + 1) * 128], Pb[:, kt * 128:(kt + 1) * 128], identb[:, :], True, True, [b_P, bc], [pb])
                src = pt[:, 0:nq * 128].rearrange("p (a b) -> p a b", a=nq)
                if g4 % 2 == 0:
                    k.cp("act", PT[:, g4 * 4:g4 * 4 + nq, :], src, [pb], [b_PT])
                else:
                    k.cp("dve", PT[:, g4 * 4:g4 * 4 + nq, :], src, [pb], [b_PT])
            po, pob = k.bank()
            for kt in range(qt + 1):
                k.mm(po[:, 0:128], PT[:, kt, :], VT[:, kt, h * 128:(h + 1) * 128], kt == 0, kt == qt, [b_PT, b_V[kt]], [pob])
            j = it % 2; it += 1
            k.ts("dve", osb[j][:, :], po[:, 0:128], sm[:, 2:3], None, ALU.mult, None, [pob, b_sm], [b_o[j]])
            k.dma("sp", o[qt * 128:(qt + 1) * 128, h * 128:(h + 1) * 128], osb[j][:, :], [b_o[j]], [], b_o[j])
    k.finish_build()
    return nc


def _ag_chunks(k, loc, rows, cols, name, groups, cbuf):
    rpc = (1 << 20) // (cols * 4)
    n = rows // rpc
    gs = [k.scratch("%s_%d" % (name, i), [2 * rpc, cols]) for i in range(n)]
    for i in range(n):
        k.allgather(gs[i][:, :], loc[i * rpc:(i + 1) * rpc, :], groups, [], [], cbuf)
    return gs, rpc


def _grow(gs, rpc, r, tau):
    ci = tau // rpc; w = tau % rpc
    return gs[ci][r * rpc + w: r * rpc + w + 128, :]


def build_fused(D, F, T, HOA, HOM, AW, NG, NCORES=8, SEG=512, HCF=256, TN=512):
    k = KB(); k.fused = True
    NTo = T // 2; CH = 64 * HOA; HC = 128 * HOM
    groups = [[2 * i, 2 * i + 1] for i in range(NCORES // 2)]
    k.psum_banks()
    ya_loc = k.scratch("ya_loc", [NTo, AW]); yb_loc = k.scratch("yb_loc", [T, CH])
    x1loc = k.scratch("x1loc", [NTo, D]); o_loc = k.scratch("o_loc", [T, HC])
    xmid0 = k.scratch("xmid0", [NTo, D]); xmid1 = k.scratch("xmid1", [NTo, D])
    cb = k.s.buf()
    k.prefix = "gm_"; k.alias["gm_ya"] = ya_loc
    build_gm(D, NTo, AW, NG, k=k); k.end_phase()
    k.prefix = "rw_"; k.alias["rw_yb"] = yb_loc
    build_rw(D, T, HOA, SEG, k=k); k.end_phase()
    ybg, rp0 = _ag_chunks(k, yb_loc, T, CH, "ybg", groups, cb); k.s.barrier()
    k.prefix = "b0_"; k.alias["b0_xmid"] = xmid0; k.alias["b0_xout"] = x1loc
    ys0 = [(0, AW, lambda t: ya_loc[t * 128:(t + 1) * 128, :], None)]
    for r in range(2):
        ys0.append((AW + r * CH, CH, (lambda t, r=r: _grow(ybg, rp0, r, t * 128)), (lambda t, r=r: _grow(ybg, rp0, r, NTo + t * 128))))
    build_bd(D, F, NTo, HCF, TN, k=k, ysrc=ys0); k.end_phase()
    x1g, rp1 = _ag_chunks(k, x1loc, NTo, D, "x1g", groups, cb); k.s.barrier()
    k.prefix = "mb_"; k.alias["mb_o"] = o_loc; k.alias["mb_x"] = x1loc
    NTT = NTo // 128
    build_mb(D, T, HOM, SEG, k=k, xfn=lambda tt: _grow(x1g, rp1, tt // NTT, (tt % NTT) * 128)); k.end_phase()
    ogs, rp2 = _ag_chunks(k, o_loc, T, HC, "og", groups, cb); k.s.barrier()
    k.prefix = "b1_"; k.alias["b1_xmid"] = xmid1; k.alias["b1_xin"] = x1loc
    ys1 = []
    for r in range(2):
        ys1.append((r * HC, HC, (lambda t, r=r: _grow(ogs, rp2, r, t * 128)), (lambda t, r=r: _grow(ogs, rp2, r, NTo + t * 128))))
    build_bd(D, F, NTo, HCF, TN, k=k, ysrc=ys1); k.end_phase()
    k.s.finish()
    return k.nc


from concourse.bass_utils import run_bass_kernel_spmd

_D = 2048; _F = 5632; _S = 2048; _NB = 4; _NC = 8
_PROG = {}


def _c(a):
    return np.ascontiguousarray(a, dtype=np.float32)


def kernel(x, c, w_ada, b_ada, g_pre_mix, g_post_mix, g_pre_ffn, g_post_ffn, w_ffn_in, w_ffn_out, w_in_ab, w_out_ab,
           a_v_gain, a_v_bias, a_w_s, a_b_s, b_mu, b_w0, b_w2, b_a0, b_a2, b_g2, b_k_k, b_k_a, b_r_k, b_lnx_gain,
           b_lnx_bias, w_qkv, w_o):
    f = np.float32
    x = np.asarray(x, f); c = np.asarray(c, f); w_ada = np.asarray(w_ada, f); b_ada = np.asarray(b_ada, f)
    D = _D
    if "f" not in _PROG:
        _PROG["f"] = build_fused(_D, _F, _S, 8, 8, 1024, 8)
    nc = _PROG["f"]
    ident = np.eye(128, dtype=f)
    own = lambda hh: slice(hh * 1024, (hh + 1) * 1024)
    su = np.triu(np.ones((64, 64)), 1); iu = np.triu(np.ones((64, 64)), 0)
    mask5 = np.concatenate([su, iu, su, iu, su.T], axis=1).astype(f)
    blk = np.kron(np.eye(2), np.ones((64, 64))).astype(f)
    tri2 = (blk * np.triu(np.ones((128, 128)))).astype(f)
    hsel = np.kron(np.eye(2), np.ones((64, 1))).astype(f)
    triu = np.triu(np.ones((128, 128))).astype(f)
    kpos = np.tile(np.arange(_S, dtype=f)[None, :], (128, 1))
    cmask = np.where(np.arange(128)[None, :] <= np.arange(128)[:, None], 0.0, -1e30).astype(f)
    gmask = np.zeros((128, 64), f)
    for qb in range(8):
        for n in range(8):
            gmask[:, qb * 8 + n] = 0.0 if n < qb else -1e30
    w_in_ab0 = np.asarray(w_in_ab[0], f)
    wada0_a = _c(w_ada[0][:, 0:2 * D]); bada0_a = _c(b_ada[0][None, 0:2 * D])
    wada0_b = _c(w_ada[0][:, 2 * D:6 * D]); bada0_b = _c(b_ada[0][None, 2 * D:6 * D])
    wada1_a = _c(w_ada[1][:, 0:2 * D]); bada1_a = _c(b_ada[1][None, 0:2 * D])
    wada1_b = _c(w_ada[1][:, 2 * D:6 * D]); bada1_b = _c(b_ada[1][None, 2 * D:6 * D])
    wu = _c(w_in_ab0[:, 0:1024]); wvv = _c(w_in_ab0[:, 1024:2048])
    vgb = _c(np.concatenate([np.asarray(a_v_gain[0]), np.asarray(a_v_bias[0])])[None, :])
    ws = _c(a_w_s[0]); bs = _c(np.asarray(a_b_s[0]).reshape(1, 1024))
    gpre0 = _c(np.asarray(g_pre_mix[0])[None, :]); gpre1 = _c(np.asarray(g_pre_mix[1])[None, :])
    mu = np.asarray(b_mu[0], f); rk = np.asarray(b_r_k[0], f).reshape(1024)
    wqkv = np.asarray(w_qkv[0], f)
    shared = {}
    rwp = []; mbp = []
    for hh in range(2):
        cs = slice(hh * 512, (hh + 1) * 512)
        mu_cat = np.zeros((1, 15 * 128), f)
        mu_cat[0, 0:512] = mu[0:1024][cs]; mu_cat[0, 512:1024] = mu[1024:2048][cs]; mu_cat[0, 1024:1536] = mu[2048:3072][cs]
        mu_cat[0, 1536:1536 + 288] = mu[3072:3360]
        pvec = np.concatenate([np.asarray(b_w0[0], f)[cs], np.asarray(b_a0[0], f)[cs], np.asarray(b_k_k[0], f)[cs],
                               np.asarray(b_k_a[0], f)[cs], rk[cs]])[None, :]
        lnx = np.concatenate([np.asarray(b_lnx_gain[0], f)[cs], np.asarray(b_lnx_bias[0], f)[cs]])[None, :]
        rwp.append(dict(rw_wr=_c(w_in_ab0[:, 2048:3072][:, cs]), rw_wk=_c(w_in_ab0[:, 3072:4096][:, cs]), rw_wv=_c(w_in_ab0[:, 4096:5120][:, cs]),
                        rw_mu_cat=mu_cat, rw_pvec=_c(pvec), rw_lnx=_c(lnx), rw_w2=_c(np.asarray(b_w2[0], f)[:, cs]),
                        rw_a2=_c(np.asarray(b_a2[0], f)[:, cs]), rw_g2=_c(np.asarray(b_g2[0], f)[:, cs])))
        cs2 = slice(hh * 1024, (hh + 1) * 1024)
        sl = np.zeros((1, 128), f)
        sl[0, 0:8] = 2.0 ** (-8.0 * (np.arange(8) + hh * 8 + 1) / 16.0)
        mbp.append(dict(mb_wq=_c(wqkv[:, 0:2048][:, cs2]), mb_wk=_c(wqkv[:, 2048:4096][:, cs2]), mb_wv=_c(wqkv[:, 4096:6144][:, cs2]), mb_slope=sl))
    wl = _c(w_in_ab0[:, 5120:5408])
    com = dict(
        gm_wada2=wada0_a, gm_bada2=bada0_a, gm_gpre=gpre0, gm_ident=ident, gm_wu=wu, gm_wv=wvv, gm_vgb=vgb, gm_ws=ws, gm_bs=bs, gm_triu=triu,
        rw_wada2=wada0_a, rw_bada2=bada0_a, rw_gpre=gpre0, rw_ident=ident, rw_wl=wl, rw_mask5=mask5, rw_tri2=tri2, rw_bones=blk, rw_hsel=hsel,
        b0_wada=wada0_b, b0_bada=bada0_b, b0_wout=_c(w_out_ab[0]), b0_gpost=_c(np.asarray(g_post_mix[0])[None]), b0_gpre=_c(np.asarray(g_pre_ffn[0])[None]),
        b0_gpost2=_c(np.asarray(g_post_ffn[0])[None]), b0_wfi=_c(w_ffn_in[0]), b0_wfo=_c(w_ffn_out[0]), b0_ident=ident,
        mb_wada2=wada1_a, mb_bada2=bada1_a, mb_gpre=gpre1, mb_ident=ident, mb_kpos=kpos, mb_cmask=cmask, mb_gmask=gmask,
        b1_wada=wada1_b, b1_bada=bada1_b, b1_wout=_c(w_o[0]), b1_gpost=_c(np.asarray(g_post_mix[1])[None]), b1_gpre=_c(np.asarray(g_pre_ffn[1])[None]),
        b1_gpost2=_c(np.asarray(g_post_ffn[1])[None]), b1_wfi=_c(w_ffn_in[1]), b1_wfo=_c(w_ffn_out[1]), b1_ident=ident)
    maps = []
    for core in range(_NC):
        b, hh = core // 2, core % 2
        sel = np.zeros((128, 2), f); sel[:, hh] = 1.0
        cv = _c(c[b][None, :])
        m = dict(com)
        m.update(rwp[hh]); m.update(mbp[hh])
        m.update(gm_x=_c(x[b, own(hh)]), gm_cvec=cv, rw_x=_c(x[b]), rw_cvec=cv, b0_xin=_c(x[b, own(hh)]), b0_cvec=cv, b0_sel=sel,
                 mb_cvec=cv, b1_cvec=cv, b1_sel=sel)
        maps.append(m)
    res = run_bass_kernel_spmd(nc, maps, core_ids=list(range(_NC))).results
    out = np.zeros((_NB, _S, D), f)
    for core in range(_NC):
        b, hh = core // 2, core % 2
        out[b, own(hh)] = res[core]["b1_xout"]
    return out
```

```python
import numpy as np
import concourse.bass as bass
import concourse.mybir as mybir

F32 = mybir.dt.float32
BF16 = mybir.dt.bfloat16
AF = mybir.ActivationFunctionType
ALU = mybir.AluOpType
AX = mybir.AxisListType


class Buf:
    __slots__ = ("name", "lw", "rd", "dsem", "demit")

    def __init__(self, name):
        self.name = name
        self.lw = None
        self.rd = []
        self.dsem = {}
        self.demit = {}


class Op:
    __slots__ = ("eng", "emit", "deps", "dbuf", "needed", "sig", "inc", "barrier")

    def __init__(self, eng, emit, deps, dbuf, inc=16):
        self.eng = eng
        self.emit = emit
        self.deps = deps
        self.dbuf = dbuf
        self.needed = False
        self.sig = 0
        self.inc = inc
        self.barrier = False


class Sch:
    def __init__(self, nc, same_engine_sync=True):
        self.nc = nc
        self.E = {"pe": nc.tensor, "act": nc.scalar, "dve": nc.vector, "pool": nc.gpsimd, "sp": nc.sync}
        self.ops = []
        self.same = same_engine_sync
        self.nbuf = 0

    def buf(self, name=None):
        self.nbuf += 1
        return Buf(name or "b%d" % self.nbuf)

    def barrier(self):
        op = Op("sp", None, [], None)
        op.barrier = True
        self.ops.append(op)

    def add(self, eng, emit, reads=(), writes=(), dma=None, inc=16):
        deps = []
        for b in reads:
            if b.lw is not None:
                deps.append(b.lw)
        for b in writes:
            if b.lw is not None:
                deps.append(b.lw)
            deps.extend(b.rd)
        op = Op(eng, emit, deps, dma, inc)
        for d in deps:
            if d.dbuf is None and d.eng == eng and (eng == "pe" or not self.same):
                continue
            d.needed = True
        for b in reads:
            b.rd.append(op)
        for b in writes:
            b.lw = op
            b.rd = []
        self.ops.append(op)
        return op

    def finish(self):
        nc = self.nc
        esem = {k: nc.alloc_semaphore("es_" + k) for k in self.E}
        cnt = {k: 0 for k in self.E}
        last = {}
        for op in self.ops:
            if op.barrier:
                for o in last.values():
                    o.needed = True
            elif op.dbuf is None:
                last[op.eng] = op
        for op in self.ops:
            if op.barrier:
                continue
            if op.dbuf is None and op.needed:
                cnt[op.eng] += 1
                op.sig = cnt[op.eng]
        waited = {k: {} for k in self.E}
        dbufs = []
        nsem = len(esem)
        lastsig = {k: 0 for k in self.E}
        for op in self.ops:
            if op.barrier:
                for en, e in self.E.items():
                    w = waited[en]
                    for x in self.E:
                        if x == en or lastsig[x] == 0:
                            continue
                        key = ("e", x)
                        if w.get(key, 0) < lastsig[x]:
                            e.wait_ge(esem[x], lastsig[x]); w[key] = lastsig[x]
                    for b, q in dbufs:
                        key = ("d", id(b), q)
                        if w.get(key, 0) < b.demit[q]:
                            e.wait_ge(b.dsem[q], b.demit[q]); w[key] = b.demit[q]
                continue
            eng = self.E[op.eng]
            need = {}
            for d in op.deps:
                if d.dbuf is not None:
                    key = ("d", id(d.dbuf), d.eng)
                    sem = d.dbuf.dsem[d.eng]
                    val = d.dbuf.demit[d.eng]
                else:
                    if d.eng == op.eng and (d.eng == "pe" or not self.same):
                        continue
                    key = ("e", d.eng)
                    sem = esem[d.eng]
                    val = d.sig
                if key not in need or need[key][1] < val:
                    need[key] = (sem, val)
            w = waited[op.eng]
            for key, (sem, val) in need.items():
                if w.get(key, 0) >= val:
                    continue
                eng.wait_ge(sem, val)
                w[key] = val
            inst = op.emit()
            if op.dbuf is not None:
                b = op.dbuf
                if op.eng not in b.dsem:
                    b.dsem[op.eng] = nc.alloc_semaphore("ds_%d" % nsem)
                    b.demit[op.eng] = 0
                    nsem += 1
                    dbufs.append((b, op.eng))
                inst.then_inc(b.dsem[op.eng], op.inc)
                b.demit[op.eng] += op.inc
            elif op.needed:
                inst.then_inc(esem[op.eng], 1)
                lastsig[op.eng] = op.sig
        for b, q in dbufs:
            nc.sync.wait_ge(b.dsem[q], b.demit[q])
        self.nsem = nsem
        return nsem


class KB:
    def __init__(self, name="k"):
        self.nc = bass.Bass("TRN2", target_bir_lowering=False)
        self.s = Sch(self.nc)
        self.nps = 0
        self.ps_banks = None
        self.prefix = ""
        self.alias = {}
        self.cms = []
        self.fused = False

    def dram(self, name, shape, dt=F32, kind="ExternalInput"):
        name = self.prefix + name
        if name in self.alias:
            return self.alias[name]
        return self.nc.dram_tensor(name, list(shape), dt, kind=kind).ap()

    def scratch(self, name, shape, dt=F32, shared=False):
        if shared:
            return self.nc.dram_tensor(name, list(shape), dt, addr_space="Shared").ap()
        return self.nc.dram_tensor(name, list(shape), dt).ap()

    def sb(self, name, shape, dt=F32):
        if not self.fused:
            return self.nc.alloc_sbuf_tensor(self.prefix + name, list(shape), dt)
        cm = self.nc.sbuf_tensor(self.prefix + name, list(shape), dt)
        t = cm.__enter__()
        self.cms.append(cm)
        return t

    def end_phase(self):
        self.s.barrier()
        for cm in reversed(self.cms):
            cm.__exit__(None, None, None)
        self.cms = []

    def finish_build(self):
        if not self.fused:
            self.s.finish()

    def allgather(self, out_ap, in_ap, groups, R, W, cbuf):
        nc = self.nc
        return self.s.add("pool", lambda: nc.gpsimd.collective_compute("AllGather", mybir.AluOpType.bypass, replica_groups=groups, ins=[in_ap], outs=[out_ap]), R, W, dma=cbuf, inc=1)

    def psum_banks(self):
        if self.ps_banks is not None:
            return
        self.ps_banks = []
        for i in range(8):
            t = self.nc.alloc_psum_tensor("psb%d" % i, [128, 512], F32)
            self.ps_banks.append((t, self.s.buf("ps%d" % i)))
        self.ps_i = 0

    def bank(self):
        n = len(self.ps_banks)
        b = self.ps_banks[self.ps_i % n]
        self.ps_i += 1
        return b

    def reserve(self):
        return self.ps_banks.pop()

    def unreserve(self, b):
        self.ps_banks.append(b)

    def mm(self, out, lhsT, rhs, start, stop, R, W):
        nc = self.nc
        return self.s.add("pe", lambda: nc.tensor.matmul(out, lhsT, rhs, start=start, stop=stop), R, W)

    def act(self, out, in_, func, R, W, bias=None, scale=None, accum=None):
        nc = self.nc
        kw = {}
        if bias is not None:
            kw["bias"] = bias
        if scale is not None:
            kw["scale"] = scale
        if accum is not None:
            kw["accum_out"] = accum
        return self.s.add("act", lambda: nc.scalar.activation(out=out, in_=in_, func=func, **kw), R, W)

    def _e(self, eng):
        return {"dve": self.nc.vector, "pool": self.nc.gpsimd}[eng]

    def tt(self, eng, out, in0, in1, op, R, W):
        e = self._e(eng)
        return self.s.add(eng, lambda: e.tensor_tensor(out=out, in0=in0, in1=in1, op=op), R, W)

    def ts(self, eng, out, in0, s1, s2, op0, op1, R, W, accum=None):
        e = self._e(eng)
        if op1 is None:
            return self.s.add(eng, lambda: e.tensor_scalar(out=out, in0=in0, scalar1=s1, scalar2=None, op0=op0), R, W)
        if accum is not None:
            return self.s.add(eng, lambda: e.tensor_scalar(out=out, in0=in0, scalar1=s1, scalar2=s2, op0=op0, op1=op1, accum_out=accum), R, W)
        return self.s.add(eng, lambda: e.tensor_scalar(out=out, in0=in0, scalar1=s1, scalar2=s2, op0=op0, op1=op1), R, W)

    def stt(self, eng, out, in0, scalar, in1, op0, op1, R, W):
        e = self._e(eng)
        return self.s.add(eng, lambda: e.scalar_tensor_tensor(out=out, in0=in0, scalar=scalar, in1=in1, op0=op0, op1=op1), R, W)

    def cp(self, eng, out, in_, R, W):
        if eng == "act":
            nc = self.nc
            return self.s.add("act", lambda: nc.scalar.copy(out=out, in_=in_), R, W)
        e = self._e(eng)
        return self.s.add(eng, lambda: e.tensor_copy(out=out, in_=in_), R, W)

    def red(self, eng, out, in_, op, axis, R, W):
        e = self._e(eng)
        return self.s.add(eng, lambda: e.tensor_reduce(out=out, in_=in_, axis=axis, op=op), R, W)

    def memset(self, eng, ap, val, W):
        e = self._e(eng)
        return self.s.add(eng, lambda: e.memset(ap, val), (), W)

    def dma(self, q, out, in_, R, W, dbuf):
        e = {"sp": self.nc.sync, "pool": self.nc.gpsimd, "act": self.nc.scalar}[q]
        return self.s.add(q, lambda: e.dma_start(out=out, in_=in_), R, W, dma=dbuf)


def _kb_recip(self, eng, out, in_, R, W):
    e = self._e(eng)
    return self.s.add(eng, lambda: e.reciprocal(out=out, in_=in_), R, W)


KB.recip = _kb_recip


def _kb_max8(self, out, in_, R, W):
    nc = self.nc
    return self.s.add("dve", lambda: nc.vector.max(out=out, in_=in_), R, W)


KB.max8 = _kb_max8


EPS = 1e-6


def row_to_col(k, row_t, row_b, ncols, ps_ap, ps_b, one_f, b_const):
    for i in range(ncols):
        k.mm(ps_ap[:, i:i + 1], row_t[0:1, i * 128:(i + 1) * 128], one_f[0:1, 0:1], True, True, [row_b, b_const], [ps_b])


class Pre:
    def __init__(self, k, D, wada2, bada2, cvec, gpre, ident, nwsl=3):
        self.k = k; s = k.s; B = s.buf
        self.D = D; DC = D // 128; self.DC = DC
        self.identf = k.sb("identf", [128, 128]); self.identb = k.sb("identb", [128, 128], BF16)
        self.ones_f = k.sb("ones_f", [1, 128]); self.ones_b = k.sb("ones_b", [1, 128], BF16)
        self.rowf = [k.sb("rowf%d" % i, [1, 512]) for i in range(2)]
        self.rowb = [k.sb("rowb%d" % i, [1, 512], BF16) for i in range(2)]
        self.condT_f = k.sb("condT_f", [128, DC]); self.condT_b = k.sb("condT_b", [128, DC], BF16)
        self.modc = k.sb("modc", [128, 2 * DC]); self.gpre_c = k.sb("gpre_c", [128, DC]); self.gs_c = k.sb("gs_c", [128, DC])
        self.wsl = [k.sb("wsl%d" % i, [128, DC, 512], BF16) for i in range(nwsl)]
        self.xt = k.sb("xt", [128, D]); self.xn = k.sb("xn", [128, D], BF16); self.st = k.sb("st", [128, 8])
        self.b_const = B(); self.b_rowf = [B(), B()]; self.b_rowb = [B(), B()]; self.b_cond = B(); self.b_modc = B()
        self.b_gpre = B(); self.b_gs = B(); self.b_wsl = [B() for _ in range(nwsl)]; self.b_xt = B(); self.b_xn = B(); self.b_st = B()
        self.nw = 0; self.nwsl = nwsl
        k.dma("sp", self.identf[:], ident[:, :], [], [self.b_const], self.b_const)
        k.cp("dve", self.identb[:], self.identf[:], [self.b_const], [self.b_const])
        k.memset("dve", self.ones_f[:], 1.0, [self.b_const]); k.memset("dve", self.ones_b[:], 1.0, [self.b_const])
        pt, pb = k.bank()
        self.col_from_dram(cvec, D, pt, pb)
        k.act(self.condT_f[:], pt[:, 0:DC], AF.Silu, [pb], [self.b_cond])
        k.cp("dve", self.condT_b[:], self.condT_f[:], [self.b_cond], [self.b_cond])
        pt, pb = k.bank()
        self.col_from_dram(gpre, D, pt, pb)
        k.cp("act", self.gpre_c[:], pt[:, 0:DC], [pb], [self.b_gpre])
        pc, pcb = k.bank()
        for j in range(2 * D // 512):
            w, wb = self.next_wsl()
            self.load_w(w, wb, wada2[:, j * 512:(j + 1) * 512], 512)
            r = j % 2
            k.dma("pool", self.rowb[r][0:1, :], bada2[0:1, j * 512:(j + 1) * 512], [], [self.b_rowb[r]], self.b_rowb[r])
            for q in range(4):
                col = j * 4 + q
                for kc in range(DC):
                    k.mm(pc[:, col:col + 1], w[:, kc, q * 128:(q + 1) * 128], self.condT_b[:, kc:kc + 1], kc == 0, False, [self.b_cond, wb], [pcb])
                k.mm(pc[:, col:col + 1], self.rowb[r][0:1, q * 128:(q + 1) * 128], self.ones_b[0:1, 0:1], False, True, [self.b_rowb[r], self.b_const], [pcb])
        k.cp("act", self.modc[:], pc[:, 0:2 * DC], [pcb], [self.b_modc])
        k.stt("dve", self.gs_c[:], self.modc[:, DC:2 * DC], 1.0, self.gpre_c[:], ALU.add, ALU.mult, [self.b_modc, self.b_gpre], [self.b_gs])

    def next_wsl(self):
        i = self.nw % self.nwsl; self.nw += 1
        return self.wsl[i], self.b_wsl[i]

    def load_w(self, dst, dbuf, src_cols, width, off=0):
        self.k.dma("pool", dst[:, :, off:off + width], src_cols.rearrange("(kc p) n -> p kc n", p=128), [], [dbuf], dbuf)

    def col_from_dram(self, vec, n, pt, pb, col0=0):
        k = self.k
        done = 0
        j = 0
        while done < n:
            w = min(512, n - done)
            r = j % 2
            k.dma("sp", self.rowf[r][0:1, 0:w], vec[0:1, done:done + w], [], [self.b_rowf[r]], self.b_rowf[r])
            row_to_col(k, self.rowf[r], self.b_rowf[r], w // 128, pt[:, col0 + done // 128: col0 + (done + w) // 128], pb, self.ones_f, self.b_const)
            done += w; j += 1

    def rstd_of(self, src_ap, src_bufs, col, n):
        k = self.k; st = self.st; b_st = self.b_st
        k.memset("dve", st[:, col:col + 1], 0.0, [b_st])
        k.act(self.xn[:, 0:n], src_ap, AF.Square, src_bufs + [b_st], [self.b_xn, b_st], accum=st[:, col:col + 1])
        k.ts("dve", st[:, col:col + 1], st[:, col:col + 1], 1.0 / n, EPS, ALU.mult, ALU.add, [b_st], [b_st])
        k.act(st[:, col:col + 1], st[:, col:col + 1], AF.Ln, [b_st], [b_st])
        k.act(st[:, col:col + 1], st[:, col:col + 1], AF.Exp, [b_st], [b_st], scale=-0.5)

    def norm_transpose(self, x_dram_tile, hT, hcol0, b_h):
        k = self.k; D = self.D; DC = self.DC
        k.dma("sp", self.xt[:], x_dram_tile, [], [self.b_xt], self.b_xt)
        self.rstd_of(self.xt[:], [self.b_xt], 1, D)
        k.act(self.xn[:], self.xt[:], AF.Copy, [self.b_xt, self.b_st], [self.b_xn], scale=self.st[:, 1:2])
        for g in range(max(1, DC // 4)):
            nq = min(4, DC)
            pt, pb = k.bank()
            for q in range(nq):
                kc = g * 4 + q
                k.mm(pt[:, q * 128:(q + 1) * 128], self.xn[:, kc * 128:(kc + 1) * 128], self.identb[:, :], True, True, [self.b_xn, self.b_const], [pb])
            for q in range(nq):
                kc = g * 4 + q
                o = hT[:, kc, hcol0:hcol0 + 128]
                i = pt[:, q * 128:(q + 1) * 128]
                if q % 2 == 0:
                    k.act(o, i, AF.Identity, [pb, self.b_gs, self.b_modc], [b_h], bias=self.modc[:, kc:kc + 1], scale=self.gs_c[:, kc:kc + 1])
                else:
                    k.ts("dve", o, i, self.gs_c[:, kc:kc + 1], self.modc[:, kc:kc + 1], ALU.mult, ALU.add, [pb, self.b_gs, self.b_modc], [b_h])


EPS = 1e-6


def row_to_col(k, row_t, row_b, ncols, ps_ap, ps_b, one_f, b_const):
    for i in range(ncols):
        k.mm(ps_ap[:, i:i + 1], row_t[0:1, i * 128:(i + 1) * 128], one_f[0:1, 0:1], True, True, [row_b, b_const], [ps_b])


def build_bd(D, F, NT, HC=256, TN=512, k=None, ysrc=None, out_kind="ExternalOutput"):
    k = k or KB()
    nc, s = k.nc, k.s
    DC = D // 128
    NTT = NT // 128
    NB = D // 512
    SUB = HC // 128
    TPB = TN // 128
    k.psum_banks()
    xin = k.dram("xin", [NT, D]); cvec = k.dram("cvec", [1, D])
    ycat = k.dram("ycat", [NT, D]) if ysrc is None else None
    seld = k.dram("sel", [128, 2]) if ysrc is not None else None
    wada = k.dram("wada", [D, 4 * D]); bada = k.dram("bada", [1, 4 * D])
    wout = k.dram("wout", [D, D]); gpost = k.dram("gpost", [1, D]); gpre = k.dram("gpre", [1, D])
    gpost2 = k.dram("gpost2", [1, D]); wfi = k.dram("wfi", [D, 2 * F]); wfo = k.dram("wfo", [F, D])
    ident = k.dram("ident", [128, 128])
    xmid = k.dram("xmid", [NT, D], kind="ExternalOutput")
    xout = k.dram("xout", [NT, D], kind=out_kind)
    identf = k.sb("identf", [128, 128]); identb = k.sb("identb", [128, 128], BF16)
    ones_f = k.sb("ones_f", [1, 128]); ones_b = k.sb("ones_b", [1, 128], BF16)
    zeros = k.sb("zeros", [128, 128])
    rowf = [k.sb("rowf%d" % i, [1, 512]) for i in range(2)]
    rowb = [k.sb("rowb%d" % i, [1, 512], BF16) for i in range(2)]
    condT_f = k.sb("condT_f", [128, DC]); condT_b = k.sb("condT_b", [128, DC], BF16)
    cond_rep = k.sb("cond_rep", [128, DC, 128], BF16)
    modc = k.sb("modc", [128, 2 * DC]); gpre_c = k.sb("gpre_c", [128, DC]); gsf_c = k.sb("gsf_c", [128, DC])
    gt_b = k.sb("gt_b", [128, D])
    wsl = [k.sb("wsl%d" % i, [128, DC, 512], BF16) for i in range(3)]
    wos = [k.sb("wos%d" % i, [128, SUB, D], BF16) for i in range(2)]
    acc = k.sb("acc", [128, NTT, D])
    hT = k.sb("hT", [128, DC, NT], BF16)
    xt = k.sb("xt", [128, D]); xn = k.sb("xn", [128, D], BF16)
    tmp = [k.sb("tmp%d" % i, [128, 512]) for i in range(2)]
    actT = [k.sb("actT%d" % i, [128, SUB, TN], BF16) for i in range(2)]
    sg = [k.sb("sg%d" % i, [128, TN]) for i in range(2)]
    st = k.sb("st", [128, 8])
    selt = k.sb("selt", [128, 2])
    B = s.buf
    b_const = B(); b_rowf = [B(), B()]; b_rowb = [B(), B()]; b_cond = B(); b_modc = B(); b_gpre = B(); b_gsf = B()
    b_gt = [B() for _ in range(NB)]
    b_wsl = [B() for _ in range(3)]; b_wos = [B() for _ in range(2)]
    b_acc = [[B() for _ in range(NB)] for _ in range(NTT)]
    b_hT = [B() for _ in range(NTT)]
    b_xt = B(); b_xn = B(); b_tmp = [B(), B()]; b_actT = [B(), B()]; b_sg = [B(), B()]; b_st = B()
    b_xmid = [B() for _ in range(NTT)]
    cnt = {"wsl": 0, "wos": 0, "row": 0, "tmp": 0, "ev": 0}

    def next_wsl():
        i = cnt["wsl"] % 3; cnt["wsl"] += 1
        return wsl[i], b_wsl[i]

    def load_w(dst, dbuf, src_cols, width, off=0):
        k.dma("pool", dst[:, :, off:off + width], src_cols.rearrange("(kc p) n -> p kc n", p=128), [], [dbuf], dbuf)

    k.dma("sp", identf[:], ident[:, :], [], [b_const], b_const)
    k.cp("dve", identb[:], identf[:], [b_const], [b_const])
    k.memset("dve", ones_f[:], 1.0, [b_const]); k.memset("dve", ones_b[:], 1.0, [b_const])
    k.memset("dve", zeros[:], 0.0, [b_const])
    one_f = ones_f
    if ysrc is not None:
        k.dma("sp", selt[:], seld[:, :], [], [b_const], b_const)

    pt, pb = k.bank()
    for j in range(D // 512):
        r = j % 2
        k.dma("sp", rowf[r][0:1, :], cvec[0:1, j * 512:(j + 1) * 512], [], [b_rowf[r]], b_rowf[r])
        row_to_col(k, rowf[r], b_rowf[r], 4, pt[:, j * 4:(j + 1) * 4], pb, one_f, b_const)
    k.act(condT_f[:], pt[:, 0:DC], AF.Silu, [pb], [b_cond])
    k.cp("dve", condT_b[:], condT_f[:], [b_cond], [b_cond])
    for kc in range(DC):
        k.ts("dve", cond_rep[:, kc, :], zeros[:], condT_f[:, kc:kc + 1], None, ALU.add, None, [b_cond, b_const], [b_cond])
    pt, pb = k.bank()
    for j in range(D // 512):
        r = j % 2
        k.dma("sp", rowf[r][0:1, :], gpre[0:1, j * 512:(j + 1) * 512], [], [b_rowf[r]], b_rowf[r])
        row_to_col(k, rowf[r], b_rowf[r], 4, pt[:, j * 4:(j + 1) * 4], pb, one_f, b_const)
    k.cp("act", gpre_c[:], pt[:, 0:DC], [pb], [b_gpre])

    def mod_bcast(col0, grow):
        for j in range(NB):
            w, wb = next_wsl()
            load_w(w, wb, wada[:, col0 + j * 512: col0 + (j + 1) * 512], 512)
            r = j % 2
            k.dma("pool", rowb[r][0:1, :], bada[0:1, col0 + j * 512: col0 + (j + 1) * 512], [], [b_rowb[r]], b_rowb[r])
            k.dma("sp", rowf[r][0:1, :], grow[0:1, j * 512:(j + 1) * 512], [], [b_rowf[r]], b_rowf[r])
            pa, pab = k.bank()
            for kc in range(DC):
                k.mm(pa[:, :], cond_rep[:, kc, :], w[:, kc, :], kc == 0, False, [b_cond, wb], [pab])
            k.mm(pa[:, :], ones_b[0:1, :], rowb[r][0:1, :], False, True, [b_const, b_rowb[r]], [pab])
            pg, pgb = k.bank()
            k.mm(pg[:, :], ones_f[0:1, :], rowf[r][0:1, :], True, True, [b_const, b_rowf[r]], [pgb])
            k.cp("act", tmp[r][:], pg[:, :], [pgb], [b_tmp[r]])
            k.tt("dve", gt_b[:, j * 512:(j + 1) * 512], pa[:, :], tmp[r][:], ALU.mult, [pab, b_tmp[r]], [b_gt[j]])

    mod_bcast(0, gpost)

    pc, pcb = k.bank()
    for j in range(2 * D // 512):
        w, wb = next_wsl()
        load_w(w, wb, wada[:, D + j * 512: D + (j + 1) * 512], 512)
        r = j % 2
        k.dma("pool", rowb[r][0:1, :], bada[0:1, D + j * 512: D + (j + 1) * 512], [], [b_rowb[r]], b_rowb[r])
        for q in range(4):
            col = j * 4 + q
            for kc in range(DC):
                k.mm(pc[:, col:col + 1], w[:, kc, q * 128:(q + 1) * 128], condT_b[:, kc:kc + 1], kc == 0, False, [b_cond, wb], [pcb])
            k.mm(pc[:, col:col + 1], rowb[r][0:1, q * 128:(q + 1) * 128], ones_b[0:1, 0:1], False, True, [b_rowb[r], b_const], [pcb])
    k.cp("act", modc[:], pc[:, 0:2 * DC], [pcb], [b_modc])
    k.stt("dve", gsf_c[:], modc[:, DC:2 * DC], 1.0, gpre_c[:], ALU.add, ALU.mult, [b_modc, b_gpre], [b_gsf])
    shf_c = modc

    def transpose_tile(t, modulate):
        for g in range(DC // 4 if DC >= 4 else 1):
            nq = min(4, DC)
            pt, pb = k.bank()
            for q in range(nq):
                kc = g * 4 + q
                k.mm(pt[:, q * 128:(q + 1) * 128], xn[:, kc * 128:(kc + 1) * 128], identb[:, :], True, True, [b_xn, b_const], [pb])
            if not modulate:
                src = pt[:, 0:nq * 128].rearrange("p (a b) -> p a b", a=nq)
                eng = "act" if g % 2 == 0 else "dve"
                k.cp(eng, hT[:, g * 4:g * 4 + nq, t * 128:(t + 1) * 128], src, [pb], [b_hT[t]])
            else:
                for q in range(nq):
                    kc = g * 4 + q
                    o = hT[:, kc, t * 128:(t + 1) * 128]
                    i = pt[:, q * 128:(q + 1) * 128]
                    if q % 2 == 0:
                        k.act(o, i, AF.Identity, [pb, b_gsf, b_modc], [b_hT[t]], bias=shf_c[:, kc:kc + 1], scale=gsf_c[:, kc:kc + 1])
                    else:
                        k.ts("dve", o, i, gsf_c[:, kc:kc + 1], shf_c[:, kc:kc + 1], ALU.mult, ALU.add, [pb, b_gsf, b_modc], [b_hT[t]])

    for t in range(NTT):
        if ysrc is None:
            k.dma("sp", xt[:], ycat[t * 128:(t + 1) * 128, :], [], [b_xt], b_xt)
        else:
            for (c0, wd, fa, fb) in ysrc:
                if fb is None:
                    k.dma("sp", xt[:, c0:c0 + wd], fa(t), [], [b_xt], b_xt)
                    continue
                for c1 in range(0, wd, 512):
                    w_ = min(512, wd - c1)
                    r = (c1 // 512) % 2
                    k.dma("sp", xt[:, c0 + c1:c0 + c1 + w_], fa(t)[:, c1:c1 + w_], [], [b_xt], b_xt)
                    k.dma("sp", tmp[r][:, 0:w_], fb(t)[:, c1:c1 + w_], [], [b_tmp[r]], b_tmp[r])
                    k.ts("dve", xt[:, c0 + c1:c0 + c1 + w_], xt[:, c0 + c1:c0 + c1 + w_], selt[:, 0:1], None, ALU.mult, None, [b_xt, b_const], [b_xt])
                    k.stt("dve", xt[:, c0 + c1:c0 + c1 + w_], tmp[r][:, 0:w_], selt[:, 1:2], xt[:, c0 + c1:c0 + c1 + w_], ALU.mult, ALU.add, [b_tmp[r], b_xt, b_const], [b_xt])
        k.cp("act", xn[:], xt[:], [b_xt], [b_xn])
        transpose_tile(t, False)

    for n in range(NB):
        w, wb = next_wsl()
        load_w(w, wb, wout[:, n * 512:(n + 1) * 512], 512)
        for t in range(NTT):
            pt, pb = k.bank()
            for kc in range(DC):
                k.mm(pt[:, :], hT[:, kc, t * 128:(t + 1) * 128], w[:, kc, :], kc == 0, kc == DC - 1, [b_hT[t], wb], [pb])
            eng = "act" if t % 2 == 0 else "dve"
            k.cp(eng, acc[:, t, n * 512:(n + 1) * 512], pt[:, :], [pb], [b_acc[t][n]])

    def rstd_of(src_ap, src_bufs, col):
        k.memset("dve", st[:, col:col + 1], 0.0, [b_st])
        k.act(xn[:], src_ap, AF.Square, src_bufs + [b_st], [b_xn, b_st], accum=st[:, col:col + 1])
        k.ts("dve", st[:, col:col + 1], st[:, col:col + 1], 1.0 / D, EPS, ALU.mult, ALU.add, [b_st], [b_st])
        k.act(st[:, col:col + 1], st[:, col:col + 1], AF.Ln, [b_st], [b_st])
        k.act(st[:, col:col + 1], st[:, col:col + 1], AF.Exp, [b_st], [b_st], scale=-0.5)

    for t in range(NTT):
        k.dma("sp", xt[:], xin[t * 128:(t + 1) * 128, :], [], [b_xt], b_xt)
        rstd_of(acc[:, t, :], b_acc[t], 0)
        k.stt("dve", acc[:, t, :], acc[:, t, :], st[:, 0:1], gt_b[:, :], ALU.mult, ALU.mult, b_acc[t] + b_gt + [b_st], b_acc[t])
        k.tt("dve", xt[:], xt[:], acc[:, t, :], ALU.add, [b_xt] + b_acc[t], [b_xt])
        k.dma("sp", xmid[t * 128:(t + 1) * 128, :], xt[:], [b_xt], [b_xmid[t]], b_xt)
        rstd_of(xt[:], [b_xt], 1)
        k.act(xn[:], xt[:], AF.Copy, [b_xt, b_st], [b_xn], scale=st[:, 1:2])
        transpose_tile(t, True)

    mod_bcast(3 * D, gpost2)

    blocks = [(hc, th) for hc in range(F // HC) for th in range(NT // TN)]
    wcur = {}

    def emit_gu(i):
        hc, th = blocks[i]
        if th == 0:
            w, wb = next_wsl()
            load_w(w, wb, wfi[:, hc * HC:(hc + 1) * HC], HC, 0)
            load_w(w, wb, wfi[:, F + hc * HC: F + (hc + 1) * HC], HC, HC)
            j = cnt["wos"] % 2; cnt["wos"] += 1
            k.dma("pool", wos[j][:, :, :], wfo[hc * HC:(hc + 1) * HC, :].rearrange("(s p) n -> p s n", p=128), [], [b_wos[j]], b_wos[j])
            wcur[hc] = (w, wb, wos[j], b_wos[j])
        w, wb, _, _ = wcur[hc]
        a = i % 2
        hbufs = [b_hT[th * TPB + q] for q in range(TPB)]
        for sub in range(SUB):
            pg, pgb = k.bank()
            for kc in range(DC):
                k.mm(pg[:, 0:TN], w[:, kc, sub * 128:(sub + 1) * 128], hT[:, kc, th * TN:(th + 1) * TN], kc == 0, kc == DC - 1, [wb] + hbufs, [pgb])
            pu, pub = k.bank()
            for kc in range(DC):
                k.mm(pu[:, 0:TN], w[:, kc, HC + sub * 128: HC + (sub + 1) * 128], hT[:, kc, th * TN:(th + 1) * TN], kc == 0, kc == DC - 1, [wb] + hbufs, [pub])
            r = sub % 2
            k.act(sg[r][:, :], pg[:, 0:TN], AF.Silu, [pgb], [b_sg[r]])
            k.tt("dve", actT[a][:, sub, :], sg[r][:, :], pu[:, 0:TN], ALU.mult, [b_sg[r], pub], [b_actT[a]])

    def emit_y(i):
        hc, th = blocks[i]
        _, _, wo, wob = wcur[hc]
        a = i % 2
        for tq in range(TPB):
            t = th * TPB + tq
            for n in range(NB):
                py, pyb = k.bank()
                for sub in range(SUB):
                    k.mm(py[:, :], actT[a][:, sub, tq * 128:(tq + 1) * 128], wo[:, sub, n * 512:(n + 1) * 512], sub == 0, sub == SUB - 1, [b_actT[a], wob], [pyb])
                dst = acc[:, t, n * 512:(n + 1) * 512]
                if hc == 0:
                    k.cp("dve", dst, py[:, :], [pyb], [b_acc[t][n]])
                else:
                    k.tt("dve", dst, dst, py[:, :], ALU.add, [pyb, b_acc[t][n]], [b_acc[t][n]])

    for i in range(len(blocks)):
        emit_gu(i)
        if i > 0:
            emit_y(i - 1)
    emit_y(len(blocks) - 1)

    for t in range(NTT):
        k.dma("sp", xt[:], xmid[t * 128:(t + 1) * 128, :], [b_xmid[t]], [b_xt], b_xt)
        rstd_of(acc[:, t, :], b_acc[t], 2)
        k.stt("dve", acc[:, t, :], acc[:, t, :], st[:, 2:3], gt_b[:, :], ALU.mult, ALU.mult, b_acc[t] + b_gt + [b_st], b_acc[t])
        k.tt("dve", xt[:], xt[:], acc[:, t, :], ALU.add, [b_xt] + b_acc[t], [b_xt])
        k.dma("sp", xout[t * 128:(t + 1) * 128, :], xt[:], [b_xt], [], b_xt)
    k.finish_build()
    return nc


def build_rw(D, T, HO, SEG=512, RDT=F32, stop=99, k=None):
    k = k or KB(); nc, s = k.nc, k.s; B = s.buf
    DC = D // 128; CH = 64 * HO; NP = HO // 2; NSEG = T // SEG; NCH = SEG // 64; NZ = 3 * NP + 3
    k.psum_banks()
    x = k.dram("x", [T, D]); cvec = k.dram("cvec", [1, D]); wada2 = k.dram("wada2", [D, 2 * D]); bada2 = k.dram("bada2", [1, 2 * D])
    gpre = k.dram("gpre", [1, D]); ident = k.dram("ident", [128, 128])
    wr = k.dram("wr", [D, CH]); wk = k.dram("wk", [D, CH]); wv = k.dram("wv", [D, CH]); wl = k.dram("wl", [D, 288])
    mu_cat = k.dram("mu_cat", [1, NZ * 128]); pvec = k.dram("pvec", [1, 5 * CH]); lnx = k.dram("lnx", [1, 2 * CH])
    w2 = k.dram("w2", [64, CH]); a2 = k.dram("a2", [64, CH]); g2 = k.dram("g2", [160, CH])
    mask5 = k.dram("mask5", [64, 320]); tri2 = k.dram("tri2", [128, 128]); bones = k.dram("bones", [128, 128]); hsel = k.dram("hsel", [128, 2])
    yb = k.dram("yb", [T, CH], kind="ExternalOutput")
    ybanks = [k.reserve(), k.reserve()]
    pre = Pre(k, D, wada2, bada2, cvec, gpre, ident, nwsl=2)
    identf = pre.identf; bc = pre.b_const
    hTs = k.sb("hTs", [128, DC, SEG], BF16); b_hT = B()
    Rt = k.sb("Rt", [128, NP, SEG]); Kt = k.sb("Kt", [128, NP, SEG]); Vt = k.sb("Vt", [128, NP, SEG], BF16)
    Rb = k.sb("Rb", [128, NP, SEG], BF16); Kb = k.sb("Kb", [128, NP, SEG], BF16)
    BTt = k.sb("BTt", [128, NP, SEG], BF16); ATt = k.sb("ATt", [128, NP, SEG], BF16); RKt = k.sb("RKt", [128, NP, SEG])
    b_Rb = [B() for _ in range(NP)]; b_Kb = [B() for _ in range(NP)]
    b_R = [B() for _ in range(NP)]; b_K = [B() for _ in range(NP)]; b_V = [B() for _ in range(NP)]
    b_BT = [B() for _ in range(NP)]; b_AT = [B() for _ in range(NP)]; b_RK = [B() for _ in range(NP)]
    zwa = k.sb("zwa", [128, SEG]); zga = k.sb("zga", [128, SEG]); zgb = k.sb("zgb", [32, SEG]); b_zl = [B(), B(), B()]
    twb = k.sb("twb", [128, SEG], BF16); sga = k.sb("sga", [128, SEG], BF16); sgb = k.sb("sgb", [32, SEG], BF16); b_lo = B()
    zraw = [k.sb("zraw%d" % i, [128, SEG + 1]) for i in range(2)]; b_zraw = [B(), B()]
    carry = k.sb("carry", [128, NZ]); b_carry = B()
    S = [k.sb("S%d" % i, [128, SEG]) for i in range(7)]; b_S = [B() for _ in range(7)]
    mu_c = k.sb("mu_c", [128, NZ]); omm_c = k.sb("omm_c", [128, NZ]); pv_c = k.sb("pv_c", [128, 5 * NP]); b_par = B()
    pcs = k.sb("pcs", [128, NP, NCH]); b_pc = [B() for _ in range(NP)]
    w2a2 = k.sb("w2a2", [128, CH], BF16); g2a = k.sb("g2a", [128, CH], BF16); g2b = k.sb("g2b", [32, CH], BF16)
    m5 = k.sb("m5", [64, 320]); tri = k.sb("tri", [128, 128]); bon1 = k.sb("bon1", [128, 128]); hs = k.sb("hs", [128, 2])
    lg_b = k.sb("lg_b", [64, CH]); lb_b = k.sb("lb_b", [64, CH])
    RtO = k.sb("RtO", [64, NP, SEG], BF16); KtO = k.sb("KtO", [64, NP, SEG], BF16); BTO = k.sb("BTO", [64, NP, SEG], BF16); ATO = k.sb("ATO", [64, NP, SEG], BF16)
    b_RO = [B() for _ in range(NP)]; b_KO = [B() for _ in range(NP)]; b_BO = [B() for _ in range(NP)]; b_AO = [B() for _ in range(NP)]
    pcsO = k.sb("pcsO", [64, NP, NCH]); b_pcO = [B() for _ in range(NP)]
    Tst = [[k.sb("Tst%d_%d" % (h, i), [64, 64]) for i in range(2)] for h in range(HO)]
    Tb = [[k.sb("Tb%d_%d" % (h, i), [64, 64], BF16) for i in range(2)] for h in range(HO)]
    Ttmp = [k.sb("Ttmp%d" % i, [64, 64]) for i in range(2)]; b_Ttmp = [B(), B()]
    b_T = [[B(), B()] for _ in range(HO)]
    TM = [k.sb("TM%d" % p, [64, 3, 128], BF16) for p in range(NP)]; b_TM = [B() for _ in range(NP)]
    NS = HO
    G = [k.sb("G%d" % i, [64, 320], BF16) for i in range(NS)]; b_G = [B() for _ in range(NS)]
    NTt = [k.sb("NT%d" % i, [64, 64], BF16) for i in range(NS)]; b_NT = [B() for _ in range(NS)]
    LP = [[k.sb("LP%d_%d" % (i, j), [64, 128], BF16) for j in range(2)] for i in range(NS)]; b_LP = [[B(), B()] for _ in range(NS)]
    Wsb = [k.sb("Wsb%d" % i, [64, 64], BF16) for i in range(NS)]; b_W = [B() for _ in range(NS)]
    Usb = [k.sb("Usb%d" % h, [64, 64], BF16) for h in range(HO)]; b_U = [B() for _ in range(HO)]
    Y1 = k.sb("Y1", [64, CH]); b_Y1 = B(); bon = k.sb("bon", [64, HO]); b_bon = B()
    stt_ = k.sb("stt_", [64, 4 * HO]); b_stt = B(); Y2 = k.sb("Y2", [64, CH]); b_Y2 = B()

    for (dst, src) in ((m5, mask5), (tri, tri2), (bon1, bones), (hs, hsel)):
        k.dma("sp", dst[:], src[:, :], [], [bc], bc)
    k.dma("pool", w2a2[0:64, :], w2[:, :], [], [bc], bc)
    k.dma("pool", w2a2[64:128, :], a2[:, :], [], [bc], bc)
    k.dma("pool", g2a[:, :], g2[0:128, :], [], [bc], bc)
    k.dma("pool", g2b[:, :], g2[128:160, :], [], [bc], bc)
    pt, pb = k.bank()
    pre.col_from_dram(mu_cat, NZ * 128, pt, pb)
    k.cp("act", mu_c[:], pt[:, 0:NZ], [pb], [b_par])
    k.ts("dve", omm_c[:], mu_c[:], -1.0, 1.0, ALU.mult, ALU.add, [b_par], [b_par])
    pt, pb = k.bank()
    pre.col_from_dram(pvec, 5 * CH, pt, pb)
    k.cp("act", pv_c[:], pt[:, 0:5 * NP], [pb], [b_par])
    for (dst, off) in ((lg_b, 0), (lb_b, CH)):
        done = 0
        while done < CH:
            w = min(512, CH - done)
            k.dma("sp", pre.rowf[0][0:1, 0:w], lnx[0:1, off + done: off + done + w], [], [pre.b_rowf[0]], pre.b_rowf[0])
            pt, pb = k.bank()
            k.mm(pt[0:64, 0:w], pre.ones_f[0:1, 0:64], pre.rowf[0][0:1, 0:w], True, True, [bc, pre.b_rowf[0]], [pb])
            k.cp("act", dst[:, done:done + w], pt[0:64, 0:w], [pb], [b_par])
            done += w
    k.memset("dve", carry[:], 0.0, [b_carry])
    for h in range(HO):
        k.memset("dve", Tst[h][0][:], 0.0, [b_T[h][0]])
        k.memset("dve", Tb[h][0][:], 0.0, [b_T[h][0]])
    W0, A0, KK, KA, RKc = [lambda p, i=i: pv_c[:, i * NP + p: i * NP + p + 1] for i in range(5)]

    if stop == 1:
        k.finish_build(); return nc
    zi = {"n": 0}

    def ztile(w, wb, c0, M, dst, dbufs, idx):
        ps, pb = k.bank()
        for kc in range(DC):
            k.mm(ps[0:M, 0:SEG], w[:, kc, c0:c0 + M], hTs[:, kc, :], kc == 0, kc == DC - 1, [wb, b_hT], [pb])
        r = zi["n"] % 2; zi["n"] += 1
        zr = zraw[r]; bz = b_zraw[r]
        k.cp("act", zr[0:M, 1:SEG + 1], ps[0:M, 0:SEG], [pb], [bz])
        k.cp("dve", zr[0:M, 0:1], carry[0:M, idx:idx + 1], [b_carry], [bz])
        k.ts("dve", S[0][0:M, :], zr[0:M, 0:SEG], mu_c[0:M, idx:idx + 1], None, ALU.mult, None, [bz, b_par], [b_S[0]])
        k.stt("dve", dst, zr[0:M, 1:SEG + 1], omm_c[0:M, idx:idx + 1], S[0][0:M, :], ALU.mult, ALU.add, [bz, b_par, b_S[0]], dbufs)
        k.cp("dve", carry[0:M, idx:idx + 1], zr[0:M, SEG:SEG + 1], [bz], [b_carry])

    for sg_ in range(NSEG):
        t0 = sg_ * SEG
        for tq in range(SEG // 128):
            pre.norm_transpose(x[t0 + tq * 128: t0 + (tq + 1) * 128, :], hTs, tq * 128, b_hT)
        for (wsrc, arr, bufs, zbase) in ((wr, Rt, b_R, 0), (wk, Kt, b_K, NP), (wv, Vt, b_V, 2 * NP)):
            w, wb = pre.next_wsl()
            pre.load_w(w, wb, wsrc[:, :], CH)
            for p in range(NP):
                ztile(w, wb, p * 128, 128, arr[:, p, :], [bufs[p]], zbase + p)
        w, wb = pre.next_wsl()
        pre.load_w(w, wb, wl[:, :], 288)
        ztile(w, wb, 0, 128, zwa[:, :], [b_zl[0]], 3 * NP)
        ztile(w, wb, 128, 128, zga[:, :], [b_zl[1]], 3 * NP + 1)
        ztile(w, wb, 256, 32, zgb[:, :], [b_zl[2]], 3 * NP + 2)
        k.act(twb[0:64, :], zwa[0:64, :], AF.Tanh, [b_zl[0]], [b_lo])
        k.cp("dve", twb[64:128, :], zwa[64:128, :], [b_zl[0]], [b_lo])
        k.act(sga[:, :], zga[:, :], AF.Sigmoid, [b_zl[1]], [b_lo])
        k.act(sgb[:, :], zgb[:, :], AF.Sigmoid, [b_zl[2]], [b_lo])
        if stop == 2:
            k.finish_build(); return nc
        for p in range(NP):
            cs = slice(p * 128, (p + 1) * 128)
            ps, pb = k.bank()
            k.mm(ps[:, 0:SEG], w2a2[0:64, cs], twb[0:64, :], True, True, [bc, b_lo], [pb])
            k.act(S[1][:, :], ps[:, 0:SEG], AF.Sigmoid, [pb, b_par], [b_S[1]], bias=W0(p))
            k.ts("dve", S[1][:, :], S[1][:, :], -0.6065306597126334, None, ALU.mult, None, [b_S[1]], [b_S[1]])
            ps, pb = k.bank()
            k.mm(ps[:, 0:SEG], w2a2[64:128, cs], twb[64:128, :], True, True, [bc, b_lo], [pb])
            k.act(S[2][:, :], ps[:, 0:SEG], AF.Sigmoid, [pb, b_par], [b_S[2]], bias=A0(p))
            k.ts("dve", S[3][:, :], Kt[:, p, :], KK(p), None, ALU.mult, None, [b_K[p], b_par], [b_S[3]])
            k.tt("dve", S[4][:, :], S[3][:, :], S[3][:, :], ALU.mult, [b_S[3]], [b_S[4]])
            ps, pb = k.bank()
            k.mm(ps[:, 0:SEG], bon1[:, :], S[4][:, :], True, True, [bc, b_S[4]], [pb])
            k.act(S[4][:, :], ps[:, 0:SEG], AF.Sqrt, [pb], [b_S[4]])
            k.ts("dve", S[4][:, :], S[4][:, :], 1e-12, None, ALU.max, None, [b_S[4]], [b_S[4]])
            k.recip("dve", S[4][:, :], S[4][:, :], [b_S[4]], [b_S[4]])
            k.tt("dve", S[3][:, :], S[3][:, :], S[4][:, :], ALU.mult, [b_S[3], b_S[4]], [b_S[3]])
            k.ts("dve", S[4][:, :], S[2][:, :], 1.0, KA(p), ALU.subtract, ALU.mult, [b_S[2], b_par], [b_S[4]])
            k.stt("dve", Kt[:, p, :], S[4][:, :], 1.0, Kt[:, p, :], ALU.add, ALU.mult, [b_S[4], b_K[p]], [b_K[p]])
            k.stt("dve", RKt[:, p, :], Rt[:, p, :], RKc(p), Kt[:, p, :], ALU.mult, ALU.mult, [b_R[p], b_K[p], b_par], [b_RK[p]])
            k.tt("dve", S[2][:, :], S[3][:, :], S[2][:, :], ALU.mult, [b_S[3], b_S[2]], [b_S[2]])
            pc_, pcb = k.bank()
            for q in range(SEG // 128):
                qs = slice(q * 128, (q + 1) * 128)
                pt, ptb = k.bank()
                k.mm(pt[:, 0:128], S[1][:, qs], identf[:, :], True, True, [b_S[1], bc], [ptb])
                k.cp("act", S[5][:, qs], pt[:, 0:128], [ptb], [b_S[5]])
                k.mm(pc_[:, qs], S[5][:, qs], tri[:, :], True, True, [b_S[5], bc], [pcb])
            k.cp("act", S[4][:, :], pc_[:, 0:SEG], [pcb], [b_S[4]])
            k.act(S[5][:, :], S[4][:, :], AF.Exp, [b_S[4]], [b_S[5]])
            k.tt("dve", Rb[:, p, :], Rt[:, p, :], S[5][:, :], ALU.mult, [b_R[p], b_S[5]], [b_Rb[p]])
            k.cp("dve", pcs[:, p, :], S[5][:, :].rearrange("p (c t) -> p c t", t=64)[:, :, 63], [b_S[5]], [b_pc[p]])
            k.act(S[6][:, :], S[4][:, :], AF.Exp, [b_S[4]], [b_S[6]], scale=-1.0)
            k.tt("dve", Kb[:, p, :], Kt[:, p, :], S[6][:, :], ALU.mult, [b_K[p], b_S[6]], [b_Kb[p]])
            k.tt("dve", BTt[:, p, :], S[2][:, :], S[6][:, :], ALU.mult, [b_S[2], b_S[6]], [b_BT[p]])
            k.tt("dve", S[4][:, :], S[4][:, :], S[1][:, :], ALU.subtract, [b_S[4], b_S[1]], [b_S[4]])
            k.act(S[4][:, :], S[4][:, :], AF.Exp, [b_S[4]], [b_S[4]])
            k.stt("dve", ATt[:, p, :], S[3][:, :], -1.0, S[4][:, :], ALU.mult, ALU.mult, [b_S[3], b_S[4]], [b_AT[p]])
            for (dst, src, bs_, bd_) in ((RtO, Rb, b_Rb, b_RO), (KtO, Kb, b_Kb, b_KO), (BTO, BTt, b_BT, b_BO), (ATO, ATt, b_AT, b_AO)):
                k.dma("sp", dst[:, p, :], src[64:128, p, :], [bs_[p]], [bd_[p]], bd_[p])
            k.dma("sp", pcsO[:, p, :], pcs[64:128, p, :], [b_pc[p]], [b_pcO[p]], b_pcO[p])
        if stop == 3:
            k.finish_build(); return nc
        for c in range(NCH):
            cg = sg_ * NCH + c
            cur = cg % 2
            cols = slice(c * 64, (c + 1) * 64)
            yps, ypb = ybanks[cg % 2]
            for p in range(NP):
                pt, ptb = k.bank()
                for i, (arr, bb) in enumerate(((Vt, b_V), (BTt, b_BT), (Kb, b_Kb))):
                    k.mm(pt[0:64, i * 128:(i + 1) * 128], arr[:, p, cols], pre.identb[:, :], True, True, [bb[p], bc], [ptb])
                k.cp("act", TM[p][:, :, :], pt[0:64, 0:384].rearrange("p (a b) -> p a b", a=3), [ptb], [b_TM[p]])
            H = []
            for h in range(HO):
                p, e = h // 2, h % 2
                rows = slice(e * 64, (e + 1) * 64)
                if e == 0:
                    bt = BTt[0:64, p, cols]; at = ATt[0:64, p, cols]; rt = Rb[0:64, p, cols]; kt = Kb[0:64, p, cols]
                    deps = [b_BT[p], b_AT[p], b_Rb[p], b_Kb[p]]
                else:
                    bt = BTO[:, p, cols]; at = ATO[:, p, cols]; rt = RtO[:, p, cols]; kt = KtO[:, p, cols]
                    deps = [b_BO[p], b_AO[p], b_RO[p], b_KO[p]]
                H.append(dict(p=p, e=e, rows=rows, bt=bt, at=at, rt=rt, kt=kt, deps=deps, b_at=deps[1], b_rt=deps[2]))
            def evac_g(g_):
                dg = H[g_]; psg, pbg = dg["ps"]
                k.tt("dve", G[g_][:, :], psg[0:64, 0:320], m5[:, :], ALU.mult, [pbg, bc], [b_G[g_]])
                k.tt("dve", NTt[g_][:, :], G[g_][:, 0:64], pre.identb[0:64, 0:64], ALU.add, [b_G[g_], bc], [b_NT[g_]])
                dg["Lk"] = G[g_][:, 256:320]; dg["Pk"] = G[g_][:, 0:64]; dg["lb"] = [b_G[g_]]
            for h in range(HO):
                d = H[h]; ps, pb = k.bank()
                k.mm(ps[0:64, 0:64], d["bt"], d["at"], True, True, d["deps"], [pb])
                k.mm(ps[0:64, 64:128], d["bt"], d["rt"], True, True, d["deps"], [pb])
                k.mm(ps[0:64, 128:192], d["kt"], d["at"], True, True, d["deps"], [pb])
                k.mm(ps[0:64, 192:256], d["kt"], d["rt"], True, True, d["deps"], [pb])
                k.mm(ps[0:64, 256:320], d["at"], d["bt"], True, True, d["deps"], [pb])
                d["ps"] = (ps, pb)
                if h >= 3:
                    evac_g(h - 3)
            for g_ in range(max(0, HO - 3), HO):
                evac_g(g_)
            for lev in range(5):
                j = lev % 2
                for h in range(HO):
                    d = H[h]; ps, pb = k.bank()
                    k.mm(ps[0:64, 0:64], d["Pk"], d["Lk"], True, True, d["lb"], [pb])
                    if lev < 4:
                        k.mm(ps[0:64, 64:128], d["Lk"], d["Pk"], True, True, d["lb"], [pb])
                    d["ps"] = (ps, pb)
                    if h >= 3:
                        g_ = h - 3; dg = H[g_]; psg, pbg = dg["ps"]
                        k.cp("act", LP[g_][j][:, :], psg[0:64, 0:128], [pbg], [b_LP[g_][j]])
                        dg["Lk"] = LP[g_][j][:, 0:64]; dg["Pk"] = LP[g_][j][:, 64:128]; dg["lb"] = [b_LP[g_][j]]
                for g_ in range(max(0, HO - 3), HO):
                    dg = H[g_]; psg, pbg = dg["ps"]
                    k.cp("act", LP[g_][j][:, :], psg[0:64, 0:128], [pbg], [b_LP[g_][j]])
                    dg["Lk"] = LP[g_][j][:, 0:64]; dg["Pk"] = LP[g_][j][:, 64:128]; dg["lb"] = [b_LP[g_][j]]
                for h in range(HO):
                    d = H[h]; ps2, pb2 = k.bank()
                    k.mm(ps2[0:64, 0:64], d["Lk"], NTt[h][:, :], True, True, d["lb"] + [b_NT[h]], [pb2])
                    d["ps2"] = (ps2, pb2)
                    if h >= 3:
                        g_ = h - 3; ps3, pb3 = H[g_]["ps2"]
                        k.tt("dve", NTt[g_][:, :], NTt[g_][:, :], ps3[0:64, 0:64], ALU.add, [pb3, b_NT[g_]], [b_NT[g_]])
                for g_ in range(max(0, HO - 3), HO):
                    ps3, pb3 = H[g_]["ps2"]
                    k.tt("dve", NTt[g_][:, :], NTt[g_][:, :], ps3[0:64, 0:64], ALU.add, [pb3, b_NT[g_]], [b_NT[g_]])
            for h in range(HO):
                d = H[h]; p = d["p"]; ps, pb = k.bank()
                vte = TM[p][:, 0, d["rows"]]
                k.mm(ps[0:64, 0:64], G[h][:, 128:192], vte, True, False, [b_G[h], b_TM[p]], [pb])
                k.mm(ps[0:64, 0:64], d["at"], Tb[h][cur][:, :], False, True, [d["b_at"], b_T[h][cur]], [pb])
                d["ps"] = (ps, pb)
                if h >= 3:
                    g_ = h - 3; psg, pbg = H[g_]["ps"]
                    k.cp("act", Wsb[g_][:, :], psg[0:64, 0:64], [pbg], [b_W[g_]])
            for g_ in range(max(0, HO - 3), HO):
                psg, pbg = H[g_]["ps"]
                k.cp("act", Wsb[g_][:, :], psg[0:64, 0:64], [pbg], [b_W[g_]])
            for h in range(HO):
                d = H[h]; ps, pb = k.bank()
                k.mm(ps[0:64, 0:64], NTt[h][:, :], Wsb[h][:, :], True, True, [b_NT[h], b_W[h]], [pb])
                d["ps"] = (ps, pb)
                if h >= 3:
                    g_ = h - 3; psg, pbg = H[g_]["ps"]
                    k.cp("act", Usb[g_][:, :], psg[0:64, 0:64], [pbg], [b_U[g_]])
            for g_ in range(max(0, HO - 3), HO):
                psg, pbg = H[g_]["ps"]
                k.cp("act", Usb[g_][:, :], psg[0:64, 0:64], [pbg], [b_U[g_]])
            for h in range(HO):
                d = H[h]; p = d["p"]
                vte = TM[p][:, 0, d["rows"]]
                yo = yps[0:64, h * 64:(h + 1) * 64]
                k.mm(yo, d["rt"], Tb[h][cur][:, :], True, False, [d["b_rt"], b_T[h][cur]], [ypb])
                k.mm(yo, G[h][:, 64:128], Usb[h][:, :], False, False, [b_G[h], b_U[h]], [ypb])
                k.mm(yo, G[h][:, 192:256], vte, False, True, [b_G[h], b_TM[p]], [ypb])
            for h in range(HO):
                d = H[h]; p = d["p"]; e = d["e"]; rows = d["rows"]
                ps, pb = k.bank()
                k.mm(ps[0:64, 0:64], TM[p][:, 1, rows], Usb[h][:, :], True, False, [b_TM[p], b_U[h]], [pb])
                k.mm(ps[0:64, 0:64], TM[p][:, 2, rows], TM[p][:, 0, rows], False, True, [b_TM[p]], [pb])
                pc_ap = pcs[0:64, p, c:c + 1] if e == 0 else pcsO[:, p, c:c + 1]
                pcb_ = b_pc[p] if e == 0 else b_pcO[p]
                tj = h % 2
                k.ts("dve", Ttmp[tj][:, :], Tst[h][cur][:, :], pc_ap, None, ALU.mult, None, [b_T[h][cur], pcb_], [b_Ttmp[tj]])
                k.stt("dve", Tst[h][1 - cur][:, :], ps[0:64, 0:64], pc_ap, Ttmp[tj][:, :], ALU.mult, ALU.add, [pb, pcb_, b_Ttmp[tj]], [b_T[h][1 - cur]])
                k.cp("act", Tb[h][1 - cur][:, :], Tst[h][1 - cur][:, :], [b_T[h][1 - cur]], [b_T[h][1 - cur]])
            if stop == 4:
                k.finish_build(); return nc
            pbn, pbnb = k.bank()
            for p in range(NP):
                k.mm(pbn[0:64, 2 * p:2 * p + 2], RKt[:, p, cols], hs[:, :], True, True, [b_RK[p], bc], [pbnb])
            k.cp("act", bon[:, :], pbn[0:64, 0:HO], [pbnb], [b_bon])
            pg, pgb = k.bank()
            k.mm(pg[0:64, 0:CH], sga[:, cols], g2a[:, :], True, False, [b_lo, bc], [pgb])
            k.mm(pg[0:64, 0:CH], sgb[:, cols], g2b[:, :], False, True, [b_lo, bc], [pgb])
            if stop == 61:
                k.finish_build(); return nc
            yv = yps[0:64, 0:CH].rearrange("p (h d) -> p h d", d=64)
            k.cp("act", Y1[:, :], yps[0:64, 0:CH], [ypb], [b_Y1])
            k.red("dve", stt_[:, 0:HO], Y1[:, :].rearrange("p (h d) -> p h d", d=64), ALU.add, AX.X, [b_Y1], [b_stt])
            if stop == 615:
                k.finish_build(); return nc
            k.tt("dve", Y2[:, :], Y1[:, :], Y1[:, :], ALU.mult, [b_Y1], [b_Y2])
            k.red("dve", stt_[:, HO:2 * HO], Y2[:, :].rearrange("p (h d) -> p h d", d=64), ALU.add, AX.X, [b_Y2], [b_stt])
            if stop == 616:
                k.finish_build(); return nc
            k.ts("dve", stt_[:, 0:HO], stt_[:, 0:HO], 1.0 / 64, None, ALU.mult, None, [b_stt], [b_stt])
            k.tt("dve", stt_[:, 2 * HO:3 * HO], stt_[:, 0:HO], stt_[:, 0:HO], ALU.mult, [b_stt], [b_stt])
            k.stt("dve", stt_[:, HO:2 * HO], stt_[:, HO:2 * HO], 1.0 / 64, stt_[:, 2 * HO:3 * HO], ALU.mult, ALU.subtract, [b_stt], [b_stt])
            if stop == 617:
                k.finish_build(); return nc
            k.ts("dve", stt_[:, HO:2 * HO], stt_[:, HO:2 * HO], 64e-5, None, ALU.add, None, [b_stt], [b_stt])
            k.act(stt_[:, HO:2 * HO], stt_[:, HO:2 * HO], AF.Ln, [b_stt], [b_stt])
            k.act(stt_[:, HO:2 * HO], stt_[:, HO:2 * HO], AF.Exp, [b_stt], [b_stt], scale=-0.5)
            if stop == 62:
                k.finish_build(); return nc
            for h in range(HO):
                hsl = slice(h * 64, (h + 1) * 64)
                k.ts("dve", Y1[:, hsl], Y1[:, hsl], stt_[:, h:h + 1], stt_[:, HO + h:HO + h + 1], ALU.subtract, ALU.mult, [b_Y1, b_stt], [b_Y1])
            k.tt("dve", Y1[:, :], Y1[:, :], lg_b[:, :], ALU.mult, [b_Y1, b_par], [b_Y1])
            k.tt("dve", Y1[:, :], Y1[:, :], lb_b[:, :], ALU.add, [b_Y1, b_par], [b_Y1])
            for h in range(HO):
                p, e = h // 2, h % 2
                hsl = slice(h * 64, (h + 1) * 64)
                k.stt("dve", Y1[:, hsl], TM[p][:, 0, e * 64:(e + 1) * 64], bon[:, h:h + 1], Y1[:, hsl], ALU.mult, ALU.add, [b_TM[p], b_bon, b_Y1], [b_Y1])
            k.tt("dve", Y2[:, :], Y1[:, :], pg[0:64, 0:CH], ALU.mult, [b_Y1, pgb], [b_Y2])
            if stop == 63:
                k.finish_build(); return nc
            k.dma("sp", yb[t0 + c * 64: t0 + (c + 1) * 64, :], Y2[:, :], [b_Y2], [], b_Y2)
            if stop == 64 + cg:
                k.finish_build(); return nc
    for b_ in ybanks:
        k.unreserve(b_)
    k.finish_build()
    return nc


def build_gm(D, NT, AW, NG, k=None):
    k = k or KB(); nc, s = k.nc, k.s; B = s.buf
    DC = D // 128; NTT = NT // 128; NB = AW // 512 if AW >= 512 else 1; CW = min(512, AW)
    k.psum_banks()
    x = k.dram("x", [NT, D]); cvec = k.dram("cvec", [1, D]); wada2 = k.dram("wada2", [D, 2 * D]); bada2 = k.dram("bada2", [1, 2 * D])
    gpre = k.dram("gpre", [1, D]); ident = k.dram("ident", [128, 128])
    wu = k.dram("wu", [D, AW]); wv = k.dram("wv", [D, AW]); vgb = k.dram("vgb", [1, 2 * AW])
    ws = k.dram("ws", [NG, 128, 128]); bs = k.dram("bs", [1, NG * 128]); triu = k.dram("triu", [128, 128])
    ya = k.dram("ya", [NT, AW], kind="ExternalOutput")
    pre = Pre(k, D, wada2, bada2, cvec, gpre, ident, nwsl=3)
    identf = pre.identf; bc = pre.b_const
    hT = k.sb("hT", [128, DC, NT], BF16); b_hT = [B() for _ in range(NTT)]
    U = k.sb("U", [128, NTT, AW]); V = k.sb("V", [128, NTT, AW]); b_U = [B() for _ in range(NTT)]; b_V = [B() for _ in range(NTT)]
    wsT = k.sb("wsT", [128, NG, 128], BF16); tmpw = k.sb("tmpw", [128, 128]); b_tw = B(); tru = k.sb("tru", [128, 128])
    bs_c = k.sb("bs_c", [128, NG]); vg_b = k.sb("vg_b", [128, AW]); vb_b = k.sb("vb_b", [128, AW]); b_par = B()
    vnb = k.sb("vnb", [128, AW], BF16); b_vn = B(); st2 = k.sb("st2", [128, 4]); b_st2 = B(); yo = k.sb("yo", [128, AW]); b_yo = B()
    junk = k.sb("junk", [128, AW], BF16); b_junk = B()
    k.dma("sp", tru[:], triu[:, :], [], [bc], bc)
    for g in range(NG):
        k.dma("sp", tmpw[:], ws[g, :, :], [], [b_tw], b_tw)
        pt, pb = k.bank()
        k.mm(pt[:, 0:128], tmpw[:, :], identf[:, :], True, True, [b_tw, bc], [pb])
        k.tt("dve", wsT[:, g, :], pt[:, 0:128], tru[:, :], ALU.mult, [pb, bc], [b_par])
    pt, pb = k.bank()
    pre.col_from_dram(bs, NG * 128, pt, pb)
    k.cp("act", bs_c[:], pt[:, 0:NG], [pb], [b_par])
    for (dst, off) in ((vg_b, 0), (vb_b, AW)):
        done = 0
        while done < AW:
            w = min(512, AW - done)
            k.dma("sp", pre.rowf[0][0:1, 0:w], vgb[0:1, off + done: off + done + w], [], [pre.b_rowf[0]], pre.b_rowf[0])
            pt, pb = k.bank()
            k.mm(pt[:, 0:w], pre.ones_f[0:1, :], pre.rowf[0][0:1, 0:w], True, True, [bc, pre.b_rowf[0]], [pb])
            k.cp("act", dst[:, done:done + w], pt[:, 0:w], [pb], [b_par])
            done += w
    for t in range(NTT):
        pre.norm_transpose(x[t * 128:(t + 1) * 128, :], hT, t * 128, b_hT[t])
    for (wsrc, dst, bufs) in ((wu, U, b_U), (wv, V, b_V)):
        for n in range(AW // CW):
            w, wb = pre.next_wsl()
            pre.load_w(w, wb, wsrc[:, n * CW:(n + 1) * CW], CW)
            for t in range(NTT):
                pt, pb = k.bank()
                for kc in range(DC):
                    k.mm(pt[:, 0:CW], hT[:, kc, t * 128:(t + 1) * 128], w[:, kc, 0:CW], kc == 0, kc == DC - 1, [b_hT[t], wb], [pb])
                k.act(dst[:, t, n * CW:(n + 1) * CW], pt[:, 0:CW], AF.Gelu, [pb], [bufs[t]])
    for t in range(NTT):
        v = V[:, t, :]
        k.red("dve", st2[:, 0:1], v, ALU.add, AX.X, [b_V[t]], [b_st2])
        k.memset("dve", st2[:, 1:2], 0.0, [b_st2])
        k.act(junk[:, :], v, AF.Square, [b_V[t], b_st2], [b_junk, b_st2], accum=st2[:, 1:2])
        k.ts("dve", st2[:, 0:1], st2[:, 0:1], 1.0 / AW, None, ALU.mult, None, [b_st2], [b_st2])
        k.tt("dve", st2[:, 2:3], st2[:, 0:1], st2[:, 0:1], ALU.mult, [b_st2], [b_st2])
        k.stt("dve", st2[:, 1:2], st2[:, 1:2], 1.0 / AW, st2[:, 2:3], ALU.mult, ALU.subtract, [b_st2], [b_st2])
        k.ts("dve", st2[:, 1:2], st2[:, 1:2], 1e-5, None, ALU.add, None, [b_st2], [b_st2])
        k.act(st2[:, 1:2], st2[:, 1:2], AF.Ln, [b_st2], [b_st2])
        k.act(st2[:, 1:2], st2[:, 1:2], AF.Exp, [b_st2], [b_st2], scale=-0.5)
        k.ts("dve", v, v, st2[:, 0:1], st2[:, 1:2], ALU.subtract, ALU.mult, [b_V[t], b_st2], [b_V[t]])
        k.tt("dve", v, v, vg_b[:, :], ALU.mult, [b_V[t], b_par], [b_V[t]])
        k.tt("dve", vnb[:, :], v, vb_b[:, :], ALU.add, [b_V[t], b_par], [b_vn])
        for g4 in range(max(1, NG // 4)):
            pt, pb = k.bank()
            ng = min(4, NG)
            for q in range(ng):
                g = g4 * 4 + q
                k.mm(pt[:, q * 128:(q + 1) * 128], wsT[:, g, :], vnb[:, g * 128:(g + 1) * 128], True, True, [b_par, b_vn], [pb])
            for q in range(ng):
                g = g4 * 4 + q
                gs = slice(g * 128, (g + 1) * 128)
                k.stt("dve", yo[:, gs], pt[:, q * 128:(q + 1) * 128], bs_c[:, g:g + 1], U[:, t, gs], ALU.add, ALU.mult, [pb, b_par, b_U[t]], [b_yo])
        k.dma("sp", ya[t * 128:(t + 1) * 128, :], yo[:, :], [b_yo], [], b_yo)
    k.finish_build()
    return nc


NEG = -1.0e30


def build_mb(D, T, HO, SEG=512, k=None, xfn=None):
    k = k or KB(); nc, s = k.nc, k.s; B = s.buf
    DC = D // 128; DH = 128; BLK = 256; NBLK = T // BLK; NQT = T // 128; HC = HO * DH; NSEG = T // SEG; CW = min(512, HC)
    assert NBLK == 8
    k.psum_banks()
    x = k.dram("x", [T, D]); cvec = k.dram("cvec", [1, D]); wada2 = k.dram("wada2", [D, 2 * D]); bada2 = k.dram("bada2", [1, 2 * D])
    gpre = k.dram("gpre", [1, D]); ident = k.dram("ident", [128, 128])
    wq = k.dram("wq", [D, HC]); wk = k.dram("wk", [D, HC]); wv = k.dram("wv", [D, HC])
    slope = k.dram("slope", [1, 128]); kpos = k.dram("kpos", [128, T]); cmask = k.dram("cmask", [128, 128]); gmask = k.dram("gmask", [128, 64])
    o = k.dram("o", [T, HC], kind="ExternalOutput")
    pre = Pre(k, D, wada2, bada2, cvec, gpre, ident, nwsl=2)
    identb = pre.identb; bc = pre.b_const
    hTs = k.sb("hTs", [128, DC, SEG], BF16); b_hT = B()
    QF = k.sb("QF", [128, HO, T], BF16); KF = k.sb("KF", [128, HO, T], BF16); VT = k.sb("VT", [128, NQT, HC], BF16)
    b_Q = [B() for _ in range(HO)]; b_K = [B() for _ in range(HO)]; b_V = [B() for _ in range(NQT)]
    kp = k.sb("kp", [128, T]); cm = k.sb("cm", [128, 128]); gmc = k.sb("gmc", [128, 64]); slc = k.sb("slc", [128, 128])
    kmf = k.sb("kmf", [128, 8]); kmb = k.sb("kmb", [128, HO, 8], BF16); b_km = B()
    ali = k.sb("ali", [128, T]); b_ali = B()
    Ssb = k.sb("Ssb", [128, T]); b_S = B(); Pb = k.sb("Pb", [128, T], BF16); b_P = B()
    PT = k.sb("PT", [128, NQT, 128], BF16); b_PT = B()
    gsb = k.sb("gsb", [128, 8]); mx8 = k.sb("mx8", [128, 8]); selb = k.sb("selb", [128, 8]); b_g = B()
    sm = k.sb("sm", [128, 4]); b_sm = B(); osb = [k.sb("osb%d" % i, [128, 128]) for i in range(2)]; b_o = [B(), B()]
    for (dst, src) in ((kp, kpos), (cm, cmask), (gmc, gmask)):
        k.dma("sp", dst[:], src[:, :], [], [bc], bc)
    k.dma("sp", pre.rowf[0][0:1, 0:128], slope[0:1, :], [], [pre.b_rowf[0]], pre.b_rowf[0])
    pt, pb = k.bank()
    k.mm(pt[:, 0:128], pre.ones_f[0:1, :], pre.rowf[0][0:1, 0:128], True, True, [bc, pre.b_rowf[0]], [pb])
    k.cp("act", slc[:, :], pt[:, 0:128], [pb], [bc])
    for sg_ in range(NSEG):
        t0 = sg_ * SEG
        for tq in range(SEG // 128):
            xsrc_ = xfn(sg_ * (SEG // 128) + tq) if xfn is not None else x[t0 + tq * 128: t0 + (tq + 1) * 128, :]
            pre.norm_transpose(xsrc_, hTs, tq * 128, b_hT)
        for (wsrc, dst, bufs, sc_) in ((wq, QF, b_Q, DH ** -0.5), (wk, KF, b_K, None)):
            for n in range(HC // CW):
                w, wb = pre.next_wsl()
                pre.load_w(w, wb, wsrc[:, n * CW:(n + 1) * CW], CW)
                for hh in range(CW // 128):
                    h = n * (CW // 128) + hh
                    pt, pb = k.bank()
                    for kc in range(DC):
                        k.mm(pt[:, 0:SEG], w[:, kc, hh * 128:(hh + 1) * 128], hTs[:, kc, :], kc == 0, kc == DC - 1, [wb, b_hT], [pb])
                    if sc_ is not None:
                        k.act(dst[:, h, t0:t0 + SEG], pt[:, 0:SEG], AF.Copy, [pb], [bufs[h]], scale=sc_)
                    else:
                        k.cp("dve", dst[:, h, t0:t0 + SEG], pt[:, 0:SEG], [pb], [bufs[h]])
        for n in range(HC // CW):
            w, wb = pre.next_wsl()
            pre.load_w(w, wb, wv[:, n * CW:(n + 1) * CW], CW)
            for tq in range(SEG // 128):
                tt_ = sg_ * (SEG // 128) + tq
                pt, pb = k.bank()
                for kc in range(DC):
                    k.mm(pt[:, 0:CW], hTs[:, kc, tq * 128:(tq + 1) * 128], w[:, kc, 0:CW], kc == 0, kc == DC - 1, [wb, b_hT], [pb])
                if tq % 2 == 0:
                    k.cp("act", VT[:, tt_, n * CW:(n + 1) * CW], pt[:, 0:CW], [pb], [b_V[tt_]])
                else:
                    k.cp("dve", VT[:, tt_, n * CW:(n + 1) * CW], pt[:, 0:CW], [pb], [b_V[tt_]])
    for h in range(HO):
        k.red("dve", kmf[:, :], KF[:, h, :].rearrange("p (n s) -> p n s", s=BLK), ALU.add, AX.X, [b_K[h]], [b_km])
        k.ts("dve", kmb[:, h, :], kmf[:, :], 1.0 / BLK, None, ALU.mult, None, [b_km], [b_km])
    it = 0
    for h in range(HO):
        k.ts("dve", ali[:, :], kp[:, :], slc[:, h:h + 1], None, ALU.mult, None, [bc], [b_ali])
        for qt in range(NQT):
            qb = qt // 2; nk = (qt + 1) * 128
            ql = QF[:, h, qt * 128:(qt + 1) * 128]
            pg, pgb = k.bank()
            k.mm(pg[:, 0:8], ql, kmb[:, h, :], True, True, [b_Q[h], b_km], [pgb])
            k.tt("dve", gsb[:, :], pg[:, 0:8], gmc[:, qb * 8:(qb + 1) * 8], ALU.add, [pgb, bc], [b_g])
            k.max8(mx8[:, :], gsb[:, :], [b_g], [b_g])
            k.ts("dve", selb[:, :], gsb[:, :], mx8[:, 2:3], 1.0, ALU.is_ge, ALU.subtract, [b_g], [b_g])
            k.ts("dve", selb[:, :], selb[:, :], 1.0e30, None, ALU.mult, None, [b_g], [b_g])
            for kg in range((nk + 511) // 512):
                w_ = min(512, nk - kg * 512)
                ps, pb = k.bank()
                k.mm(ps[:, 0:w_], ql, KF[:, h, kg * 512: kg * 512 + w_], True, True, [b_Q[h], b_K[h]], [pb])
                for kt in range(kg * 4, kg * 4 + w_ // 128):
                    n = kt // 2
                    lo = (kt - kg * 4) * 128
                    ks = slice(kt * 128, (kt + 1) * 128)
                    if n < qb:
                        k.stt("dve", Ssb[:, ks], ps[:, lo:lo + 128], selb[:, n:n + 1], ali[:, ks], ALU.add, ALU.add, [pb, b_g, b_ali], [b_S])
                    else:
                        k.tt("dve", Ssb[:, ks], ps[:, lo:lo + 128], ali[:, ks], ALU.add, [pb, b_ali], [b_S])
                        if kt == qt:
                            k.tt("dve", Ssb[:, ks], Ssb[:, ks], cm[:, :], ALU.add, [b_S, bc], [b_S])
            k.red("dve", sm[:, 0:1], Ssb[:, 0:nk], ALU.max, AX.X, [b_S], [b_sm])
            k.ts("dve", sm[:, 0:1], sm[:, 0:1], -1.0, None, ALU.mult, None, [b_sm], [b_sm])
            k.memset("dve", sm[:, 1:2], 0.0, [b_sm])
            k.act(Pb[:, 0:nk], Ssb[:, 0:nk], AF.Exp, [b_S, b_sm], [b_P, b_sm], bias=sm[:, 0:1], accum=sm[:, 1:2])
            k.recip("dve", sm[:, 2:3], sm[:, 1:2], [b_sm], [b_sm])
            for g4 in range((qt + 4) // 4):
                nq = min(4, qt + 1 - g4 * 4)
                pt, pb = k.bank()
                for q in range(nq):
                    kt = g4 * 4 + q
                    k.mm(pt[:, q * 128:(q + 1) * 128], Pb[:, kt * 128:(kt + 1) * 128], identb[:, :], True, True, [b_P, bc], [pb])
                src = pt[:, 0:nq * 128].rearrange("p (a b) -> p a b", a=nq)
                if g4 % 2 == 0:
                    k.cp("act", PT[:, g4 * 4:g4 * 4 + nq, :], src, [pb], [b_PT])
                else:
                    k.cp("dve", PT[:, g4 * 4:g4 * 4 + nq, :], src, [pb], [b_PT])
            po, pob = k.bank()
            for kt in range(qt + 1):
                k.mm(po[:, 0:128], PT[:, kt, :], VT[:, kt, h * 128:(h + 1) * 128], kt == 0, kt == qt, [b_PT, b_V[kt]], [pob])
            j = it % 2; it += 1
            k.ts("dve", osb[j][:, :], po[:, 0:128], sm[:, 2:3], None, ALU.mult, None, [pob, b_sm], [b_o[j]])
            k.dma("sp", o[qt * 128:(qt + 1) * 128, h * 128:(h + 1) * 128], osb[j][:, :], [b_o[j]], [], b_o[j])
    k.finish_build()
    return nc


def _ag_chunks(k, loc, rows, cols, name, groups, cbuf):
    rpc = (1 << 20) // (cols * 4)
    n = rows // rpc
    gs = [k.scratch("%s_%d" % (name, i), [2 * rpc, cols]) for i in range(n)]
    for i in range(n):
        k.allgather(gs[i][:, :], loc[i * rpc:(i + 1) * rpc, :], groups, [], [], cbuf)
    return gs, rpc


def _grow(gs, rpc, r, tau):
    ci = tau // rpc; w = tau % rpc
    return gs[ci][r * rpc + w: r * rpc + w + 128, :]


def build_fused(D, F, T, HOA, HOM, AW, NG, NCORES=8, SEG=512, HCF=256, TN=512):
    k = KB(); k.fused = True
    NTo = T // 2; CH = 64 * HOA; HC = 128 * HOM
    groups = [[2 * i, 2 * i + 1] for i in range(NCORES // 2)]
    k.psum_banks()
    ya_loc = k.scratch("ya_loc", [NTo, AW]); yb_loc = k.scratch("yb_loc", [T, CH])
    x1loc = k.scratch("x1loc", [NTo, D]); o_loc = k.scratch("o_loc", [T, HC])
    xmid0 = k.scratch("xmid0", [NTo, D]); xmid1 = k.scratch("xmid1", [NTo, D])
    cb = k.s.buf()
    k.prefix = "gm_"; k.alias["gm_ya"] = ya_loc
    build_gm(D, NTo, AW, NG, k=k); k.end_phase()
    k.prefix = "rw_"; k.alias["rw_yb"] = yb_loc
    build_rw(D, T, HOA, SEG, k=k); k.end_phase()
    ybg, rp0 = _ag_chunks(k, yb_loc, T, CH, "ybg", groups, cb); k.s.barrier()
    k.prefix = "b0_"; k.alias["b0_xmid"] = xmid0; k.alias["b0_xout"] = x1loc
    ys0 = [(0, AW, lambda t: ya_loc[t * 128:(t + 1) * 128, :], None)]
    for r in range(2):
        ys0.append((AW + r * CH, CH, (lambda t, r=r: _grow(ybg, rp0, r, t * 128)), (lambda t, r=r: _grow(ybg, rp0, r, NTo + t * 128))))
    build_bd(D, F, NTo, HCF, TN, k=k, ysrc=ys0); k.end_phase()
    x1g, rp1 = _ag_chunks(k, x1loc, NTo, D, "x1g", groups, cb); k.s.barrier()
    k.prefix = "mb_"; k.alias["mb_o"] = o_loc; k.alias["mb_x"] = x1loc
    NTT = NTo // 128
    build_mb(D, T, HOM, SEG, k=k, xfn=lambda tt: _grow(x1g, rp1, tt // NTT, (tt % NTT) * 128)); k.end_phase()
    ogs, rp2 = _ag_chunks(k, o_loc, T, HC, "og", groups, cb); k.s.barrier()
    k.prefix = "b1_"; k.alias["b1_xmid"] = xmid1; k.alias["b1_xin"] = x1loc
    ys1 = []
    for r in range(2):
        ys1.append((r * HC, HC, (lambda t, r=r: _grow(ogs, rp2, r, t * 128)), (lambda t, r=r: _grow(ogs, rp2, r, NTo + t * 128))))
    build_bd(D, F, NTo, HCF, TN, k=k, ysrc=ys1); k.end_phase()
    k.s.finish()
    return k.nc


from concourse.bass_utils import run_bass_kernel_spmd

_D = 2048; _F = 5632; _S = 2048; _NB = 4; _NC = 8
_PROG = {}


def _c(a):
    return np.ascontiguousarray(a, dtype=np.float32)


def kernel(x, c, w_ada, b_ada, g_pre_mix, g_post_mix, g_pre_ffn, g_post_ffn, w_ffn_in, w_ffn_out, w_in_ab, w_out_ab,
           a_v_gain, a_v_bias, a_w_s, a_b_s, b_mu, b_w0, b_w2, b_a0, b_a2, b_g2, b_k_k, b_k_a, b_r_k, b_lnx_gain,
           b_lnx_bias, w_qkv, w_o):
    f = np.float32
    x = np.asarray(x, f); c = np.asarray(c, f); w_ada = np.asarray(w_ada, f); b_ada = np.asarray(b_ada, f)
    D = _D
    if "f" not in _PROG:
        _PROG["f"] = build_fused(_D, _F, _S, 8, 8, 1024, 8)
    nc = _PROG["f"]
    ident = np.eye(128, dtype=f)
    own = lambda hh: slice(hh * 1024, (hh + 1) * 1024)
    su = np.triu(np.ones((64, 64)), 1); iu = np.triu(np.ones((64, 64)), 0)
    mask5 = np.concatenate([su, iu, su, iu, su.T], axis=1).astype(f)
    blk = np.kron(np.eye(2), np.ones((64, 64))).astype(f)
    tri2 = (blk * np.triu(np.ones((128, 128)))).astype(f)
    hsel = np.kron(np.eye(2), np.ones((64, 1))).astype(f)
    triu = np.triu(np.ones((128, 128))).astype(f)
    kpos = np.tile(np.arange(_S, dtype=f)[None, :], (128, 1))
    cmask = np.where(np.arange(128)[None, :] <= np.arange(128)[:, None], 0.0, -1e30).astype(f)
    gmask = np.zeros((128, 64), f)
    for qb in range(8):
        for n in range(8):
            gmask[:, qb * 8 + n] = 0.0 if n < qb else -1e30
    w_in_ab0 = np.asarray(w_in_ab[0], f)
    wada0_a = _c(w_ada[0][:, 0:2 * D]); bada0_a = _c(b_ada[0][None, 0:2 * D])
    wada0_b = _c(w_ada[0][:, 2 * D:6 * D]); bada0_b = _c(b_ada[0][None, 2 * D:6 * D])
    wada1_a = _c(w_ada[1][:, 0:2 * D]); bada1_a = _c(b_ada[1][None, 0:2 * D])
    wada1_b = _c(w_ada[1][:, 2 * D:6 * D]); bada1_b = _c(b_ada[1][None, 2 * D:6 * D])
    wu = _c(w_in_ab0[:, 0:1024]); wvv = _c(w_in_ab0[:, 1024:2048])
    vgb = _c(np.concatenate([np.asarray(a_v_gain[0]), np.asarray(a_v_bias[0])])[None, :])
    ws = _c(a_w_s[0]); bs = _c(np.asarray(a_b_s[0]).reshape(1, 1024))
    gpre0 = _c(np.asarray(g_pre_mix[0])[None, :]); gpre1 = _c(np.asarray(g_pre_mix[1])[None, :])
    mu = np.asarray(b_mu[0], f); rk = np.asarray(b_r_k[0], f).reshape(1024)
    wqkv = np.asarray(w_qkv[0], f)
    shared = {}
    rwp = []; mbp = []
    for hh in range(2):
        cs = slice(hh * 512, (hh + 1) * 512)
        mu_cat = np.zeros((1, 15 * 128), f)
        mu_cat[0, 0:512] = mu[0:1024][cs]; mu_cat[0, 512:1024] = mu[1024:2048][cs]; mu_cat[0, 1024:1536] = mu[2048:3072][cs]
        mu_cat[0, 1536:1536 + 288] = mu[3072:3360]
        pvec = np.concatenate([np.asarray(b_w0[0], f)[cs], np.asarray(b_a0[0], f)[cs], np.asarray(b_k_k[0], f)[cs],
                               np.asarray(b_k_a[0], f)[cs], rk[cs]])[None, :]
        lnx = np.concatenate([np.asarray(b_lnx_gain[0], f)[cs], np.asarray(b_lnx_bias[0], f)[cs]])[None, :]
        rwp.append(dict(rw_wr=_c(w_in_ab0[:, 2048:3072][:, cs]), rw_wk=_c(w_in_ab0[:, 3072:4096][:, cs]), rw_wv=_c(w_in_ab0[:, 4096:5120][:, cs]),
                        rw_mu_cat=mu_cat, rw_pvec=_c(pvec), rw_lnx=_c(lnx), rw_w2=_c(np.asarray(b_w2[0], f)[:, cs]),
                        rw_a2=_c(np.asarray(b_a2[0], f)[:, cs]), rw_g2=_c(np.asarray(b_g2[0], f)[:, cs])))
        cs2 = slice(hh * 1024, (hh + 1) * 1024)
        sl = np.zeros((1, 128), f)
        sl[0, 0:8] = 2.0 ** (-8.0 * (np.arange(8) + hh * 8 + 1) / 16.0)
        mbp.append(dict(mb_wq=_c(wqkv[:, 0:2048][:, cs2]), mb_wk=_c(wqkv[:, 2048:4096][:, cs2]), mb_wv=_c(wqkv[:, 4096:6144][:, cs2]), mb_slope=sl))
    wl = _c(w_in_ab0[:, 5120:5408])
    com = dict(
        gm_wada2=wada0_a, gm_bada2=bada0_a, gm_gpre=gpre0, gm_ident=ident, gm_wu=wu, gm_wv=wvv, gm_vgb=vgb, gm_ws=ws, gm_bs=bs, gm_triu=triu,
        rw_wada2=wada0_a, rw_bada2=bada0_a, rw_gpre=gpre0, rw_ident=ident, rw_wl=wl, rw_mask5=mask5, rw_tri2=tri2, rw_bones=blk, rw_hsel=hsel,
        b0_wada=wada0_b, b0_bada=bada0_b, b0_wout=_c(w_out_ab[0]), b0_gpost=_c(np.asarray(g_post_mix[0])[None]), b0_gpre=_c(np.asarray(g_pre_ffn[0])[None]),
        b0_gpost2=_c(np.asarray(g_post_ffn[0])[None]), b0_wfi=_c(w_ffn_in[0]), b0_wfo=_c(w_ffn_out[0]), b0_ident=ident,
        mb_wada2=wada1_a, mb_bada2=bada1_a, mb_gpre=gpre1, mb_ident=ident, mb_kpos=kpos, mb_cmask=cmask, mb_gmask=gmask,
        b1_wada=wada1_b, b1_bada=bada1_b, b1_wout=_c(w_o[0]), b1_gpost=_c(np.asarray(g_post_mix[1])[None]), b1_gpre=_c(np.asarray(g_pre_ffn[1])[None]),
        b1_gpost2=_c(np.asarray(g_post_ffn[1])[None]), b1_wfi=_c(w_ffn_in[1]), b1_wfo=_c(w_ffn_out[1]), b1_ident=ident)
    maps = []
    for core in range(_NC):
        b, hh = core // 2, core % 2
        sel = np.zeros((128, 2), f); sel[:, hh] = 1.0
        cv = _c(c[b][None, :])
        m = dict(com)
        m.update(rwp[hh]); m.update(mbp[hh])
        m.update(gm_x=_c(x[b, own(hh)]), gm_cvec=cv, rw_x=_c(x[b]), rw_cvec=cv, b0_xin=_c(x[b, own(hh)]), b0_cvec=cv, b0_sel=sel,
                 mb_cvec=cv, b1_cvec=cv, b1_sel=sel)
        maps.append(m)
    res = run_bass_kernel_spmd(nc, maps, core_ids=list(range(_NC))).results
    out = np.zeros((_NB, _S, D), f)
    for core in range(_NC):
        b, hh = core // 2, core % 2
        out[b, own(hh)] = res[core]["b1_xout"]
    return out
```

```python
import numpy as np
import concourse.bass as bass
import concourse.mybir as mybir

F32 = mybir.dt.float32
BF16 = mybir.dt.bfloat16
AF = mybir.ActivationFunctionType
ALU = mybir.AluOpType
AX = mybir.AxisListType


class Buf:
    __slots__ = ("name", "lw", "rd", "dsem", "demit")

    def __init__(self, name):
        self.name = name
        self.lw = None
        self.rd = []
        self.dsem = {}
        self.demit = {}


class Op:
    __slots__ = ("eng", "emit", "deps", "dbuf", "needed", "sig", "inc", "barrier")

    def __init__(self, eng, emit, deps, dbuf, inc=16):
        self.eng = eng
        self.emit = emit
        self.deps = deps
        self.dbuf = dbuf
        self.needed = False
        self.sig = 0
        self.inc = inc
        self.barrier = False


class Sch:
    def __init__(self, nc, same_engine_sync=True):
        self.nc = nc
        self.E = {"pe": nc.tensor, "act": nc.scalar, "dve": nc.vector, "pool": nc.gpsimd, "sp": nc.sync}
        self.ops = []
        self.same = same_engine_sync
        self.nbuf = 0

    def buf(self, name=None):
        self.nbuf += 1
        return Buf(name or "b%d" % self.nbuf)

    def barrier(self):
        op = Op("sp", None, [], None)
        op.barrier = True
        self.ops.append(op)

    def add(self, eng, emit, reads=(), writes=(), dma=None, inc=16):
        deps = []
        for b in reads:
            if b.lw is not None:
                deps.append(b.lw)
        for b in writes:
            if b.lw is not None:
                deps.append(b.lw)
            deps.extend(b.rd)
        op = Op(eng, emit, deps, dma, inc)
        for d in deps:
            if d.dbuf is None and d.eng == eng and (eng == "pe" or not self.same):
                continue
            d.needed = True
        for b in reads:
            b.rd.append(op)
        for b in writes:
            b.lw = op
            b.rd = []
        self.ops.append(op)
        return op

    def finish(self):
        nc = self.nc
        esem = {k: nc.alloc_semaphore("es_" + k) for k in self.E}
        cnt = {k: 0 for k in self.E}
        last = {}
        for op in self.ops:
            if op.barrier:
                for o in last.values():
                    o.needed = True
            elif op.dbuf is None:
                last[op.eng] = op
        for op in self.ops:
            if op.barrier:
                continue
            if op.dbuf is None and op.needed:
                cnt[op.eng] += 1
                op.sig = cnt[op.eng]
        waited = {k: {} for k in self.E}
        dbufs = []
        nsem = len(esem)
        lastsig = {k: 0 for k in self.E}
        for op in self.ops:
            if op.barrier:
                for en, e in self.E.items():
                    w = waited[en]
                    for x in self.E:
                        if x == en or lastsig[x] == 0:
                            continue
                        key = ("e", x)
                        if w.get(key, 0) < lastsig[x]:
                            e.wait_ge(esem[x], lastsig[x]); w[key] = lastsig[x]
                    for b, q in dbufs:
                        key = ("d", id(b), q)
                        if w.get(key, 0) < b.demit[q]:
                            e.wait_ge(b.dsem[q], b.demit[q]); w[key] = b.demit[q]
                continue
            eng = self.E[op.eng]
            need = {}
            for d in op.deps:
                if d.dbuf is not None:
                    key = ("d", id(d.dbuf), d.eng)
                    sem = d.dbuf.dsem[d.eng]
                    val = d.dbuf.demit[d.eng]
                else:
                    if d.eng == op.eng and (d.eng == "pe" or not self.same):
                        continue
                    key = ("e", d.eng)
                    sem = esem[d.eng]
                    val = d.sig
                if key not in need or need[key][1] < val:
                    need[key] = (sem, val)
            w = waited[op.eng]
            for key, (sem, val) in need.items():
                if w.get(key, 0) >= val:
                    continue
                eng.wait_ge(sem, val)
                w[key] = val
            inst = op.emit()
            if op.dbuf is not None:
                b = op.dbuf
                if op.eng not in b.dsem:
                    b.dsem[op.eng] = nc.alloc_semaphore("ds_%d" % nsem)
                    b.demit[op.eng] = 0
                    nsem += 1
                    dbufs.append((b, op.eng))
                inst.then_inc(b.dsem[op.eng], op.inc)
                b.demit[op.eng] += op.inc
            elif op.needed:
                inst.then_inc(esem[op.eng], 1)
                lastsig[op.eng] = op.sig
        for b, q in dbufs:
            nc.sync.wait_ge(b.dsem[q], b.demit[q])
        self.nsem = nsem
        return nsem


class KB:
    def __init__(self, name="k"):
        self.nc = bass.Bass("TRN2", target_bir_lowering=False)
        self.s = Sch(self.nc)
        self.nps = 0
        self.ps_banks = None
        self.prefix = ""
        self.alias = {}
        self.cms = []
        self.fused = False

    def dram(self, name, shape, dt=F32, kind="ExternalInput"):
        name = self.prefix + name
        if name in self.alias:
            return self.alias[name]
        return self.nc.dram_tensor(name, list(shape), dt, kind=kind).ap()

    def scratch(self, name, shape, dt=F32, shared=False):
        if shared:
            return self.nc.dram_tensor(name, list(shape), dt, addr_space="Shared").ap()
        return self.nc.dram_tensor(name, list(shape), dt).ap()

    def sb(self, name, shape, dt=F32):
        if not self.fused:
            return self.nc.alloc_sbuf_tensor(self.prefix + name, list(shape), dt)
        cm = self.nc.sbuf_tensor(self.prefix + name, list(shape), dt)
        t = cm.__enter__()
        self.cms.append(cm)
        return t

    def end_phase(self):
        self.s.barrier()
        for cm in reversed(self.cms):
            cm.__exit__(None, None, None)
        self.cms = []

    def finish_build(self):
        if not self.fused:
            self.s.finish()

    def allgather(self, out_ap, in_ap, groups, R, W, cbuf):
        nc = self.nc
        return self.s.add("pool", lambda: nc.gpsimd.collective_compute("AllGather", mybir.AluOpType.bypass, replica_groups=groups, ins=[in_ap], outs=[out_ap]), R, W, dma=cbuf, inc=1)

    def psum_banks(self):
        if self.ps_banks is not None:
            return
        self.ps_banks = []
        for i in range(8):
            t = self.nc.alloc_psum_tensor("psb%d" % i, [128, 512], F32)
            self.ps_banks.append((t, self.s.buf("ps%d" % i)))
        self.ps_i = 0

    def bank(self):
        n = len(self.ps_banks)
        b = self.ps_banks[self.ps_i % n]
        self.ps_i += 1
        return b

    def reserve(self):
        return self.ps_banks.pop()

    def unreserve(self, b):
        self.ps_banks.append(b)

    def mm(self, out, lhsT, rhs, start, stop, R, W):
        nc = self.nc
        return self.s.add("pe", lambda: nc.tensor.matmul(out, lhsT, rhs, start=start, stop=stop), R, W)

    def act(self, out, in_, func, R, W, bias=None, scale=None, accum=None):
        nc = self.nc
        kw = {}
        if bias is not None:
            kw["bias"] = bias
        if scale is not None:
            kw["scale"] = scale
        if accum is not None:
            kw["accum_out"] = accum
        return self.s.add("act", lambda: nc.scalar.activation(out=out, in_=in_, func=func, **kw), R, W)

    def _e(self, eng):
        return {"dve": self.nc.vector, "pool": self.nc.gpsimd}[eng]

    def tt(self, eng, out, in0, in1, op, R, W):
        e = self._e(eng)
        return self.s.add(eng, lambda: e.tensor_tensor(out=out, in0=in0, in1=in1, op=op), R, W)

    def ts(self, eng, out, in0, s1, s2, op0, op1, R, W, accum=None):
        e = self._e(eng)
        if op1 is None:
            return self.s.add(eng, lambda: e.tensor_scalar(out=out, in0=in0, scalar1=s1, scalar2=None, op0=op0), R, W)
        if accum is not None:
            return self.s.add(eng, lambda: e.tensor_scalar(out=out, in0=in0, scalar1=s1, scalar2=s2, op0=op0, op1=op1, accum_out=accum), R, W)
        return self.s.add(eng, lambda: e.tensor_scalar(out=out, in0=in0, scalar1=s1, scalar2=s2, op0=op0, op1=op1), R, W)

    def stt(self, eng, out, in0, scalar, in1, op0, op1, R, W):
        e = self._e(eng)
        return self.s.add(eng, lambda: e.scalar_tensor_tensor(out=out, in0=in0, scalar=scalar, in1=in1, op0=op0, op1=op1), R, W)

    def cp(self, eng, out, in_, R, W):
        if eng == "act":
            nc = self.nc
            return self.s.add("act", lambda: nc.scalar.copy(out=out, in_=in_), R, W)
        e = self._e(eng)
        return self.s.add(eng, lambda: e.tensor_copy(out=out, in_=in_), R, W)

    def red(self, eng, out, in_, op, axis, R, W):
        e = self._e(eng)
        return self.s.add(eng, lambda: e.tensor_reduce(out=out, in_=in_, axis=axis, op=op), R, W)

    def memset(self, eng, ap, val, W):
        e = self._e(eng)
        return self.s.add(eng, lambda: e.memset(ap, val), (), W)

    def dma(self, q, out, in_, R, W, dbuf):
        e = {"sp": self.nc.sync, "pool": self.nc.gpsimd, "act": self.nc.scalar}[q]
        return self.s.add(q, lambda: e.dma_start(out=out, in_=in_), R, W, dma=dbuf)


def _kb_recip(self, eng, out, in_, R, W):
    e = self._e(eng)
    return self.s.add(eng, lambda: e.reciprocal(out=out, in_=in_), R, W)


KB.recip = _kb_recip


def _kb_max8(self, out, in_, R, W):
    nc = self.nc
    return self.s.add("dve", lambda: nc.vector.max(out=out, in_=in_), R, W)


KB.max8 = _kb_max8


EPS = 1e-6


def row_to_col(k, row_t, row_b, ncols, ps_ap, ps_b, one_f, b_const):
    for i in range(ncols):
        k.mm(ps_ap[:, i:i + 1], row_t[0:1, i * 128:(i + 1) * 128], one_f[0:1, 0:1], True, True, [row_b, b_const], [ps_b])


class Pre:
    def __init__(self, k, D, wada2, bada2, cvec, gpre, ident, nwsl=3):
        self.k = k; s = k.s; B = s.buf
        self.D = D; DC = D // 128; self.DC = DC
        self.identf = k.sb("identf", [128, 128]); self.identb = k.sb("identb", [128, 128], BF16)
        self.ones_f = k.sb("ones_f", [1, 128]); self.ones_b = k.sb("ones_b", [1, 128], BF16)
        self.rowf = [k.sb("rowf%d" % i, [1, 512]) for i in range(2)]
        self.rowb = [k.sb("rowb%d" % i, [1, 512], BF16) for i in range(2)]
        self.condT_f = k.sb("condT_f", [128, DC]); self.condT_b = k.sb("condT_b", [128, DC], BF16)
        self.modc = k.sb("modc", [128, 2 * DC]); self.gpre_c = k.sb("gpre_c", [128, DC]); self.gs_c = k.sb("gs_c", [128, DC])
        self.wsl = [k.sb("wsl%d" % i, [128, DC, 512], BF16) for i in range(nwsl)]
        self.xt = k.sb("xt", [128, D]); self.xn = k.sb("xn", [128, D], BF16); self.st = k.sb("st", [128, 8])
        self.b_const = B(); self.b_rowf = [B(), B()]; self.b_rowb = [B(), B()]; self.b_cond = B(); self.b_modc = B()
        self.b_gpre = B(); self.b_gs = B(); self.b_wsl = [B() for _ in range(nwsl)]; self.b_xt = B(); self.b_xn = B(); self.b_st = B()
        self.nw = 0; self.nwsl = nwsl
        k.dma("sp", self.identf[:], ident[:, :], [], [self.b_const], self.b_const)
        k.cp("dve", self.identb[:], self.identf[:], [self.b_const], [self.b_const])
        k.memset("dve", self.ones_f[:], 1.0, [self.b_const]); k.memset("dve", self.ones_b[:], 1.0, [self.b_const])
        pt, pb = k.bank()
        self.col_from_dram(cvec, D, pt, pb)
        k.act(self.condT_f[:], pt[:, 0:DC], AF.Silu, [pb], [self.b_cond])
        k.cp("dve", self.condT_b[:], self.condT_f[:], [self.b_cond], [self.b_cond])
        pt, pb = k.bank()
        self.col_from_dram(gpre, D, pt, pb)
        k.cp("act", self.gpre_c[:], pt[:, 0:DC], [pb], [self.b_gpre])
        pc, pcb = k.bank()
        for j in range(2 * D // 512):
            w, wb = self.next_wsl()
            self.load_w(w, wb, wada2[:, j * 512:(j + 1) * 512], 512)
            r = j % 2
            k.dma("pool", self.rowb[r][0:1, :], bada2[0:1, j * 512:(j + 1) * 512], [], [self.b_rowb[r]], self.b_rowb[r])
            for q in range(4):
                col = j * 4 + q
                for kc in range(DC):
                    k.mm(pc[:, col:col + 1], w[:, kc, q * 128:(q + 1) * 128], self.condT_b[:, kc:kc + 1], kc == 0, False, [self.b_cond, wb], [pcb])
                k.mm(pc[:, col:col + 1], self.rowb[r][0:1, q * 128:(q + 1) * 128], self.ones_b[0:1, 0:1], False, True, [self.b_rowb[r], self.b_const], [pcb])
        k.cp("act", self.modc[:], pc[:, 0:2 * DC], [pcb], [self.b_modc])
        k.stt("dve", self.gs_c[:], self.modc[:, DC:2 * DC], 1.0, self.gpre_c[:], ALU.add, ALU.mult, [self.b_modc, self.b_gpre], [self.b_gs])

    def next_wsl(self):
        i = self.nw % self.nwsl; self.nw += 1
        return self.wsl[i], self.b_wsl[i]

    def load_w(self, dst, dbuf, src_cols, width, off=0):
        self.k.dma("pool", dst[:, :, off:off + width], src_cols.rearrange("(kc p) n -> p kc n", p=128), [], [dbuf], dbuf)

    def col_from_dram(self, vec, n, pt, pb, col0=0):
        k = self.k
        done = 0
        j = 0
        while done < n:
            w = min(512, n - done)
            r = j % 2
            k.dma("sp", self.rowf[r][0:1, 0:w], vec[0:1, done:done + w], [], [self.b_rowf[r]], self.b_rowf[r])
            row_to_col(k, self.rowf[r], self.b_rowf[r], w // 128, pt[:, col0 + done // 128: col0 + (done + w) // 128], pb, self.ones_f, self.b_const)
            done += w; j += 1

    def rstd_of(self, src_ap, src_bufs, col, n):
        k = self.k; st = self.st; b_st = self.b_st
        k.memset("dve", st[:, col:col + 1], 0.0, [b_st])
        k.act(self.xn[:, 0:n], src_ap, AF.Square, src_bufs + [b_st], [self.b_xn, b_st], accum=st[:, col:col + 1])
        k.ts("dve", st[:, col:col + 1], st[:, col:col + 1], 1.0 / n, EPS, ALU.mult, ALU.add, [b_st], [b_st])
        k.act(st[:, col:col + 1], st[:, col:col + 1], AF.Ln, [b_st], [b_st])
        k.act(st[:, col:col + 1], st[:, col:col + 1], AF.Exp, [b_st], [b_st], scale=-0.5)

    def norm_transpose(self, x_dram_tile, hT, hcol0, b_h):
        k = self.k; D = self.D; DC = self.DC
        k.dma("sp", self.xt[:], x_dram_tile, [], [self.b_xt], self.b_xt)
        self.rstd_of(self.xt[:], [self.b_xt], 1, D)
        k.act(self.xn[:], self.xt[:], AF.Copy, [self.b_xt, self.b_st], [self.b_xn], scale=self.st[:, 1:2])
        for g in range(max(1, DC // 4)):
            nq = min(4, DC)
            pt, pb = k.bank()
            for q in range(nq):
                kc = g * 4 + q
                k.mm(pt[:, q * 128:(q + 1) * 128], self.xn[:, kc * 128:(kc + 1) * 128], self.identb[:, :], True, True, [self.b_xn, self.b_const], [pb])
            for q in range(nq):
                kc = g * 4 + q
                o = hT[:, kc, hcol0:hcol0 + 128]
                i = pt[:, q * 128:(q + 1) * 128]
                if q % 2 == 0:
                    k.act(o, i, AF.Identity, [pb, self.b_gs, self.b_modc], [b_h], bias=self.modc[:, kc:kc + 1], scale=self.gs_c[:, kc:kc + 1])
                else:
                    k.ts("dve", o, i, self.gs_c[:, kc:kc + 1], self.modc[:, kc:kc + 1], ALU.mult, ALU.add, [pb, self.b_gs, self.b_modc], [b_h])


EPS = 1e-6


def row_to_col(k, row_t, row_b, ncols, ps_ap, ps_b, one_f, b_const):
    for i in range(ncols):
        k.mm(ps_ap[:, i:i + 1], row_t[0:1, i * 128:(i + 1) * 128], one_f[0:1, 0:1], True, True, [row_b, b_const], [ps_b])


def build_bd(D, F, NT, HC=256, TN=512, k=None, ysrc=None, out_kind="ExternalOutput"):
    k = k or KB()
    nc, s = k.nc, k.s
    DC = D // 128
    NTT = NT // 128
    NB = D // 512
    SUB = HC // 128
    TPB = TN // 128
    k.psum_banks()
    xin = k.dram("xin", [NT, D]); cvec = k.dram("cvec", [1, D])
    ycat = k.dram("ycat", [NT, D]) if ysrc is None else None
    seld = k.dram("sel", [128, 2]) if ysrc is not None else None
    wada = k.dram("wada", [D, 4 * D]); bada = k.dram("bada", [1, 4 * D])
    wout = k.dram("wout", [D, D]); gpost = k.dram("gpost", [1, D]); gpre = k.dram("gpre", [1, D])
    gpost2 = k.dram("gpost2", [1, D]); wfi = k.dram("wfi", [D, 2 * F]); wfo = k.dram("wfo", [F, D])
    ident = k.dram("ident", [128, 128])
    xmid = k.dram("xmid", [NT, D], kind="ExternalOutput")
    xout = k.dram("xout", [NT, D], kind=out_kind)
    identf = k.sb("identf", [128, 128]); identb = k.sb("identb", [128, 128], BF16)
    ones_f = k.sb("ones_f", [1, 128]); ones_b = k.sb("ones_b", [1, 128], BF16)
    zeros = k.sb("zeros", [128, 128])
    rowf = [k.sb("rowf%d" % i, [1, 512]) for i in range(2)]
    rowb = [k.sb("rowb%d" % i, [1, 512], BF16) for i in range(2)]
    condT_f = k.sb("condT_f", [128, DC]); condT_b = k.sb("condT_b", [128, DC], BF16)
    cond_rep = k.sb("cond_rep", [128, DC, 128], BF16)
    modc = k.sb("modc", [128, 2 * DC]); gpre_c = k.sb("gpre_c", [128, DC]); gsf_c = k.sb("gsf_c", [128, DC])
    gt_b = k.sb("gt_b", [128, D])
    wsl = [k.sb("wsl%d" % i, [128, DC, 512], BF16) for i in range(3)]
    wos = [k.sb("wos%d" % i, [128, SUB, D], BF16) for i in range(2)]
    acc = k.sb("acc", [128, NTT, D])
    hT = k.sb("hT", [128, DC, NT], BF16)
    xt = k.sb("xt", [128, D]); xn = k.sb("xn", [128, D], BF16)
    tmp = [k.sb("tmp%d" % i, [128, 512]) for i in range(2)]
    actT = [k.sb("actT%d" % i, [128, SUB, TN], BF16) for i in range(2)]
    sg = [k.sb("sg%d" % i, [128, TN]) for i in range(2)]
    st = k.sb("st", [128, 8])
    selt = k.sb("selt", [128, 2])
    B = s.buf
    b_const = B(); b_rowf = [B(), B()]; b_rowb = [B(), B()]; b_cond = B(); b_modc = B(); b_gpre = B(); b_gsf = B()
    b_gt = [B() for _ in range(NB)]
    b_wsl = [B() for _ in range(3)]; b_wos = [B() for _ in range(2)]
    b_acc = [[B() for _ in range(NB)] for _ in range(NTT)]
    b_hT = [B() for _ in range(NTT)]
    b_xt = B(); b_xn = B(); b_tmp = [B(), B()]; b_actT = [B(), B()]; b_sg = [B(), B()]; b_st = B()
    b_xmid = [B() for _ in range(NTT)]
    cnt = {"wsl": 0, "wos": 0, "row": 0, "tmp": 0, "ev": 0}

    def next_wsl():
        i = cnt["wsl"] % 3; cnt["wsl"] += 1
        return wsl[i], b_wsl[i]

    def load_w(dst, dbuf, src_cols, width, off=0):
        k.dma("pool", dst[:, :, off:off + width], src_cols.rearrange("(kc p) n -> p kc n", p=128), [], [dbuf], dbuf)

    k.dma("sp", identf[:], ident[:, :], [], [b_const], b_const)
    k.cp("dve", identb[:], identf[:], [b_const], [b_const])
    k.memset("dve", ones_f[:], 1.0, [b_const]); k.memset("dve", ones_b[:], 1.0, [b_const])
    k.memset("dve", zeros[:], 0.0, [b_const])
    one_f = ones_f
    if ysrc is not None:
        k.dma("sp", selt[:], seld[:, :], [], [b_const], b_const)

    pt, pb = k.bank()
    for j in range(D // 512):
        r = j % 2
        k.dma("sp", rowf[r][0:1, :], cvec[0:1, j * 512:(j + 1) * 512], [], [b_rowf[r]], b_rowf[r])
        row_to_col(k, rowf[r], b_rowf[r], 4, pt[:, j * 4:(j + 1) * 4], pb, one_f, b_const)
    k.act(condT_f[:], pt[:, 0:DC], AF.Silu, [pb], [b_cond])
    k.cp("dve", condT_b[:], condT_f[:], [b_cond], [b_cond])
    for kc in range(DC):
        k.ts("dve", cond_rep[:, kc, :], zeros[:], condT_f[:, kc:kc + 1], None, ALU.add, None, [b_cond, b_const], [b_cond])
    pt, pb = k.bank()
    for j in range(D // 512):
        r = j % 2
        k.dma("sp", rowf[r][0:1, :], gpre[0:1, j * 512:(j + 1) * 512], [], [b_rowf[r]], b_rowf[r])
        row_to_col(k, rowf[r], b_rowf[r], 4, pt[:, j * 4:(j + 1) * 4], pb, one_f, b_const)
    k.cp("act", gpre_c[:], pt[:, 0:DC], [pb], [b_gpre])

    def mod_bcast(col0, grow):
        for j in range(NB):
            w, wb = next_wsl()
            load_w(w, wb, wada[:, col0 + j * 512: col0 + (j + 1) * 512], 512)
            r = j % 2
            k.dma("pool", rowb[r][0:1, :], bada[0:1, col0 + j * 512: col0 + (j + 1) * 512], [], [b_rowb[r]], b_rowb[r])
            k.dma("sp", rowf[r][0:1, :], grow[0:1, j * 512:(j + 1) * 512], [], [b_rowf[r]], b_rowf[r])
            pa, pab = k.bank()
            for kc in range(DC):
                k.mm(pa[:, :], cond_rep[:, kc, :], w[:, kc, :], kc == 0, False, [b_cond, wb], [pab])
            k.mm(pa[:, :], ones_b[0:1, :], rowb[r][0:1, :], False, True, [b_const, b_rowb[r]], [pab])
            pg, pgb = k.bank()
            k.mm(pg[:, :], ones_f[0:1, :], rowf[r][0:1, :], True, True, [b_const, b_rowf[r]], [pgb])
            k.cp("act", tmp[r][:], pg[:, :], [pgb], [b_tmp[r]])
            k.tt("dve", gt_b[:, j * 512:(j + 1) * 512], pa[:, :], tmp[r][:], ALU.mult, [pab, b_tmp[r]], [b_gt[j]])

    mod_bcast(0, gpost)

    pc, pcb = k.bank()
    for j in range(2 * D // 512):
        w, wb = next_wsl()
        load_w(w, wb, wada[:, D + j * 512: D + (j + 1) * 512], 512)
        r = j % 2
        k.dma("pool", rowb[r][0:1, :], bada[0:1, D + j * 512: D + (j + 1) * 512], [], [b_rowb[r]], b_rowb[r])
        for q in range(4):
            col = j * 4 + q
            for kc in range(DC):
                k.mm(pc[:, col:col + 1], w[:, kc, q * 128:(q + 1) * 128], condT_b[:, kc:kc + 1], kc == 0, False, [b_cond, wb], [pcb])
            k.mm(pc[:, col:col + 1], rowb[r][0:1, q * 128:(q + 1) * 128], ones_b[0:1, 0:1], False, True, [b_rowb[r], b_const], [pcb])
    k.cp("act", modc[:], pc[:, 0:2 * DC], [pcb], [b_modc])
    k.stt("dve", gsf_c[:], modc[:, DC:2 * DC], 1.0, gpre_c[:], ALU.add, ALU.mult, [b_modc, b_gpre], [b_gsf])
    shf_c = modc

    def transpose_tile(t, modulate):
        for g in range(DC // 4 if DC >= 4 else 1):
            nq = min(4, DC)
            pt, pb = k.bank()
            for q in range(nq):
                kc = g * 4 + q
                k.mm(pt[:, q * 128:(q + 1) * 128], xn[:, kc * 128:(kc + 1) * 128], identb[:, :], True, True, [b_xn, b_const], [pb])
            if not modulate:
                src = pt[:, 0:nq * 128].rearrange("p (a b) -> p a b", a=nq)
                eng = "act" if g % 2 == 0 else "dve"
                k.cp(eng, hT[:, g * 4:g * 4 + nq, t * 128:(t + 1) * 128], src, [pb], [b_hT[t]])
            else:
                for q in range(nq):
                    kc = g * 4 + q
                    o = hT[:, kc, t * 128:(t + 1) * 128]
                    i = pt[:, q * 128:(q + 1) * 128]
                    if q % 2 == 0:
                        k.act(o, i, AF.Identity, [pb, b_gsf, b_modc], [b_hT[t]], bias=shf_c[:, kc:kc + 1], scale=gsf_c[:, kc:kc + 1])
                    else:
                        k.ts("dve", o, i, gsf_c[:, kc:kc + 1], shf_c[:, kc:kc + 1], ALU.mult, ALU.add, [pb, b_gsf, b_modc], [b_hT[t]])

    for t in range(NTT):
        if ysrc is None:
            k.dma("sp", xt[:], ycat[t * 128:(t + 1) * 128, :], [], [b_xt], b_xt)
        else:
            for (c0, wd, fa, fb) in ysrc:
                if fb is None:
                    k.dma("sp", xt[:, c0:c0 + wd], fa(t), [], [b_xt], b_xt)
                    continue
                for c1 in range(0, wd, 512):
                    w_ = min(512, wd - c1)
                    r = (c1 // 512) % 2
                    k.dma("sp", xt[:, c0 + c1:c0 + c1 + w_], fa(t)[:, c1:c1 + w_], [], [b_xt], b_xt)
                    k.dma("sp", tmp[r][:, 0:w_], fb(t)[:, c1:c1 + w_], [], [b_tmp[r]], b_tmp[r])
                    k.ts("dve", xt[:, c0 + c1:c0 + c1 + w_], xt[:, c0 + c1:c0 + c1 + w_], selt[:, 0:1], None, ALU.mult, None, [b_xt, b_const], [b_xt])
                    k.stt("dve", xt[:, c0 + c1:c0 + c1 + w_], tmp[r][:, 0:w_], selt[:, 1:2], xt[:, c0 + c1:c0 + c1 + w_], ALU.mult, ALU.add, [b_tmp[r], b_xt, b_const], [b_xt])
        k.cp("act", xn[:], xt[:], [b_xt], [b_xn])
        transpose_tile(t, False)

    for n in range(NB):
        w, wb = next_wsl()
        load_w(w, wb, wout[:, n * 512:(n + 1) * 512], 512)
        for t in range(NTT):
            pt, pb = k.bank()
            for kc in range(DC):
                k.mm(pt[:, :], hT[:, kc, t * 128:(t + 1) * 128], w[:, kc, :], kc == 0, kc == DC - 1, [b_hT[t], wb], [pb])
            eng = "act" if t % 2 == 0 else "dve"
            k.cp(eng, acc[:, t, n * 512:(n + 1) * 512], pt[:, :], [pb], [b_acc[t][n]])

    def rstd_of(src_ap, src_bufs, col):
        k.memset("dve", st[:, col:col + 1], 0.0, [b_st])
        k.act(xn[:], src_ap, AF.Square, src_bufs + [b_st], [b_xn, b_st], accum=st[:, col:col + 1])
        k.ts("dve", st[:, col:col + 1], st[:, col:col + 1], 1.0 / D, EPS, ALU.mult, ALU.add, [b_st], [b_st])
        k.act(st[:, col:col + 1], st[:, col:col + 1], AF.Ln, [b_st], [b_st])
        k.act(st[:, col:col + 1], st[:, col:col + 1], AF.Exp, [b_st], [b_st], scale=-0.5)

    for t in range(NTT):
        k.dma("sp", xt[:], xin[t * 128:(t + 1) * 128, :], [], [b_xt], b_xt)
        rstd_of(acc[:, t, :], b_acc[t], 0)
        k.stt("dve", acc[:, t, :], acc[:, t, :], st[:, 0:1], gt_b[:, :], ALU.mult, ALU.mult, b_acc[t] + b_gt + [b_st], b_acc[t])
        k.tt("dve", xt[:], xt[:], acc[:, t, :], ALU.add, [b_xt] + b_acc[t], [b_xt])
        k.dma("sp", xmid[t * 128:(t + 1) * 128, :], xt[:], [b_xt], [b_xmid[t]], b_xt)
        rstd_of(xt[:], [b_xt], 1)
        k.act(xn[:], xt[:], AF.Copy, [b_xt, b_st], [b_xn], scale=st[:, 1:2])
        transpose_tile(t, True)

    mod_bcast(3 * D, gpost2)

    blocks = [(hc, th) for hc in range(F // HC) for th in range(NT // TN)]
    wcur = {}

    def emit_gu(i):
        hc, th = blocks[i]
        if th == 0:
            w, wb = next_wsl()
            load_w(w, wb, wfi[:, hc * HC:(hc + 1) * HC], HC, 0)
            load_w(w, wb, wfi[:, F + hc * HC: F + (hc + 1) * HC], HC, HC)
            j = cnt["wos"] % 2; cnt["wos"] += 1
            k.dma("pool", wos[j][:, :, :], wfo[hc * HC:(hc + 1) * HC, :].rearrange("(s p) n -> p s n", p=128), [], [b_wos[j]], b_wos[j])
            wcur[hc] = (w, wb, wos[j], b_wos[j])
        w, wb, _, _ = wcur[hc]
        a = i % 2
        hbufs = [b_hT[th * TPB + q] for q in range(TPB)]
        for sub in range(SUB):
            pg, pgb = k.bank()
            for kc in range(DC):
                k.mm(pg[:, 0:TN], w[:, kc, sub * 128:(sub + 1) * 128], hT[:, kc, th * TN:(th + 1) * TN], kc == 0, kc == DC - 1, [wb] + hbufs, [pgb])
            pu, pub = k.bank()
            for kc in range(DC):
                k.mm(pu[:, 0:TN], w[:, kc, HC + sub * 128: HC + (sub + 1) * 128], hT[:, kc, th * TN:(th + 1) * TN], kc == 0, kc == DC - 1, [wb] + hbufs, [pub])
            r = sub % 2
            k.act(sg[r][:, :], pg[:, 0:TN], AF.Silu, [pgb], [b_sg[r]])
            k.tt("dve", actT[a][:, sub, :], sg[r][:, :], pu[:, 0:TN], ALU.mult, [b_sg[r], pub], [b_actT[a]])

    def emit_y(i):
        hc, th = blocks[i]
        _, _, wo, wob = wcur[hc]
        a = i % 2
        for tq in range(TPB):
            t = th * TPB + tq
            for n in range(NB):
                py, pyb = k.bank()
                for sub in range(SUB):
                    k.mm(py[:, :], actT[a][:, sub, tq * 128:(tq + 1) * 128], wo[:, sub, n * 512:(n + 1) * 512], sub == 0, sub == SUB - 1, [b_actT[a], wob], [pyb])
                dst = acc[:, t, n * 512:(n + 1) * 512]
                if hc == 0:
                    k.cp("dve", dst, py[:, :], [pyb], [b_acc[t][n]])
                else:
                    k.tt("dve", dst, dst, py[:, :], ALU.add, [pyb, b_acc[t][n]], [b_acc[t][n]])

    for i in range(len(blocks)):
        emit_gu(i)
        if i > 0:
            emit_y(i - 1)
    emit_y(len(blocks) - 1)

    for t in range(NTT):
        k.dma("sp", xt[:], xmid[t * 128:(t + 1) * 128, :], [b_xmid[t]], [b_xt], b_xt)
        rstd_of(acc[:, t, :], b_acc[t], 2)
        k.stt("dve", acc[:, t, :], acc[:, t, :], st[:, 2:3], gt_b[:, :], ALU.mult, ALU.mult, b_acc[t] + b_gt + [b_st], b_acc[t])
        k.tt("dve", xt[:], xt[:], acc[:, t, :], ALU.add, [b_xt] + b_acc[t], [b_xt])
        k.dma("sp", xout[t * 128:(t + 1) * 128, :], xt[:], [b_xt], [], b_xt)
    k.finish_build()
    return nc


def build_rw(D, T, HO, SEG=512, RDT=F32, stop=99, k=None):
    k = k or KB(); nc, s = k.nc, k.s; B = s.buf
    DC = D // 128; CH = 64 * HO; NP = HO // 2; NSEG = T // SEG; NCH = SEG // 64; NZ = 3 * NP + 3
    k.psum_banks()
    x = k.dram("x", [T, D]); cvec = k.dram("cvec", [1, D]); wada2 = k.dram("wada2", [D, 2 * D]); bada2 = k.dram("bada2", [1, 2 * D])
    gpre = k.dram("gpre", [1, D]); ident = k.dram("ident", [128, 128])
    wr = k.dram("wr", [D, CH]); wk = k.dram("wk", [D, CH]); wv = k.dram("wv", [D, CH]); wl = k.dram("wl", [D, 288])
    mu_cat = k.dram("mu_cat", [1, NZ * 128]); pvec = k.dram("pvec", [1, 5 * CH]); lnx = k.dram("lnx", [1, 2 * CH])
    w2 = k.dram("w2", [64, CH]); a2 = k.dram("a2", [64, CH]); g2 = k.dram("g2", [160, CH])
    mask5 = k.dram("mask5", [64, 320]); tri2 = k.dram("tri2", [128, 128]); bones = k.dram("bones", [128, 128]); hsel = k.dram("hsel", [128, 2])
    yb = k.dram("yb", [T, CH], kind="ExternalOutput")
    ybanks = [k.reserve(), k.reserve()]
    pre = Pre(k, D, wada2, bada2, cvec, gpre, ident, nwsl=2)
    identf = pre.identf; bc = pre.b_const
    hTs = k.sb("hTs", [128, DC, SEG], BF16); b_hT = B()
    Rt = k.sb("Rt", [128, NP, SEG]); Kt = k.sb("Kt", [128, NP, SEG]); Vt = k.sb("Vt", [128, NP, SEG], BF16)
    Rb = k.sb("Rb", [128, NP, SEG], BF16); Kb = k.sb("Kb", [128, NP, SEG], BF16)
    BTt = k.sb("BTt", [128, NP, SEG], BF16); ATt = k.sb("ATt", [128, NP, SEG], BF16); RKt = k.sb("RKt", [128, NP, SEG])
    b_Rb = [B() for _ in range(NP)]; b_Kb = [B() for _ in range(NP)]
    b_R = [B() for _ in range(NP)]; b_K = [B() for _ in range(NP)]; b_V = [B() for _ in range(NP)]
    b_BT = [B() for _ in range(NP)]; b_AT = [B() for _ in range(NP)]; b_RK = [B() for _ in range(NP)]
    zwa = k.sb("zwa", [128, SEG]); zga = k.sb("zga", [128, SEG]); zgb = k.sb("zgb", [32, SEG]); b_zl = [B(), B(), B()]
    twb = k.sb("twb", [128, SEG], BF16); sga = k.sb("sga", [128, SEG], BF16); sgb = k.sb("sgb", [32, SEG], BF16); b_lo = B()
    zraw = [k.sb("zraw%d" % i, [128, SEG + 1]) for i in range(2)]; b_zraw = [B(), B()]
    carry = k.sb("carry", [128, NZ]); b_carry = B()
    S = [k.sb("S%d" % i, [128, SEG]) for i in range(7)]; b_S = [B() for _ in range(7)]
    mu_c = k.sb("mu_c", [128, NZ]); omm_c = k.sb("omm_c", [128, NZ]); pv_c = k.sb("pv_c", [128, 5 * NP]); b_par = B()
    pcs = k.sb("pcs", [128, NP, NCH]); b_pc = [B() for _ in range(NP)]
    w2a2 = k.sb("w2a2", [128, CH], BF16); g2a = k.sb("g2a", [128, CH], BF16); g2b = k.sb("g2b", [32, CH], BF16)
    m5 = k.sb("m5", [64, 320]); tri = k.sb("tri", [128, 128]); bon1 = k.sb("bon1", [128, 128]); hs = k.sb("hs", [128, 2])
    lg_b = k.sb("lg_b", [64, CH]); lb_b = k.sb("lb_b", [64, CH])
    RtO = k.sb("RtO", [64, NP, SEG], BF16); KtO = k.sb("KtO", [64, NP, SEG], BF16); BTO = k.sb("BTO", [64, NP, SEG], BF16); ATO = k.sb("ATO", [64, NP, SEG], BF16)
    b_RO = [B() for _ in range(NP)]; b_KO = [B() for _ in range(NP)]; b_BO = [B() for _ in range(NP)]; b_AO = [B() for _ in range(NP)]
    pcsO = k.sb("pcsO", [64, NP, NCH]); b_pcO = [B() for _ in range(NP)]
    Tst = [[k.sb("Tst%d_%d" % (h, i), [64, 64]) for i in range(2)] for h in range(HO)]
    Tb = [[k.sb("Tb%d_%d" % (h, i), [64, 64], BF16) for i in range(2)] for h in range(HO)]
    Ttmp = [k.sb("Ttmp%d" % i, [64, 64]) for i in range(2)]; b_Ttmp = [B(), B()]
    b_T = [[B(), B()] for _ in range(HO)]
    TM = [k.sb("TM%d" % p, [64, 3, 128], BF16) for p in range(NP)]; b_TM = [B() for _ in range(NP)]
    NS = HO
    G = [k.sb("G%d" % i, [64, 320], BF16) for i in range(NS)]; b_G = [B() for _ in range(NS)]
    NTt = [k.sb("NT%d" % i, [64, 64], BF16) for i in range(NS)]; b_NT = [B() for _ in range(NS)]
    LP = [[k.sb("LP%d_%d" % (i, j), [64, 128], BF16) for j in range(2)] for i in range(NS)]; b_LP = [[B(), B()] for _ in range(NS)]
    Wsb = [k.sb("Wsb%d" % i, [64, 64], BF16) for i in range(NS)]; b_W = [B() for _ in range(NS)]
    Usb = [k.sb("Usb%d" % h, [64, 64], BF16) for h in range(HO)]; b_U = [B() for _ in range(HO)]
    Y1 = k.sb("Y1", [64, CH]); b_Y1 = B(); bon = k.sb("bon", [64, HO]); b_bon = B()
    stt_ = k.sb("stt_", [64, 4 * HO]); b_stt = B(); Y2 = k.sb("Y2", [64, CH]); b_Y2 = B()

    for (dst, src) in ((m5, mask5), (tri, tri2), (bon1, bones), (hs, hsel)):
        k.dma("sp", dst[:], src[:, :], [], [bc], bc)
    k.dma("pool", w2a2[0:64, :], w2[:, :], [], [bc], bc)
    k.dma("pool", w2a2[64:128, :], a2[:, :], [], [bc], bc)
    k.dma("pool", g2a[:, :], g2[0:128, :], [], [bc], bc)
    k.dma("pool", g2b[:, :], g2[128:160, :], [], [bc], bc)
    pt, pb = k.bank()
    pre.col_from_dram(mu_cat, NZ * 128, pt, pb)
    k.cp("act", mu_c[:], pt[:, 0:NZ], [pb], [b_par])
    k.ts("dve", omm_c[:], mu_c[:], -1.0, 1.0, ALU.mult, ALU.add, [b_par], [b_par])
    pt, pb = k.bank()
    pre.col_from_dram(pvec, 5 * CH, pt, pb)
    k.cp("act", pv_c[:], pt[:, 0:5 * NP], [pb], [b_par])
    for (dst, off) in ((lg_b, 0), (lb_b, CH)):
        done = 0
        while done < CH:
            w = min(512, CH - done)
            k.dma("sp", pre.rowf[0][0:1, 0:w], lnx[0:1, off + done: off + done + w], [], [pre.b_rowf[0]], pre.b_rowf[0])
            pt, pb = k.bank()
            k.mm(pt[0:64, 0:w], pre.ones_f[0:1, 0:64], pre.rowf[0][0:1, 0:w], True, True, [bc, pre.b_rowf[0]], [pb])
            k.cp("act", dst[:, done:done + w], pt[0:64, 0:w], [pb], [b_par])
            done += w
    k.memset("dve", carry[:], 0.0, [b_carry])
    for h in range(HO):
        k.memset("dve", Tst[h][0][:], 0.0, [b_T[h][0]])
        k.memset("dve", Tb[h][0][:], 0.0, [b_T[h][0]])
    W0, A0, KK, KA, RKc = [lambda p, i=i: pv_c[:, i * NP + p: i * NP + p + 1] for i in range(5)]

    if stop == 1:
        k.finish_build(); return nc
    zi = {"n": 0}

    def ztile(w, wb, c0, M, dst, dbufs, idx):
        ps, pb = k.bank()
        for kc in range(DC):
            k.mm(ps[0:M, 0:SEG], w[:, kc, c0:c0 + M], hTs[:, kc, :], kc == 0, kc == DC - 1, [wb, b_hT], [pb])
        r = zi["n"] % 2; zi["n"] += 1
        zr = zraw[r]; bz = b_zraw[r]
        k.cp("act", zr[0:M, 1:SEG + 1], ps[0:M, 0:SEG], [pb], [bz])
        k.cp("dve", zr[0:M, 0:1], carry[0:M, idx:idx + 1], [b_carry], [bz])
        k.ts("dve", S[0][0:M, :], zr[0:M, 0:SEG], mu_c[0:M, idx:idx + 1], None, ALU.mult, None, [bz, b_par], [b_S[0]])
        k.stt("dve", dst, zr[0:M, 1:SEG + 1], omm_c[0:M, idx:idx + 1], S[0][0:M, :], ALU.mult, ALU.add, [bz, b_par, b_S[0]], dbufs)
        k.cp("dve", carry[0:M, idx:idx + 1], zr[0:M, SEG:SEG + 1], [bz], [b_carry])

    for sg_ in range(NSEG):
        t0 = sg_ * SEG
        for tq in range(SEG // 128):
            pre.norm_transpose(x[t0 + tq * 128: t0 + (tq + 1) * 128, :], hTs, tq * 128, b_hT)
        for (wsrc, arr, bufs, zbase) in ((wr, Rt, b_R, 0), (wk, Kt, b_K, NP), (wv, Vt, b_V, 2 * NP)):
            w, wb = pre.next_wsl()
            pre.load_w(w, wb, wsrc[:, :], CH)
            for p in range(NP):
                ztile(w, wb, p * 128, 128, arr[:, p, :], [bufs[p]], zbase + p)
        w, wb = pre.next_wsl()
        pre.load_w(w, wb, wl[:, :], 288)
        ztile(w, wb, 0, 128, zwa[:, :], [b_zl[0]], 3 * NP)
        ztile(w, wb, 128, 128, zga[:, :], [b_zl[1]], 3 * NP + 1)
        ztile(w, wb, 256, 32, zgb[:, :], [b_zl[2]], 3 * NP + 2)
        k.act(twb[0:64, :], zwa[0:64, :], AF.Tanh, [b_zl[0]], [b_lo])
        k.cp("dve", twb[64:128, :], zwa[64:128, :], [b_zl[0]], [b_lo])
        k.act(sga[:, :], zga[:, :], AF.Sigmoid, [b_zl[1]], [b_lo])
        k.act(sgb[:, :], zgb[:, :], AF.Sigmoid, [b_zl[2]], [b_lo])
        if stop == 2:
            k.finish_build(); return nc
        for p in range(NP):
            cs = slice(p * 128, (p + 1) * 128)
            ps, pb = k.bank()
            k.mm(ps[:, 0:SEG], w2a2[0:64, cs], twb[0:64, :], True, True, [bc, b_lo], [pb])
            k.act(S[1][:, :], ps[:, 0:SEG], AF.Sigmoid, [pb, b_par], [b_S[1]], bias=W0(p))
            k.ts("dve", S[1][:, :], S[1][:, :], -0.6065306597126334, None, ALU.mult, None, [b_S[1]], [b_S[1]])
            ps, pb = k.bank()
            k.mm(ps[:, 0:SEG], w2a2[64:128, cs], twb[64:128, :], True, True, [bc, b_lo], [pb])
            k.act(S[2][:, :], ps[:, 0:SEG], AF.Sigmoid, [pb, b_par], [b_S[2]], bias=A0(p))
            k.ts("dve", S[3][:, :], Kt[:, p, :], KK(p), None, ALU.mult, None, [b_K[p], b_par], [b_S[3]])
            k.tt("dve", S[4][:, :], S[3][:, :], S[3][:, :], ALU.mult, [b_S[3]], [b_S[4]])
            ps, pb = k.bank()
            k.mm(ps[:, 0:SEG], bon1[:, :], S[4][:, :], True, True, [bc, b_S[4]], [pb])
            k.act(S[4][:, :], ps[:, 0:SEG], AF.Sqrt, [pb], [b_S[4]])
            k.ts("dve", S[4][:, :], S[4][:, :], 1e-12, None, ALU.max, None, [b_S[4]], [b_S[4]])
            k.recip("dve", S[4][:, :], S[4][:, :], [b_S[4]], [b_S[4]])
            k.tt("dve", S[3][:, :], S[3][:, :], S[4][:, :], ALU.mult, [b_S[3], b_S[4]], [b_S[3]])
            k.ts("dve", S[4][:, :], S[2][:, :], 1.0, KA(p), ALU.subtract, ALU.mult, [b_S[2], b_par], [b_S[4]])
            k.stt("dve", Kt[:, p, :], S[4][:, :], 1.0, Kt[:, p, :], ALU.add, ALU.mult, [b_S[4], b_K[p]], [b_K[p]])
            k.stt("dve", RKt[:, p, :], Rt[:, p, :], RKc(p), Kt[:, p, :], ALU.mult, ALU.mult, [b_R[p], b_K[p], b_par], [b_RK[p]])
            k.tt("dve", S[2][:, :], S[3][:, :], S[2][:, :], ALU.mult, [b_S[3], b_S[2]], [b_S[2]])
            pc_, pcb = k.bank()
            for q in range(SEG // 128):
                qs = slice(q * 128, (q + 1) * 128)
                pt, ptb = k.bank()
                k.mm(pt[:, 0:128], S[1][:, qs], identf[:, :], True, True, [b_S[1], bc], [ptb])
                k.cp("act", S[5][:, qs], pt[:, 0:128], [ptb], [b_S[5]])
                k.mm(pc_[:, qs], S[5][:, qs], tri[:, :], True, True, [b_S[5], bc], [pcb])
            k.cp("act", S[4][:, :], pc_[:, 0:SEG], [pcb], [b_S[4]])
            k.act(S[5][:, :], S[4][:, :], AF.Exp, [b_S[4]], [b_S[5]])
            k.tt("dve", Rb[:, p, :], Rt[:, p, :], S[5][:, :], ALU.mult, [b_R[p], b_S[5]], [b_Rb[p]])
            k.cp("dve", pcs[:, p, :], S[5][:, :].rearrange("p (c t) -> p c t", t=64)[:, :, 63], [b_S[5]], [b_pc[p]])
            k.act(S[6][:, :], S[4][:, :], AF.Exp, [b_S[4]], [b_S[6]], scale=-1.0)
            k.tt("dve", Kb[:, p, :], Kt[:, p, :], S[6][:, :], ALU.mult, [b_K[p], b_S[6]], [b_Kb[p]])
            k.tt("dve", BTt[:, p, :], S[2][:, :], S[6][:, :], ALU.mult, [b_S[2], b_S[6]], [b_BT[p]])
            k.tt("dve", S[4][:, :], S[4][:, :], S[1][:, :], ALU.subtract, [b_S[4], b_S[1]], [b_S[4]])
            k.act(S[4][:, :], S[4][:, :], AF.Exp, [b_S[4]], [b_S[4]])
            k.stt("dve", ATt[:, p, :], S[3][:, :], -1.0, S[4][:, :], ALU.mult, ALU.mult, [b_S[3], b_S[4]], [b_AT[p]])
            for (dst, src, bs_, bd_) in ((RtO, Rb, b_Rb, b_RO), (KtO, Kb, b_Kb, b_KO), (BTO, BTt, b_BT, b_BO), (ATO, ATt, b_AT, b_AO)):
                k.dma("sp", dst[:, p, :], src[64:128, p, :], [bs_[p]], [bd_[p]], bd_[p])
            k.dma("sp", pcsO[:, p, :], pcs[64:128, p, :], [b_pc[p]], [b_pcO[p]], b_pcO[p])
        if stop == 3:
            k.finish_build(); return nc
        for c in range(NCH):
            cg = sg_ * NCH + c
            cur = cg % 2
            cols = slice(c * 64, (c + 1) * 64)
            yps, ypb = ybanks[cg % 2]
            for p in range(NP):
                pt, ptb = k.bank()
                for i, (arr, bb) in enumerate(((Vt, b_V), (BTt, b_BT), (Kb, b_Kb))):
                    k.mm(pt[0:64, i * 128:(i + 1) * 128], arr[:, p, cols], pre.identb[:, :], True, True, [bb[p], bc], [ptb])
                k.cp("act", TM[p][:, :, :], pt[0:64, 0:384].rearrange("p (a b) -> p a b", a=3), [ptb], [b_TM[p]])
            H = []
            for h in range(HO):
                p, e = h // 2, h % 2
                rows = slice(e * 64, (e + 1) * 64)
                if e == 0:
                    bt = BTt[0:64, p, cols]; at = ATt[0:64, p, cols]; rt = Rb[0:64, p, cols]; kt = Kb[0:64, p, cols]
                    deps = [b_BT[p], b_AT[p], b_Rb[p], b_Kb[p]]
                else:
                    bt = BTO[:, p, cols]; at = ATO[:, p, cols]; rt = RtO[:, p, cols]; kt = KtO[:, p, cols]
                    deps = [b_BO[p], b_AO[p], b_RO[p], b_KO[p]]
                H.append(dict(p=p, e=e, rows=rows, bt=bt, at=at, rt=rt, kt=kt, deps=deps, b_at=deps[1], b_rt=deps[2]))
            def evac_g(g_):
                dg = H[g_]; psg, pbg = dg["ps"]
                k.tt("dve", G[g_][:, :], psg[0:64, 0:320], m5[:, :], ALU.mult, [pbg, bc], [b_G[g_]])
                k.tt("dve", NTt[g_][:, :], G[g_][:, 0:64], pre.identb[0:64, 0:64], ALU.add, [b_G[g_], bc], [b_NT[g_]])
                dg["Lk"] = G[g_][:, 256:320]; dg["Pk"] = G[g_][:, 0:64]; dg["lb"] = [b_G[g_]]
            for h in range(HO):
                d = H[h]; ps, pb = k.bank()
                k.mm(ps[0:64, 0:64], d["bt"], d["at"], True, True, d["deps"], [pb])
                k.mm(ps[0:64, 64:128], d["bt"], d["rt"], True, True, d["deps"], [pb])
                k.mm(ps[0:64, 128:192], d["kt"], d["at"], True, True, d["deps"], [pb])
                k.mm(ps[0:64, 192:256], d["kt"], d["rt"], True, True, d["deps"], [pb])
                k.mm(ps[0:64, 256:320], d["at"], d["bt"], True, True, d["deps"], [pb])
                d["ps"] = (ps, pb)
                if h >= 3:
                    evac_g(h - 3)
            for g_ in range(max(0, HO - 3), HO):
                evac_g(g_)
            for lev in range(5):
                j = lev % 2
                for h in range(HO):
                    d = H[h]; ps, pb = k.bank()
                    k.mm(ps[0:64, 0:64], d["Pk"], d["Lk"], True, True, d["lb"], [pb])
                    if lev < 4:
                        k.mm(ps[0:64, 64:128], d["Lk"], d["Pk"], True, True, d["lb"], [pb])
                    d["ps"] = (ps, pb)
                    if h >= 3:
                        g_ = h - 3; dg = H[g_]; psg, pbg = dg["ps"]
                        k.cp("act", LP[g_][j][:, :], psg[0:64, 0:128], [pbg], [b_LP[g_][j]])
                        dg["Lk"] = LP[g_][j][:, 0:64]; dg["Pk"] = LP[g_][j][:, 64:128]; dg["lb"] = [b_LP[g_][j]]
                for g_ in range(max(0, HO - 3), HO):
                    dg = H[g_]; psg, pbg = dg["ps"]
                    k.cp("act", LP[g_][j][:, :], psg[0:64, 0:128], [pbg], [b_LP[g_][j]])
                    dg["Lk"] = LP[g_][j][:, 0:64]; dg["Pk"] = LP[g_][j][:, 64:128]; dg["lb"] = [b_LP[g_][j]]
                for h in range(HO):
                    d = H[h]; ps2, pb2 = k.bank()
                    k.mm(ps2[0:64, 0:64], d["Lk"], NTt[h][:, :], True, True, d["lb"] + [b_NT[h]], [pb2])
                    d["ps2"] = (ps2, pb2)
                    if h >= 3:
                        g_ = h - 3; ps3, pb3 = H[g_]["ps2"]
                        k.tt("dve", NTt[g_][:, :], NTt[g_][:, :], ps3[0:64, 0:64], ALU.add, [pb3, b_NT[g_]], [b_NT[g_]])
                for g_ in range(max(0, HO - 3), HO):
                    ps3, pb3 = H[g_]["ps2"]
                    k.tt("dve", NTt[g_][:, :], NTt[g_][:, :], ps3[0:64, 0:64], ALU.add, [pb3, b_NT[g_]], [b_NT[g_]])
            for h in range(HO):
                d = H[h]; p = d["p"]; ps, pb = k.bank()
                vte = TM[p][:, 0, d["rows"]]
                k.mm(ps[0:64, 0:64], G[h][:, 128:192], vte, True, False, [b_G[h], b_TM[p]], [pb])
                k.mm(ps[0:64, 0:64], d["at"], Tb[h][cur][:, :], False, True, [d["b_at"], b_T[h][cur]], [pb])
                d["ps"] = (ps, pb)
                if h >= 3:
                    g_ = h - 3; psg, pbg = H[g_]["ps"]
                    k.cp("act", Wsb[g_][:, :], psg[0:64, 0:64], [pbg], [b_W[g_]])
            for g_ in range(max(0, HO - 3), HO):
                psg, pbg = H[g_]["ps"]
                k.cp("act", Wsb[g_][:, :], psg[0:64, 0:64], [pbg], [b_W[g_]])
            for h in range(HO):
                d = H[h]; ps, pb = k.bank()
                k.mm(ps[0:64, 0:64], NTt[h][:, :], Wsb[h][:, :], True, True, [b_NT[h], b_W[h]], [pb])
                d["ps"] = (ps, pb)
                if h >= 3:
                    g_ = h - 3; psg, pbg = H[g_]["ps"]
                    k.cp("act", Usb[g_][:, :], psg[0:64, 0:64], [pbg], [b_U[g_]])
            for g_ in range(max(0, HO - 3), HO):
                psg, pbg = H[g_]["ps"]
                k.cp("act", Usb[g_][:, :], psg[0:64, 0:64], [pbg], [b_U[g_]])
            for h in range(HO):
                d = H[h]; p = d["p"]
                vte = TM[p][:, 0, d["rows"]]
                yo = yps[0:64, h * 64:(h + 1) * 64]
                k.mm(yo, d["rt"], Tb[h][cur][:, :], True, False, [d["b_rt"], b_T[h][cur]], [ypb])
                k.mm(yo, G[h][:, 64:128], Usb[h][:, :], False, False, [b_G[h], b_U[h]], [ypb])
                k.mm(yo, G[h][:, 192:256], vte, False, True, [b_G[h], b_TM[p]], [ypb])
            for h in range(HO):
                d = H[h]; p = d["p"]; e = d["e"]; rows = d["rows"]
                ps, pb = k.bank()
                k.mm(ps[0:64, 0:64], TM[p][:, 1, rows], Usb[h][:, :], True, False, [b_TM[p], b_U[h]], [pb])
                k.mm(ps[0:64, 0:64], TM[p][:, 2, rows], TM[p][:, 0, rows], False, True, [b_TM[p]], [pb])
                pc_ap = pcs[0:64, p, c:c + 1] if e == 0 else pcsO[:, p, c:c + 1]
                pcb_ = b_pc[p] if e == 0 else b_pcO[p]
                tj = h % 2
                k.ts("dve", Ttmp[tj][:, :], Tst[h][cur][:, :], pc_ap, None, ALU.mult, None, [b_T[h][cur], pcb_], [b_Ttmp[tj]])
                k.stt("dve", Tst[h][1 - cur][:, :], ps[0:64, 0:64], pc_ap, Ttmp[tj][:, :], ALU.mult, ALU.add, [pb, pcb_, b_Ttmp[tj]], [b_T[h][1 - cur]])
                k.cp("act", Tb[h][1 - cur][:, :], Tst[h][1 - cur][:, :], [b_T[h][1 - cur]], [b_T[h][1 - cur]])
            if stop == 4:
                k.finish_build(); return nc
            pbn, pbnb = k.bank()
            for p in range(NP):
                k.mm(pbn[0:64, 2 * p:2 * p + 2], RKt[:, p, cols], hs[:, :], True, True, [b_RK[p], bc], [pbnb])
            k.cp("act", bon[:, :], pbn[0:64, 0:HO], [pbnb], [b_bon])
            pg, pgb = k.bank()
            k.mm(pg[0:64, 0:CH], sga[:, cols], g2a[:, :], True, False, [b_lo, bc], [pgb])
            k.mm(pg[0:64, 0:CH], sgb[:, cols], g2b[:, :], False, True, [b_lo, bc], [pgb])
            if stop == 61:
                k.finish_build(); return nc
            yv = yps[0:64, 0:CH].rearrange("p (h d) -> p h d", d=64)
            k.cp("act", Y1[:, :], yps[0:64, 0:CH], [ypb], [b_Y1])
            k.red("dve", stt_[:, 0:HO], Y1[:, :].rearrange("p (h d) -> p h d", d=64), ALU.add, AX.X, [b_Y1], [b_stt])
            if stop == 615:
                k.finish_build(); return nc
            k.tt("dve", Y2[:, :], Y1[:, :], Y1[:, :], ALU.mult, [b_Y1], [b_Y2])
            k.red("dve", stt_[:, HO:2 * HO], Y2[:, :].rearrange("p (h d) -> p h d", d=64), ALU.add, AX.X, [b_Y2], [b_stt])
            if stop == 616:
                k.finish_build(); return nc
            k.ts("dve", stt_[:, 0:HO], stt_[:, 0:HO], 1.0 / 64, None, ALU.mult, None, [b_stt], [b_stt])
            k.tt("dve", stt_[:, 2 * HO:3 * HO], stt_[:, 0:HO], stt_[:, 0:HO], ALU.mult, [b_stt], [b_stt])
            k.stt("dve", stt_[:, HO:2 * HO], stt_[:, HO:2 * HO], 1.0 / 64, stt_[:, 2 * HO:3 * HO], ALU.mult, ALU.subtract, [b_stt], [b_stt])
            if stop == 617:
                k.finish_build(); return nc
            k.ts("dve", stt_[:, HO:2 * HO], stt_[:, HO:2 * HO], 64e-5, None, ALU.add, None, [b_stt], [b_stt])
            k.act(stt_[:, HO:2 * HO], stt_[:, HO:2 * HO], AF.Ln, [b_stt], [b_stt])
            k.act(stt_[:, HO:2 * HO], stt_[:, HO:2 * HO], AF.Exp, [b_stt], [b_stt], scale=-0.5)
            if stop == 62:
                k.finish_build(); return nc
            for h in range(HO):
                hsl = slice(h * 64, (h + 1) * 64)
                k.ts("dve", Y1[:, hsl], Y1[:, hsl], stt_[:, h:h + 1], stt_[:, HO + h:HO + h + 1], ALU.subtract, ALU.mult, [b_Y1, b_stt], [b_Y1])
            k.tt("dve", Y1[:, :], Y1[:, :], lg_b[:, :], ALU.mult, [b_Y1, b_par], [b_Y1])
            k.tt("dve", Y1[:, :], Y1[:, :], lb_b[:, :], ALU.add, [b_Y1, b_par], [b_Y1])
            for h in range(HO):
                p, e = h // 2, h % 2
                hsl = slice(h * 64, (h + 1) * 64)
                k.stt("dve", Y1[:, hsl], TM[p][:, 0, e * 64:(e + 1) * 64], bon[:, h:h + 1], Y1[:, hsl], ALU.mult, ALU.add, [b_TM[p], b_bon, b_Y1], [b_Y1])
            k.tt("dve", Y2[:, :], Y1[:, :], pg[0:64, 0:CH], ALU.mult, [b_Y1, pgb], [b_Y2])
            if stop == 63:
                k.finish_build(); return nc
            k.dma("sp", yb[t0 + c * 64: t0 + (c + 1) * 64, :], Y2[:, :], [b_Y2], [], b_Y2)
            if stop == 64 + cg:
                k.finish_build(); return nc
    for b_ in ybanks:
        k.unreserve(b_)
    k.finish_build()
    return nc


def build_gm(D, NT, AW, NG, k=None):
    k = k or KB(); nc, s = k.nc, k.s; B = s.buf
    DC = D // 128; NTT = NT // 128; NB = AW // 512 if AW >= 512 else 1; CW = min(512, AW)
    k.psum_banks()
    x = k.dram("x", [NT, D]); cvec = k.dram("cvec", [1, D]); wada2 = k.dram("wada2", [D, 2 * D]); bada2 = k.dram("bada2", [1, 2 * D])
    gpre = k.dram("gpre", [1, D]); ident = k.dram("ident", [128, 128])
    wu = k.dram("wu", [D, AW]); wv = k.dram("wv", [D, AW]); vgb = k.dram("vgb", [1, 2 * AW])
    ws = k.dram("ws", [NG, 128, 128]); bs = k.dram("bs", [1, NG * 128]); triu = k.dram("triu", [128, 128])
    ya = k.dram("ya", [NT, AW], kind="ExternalOutput")
    pre = Pre(k, D, wada2, bada2, cvec, gpre, ident, nwsl=3)
    identf = pre.identf; bc = pre.b_const
    hT = k.sb("hT", [128, DC, NT], BF16); b_hT = [B() for _ in range(NTT)]
    U = k.sb("U", [128, NTT, AW]); V = k.sb("V", [128, NTT, AW]); b_U = [B() for _ in range(NTT)]; b_V = [B() for _ in range(NTT)]
    wsT = k.sb("wsT", [128, NG, 128], BF16); tmpw = k.sb("tmpw", [128, 128]); b_tw = B(); tru = k.sb("tru", [128, 128])
    bs_c = k.sb("bs_c", [128, NG]); vg_b = k.sb("vg_b", [128, AW]); vb_b = k.sb("vb_b", [128, AW]); b_par = B()
    vnb = k.sb("vnb", [128, AW], BF16); b_vn = B(); st2 = k.sb("st2", [128, 4]); b_st2 = B(); yo = k.sb("yo", [128, AW]); b_yo = B()
    junk = k.sb("junk", [128, AW], BF16); b_junk = B()
    k.dma("sp", tru[:], triu[:, :], [], [bc], bc)
    for g in range(NG):
        k.dma("sp", tmpw[:], ws[g, :, :], [], [b_tw], b_tw)
        pt, pb = k.bank()
        k.mm(pt[:, 0:128], tmpw[:, :], identf[:, :], True, True, [b_tw, bc], [pb])
        k.tt("dve", wsT[:, g, :], pt[:, 0:128], tru[:, :], ALU.mult, [pb, bc], [b_par])
    pt, pb = k.bank()
    pre.col_from_dram(bs, NG * 128, pt, pb)
    k.cp("act", bs_c[:], pt[:, 0:NG], [pb], [b_par])
    for (dst, off) in ((vg_b, 0), (vb_b, AW)):
        done = 0
        while done < AW:
            w = min(512, AW - done)
            k.dma("sp", pre.rowf[0][0:1, 0:w], vgb[0:1, off + done: off + done + w], [], [pre.b_rowf[0]], pre.b_rowf[0])
            pt, pb = k.bank()
            k.mm(pt[:, 0:w], pre.ones_f[0:1, :], pre.rowf[0][0:1, 0:w], True, True, [bc, pre.b_rowf[0]], [pb])
            k.cp("act", dst[:, done:done + w], pt[:, 0:w], [pb], [b_par])
            done += w
    for t in range(NTT):
        pre.norm_transpose(x[t * 128:(t + 1) * 128, :], hT, t * 128, b_hT[t])
    for (wsrc, dst, bufs) in ((wu, U, b_U), (wv, V, b_V)):
        for n in range(AW // CW):
            w, wb = pre.next_wsl()
            pre.load_w(w, wb, wsrc[:, n * CW:(n + 1) * CW], CW)
            for t in range(NTT):
                pt, pb = k.bank()
                for kc in range(DC):
                    k.mm(pt[:, 0:CW], hT[:, kc, t * 128:(t + 1) * 128], w[:, kc, 0:CW], kc == 0, kc == DC - 1, [b_hT[t], wb], [pb])
                k.act(dst[:, t, n * CW:(n + 1) * CW], pt[:, 0:CW], AF.Gelu, [pb], [bufs[t]])
    for t in range(NTT):
        v = V[:, t, :]
        k.red("dve", st2[:, 0:1], v, ALU.add, AX.X, [b_V[t]], [b_st2])
        k.memset("dve", st2[:, 1:2], 0.0, [b_st2])
        k.act(junk[:, :], v, AF.Square, [b_V[t], b_st2], [b_junk, b_st2], accum=st2[:, 1:2])
        k.ts("dve", st2[:, 0:1], st2[:, 0:1], 1.0 / AW, None, ALU.mult, None, [b_st2], [b_st2])
        k.tt("dve", st2[:, 2:3], st2[:, 0:1], st2[:, 0:1], ALU.mult, [b_st2], [b_st2])
        k.stt("dve", st2[:, 1:2], st2[:, 1:2], 1.0 / AW, st2[:, 2:3], ALU.mult, ALU.subtract, [b_st2], [b_st2])
        k.ts("dve", st2[:, 1:2], st2[:, 1:2], 1e-5, None, ALU.add, None, [b_st2], [b_st2])
        k.act(st2[:, 1:2], st2[:, 1:2], AF.Ln, [b_st2], [b_st2])
        k.act(st2[:, 1:2], st2[:, 1:2], AF.Exp, [b_st2], [b_st2], scale=-0.5)
        k.ts("dve", v, v, st2[:, 0:1], st2[:, 1:2], ALU.subtract, ALU.mult, [b_V[t], b_st2], [b_V[t]])
        k.tt("dve", v, v, vg_b[:, :], ALU.mult, [b_V[t], b_par], [b_V[t]])
        k.tt("dve", vnb[:, :], v, vb_b[:, :], ALU.add, [b_V[t], b_par], [b_vn])
        for g4 in range(max(1, NG // 4)):
            pt, pb = k.bank()
            ng = min(4, NG)
            for q in range(ng):
                g = g4 * 4 + q
                k.mm(pt[:, q * 128:(q + 1) * 128], wsT[:, g, :], vnb[:, g * 128:(g + 1) * 128], True, True, [b_par, b_vn], [pb])
            for q in range(ng):
                g = g4 * 4 + q
                gs = slice(g * 128, (g + 1) * 128)
                k.stt("dve", yo[:, gs], pt[:, q * 128:(q + 1) * 128], bs_c[:, g:g + 1], U[:, t, gs], ALU.add, ALU.mult, [pb, b_par, b_U[t]], [b_yo])
        k.dma("sp", ya[t * 128:(t + 1) * 128, :], yo[:, :], [b_yo], [], b_yo)
    k.finish_build()
    return nc


NEG = -1.0e30


def build_mb(D, T, HO, SEG=512, k=None, xfn=None):
    k = k or KB(); nc, s = k.nc, k.s; B = s.buf
    DC = D // 128; DH = 128; BLK = 256; NBLK = T // BLK; NQT = T // 128; HC = HO * DH; NSEG = T // SEG; CW = min(512, HC)
    assert NBLK == 8
    k.psum_banks()
    x = k.dram("x", [T, D]); cvec = k.dram("cvec", [1, D]); wada2 = k.dram("wada2", [D, 2 * D]); bada2 = k.dram("bada2", [1, 2 * D])
    gpre = k.dram("gpre", [1, D]); ident = k.dram("ident", [128, 128])
    wq = k.dram("wq", [D, HC]); wk = k.dram("wk", [D, HC]); wv = k.dram("wv", [D, HC])
    slope = k.dram("slope", [1, 128]); kpos = k.dram("kpos", [128, T]); cmask = k.dram("cmask", [128, 128]); gmask = k.dram("gmask", [128, 64])
    o = k.dram("o", [T, HC], kind="ExternalOutput")
    pre = Pre(k, D, wada2, bada2, cvec, gpre, ident, nwsl=2)
    identb = pre.identb; bc = pre.b_const
    hTs = k.sb("hTs", [128, DC, SEG], BF16); b_hT = B()
    QF = k.sb("QF", [128, HO, T], BF16); KF = k.sb("KF", [128, HO, T], BF16); VT = k.sb("VT", [128, NQT, HC], BF16)
    b_Q = [B() for _ in range(HO)]; b_K = [B() for _ in range(HO)]; b_V = [B() for _ in range(NQT)]
    kp = k.sb("kp", [128, T]); cm = k.sb("cm", [128, 128]); gmc = k.sb("gmc", [128, 64]); slc = k.sb("slc", [128, 128])
    kmf = k.sb("kmf", [128, 8]); kmb = k.sb("kmb", [128, HO, 8], BF16); b_km = B()
    ali = k.sb("ali", [128, T]); b_ali = B()
    Ssb = k.sb("Ssb", [128, T]); b_S = B()
    Pb2 = [k.sb("Pb%d" % i, [128, T], BF16) for i in range(2)]; b_P2 = [B(), B()]
    PT2 = [k.sb("PT%d" % i, [128, NQT, 128], BF16) for i in range(2)]; b_PT2 = [B(), B()]
    gsb2 = [k.sb("gsb%d" % i, [128, 8]) for i in range(2)]; mx82 = [k.sb("mx8%d" % i, [128, 8]) for i in range(2)]
    selb2 = [k.sb("selb%d" % i, [128, 8]) for i in range(2)]; b_g2 = [B(), B()]
    sm2 = [k.sb("sm%d" % i, [128, 4]) for i in range(2)]; b_sm2 = [B(), B()]
    osb = [k.sb("osb%d" % i, [128, 128]) for i in range(2)]; b_o = [B(), B()]
    for (dst, src) in ((kp, kpos), (cm, cmask), (gmc, gmask)):
        k.dma("sp", dst[:], src[:, :], [], [bc], bc)
    k.dma("sp", pre.rowf[0][0:1, 0:128], slope[0:1, :], [], [pre.b_rowf[0]], pre.b_rowf[0])
    pt, pb = k.bank()
    k.mm(pt[:, 0:128], pre.ones_f[0:1, :], pre.rowf[0][0:1, 0:128], True, True, [bc, pre.b_rowf[0]], [pb])
    k.cp("act", slc[:, :], pt[:, 0:128], [pb], [bc])
    for sg_ in range(NSEG):
        t0 = sg_ * SEG
        for tq in range(SEG // 128):
            xsrc_ = xfn(sg_ * (SEG // 128) + tq) if xfn is not None else x[t0 + tq * 128: t0 + (tq + 1) * 128, :]
            pre.norm_transpose(xsrc_, hTs, tq * 128, b_hT)
        for (wsrc, dst, bufs, sc_) in ((wq, QF, b_Q, DH ** -0.5), (wk, KF, b_K, None)):
            for n in range(HC // CW):
                w, wb = pre.next_wsl()
                pre.load_w(w, wb, wsrc[:, n * CW:(n + 1) * CW], CW)
                for hh in range(CW // 128):
                    h = n * (CW // 128) + hh
                    pt, pb = k.bank()
                    for kc in range(DC):
                        k.mm(pt[:, 0:SEG], w[:, kc, hh * 128:(hh + 1) * 128], hTs[:, kc, :], kc == 0, kc == DC - 1, [wb, b_hT], [pb])
                    if sc_ is not None:
                        k.act(dst[:, h, t0:t0 + SEG], pt[:, 0:SEG], AF.Copy, [pb], [bufs[h]], scale=sc_)
                    else:
                        k.cp("dve", dst[:, h, t0:t0 + SEG], pt[:, 0:SEG], [pb], [bufs[h]])
        for n in range(HC // CW):
            w, wb = pre.next_wsl()
            pre.load_w(w, wb, wv[:, n * CW:(n + 1) * CW], CW)
            for tq in range(SEG // 128):
                tt_ = sg_ * (SEG // 128) + tq
                pt, pb = k.bank()
                for kc in range(DC):
                    k.mm(pt[:, 0:CW], hTs[:, kc, tq * 128:(tq + 1) * 128], w[:, kc, 0:CW], kc == 0, kc == DC - 1, [wb, b_hT], [pb])
                if tq % 2 == 0:
                    k.cp("act", VT[:, tt_, n * CW:(n + 1) * CW], pt[:, 0:CW], [pb], [b_V[tt_]])
                else:
                    k.cp("dve", VT[:, tt_, n * CW:(n + 1) * CW], pt[:, 0:CW], [pb], [b_V[tt_]])
    for h in range(HO):
        k.red("dve", kmf[:, :], KF[:, h, :].rearrange("p (n s) -> p n s", s=BLK), ALU.add, AX.X, [b_K[h]], [b_km])
        k.ts("dve", kmb[:, h, :], kmf[:, :], 1.0 / BLK, None, ALU.mult, None, [b_km], [b_km])
    it = 0
    for h in range(HO):
        k.ts("dve", ali[:, :], kp[:, :], slc[:, h:h + 1], None, ALU.mult, None, [bc], [b_ali])
        for qt in range(NQT):
            qb = qt // 2; nk = (qt + 1) * 128
            jj = it % 2
            Pb = Pb2[jj]; b_P = b_P2[jj]; PT = PT2[jj]; b_PT = b_PT2[jj]; gsb = gsb2[jj]; mx8 = mx82[jj]; selb = selb2[jj]; b_g = b_g2[jj]
            sm = sm2[jj]; b_sm = b_sm2[jj]
            ql = QF[:, h, qt * 128:(qt + 1) * 128]
            pg, pgb = k.bank()
            k.mm(pg[:, 0:8], ql, kmb[:, h, :], True, True, [b_Q[h], b_km], [pgb])
            k.tt("dve", gsb[:, :], pg[:, 0:8], gmc[:, qb * 8:(qb + 1) * 8], ALU.add, [pgb, bc], [b_g])
            k.max8(mx8[:, :], gsb[:, :], [b_g], [b_g])
            k.ts("dve", selb[:, :], gsb[:, :], mx8[:, 2:3], 1.0, ALU.is_ge, ALU.subtract, [b_g], [b_g])
            k.ts("dve", selb[:, :], selb[:, :], 1.0e30, None, ALU.mult, None, [b_g], [b_g])
            for kg in range((nk + 511) // 512):
                w_ = min(512, nk - kg * 512)
                ps, pb = k.bank()
                k.mm(ps[:, 0:w_], ql, KF[:, h, kg * 512: kg * 512 + w_], True, True, [b_Q[h], b_K[h]], [pb])
                for kt in range(kg * 4, kg * 4 + w_ // 128):
                    n = kt // 2
                    lo = (kt - kg * 4) * 128
                    ks = slice(kt * 128, (kt + 1) * 128)
                    if n < qb:
                        k.stt("dve", Ssb[:, ks], ps[:, lo:lo + 128], selb[:, n:n + 1], ali[:, ks], ALU.add, ALU.add, [pb, b_g, b_ali], [b_S])
                    else:
                        k.tt("dve", Ssb[:, ks], ps[:, lo:lo + 128], ali[:, ks], ALU.add, [pb, b_ali], [b_S])
                        if kt == qt:
                            k.tt("dve", Ssb[:, ks], Ssb[:, ks], cm[:, :], ALU.add, [b_S, bc], [b_S])
            k.red("dve", sm[:, 0:1], Ssb[:, 0:nk], ALU.max, AX.X, [b_S], [b_sm])
            k.ts("dve", sm[:, 0:1], sm[:, 0:1], -1.0, None, ALU.mult, None, [b_sm], [b_sm])
            k.memset("dve", sm[:, 1:2], 0.0, [b_sm])
            k.act(Pb[:, 0:nk], Ssb[:, 0:nk], AF.Exp, [b_S, b_sm], [b_P, b_sm], bias=sm[:, 0:1], accum=sm[:, 1:2])
            k.recip("dve", sm[:, 2:3], sm[:, 1:2], [b_sm], [b_sm])
            for g4 in range((qt + 4) // 4):
                nq = min(4, qt + 1 - g4 * 4)
                pt, pb = k.bank()
                for q in range(nq):
                    kt = g4 * 4 + q
                    k.mm(pt[:, q * 128:(q + 1) * 128], Pb[:, kt * 128:(kt + 1) * 128], identb[:, :], True, True, [b_P, bc], [pb])
                src = pt[:, 0:nq * 128].rearrange("p (a b) -> p a b", a=nq)
                if g4 % 2 == 0:
                    k.cp("act", PT[:, g4 * 4:g4 * 4 + nq, :], src, [pb], [b_PT])
                else:
                    k.cp("dve", PT[:, g4 * 4:g4 * 4 + nq, :], src, [pb], [b_PT])
            po, pob = k.bank()
            for kt in range(qt + 1):
                k.mm(po[:, 0:128], PT[:, kt, :], VT[:, kt, h * 128:(h + 1) * 128], kt == 0, kt == qt, [b_PT, b_V[kt]], [pob])
            j = it % 2; it += 1
            k.ts("dve", osb[j][:, :], po[:, 0:128], sm[:, 2:3], None, ALU.mult, None, [pob, b_sm], [b_o[j]])
            k.dma("sp", o[qt * 128:(qt + 1) * 128, h * 128:(h + 1) * 128], osb[j][:, :], [b_o[j]], [], b_o[j])
    k.finish_build()
    return nc


def _ag_chunks(k, loc, rows, cols, name, groups, cbuf):
    rpc = (1 << 20) // (cols * 4)
    n = rows // rpc
    gs = [k.scratch("%s_%d" % (name, i), [2 * rpc, cols]) for i in range(n)]
    for i in range(n):
        k.allgather(gs[i][:, :], loc[i * rpc:(i + 1) * rpc, :], groups, [], [], cbuf)
    return gs, rpc


def _grow(gs, rpc, r, tau):
    ci = tau // rpc; w = tau % rpc
    return gs[ci][r * rpc + w: r * rpc + w + 128, :]


def build_fused(D, F, T, HOA, HOM, AW, NG, NCORES=8, SEG=512, HCF=256, TN=512):
    k = KB(); k.fused = True
    NTo = T // 2; CH = 64 * HOA; HC = 128 * HOM
    groups = [[2 * i, 2 * i + 1] for i in range(NCORES // 2)]
    k.psum_banks()
    ya_loc = k.scratch("ya_loc", [NTo, AW]); yb_loc = k.scratch("yb_loc", [T, CH])
    x1loc = k.scratch("x1loc", [NTo, D]); o_loc = k.scratch("o_loc", [T, HC])
    xmid0 = k.scratch("xmid0", [NTo, D]); xmid1 = k.scratch("xmid1", [NTo, D])
    cb = k.s.buf()
    k.prefix = "gm_"; k.alias["gm_ya"] = ya_loc
    build_gm(D, NTo, AW, NG, k=k); k.end_phase()
    k.prefix = "rw_"; k.alias["rw_yb"] = yb_loc
    build_rw(D, T, HOA, SEG, k=k); k.end_phase()
    ybg, rp0 = _ag_chunks(k, yb_loc, T, CH, "ybg", groups, cb); k.s.barrier()
    k.prefix = "b0_"; k.alias["b0_xmid"] = xmid0; k.alias["b0_xout"] = x1loc
    ys0 = [(0, AW, lambda t: ya_loc[t * 128:(t + 1) * 128, :], None)]
    for r in range(2):
        ys0.append((AW + r * CH, CH, (lambda t, r=r: _grow(ybg, rp0, r, t * 128)), (lambda t, r=r: _grow(ybg, rp0, r, NTo + t * 128))))
    build_bd(D, F, NTo, HCF, TN, k=k, ysrc=ys0); k.end_phase()
    x1g, rp1 = _ag_chunks(k, x1loc, NTo, D, "x1g", groups, cb); k.s.barrier()
    k.prefix = "mb_"; k.alias["mb_o"] = o_loc; k.alias["mb_x"] = x1loc
    NTT = NTo // 128
    build_mb(D, T, HOM, SEG, k=k, xfn=lambda tt: _grow(x1g, rp1, tt // NTT, (tt % NTT) * 128)); k.end_phase()
    ogs, rp2 = _ag_chunks(k, o_loc, T, HC, "og", groups, cb); k.s.barrier()
    k.prefix = "b1_"; k.alias["b1_xmid"] = xmid1; k.alias["b1_xin"] = x1loc
    ys1 = []
    for r in range(2):
        ys1.append((r * HC, HC, (lambda t, r=r: _grow(ogs, rp2, r, t * 128)), (lambda t, r=r: _grow(ogs, rp2, r, NTo + t * 128))))
    build_bd(D, F, NTo, HCF, TN, k=k, ysrc=ys1); k.end_phase()
    k.s.finish()
    return k.nc


from concourse.bass_utils import run_bass_kernel_spmd

_D = 2048; _F = 5632; _S = 2048; _NB = 4; _NC = 8
_PROG = {}


def _c(a):
    return np.ascontiguousarray(a, dtype=np.float32)


def kernel(x, c, w_ada, b_ada, g_pre_mix, g_post_mix, g_pre_ffn, g_post_ffn, w_ffn_in, w_ffn_out, w_in_ab, w_out_ab,
           a_v_gain, a_v_bias, a_w_s, a_b_s, b_mu, b_w0, b_w2, b_a0, b_a2, b_g2, b_k_k, b_k_a, b_r_k, b_lnx_gain,
           b_lnx_bias, w_qkv, w_o):
    f = np.float32
    x = np.asarray(x, f); c = np.asarray(c, f); w_ada = np.asarray(w_ada, f); b_ada = np.asarray(b_ada, f)
    D = _D
    if "f" not in _PROG:
        _PROG["f"] = build_fused(_D, _F, _S, 8, 8, 1024, 8)
    nc = _PROG["f"]
    ident = np.eye(128, dtype=f)
    own = lambda hh: slice(hh * 1024, (hh + 1) * 1024)
    su = np.triu(np.ones((64, 64)), 1); iu = np.triu(np.ones((64, 64)), 0)
    mask5 = np.concatenate([su, iu, su, iu, su.T], axis=1).astype(f)
    blk = np.kron(np.eye(2), np.ones((64, 64))).astype(f)
    tri2 = (blk * np.triu(np.ones((128, 128)))).astype(f)
    hsel = np.kron(np.eye(2), np.ones((64, 1))).astype(f)
    triu = np.triu(np.ones((128, 128))).astype(f)
    kpos = np.tile(np.arange(_S, dtype=f)[None, :], (128, 1))
    cmask = np.where(np.arange(128)[None, :] <= np.arange(128)[:, None], 0.0, -1e30).astype(f)
    gmask = np.zeros((128, 64), f)
    for qb in range(8):
        for n in range(8):
            gmask[:, qb * 8 + n] = 0.0 if n < qb else -1e30
    w_in_ab0 = np.asarray(w_in_ab[0], f)
    wada0_a = _c(w_ada[0][:, 0:2 * D]); bada0_a = _c(b_ada[0][None, 0:2 * D])
    wada0_b = _c(w_ada[0][:, 2 * D:6 * D]); bada0_b = _c(b_ada[0][None, 2 * D:6 * D])
    wada1_a = _c(w_ada[1][:, 0:2 * D]); bada1_a = _c(b_ada[1][None, 0:2 * D])
    wada1_b = _c(w_ada[1][:, 2 * D:6 * D]); bada1_b = _c(b_ada[1][None, 2 * D:6 * D])
    wu = _c(w_in_ab0[:, 0:1024]); wvv = _c(w_in_ab0[:, 1024:2048])
    vgb = _c(np.concatenate([np.asarray(a_v_gain[0]), np.asarray(a_v_bias[0])])[None, :])
    ws = _c(a_w_s[0]); bs = _c(np.asarray(a_b_s[0]).reshape(1, 1024))
    gpre0 = _c(np.asarray(g_pre_mix[0])[None, :]); gpre1 = _c(np.asarray(g_pre_mix[1])[None, :])
    mu = np.asarray(b_mu[0], f); rk = np.asarray(b_r_k[0], f).reshape(1024)
    wqkv = np.asarray(w_qkv[0], f)
    shared = {}
    rwp = []; mbp = []
    for hh in range(2):
        cs = slice(hh * 512, (hh + 1) * 512)
        mu_cat = np.zeros((1, 15 * 128), f)
        mu_cat[0, 0:512] = mu[0:1024][cs]; mu_cat[0, 512:1024] = mu[1024:2048][cs]; mu_cat[0, 1024:1536] = mu[2048:3072][cs]
        mu_cat[0, 1536:1536 + 288] = mu[3072:3360]
        pvec = np.concatenate([np.asarray(b_w0[0], f)[cs], np.asarray(b_a0[0], f)[cs], np.asarray(b_k_k[0], f)[cs],
                               np.asarray(b_k_a[0], f)[cs], rk[cs]])[None, :]
        lnx = np.concatenate([np.asarray(b_lnx_gain[0], f)[cs], np.asarray(b_lnx_bias[0], f)[cs]])[None, :]
        rwp.append(dict(rw_wr=_c(w_in_ab0[:, 2048:3072][:, cs]), rw_wk=_c(w_in_ab0[:, 3072:4096][:, cs]), rw_wv=_c(w_in_ab0[:, 4096:5120][:, cs]),
                        rw_mu_cat=mu_cat, rw_pvec=_c(pvec), rw_lnx=_c(lnx), rw_w2=_c(np.asarray(b_w2[0], f)[:, cs]),
                        rw_a2=_c(np.asarray(b_a2[0], f)[:, cs]), rw_g2=_c(np.asarray(b_g2[0], f)[:, cs])))
        cs2 = slice(hh * 1024, (hh + 1) * 1024)
        sl = np.zeros((1, 128), f)
        sl[0, 0:8] = 2.0 ** (-8.0 * (np.arange(8) + hh * 8 + 1) / 16.0)
        mbp.append(dict(mb_wq=_c(wqkv[:, 0:2048][:, cs2]), mb_wk=_c(wqkv[:, 2048:4096][:, cs2]), mb_wv=_c(wqkv[:, 4096:6144][:, cs2]), mb_slope=sl))
    wl = _c(w_in_ab0[:, 5120:5408])
    com = dict(
        gm_wada2=wada0_a, gm_bada2=bada0_a, gm_gpre=gpre0, gm_ident=ident, gm_wu=wu, gm_wv=wvv, gm_vgb=vgb, gm_ws=ws, gm_bs=bs, gm_triu=triu,
        rw_wada2=wada0_a, rw_bada2=bada0_a, rw_gpre=gpre0, rw_ident=ident, rw_wl=wl, rw_mask5=mask5, rw_tri2=tri2, rw_bones=blk, rw_hsel=hsel,
        b0_wada=wada0_b, b0_bada=bada0_b, b0_wout=_c(w_out_ab[0]), b0_gpost=_c(np.asarray(g_post_mix[0])[None]), b0_gpre=_c(np.asarray(g_pre_ffn[0])[None]),
        b0_gpost2=_c(np.asarray(g_post_ffn[0])[None]), b0_wfi=_c(w_ffn_in[0]), b0_wfo=_c(w_ffn_out[0]), b0_ident=ident,
        mb_wada2=wada1_a, mb_bada2=bada1_a, mb_gpre=gpre1, mb_ident=ident, mb_kpos=kpos, mb_cmask=cmask, mb_gmask=gmask,
        b1_wada=wada1_b, b1_bada=bada1_b, b1_wout=_c(w_o[0]), b1_gpost=_c(np.asarray(g_post_mix[1])[None]), b1_gpre=_c(np.asarray(g_pre_ffn[1])[None]),
        b1_gpost2=_c(np.asarray(g_post_ffn[1])[None]), b1_wfi=_c(w_ffn_in[1]), b1_wfo=_c(w_ffn_out[1]), b1_ident=ident)
    maps = []
    for core in range(_NC):
        b, hh = core // 2, core % 2
        sel = np.zeros((128, 2), f); sel[:, hh] = 1.0
        cv = _c(c[b][None, :])
        m = dict(com)
        m.update(rwp[hh]); m.update(mbp[hh])
        m.update(gm_x=_c(x[b, own(hh)]), gm_cvec=cv, rw_x=_c(x[b]), rw_cvec=cv, b0_xin=_c(x[b, own(hh)]), b0_cvec=cv, b0_sel=sel,
                 mb_cvec=cv, b1_cvec=cv, b1_sel=sel)
        maps.append(m)
    res = run_bass_kernel_spmd(nc, maps, core_ids=list(range(_NC))).results
    out = np.zeros((_NB, _S, D), f)
    for core in range(_NC):
        b, hh = core // 2, core % 2
        out[b, own(hh)] = res[core]["b1_xout"]
    return out
```

```python
import numpy as np
import concourse.bass as bass
import concourse.mybir as mybir

F32 = mybir.dt.float32
BF16 = mybir.dt.bfloat16
AF = mybir.ActivationFunctionType
ALU = mybir.AluOpType
AX = mybir.AxisListType


class Buf:
    __slots__ = ("name", "lw", "rd", "dsem", "demit")

    def __init__(self, name):
        self.name = name
        self.lw = None
        self.rd = []
        self.dsem = {}
        self.demit = {}


class Op:
    __slots__ = ("eng", "emit", "deps", "dbuf", "needed", "sig", "inc", "barrier")

    def __init__(self, eng, emit, deps, dbuf, inc=16):
        self.eng = eng
        self.emit = emit
        self.deps = deps
        self.dbuf = dbuf
        self.needed = False
        self.sig = 0
        self.inc = inc
        self.barrier = False


class Sch:
    def __init__(self, nc, same_engine_sync=True):
        self.nc = nc
        self.E = {"pe": nc.tensor, "act": nc.scalar, "dve": nc.vector, "pool": nc.gpsimd, "sp": nc.sync}
        self.ops = []
        self.same = same_engine_sync
        self.nbuf = 0

    def buf(self, name=None):
        self.nbuf += 1
        return Buf(name or "b%d" % self.nbuf)

    def barrier(self):
        op = Op("sp", None, [], None)
        op.barrier = True
        self.ops.append(op)

    def add(self, eng, emit, reads=(), writes=(), dma=None, inc=16):
        deps = []
        for b in reads:
            if b.lw is not None:
                deps.append(b.lw)
        for b in writes:
            if b.lw is not None:
                deps.append(b.lw)
            deps.extend(b.rd)
        op = Op(eng, emit, deps, dma, inc)
        for d in deps:
            if d.dbuf is None and d.eng == eng and (eng == "pe" or not self.same):
                continue
            d.needed = True
        for b in reads:
            b.rd.append(op)
        for b in writes:
            b.lw = op
            b.rd = []
        self.ops.append(op)
        return op

    def finish(self):
        nc = self.nc
        esem = {k: nc.alloc_semaphore("es_" + k) for k in self.E}
        cnt = {k: 0 for k in self.E}
        last = {}
        for op in self.ops:
            if op.barrier:
                for o in last.values():
                    o.needed = True
            elif op.dbuf is None:
                last[op.eng] = op
        for op in self.ops:
            if op.barrier:
                continue
            if op.dbuf is None and op.needed:
                cnt[op.eng] += 1
                op.sig = cnt[op.eng]
        waited = {k: {} for k in self.E}
        dbufs = []
        nsem = len(esem)
        lastsig = {k: 0 for k in self.E}
        for op in self.ops:
            if op.barrier:
                for en, e in self.E.items():
                    w = waited[en]
                    for x in self.E:
                        if x == en or lastsig[x] == 0:
                            continue
                        key = ("e", x)
                        if w.get(key, 0) < lastsig[x]:
                            e.wait_ge(esem[x], lastsig[x]); w[key] = lastsig[x]
                    for b, q in dbufs:
                        key = ("d", id(b), q)
                        if w.get(key, 0) < b.demit[q]:
                            e.wait_ge(b.dsem[q], b.demit[q]); w[key] = b.demit[q]
                continue
            eng = self.E[op.eng]
            need = {}
            for d in op.deps:
                if d.dbuf is not None:
                    key = ("d", id(d.dbuf), d.eng)
                    sem = d.dbuf.dsem[d.eng]
                    val = d.dbuf.demit[d.eng]
                else:
                    if d.eng == op.eng and (d.eng == "pe" or not self.same):
                        continue
                    key = ("e", d.eng)
                    sem = esem[d.eng]
                    val = d.sig
                if key not in need or need[key][1] < val:
                    need[key] = (sem, val)
            w = waited[op.eng]
            for key, (sem, val) in need.items():
                if w.get(key, 0) >= val:
                    continue
                eng.wait_ge(sem, val)
                w[key] = val
            inst = op.emit()
            if op.dbuf is not None:
                b = op.dbuf
                if op.eng not in b.dsem:
                    b.dsem[op.eng] = nc.alloc_semaphore("ds_%d" % nsem)
                    b.demit[op.eng] = 0
                    nsem += 1
                    dbufs.append((b, op.eng))
                inst.then_inc(b.dsem[op.eng], op.inc)
                b.demit[op.eng] += op.inc
            elif op.needed:
                inst.then_inc(esem[op.eng], 1)
                lastsig[op.eng] = op.sig
        for b, q in dbufs:
            nc.sync.wait_ge(b.dsem[q], b.demit[q])
        self.nsem = nsem
        return nsem


class KB:
    def __init__(self, name="k"):
        self.nc = bass.Bass("TRN2", target_bir_lowering=False)
        self.s = Sch(self.nc)
        self.nps = 0
        self.ps_banks = None
        self.prefix = ""
        self.alias = {}
        self.cms = []
        self.fused = False

    def dram(self, name, shape, dt=F32, kind="ExternalInput"):
        name = self.prefix + name
        if name in self.alias:
            return self.alias[name]
        return self.nc.dram_tensor(name, list(shape), dt, kind=kind).ap()

    def scratch(self, name, shape, dt=F32, shared=False):
        if shared:
            return self.nc.dram_tensor(name, list(shape), dt, addr_space="Shared").ap()
        return self.nc.dram_tensor(name, list(shape), dt).ap()

    def sb(self, name, shape, dt=F32):
        if not self.fused:
            return self.nc.alloc_sbuf_tensor(self.prefix + name, list(shape), dt)
        cm = self.nc.sbuf_tensor(self.prefix + name, list(shape), dt)
        t = cm.__enter__()
        self.cms.append(cm)
        return t

    def end_phase(self):
        self.s.barrier()
        for cm in reversed(self.cms):
            cm.__exit__(None, None, None)
        self.cms = []

    def finish_build(self):
        if not self.fused:
            self.s.finish()

    def allgather(self, out_ap, in_ap, groups, R, W, cbuf):
        nc = self.nc
        return self.s.add("pool", lambda: nc.gpsimd.collective_compute("AllGather", mybir.AluOpType.bypass, replica_groups=groups, ins=[in_ap], outs=[out_ap]), R, W, dma=cbuf, inc=1)

    def psum_banks(self):
        if self.ps_banks is not None:
            return
        self.ps_banks = []
        for i in range(8):
            t = self.nc.alloc_psum_tensor("psb%d" % i, [128, 512], F32)
            self.ps_banks.append((t, self.s.buf("ps%d" % i)))
        self.ps_i = 0

    def bank(self):
        n = len(self.ps_banks)
        b = self.ps_banks[self.ps_i % n]
        self.ps_i += 1
        return b

    def reserve(self):
        return self.ps_banks.pop()

    def unreserve(self, b):
        self.ps_banks.append(b)

    def mm(self, out, lhsT, rhs, start, stop, R, W):
        nc = self.nc
        return self.s.add("pe", lambda: nc.tensor.matmul(out, lhsT, rhs, start=start, stop=stop), R, W)

    def act(self, out, in_, func, R, W, bias=None, scale=None, accum=None):
        nc = self.nc
        kw = {}
        if bias is not None:
            kw["bias"] = bias
        if scale is not None:
            kw["scale"] = scale
        if accum is not None:
            kw["accum_out"] = accum
        return self.s.add("act", lambda: nc.scalar.activation(out=out, in_=in_, func=func, **kw), R, W)

    def _e(self, eng):
        return {"dve": self.nc.vector, "pool": self.nc.gpsimd}[eng]

    def tt(self, eng, out, in0, in1, op, R, W):
        e = self._e(eng)
        return self.s.add(eng, lambda: e.tensor_tensor(out=out, in0=in0, in1=in1, op=op), R, W)

    def ts(self, eng, out, in0, s1, s2, op0, op1, R, W, accum=None):
        e = self._e(eng)
        if op1 is None:
            return self.s.add(eng, lambda: e.tensor_scalar(out=out, in0=in0, scalar1=s1, scalar2=None, op0=op0), R, W)
        if accum is not None:
            return self.s.add(eng, lambda: e.tensor_scalar(out=out, in0=in0, scalar1=s1, scalar2=s2, op0=op0, op1=op1, accum_out=accum), R, W)
        return self.s.add(eng, lambda: e.tensor_scalar(out=out, in0=in0, scalar1=s1, scalar2=s2, op0=op0, op1=op1), R, W)

    def stt(self, eng, out, in0, scalar, in1, op0, op1, R, W):
        e = self._e(eng)
        return self.s.add(eng, lambda: e.scalar_tensor_tensor(out=out, in0=in0, scalar=scalar, in1=in1, op0=op0, op1=op1), R, W)

    def cp(self, eng, out, in_, R, W):
        if eng == "act":
            nc = self.nc
            return self.s.add("act", lambda: nc.scalar.copy(out=out, in_=in_), R, W)
        e = self._e(eng)
        return self.s.add(eng, lambda: e.tensor_copy(out=out, in_=in_), R, W)

    def red(self, eng, out, in_, op, axis, R, W):
        e = self._e(eng)
        return self.s.add(eng, lambda: e.tensor_reduce(out=out, in_=in_, axis=axis, op=op), R, W)

    def memset(self, eng, ap, val, W):
        e = self._e(eng)
        return self.s.add(eng, lambda: e.memset(ap, val), (), W)

    def dma(self, q, out, in_, R, W, dbuf):
        e = {"sp": self.nc.sync, "pool": self.nc.gpsimd, "act": self.nc.scalar}[q]
        return self.s.add(q, lambda: e.dma_start(out=out, in_=in_), R, W, dma=dbuf)


def _kb_recip(self, eng, out, in_, R, W):
    e = self._e(eng)
    return self.s.add(eng, lambda: e.reciprocal(out=out, in_=in_), R, W)


KB.recip = _kb_recip


def _kb_max8(self, out, in_, R, W):
    nc = self.nc
    return self.s.add("dve", lambda: nc.vector.max(out=out, in_=in_), R, W)


KB.max8 = _kb_max8


EPS = 1e-6


def row_to_col(k, row_t, row_b, ncols, ps_ap, ps_b, one_f, b_const):
    for i in range(ncols):
        k.mm(ps_ap[:, i:i + 1], row_t[0:1, i * 128:(i + 1) * 128], one_f[0:1, 0:1], True, True, [row_b, b_const], [ps_b])


class Pre:
    def __init__(self, k, D, wada2, bada2, cvec, gpre, ident, nwsl=3):
        self.k = k; s = k.s; B = s.buf
        self.D = D; DC = D // 128; self.DC = DC
        self.identf = k.sb("identf", [128, 128]); self.identb = k.sb("identb", [128, 128], BF16)
        self.ones_f = k.sb("ones_f", [1, 128]); self.ones_b = k.sb("ones_b", [1, 128], BF16)
        self.rowf = [k.sb("rowf%d" % i, [1, 512]) for i in range(2)]
        self.rowb = [k.sb("rowb%d" % i, [1, 512], BF16) for i in range(2)]
        self.condT_f = k.sb("condT_f", [128, DC]); self.condT_b = k.sb("condT_b", [128, DC], BF16)
        self.modc = k.sb("modc", [128, 2 * DC]); self.gpre_c = k.sb("gpre_c", [128, DC]); self.gs_c = k.sb("gs_c", [128, DC])
        self.wsl = [k.sb("wsl%d" % i, [128, DC, 512], BF16) for i in range(nwsl)]
        self.xt = k.sb("xt", [128, D]); self.xn = k.sb("xn", [128, D], BF16); self.st = k.sb("st", [128, 8])
        self.b_const = B(); self.b_rowf = [B(), B()]; self.b_rowb = [B(), B()]; self.b_cond = B(); self.b_modc = B()
        self.b_gpre = B(); self.b_gs = B(); self.b_wsl = [B() for _ in range(nwsl)]; self.b_xt = B(); self.b_xn = B(); self.b_st = B()
        self.nw = 0; self.nwsl = nwsl
        k.dma("sp", self.identf[:], ident[:, :], [], [self.b_const], self.b_const)
        k.cp("dve", self.identb[:], self.identf[:], [self.b_const], [self.b_const])
        k.memset("dve", self.ones_f[:], 1.0, [self.b_const]); k.memset("dve", self.ones_b[:], 1.0, [self.b_const])
        pt, pb = k.bank()
        self.col_from_dram(cvec, D, pt, pb)
        k.act(self.condT_f[:], pt[:, 0:DC], AF.Silu, [pb], [self.b_cond])
        k.cp("dve", self.condT_b[:], self.condT_f[:], [self.b_cond], [self.b_cond])
        pt, pb = k.bank()
        self.col_from_dram(gpre, D, pt, pb)
        k.cp("act", self.gpre_c[:], pt[:, 0:DC], [pb], [self.b_gpre])
        pc, pcb = k.bank()
        for j in range(2 * D // 512):
            w, wb = self.next_wsl()
            self.load_w(w, wb, wada2[:, j * 512:(j + 1) * 512], 512)
            r = j % 2
            k.dma("pool", self.rowb[r][0:1, :], bada2[0:1, j * 512:(j + 1) * 512], [], [self.b_rowb[r]], self.b_rowb[r])
            for q in range(4):
                col = j * 4 + q
                for kc in range(DC):
                    k.mm(pc[:, col:col + 1], w[:, kc, q * 128:(q + 1) * 128], self.condT_b[:, kc:kc + 1], kc == 0, False, [self.b_cond, wb], [pcb])
                k.mm(pc[:, col:col + 1], self.rowb[r][0:1, q * 128:(q + 1) * 128], self.ones_b[0:1, 0:1], False, True, [self.b_rowb[r], self.b_const], [pcb])
        k.cp("act", self.modc[:], pc[:, 0:2 * DC], [pcb], [self.b_modc])
        k.stt("dve", self.gs_c[:], self.modc[:, DC:2 * DC], 1.0, self.gpre_c[:], ALU.add, ALU.mult, [self.b_modc, self.b_gpre], [self.b_gs])

    def next_wsl(self):
        i = self.nw % self.nwsl; self.nw += 1
        return self.wsl[i], self.b_wsl[i]

    def load_w(self, dst, dbuf, src_cols, width, off=0):
        self.k.dma("pool", dst[:, :, off:off + width], src_cols.rearrange("(kc p) n -> p kc n", p=128), [], [dbuf], dbuf)

    def col_from_dram(self, vec, n, pt, pb, col0=0):
        k = self.k
        done = 0
        j = 0
        while done < n:
            w = min(512, n - done)
            r = j % 2
            k.dma("sp", self.rowf[r][0:1, 0:w], vec[0:1, done:done + w], [], [self.b_rowf[r]], self.b_rowf[r])
            row_to_col(k, self.rowf[r], self.b_rowf[r], w // 128, pt[:, col0 + done // 128: col0 + (done + w) // 128], pb, self.ones_f, self.b_const)
            done += w; j += 1

    def rstd_of(self, src_ap, src_bufs, col, n):
        k = self.k; st = self.st; b_st = self.b_st
        k.memset("dve", st[:, col:col + 1], 0.0, [b_st])
        k.act(self.xn[:, 0:n], src_ap, AF.Square, src_bufs + [b_st], [self.b_xn, b_st], accum=st[:, col:col + 1])
        k.ts("dve", st[:, col:col + 1], st[:, col:col + 1], 1.0 / n, EPS, ALU.mult, ALU.add, [b_st], [b_st])
        k.act(st[:, col:col + 1], st[:, col:col + 1], AF.Ln, [b_st], [b_st])
        k.act(st[:, col:col + 1], st[:, col:col + 1], AF.Exp, [b_st], [b_st], scale=-0.5)

    def norm_transpose(self, x_dram_tile, hT, hcol0, b_h):
        k = self.k; D = self.D; DC = self.DC
        k.dma("sp", self.xt[:], x_dram_tile, [], [self.b_xt], self.b_xt)
        self.rstd_of(self.xt[:], [self.b_xt], 1, D)
        k.act(self.xn[:], self.xt[:], AF.Copy, [self.b_xt, self.b_st], [self.b_xn], scale=self.st[:, 1:2])
        for g in range(max(1, DC // 4)):
            nq = min(4, DC)
            pt, pb = k.bank()
            for q in range(nq):
                kc = g * 4 + q
                k.mm(pt[:, q * 128:(q + 1) * 128], self.xn[:, kc * 128:(kc + 1) * 128], self.identb[:, :], True, True, [self.b_xn, self.b_const], [pb])
            for q in range(nq):
                kc = g * 4 + q
                o = hT[:, kc, hcol0:hcol0 + 128]
                i = pt[:, q * 128:(q + 1) * 128]
                if q % 2 == 0:
                    k.act(o, i, AF.Identity, [pb, self.b_gs, self.b_modc], [b_h], bias=self.modc[:, kc:kc + 1], scale=self.gs_c[:, kc:kc + 1])
                else:
                    k.ts("dve", o, i, self.gs_c[:, kc:kc + 1], self.modc[:, kc:kc + 1], ALU.mult, ALU.add, [pb, self.b_gs, self.b_modc], [b_h])


EPS = 1e-6


def row_to_col(k, row_t, row_b, ncols, ps_ap, ps_b, one_f, b_const):
    for i in range(ncols):
        k.mm(ps_ap[:, i:i + 1], row_t[0:1, i * 128:(i + 1) * 128], one_f[0:1, 0:1], True, True, [row_b, b_const], [ps_b])


def build_bd(D, F, NT, HC=256, TN=512, k=None, ysrc=None, out_kind="ExternalOutput"):
    k = k or KB()
    nc, s = k.nc, k.s
    DC = D // 128
    NTT = NT // 128
    NB = D // 512
    SUB = HC // 128
    TPB = TN // 128
    k.psum_banks()
    xin = k.dram("xin", [NT, D]); cvec = k.dram("cvec", [1, D])
    ycat = k.dram("ycat", [NT, D]) if ysrc is None else None
    seld = k.dram("sel", [128, 2]) if ysrc is not None else None
    wada = k.dram("wada", [D, 4 * D]); bada = k.dram("bada", [1, 4 * D])
    wout = k.dram("wout", [D, D]); gpost = k.dram("gpost", [1, D]); gpre = k.dram("gpre", [1, D])
    gpost2 = k.dram("gpost2", [1, D]); wfi = k.dram("wfi", [D, 2 * F]); wfo = k.dram("wfo", [F, D])
    ident = k.dram("ident", [128, 128])
    xmid = k.dram("xmid", [NT, D], kind="ExternalOutput")
    xout = k.dram("xout", [NT, D], kind=out_kind)
    identf = k.sb("identf", [128, 128]); identb = k.sb("identb", [128, 128], BF16)
    ones_f = k.sb("ones_f", [1, 128]); ones_b = k.sb("ones_b", [1, 128], BF16)
    zeros = k.sb("zeros", [128, 128])
    rowf = [k.sb("rowf%d" % i, [1, 512]) for i in range(2)]
    rowb = [k.sb("rowb%d" % i, [1, 512], BF16) for i in range(2)]
    condT_f = k.sb("condT_f", [128, DC]); condT_b = k.sb("condT_b", [128, DC], BF16)
    cond_rep = k.sb("cond_rep", [128, DC, 128], BF16)
    modc = k.sb("modc", [128, 2 * DC]); gpre_c = k.sb("gpre_c", [128, DC]); gsf_c = k.sb("gsf_c", [128, DC])
    gt_b = k.sb("gt_b", [128, D])
    wsl = [k.sb("wsl%d" % i, [128, DC, 512], BF16) for i in range(3)]
    wos = [k.sb("wos%d" % i, [128, SUB, D], BF16) for i in range(2)]
    acc = k.sb("acc", [128, NTT, D])
    hT = k.sb("hT", [128, DC, NT], BF16)
    xt = k.sb("xt", [128, D]); xn = k.sb("xn", [128, D], BF16)
    tmp = [k.sb("tmp%d" % i, [128, 512]) for i in range(2)]
    actT = [k.sb("actT%d" % i, [128, SUB, TN], BF16) for i in range(2)]
    sg = [k.sb("sg%d" % i, [128, TN]) for i in range(2)]
    st = k.sb("st", [128, 8])
    selt = k.sb("selt", [128, 2])
    B = s.buf
    b_const = B(); b_rowf = [B(), B()]; b_rowb = [B(), B()]; b_cond = B(); b_modc = B(); b_gpre = B(); b_gsf = B()
    b_gt = [B() for _ in range(NB)]
    b_wsl = [B() for _ in range(3)]; b_wos = [B() for _ in range(2)]
    b_acc = [[B() for _ in range(NB)] for _ in range(NTT)]
    b_hT = [B() for _ in range(NTT)]
    b_xt = B(); b_xn = B(); b_tmp = [B(), B()]; b_actT = [B(), B()]; b_sg = [B(), B()]; b_st = B()
    b_xmid = [B() for _ in range(NTT)]
    cnt = {"wsl": 0, "wos": 0, "row": 0, "tmp": 0, "ev": 0}

    def next_wsl():
        i = cnt["wsl"] % 3; cnt["wsl"] += 1
        return wsl[i], b_wsl[i]

    def load_w(dst, dbuf, src_cols, width, off=0):
        k.dma("pool", dst[:, :, off:off + width], src_cols.rearrange("(kc p) n -> p kc n", p=128), [], [dbuf], dbuf)

    k.dma("sp", identf[:], ident[:, :], [], [b_const], b_const)
    k.cp("dve", identb[:], identf[:], [b_const], [b_const])
    k.memset("dve", ones_f[:], 1.0, [b_const]); k.memset("dve", ones_b[:], 1.0, [b_const])
    k.memset("dve", zeros[:], 0.0, [b_const])
    one_f = ones_f
    if ysrc is not None:
        k.dma("sp", selt[:], seld[:, :], [], [b_const], b_const)

    pt, pb = k.bank()
    for j in range(D // 512):
        r = j % 2
        k.dma("sp", rowf[r][0:1, :], cvec[0:1, j * 512:(j + 1) * 512], [], [b_rowf[r]], b_rowf[r])
        row_to_col(k, rowf[r], b_rowf[r], 4, pt[:, j * 4:(j + 1) * 4], pb, one_f, b_const)
    k.act(condT_f[:], pt[:, 0:DC], AF.Silu, [pb], [b_cond])
    k.cp("dve", condT_b[:], condT_f[:], [b_cond], [b_cond])
    for kc in range(DC):
        k.ts("dve", cond_rep[:, kc, :], zeros[:], condT_f[:, kc:kc + 1], None, ALU.add, None, [b_cond, b_const], [b_cond])
    pt, pb = k.bank()
    for j in range(D // 512):
        r = j % 2
        k.dma("sp", rowf[r][0:1, :], gpre[0:1, j * 512:(j + 1) * 512], [], [b_rowf[r]], b_rowf[r])
        row_to_col(k, rowf[r], b_rowf[r], 4, pt[:, j * 4:(j + 1) * 4], pb, one_f, b_const)
    k.cp("act", gpre_c[:], pt[:, 0:DC], [pb], [b_gpre])

    def mod_bcast(col0, grow):
        for j in range(NB):
            w, wb = next_wsl()
            load_w(w, wb, wada[:, col0 + j * 512: col0 + (j + 1) * 512], 512)
            r = j % 2
            k.dma("pool", rowb[r][0:1, :], bada[0:1, col0 + j * 512: col0 + (j + 1) * 512], [], [b_rowb[r]], b_rowb[r])
            k.dma("sp", rowf[r][0:1, :], grow[0:1, j * 512:(j + 1) * 512], [], [b_rowf[r]], b_rowf[r])
            pa, pab = k.bank()
            for kc in range(DC):
                k.mm(pa[:, :], cond_rep[:, kc, :], w[:, kc, :], kc == 0, False, [b_cond, wb], [pab])
            k.mm(pa[:, :], ones_b[0:1, :], rowb[r][0:1, :], False, True, [b_const, b_rowb[r]], [pab])
            pg, pgb = k.bank()
            k.mm(pg[:, :], ones_f[0:1, :], rowf[r][0:1, :], True, True, [b_const, b_rowf[r]], [pgb])
            k.cp("act", tmp[r][:], pg[:, :], [pgb], [b_tmp[r]])
            k.tt("dve", gt_b[:, j * 512:(j + 1) * 512], pa[:, :], tmp[r][:], ALU.mult, [pab, b_tmp[r]], [b_gt[j]])

    mod_bcast(0, gpost)

    pc, pcb = k.bank()
    for j in range(2 * D // 512):
        w, wb = next_wsl()
        load_w(w, wb, wada[:, D + j * 512: D + (j + 1) * 512], 512)
        r = j % 2
        k.dma("pool", rowb[r][0:1, :], bada[0:1, D + j * 512: D + (j + 1) * 512], [], [b_rowb[r]], b_rowb[r])
        for q in range(4):
            col = j * 4 + q
            for kc in range(DC):
                k.mm(pc[:, col:col + 1], w[:, kc, q * 128:(q + 1) * 128], condT_b[:, kc:kc + 1], kc == 0, False, [b_cond, wb], [pcb])
            k.mm(pc[:, col:col + 1], rowb[r][0:1, q * 128:(q + 1) * 128], ones_b[0:1, 0:1], False, True, [b_rowb[r], b_const], [pcb])
    k.cp("act", modc[:], pc[:, 0:2 * DC], [pcb], [b_modc])
    k.stt("dve", gsf_c[:], modc[:, DC:2 * DC], 1.0, gpre_c[:], ALU.add, ALU.mult, [b_modc, b_gpre], [b_gsf])
    shf_c = modc

    def transpose_tile(t, modulate):
        for g in range(DC // 4 if DC >= 4 else 1):
            nq = min(4, DC)
            pt, pb = k.bank()
            for q in range(nq):
                kc = g * 4 + q
                k.mm(pt[:, q * 128:(q + 1) * 128], xn[:, kc * 128:(kc + 1) * 128], identb[:, :], True, True, [b_xn, b_const], [pb])
            if not modulate:
                src = pt[:, 0:nq * 128].rearrange("p (a b) -> p a b", a=nq)
                eng = "act" if g % 2 == 0 else "dve"
                k.cp(eng, hT[:, g * 4:g * 4 + nq, t * 128:(t + 1) * 128], src, [pb], [b_hT[t]])
            else:
                for q in range(nq):
                    kc = g * 4 + q
                    o = hT[:, kc, t * 128:(t + 1) * 128]
                    i = pt[:, q * 128:(q + 1) * 128]
                    if q % 2 == 0:
                        k.act(o, i, AF.Identity, [pb, b_gsf, b_modc], [b_hT[t]], bias=shf_c[:, kc:kc + 1], scale=gsf_c[:, kc:kc + 1])
                    else:
                        k.ts("dve", o, i, gsf_c[:, kc:kc + 1], shf_c[:, kc:kc + 1], ALU.mult, ALU.add, [pb, b_gsf, b_modc], [b_hT[t]])

    for t in range(NTT):
        if ysrc is None:
            k.dma("sp", xt[:], ycat[t * 128:(t + 1) * 128, :], [], [b_xt], b_xt)
        else:
            for (c0, wd, fa, fb) in ysrc:
                if fb is None:
                    k.dma("sp", xt[:, c0:c0 + wd], fa(t), [], [b_xt], b_xt)
                    continue
                for c1 in range(0, wd, 512):
                    w_ = min(512, wd - c1)
                    r = (c1 // 512) % 2
                    k.dma("sp", xt[:, c0 + c1:c0 + c1 + w_], fa(t)[:, c1:c1 + w_], [], [b_xt], b_xt)
                    k.dma("sp", tmp[r][:, 0:w_], fb(t)[:, c1:c1 + w_], [], [b_tmp[r]], b_tmp[r])
                    k.ts("dve", xt[:, c0 + c1:c0 + c1 + w_], xt[:, c0 + c1:c0 + c1 + w_], selt[:, 0:1], None, ALU.mult, None, [b_xt, b_const], [b_xt])
                    k.stt("dve", xt[:, c0 + c1:c0 + c1 + w_], tmp[r][:, 0:w_], selt[:, 1:2], xt[:, c0 + c1:c0 + c1 + w_], ALU.mult, ALU.add, [b_tmp[r], b_xt, b_const], [b_xt])
        k.cp("act", xn[:], xt[:], [b_xt], [b_xn])
        transpose_tile(t, False)

    for n in range(NB):
        w, wb = next_wsl()
        load_w(w, wb, wout[:, n * 512:(n + 1) * 512], 512)
        for t in range(NTT):
            pt, pb = k.bank()
            for kc in range(DC):
                k.mm(pt[:, :], hT[:, kc, t * 128:(t + 1) * 128], w[:, kc, :], kc == 0, kc == DC - 1, [b_hT[t], wb], [pb])
            eng = "act" if t % 2 == 0 else "dve"
            k.cp(eng, acc[:, t, n * 512:(n + 1) * 512], pt[:, :], [pb], [b_acc[t][n]])

    def rstd_of(src_ap, src_bufs, col):
        k.memset("dve", st[:, col:col + 1], 0.0, [b_st])
        k.act(xn[:], src_ap, AF.Square, src_bufs + [b_st], [b_xn, b_st], accum=st[:, col:col + 1])
        k.ts("dve", st[:, col:col + 1], st[:, col:col + 1], 1.0 / D, EPS, ALU.mult, ALU.add, [b_st], [b_st])
        k.act(st[:, col:col + 1], st[:, col:col + 1], AF.Ln, [b_st], [b_st])
        k.act(st[:, col:col + 1], st[:, col:col + 1], AF.Exp, [b_st], [b_st], scale=-0.5)

    for t in range(NTT):
        k.dma("sp", xt[:], xin[t * 128:(t + 1) * 128, :], [], [b_xt], b_xt)
        rstd_of(acc[:, t, :], b_acc[t], 0)
        k.stt("dve", acc[:, t, :], acc[:, t, :], st[:, 0:1], gt_b[:, :], ALU.mult, ALU.mult, b_acc[t] + b_gt + [b_st], b_acc[t])
        k.tt("dve", xt[:], xt[:], acc[:, t, :], ALU.add, [b_xt] + b_acc[t], [b_xt])
        k.dma("sp", xmid[t * 128:(t + 1) * 128, :], xt[:], [b_xt], [b_xmid[t]], b_xt)
        rstd_of(xt[:], [b_xt], 1)
        k.act(xn[:], xt[:], AF.Copy, [b_xt, b_st], [b_xn], scale=st[:, 1:2])
        transpose_tile(t, True)


    blocks = [(hc, th) for hc in range(F // HC) for th in range(NT // TN)]
    wcur = {}

    def emit_gu(i):
        hc, th = blocks[i]
        if th == 0:
            w, wb = next_wsl()
            load_w(w, wb, wfi[:, hc * HC:(hc + 1) * HC], HC, 0)
            load_w(w, wb, wfi[:, F + hc * HC: F + (hc + 1) * HC], HC, HC)
            j = cnt["wos"] % 2; cnt["wos"] += 1
            k.dma("pool", wos[j][:, :, :], wfo[hc * HC:(hc + 1) * HC, :].rearrange("(s p) n -> p s n", p=128), [], [b_wos[j]], b_wos[j])
            wcur[hc] = (w, wb, wos[j], b_wos[j])
        w, wb, _, _ = wcur[hc]
        a = i % 2
        hbufs = [b_hT[th * TPB + q] for q in range(TPB)]
        for sub in range(SUB):
            pg, pgb = k.bank()
            for kc in range(DC):
                k.mm(pg[:, 0:TN], w[:, kc, sub * 128:(sub + 1) * 128], hT[:, kc, th * TN:(th + 1) * TN], kc == 0, kc == DC - 1, [wb] + hbufs, [pgb])
            pu, pub = k.bank()
            for kc in range(DC):
                k.mm(pu[:, 0:TN], w[:, kc, HC + sub * 128: HC + (sub + 1) * 128], hT[:, kc, th * TN:(th + 1) * TN], kc == 0, kc == DC - 1, [wb] + hbufs, [pub])
            r = sub % 2
            k.act(sg[r][:, :], pg[:, 0:TN], AF.Silu, [pgb], [b_sg[r]])
            k.tt("dve", actT[a][:, sub, :], sg[r][:, :], pu[:, 0:TN], ALU.mult, [b_sg[r], pub], [b_actT[a]])

    def emit_y(i):
        hc, th = blocks[i]
        _, _, wo, wob = wcur[hc]
        a = i % 2
        for tq in range(TPB):
            t = th * TPB + tq
            for n in range(NB):
                py, pyb = k.bank()
                for sub in range(SUB):
                    k.mm(py[:, :], actT[a][:, sub, tq * 128:(tq + 1) * 128], wo[:, sub, n * 512:(n + 1) * 512], sub == 0, sub == SUB - 1, [b_actT[a], wob], [pyb])
                dst = acc[:, t, n * 512:(n + 1) * 512]
                if hc == 0:
                    k.cp("dve", dst, py[:, :], [pyb], [b_acc[t][n]])
                else:
                    k.tt("dve", dst, dst, py[:, :], ALU.add, [pyb, b_acc[t][n]], [b_acc[t][n]])

    for i in range(len(blocks)):
        emit_gu(i)
        if i > 0:
            emit_y(i - 1)
    emit_y(len(blocks) - 1)

    mod_bcast(3 * D, gpost2)

    for t in range(NTT):
        k.dma("sp", xt[:], xmid[t * 128:(t + 1) * 128, :], [b_xmid[t]], [b_xt], b_xt)
        rstd_of(acc[:, t, :], b_acc[t], 2)
        k.stt("dve", acc[:, t, :], acc[:, t, :], st[:, 2:3], gt_b[:, :], ALU.mult, ALU.mult, b_acc[t] + b_gt + [b_st], b_acc[t])
        k.tt("dve", xt[:], xt[:], acc[:, t, :], ALU.add, [b_xt] + b_acc[t], [b_xt])
        k.dma("sp", xout[t * 128:(t + 1) * 128, :], xt[:], [b_xt], [], b_xt)
    k.finish_build()
    return nc


def build_rw(D, T, HO, SEG=512, RDT=F32, stop=99, k=None):
    k = k or KB(); nc, s = k.nc, k.s; B = s.buf
    DC = D // 128; CH = 64 * HO; NP = HO // 2; NSEG = T // SEG; NCH = SEG // 64; NZ = 3 * NP + 3
    k.psum_banks()
    x = k.dram("x", [T, D]); cvec = k.dram("cvec", [1, D]); wada2 = k.dram("wada2", [D, 2 * D]); bada2 = k.dram("bada2", [1, 2 * D])
    gpre = k.dram("gpre", [1, D]); ident = k.dram("ident", [128, 128])
    wr = k.dram("wr", [D, CH]); wk = k.dram("wk", [D, CH]); wv = k.dram("wv", [D, CH]); wl = k.dram("wl", [D, 288])
    mu_cat = k.dram("mu_cat", [1, NZ * 128]); pvec = k.dram("pvec", [1, 5 * CH]); lnx = k.dram("lnx", [1, 2 * CH])
    w2 = k.dram("w2", [64, CH]); a2 = k.dram("a2", [64, CH]); g2 = k.dram("g2", [160, CH])
    mask5 = k.dram("mask5", [64, 320]); tri2 = k.dram("tri2", [128, 128]); bones = k.dram("bones", [128, 128]); hsel = k.dram("hsel", [128, 2])
    yb = k.dram("yb", [T, CH], kind="ExternalOutput")
    ybanks = [k.reserve(), k.reserve()]
    pre = Pre(k, D, wada2, bada2, cvec, gpre, ident, nwsl=2)
    identf = pre.identf; bc = pre.b_const
    hTs = k.sb("hTs", [128, DC, SEG], BF16); b_hT = B()
    Rt = k.sb("Rt", [128, NP, SEG]); Kt = k.sb("Kt", [128, NP, SEG]); Vt = k.sb("Vt", [128, NP, SEG], BF16)
    Rb = k.sb("Rb", [128, NP, SEG], BF16); Kb = k.sb("Kb", [128, NP, SEG], BF16)
    BTt = k.sb("BTt", [128, NP, SEG], BF16); ATt = k.sb("ATt", [128, NP, SEG], BF16); RKt = k.sb("RKt", [128, NP, SEG])
    b_Rb = [B() for _ in range(NP)]; b_Kb = [B() for _ in range(NP)]
    b_R = [B() for _ in range(NP)]; b_K = [B() for _ in range(NP)]; b_V = [B() for _ in range(NP)]
    b_BT = [B() for _ in range(NP)]; b_AT = [B() for _ in range(NP)]; b_RK = [B() for _ in range(NP)]
    zwa = k.sb("zwa", [128, SEG]); zga = k.sb("zga", [128, SEG]); zgb = k.sb("zgb", [32, SEG]); b_zl = [B(), B(), B()]
    twb = k.sb("twb", [128, SEG], BF16); sga = k.sb("sga", [128, SEG], BF16); sgb = k.sb("sgb", [32, SEG], BF16); b_lo = B()
    zraw = [k.sb("zraw%d" % i, [128, SEG + 1]) for i in range(2)]; b_zraw = [B(), B()]
    carry = k.sb("carry", [128, NZ]); b_carry = B()
    S = [k.sb("S%d" % i, [128, SEG]) for i in range(7)]; b_S = [B() for _ in range(7)]
    mu_c = k.sb("mu_c", [128, NZ]); omm_c = k.sb("omm_c", [128, NZ]); pv_c = k.sb("pv_c", [128, 5 * NP]); b_par = B()
    pcs = k.sb("pcs", [128, NP, NCH]); b_pc = [B() for _ in range(NP)]
    w2a2 = k.sb("w2a2", [128, CH], BF16); g2a = k.sb("g2a", [128, CH], BF16); g2b = k.sb("g2b", [32, CH], BF16)
    m5 = k.sb("m5", [64, 320]); tri = k.sb("tri", [128, 128]); bon1 = k.sb("bon1", [128, 128]); hs = k.sb("hs", [128, 2])
    lg_b = k.sb("lg_b", [64, CH]); lb_b = k.sb("lb_b", [64, CH])
    RtO = k.sb("RtO", [64, NP, SEG], BF16); KtO = k.sb("KtO", [64, NP, SEG], BF16); BTO = k.sb("BTO", [64, NP, SEG], BF16); ATO = k.sb("ATO", [64, NP, SEG], BF16)
    b_RO = [B() for _ in range(NP)]; b_KO = [B() for _ in range(NP)]; b_BO = [B() for _ in range(NP)]; b_AO = [B() for _ in range(NP)]
    pcsO = k.sb("pcsO", [64, NP, NCH]); b_pcO = [B() for _ in range(NP)]
    Tst = [[k.sb("Tst%d_%d" % (h, i), [64, 64]) for i in range(2)] for h in range(HO)]
    Tb = [[k.sb("Tb%d_%d" % (h, i), [64, 64], BF16) for i in range(2)] for h in range(HO)]
    Ttmp = [k.sb("Ttmp%d" % i, [64, 64]) for i in range(2)]; b_Ttmp = [B(), B()]
    b_T = [[B(), B()] for _ in range(HO)]
    TM = [k.sb("TM%d" % p, [64, 3, 128], BF16) for p in range(NP)]; b_TM = [B() for _ in range(NP)]
    NS = HO
    G = [k.sb("G%d" % i, [64, 320], BF16) for i in range(NS)]; b_G = [B() for _ in range(NS)]
    NTt = [k.sb("NT%d" % i, [64, 64], BF16) for i in range(NS)]; b_NT = [B() for _ in range(NS)]
    LP = [[k.sb("LP%d_%d" % (i, j), [64, 128], BF16) for j in range(2)] for i in range(NS)]; b_LP = [[B(), B()] for _ in range(NS)]
    Wsb = [k.sb("Wsb%d" % i, [64, 64], BF16) for i in range(NS)]; b_W = [B() for _ in range(NS)]
    Usb = [k.sb("Usb%d" % h, [64, 64], BF16) for h in range(HO)]; b_U = [B() for _ in range(HO)]
    Y1 = k.sb("Y1", [64, CH]); b_Y1 = B(); bon = k.sb("bon", [64, HO]); b_bon = B()
    stt_ = k.sb("stt_", [64, 4 * HO]); b_stt = B(); Y2 = k.sb("Y2", [64, CH]); b_Y2 = B()

    for (dst, src) in ((m5, mask5), (tri, tri2), (bon1, bones), (hs, hsel)):
        k.dma("sp", dst[:], src[:, :], [], [bc], bc)
    k.dma("pool", w2a2[0:64, :], w2[:, :], [], [bc], bc)
    k.dma("pool", w2a2[64:128, :], a2[:, :], [], [bc], bc)
    k.dma("pool", g2a[:, :], g2[0:128, :], [], [bc], bc)
    k.dma("pool", g2b[:, :], g2[128:160, :], [], [bc], bc)
    pt, pb = k.bank()
    pre.col_from_dram(mu_cat, NZ * 128, pt, pb)
    k.cp("act", mu_c[:], pt[:, 0:NZ], [pb], [b_par])
    k.ts("dve", omm_c[:], mu_c[:], -1.0, 1.0, ALU.mult, ALU.add, [b_par], [b_par])
    pt, pb = k.bank()
    pre.col_from_dram(pvec, 5 * CH, pt, pb)
    k.cp("act", pv_c[:], pt[:, 0:5 * NP], [pb], [b_par])
    for (dst, off) in ((lg_b, 0), (lb_b, CH)):
        done = 0
        while done < CH:
            w = min(512, CH - done)
            k.dma("sp", pre.rowf[0][0:1, 0:w], lnx[0:1, off + done: off + done + w], [], [pre.b_rowf[0]], pre.b_rowf[0])
            pt, pb = k.bank()
            k.mm(pt[0:64, 0:w], pre.ones_f[0:1, 0:64], pre.rowf[0][0:1, 0:w], True, True, [bc, pre.b_rowf[0]], [pb])
            k.cp("act", dst[:, done:done + w], pt[0:64, 0:w], [pb], [b_par])
            done += w
    k.memset("dve", carry[:], 0.0, [b_carry])
    for h in range(HO):
        k.memset("dve", Tst[h][0][:], 0.0, [b_T[h][0]])
        k.memset("dve", Tb[h][0][:], 0.0, [b_T[h][0]])
    W0, A0, KK, KA, RKc = [lambda p, i=i: pv_c[:, i * NP + p: i * NP + p + 1] for i in range(5)]

    if stop == 1:
        k.finish_build(); return nc
    zi = {"n": 0}

    def ztile(w, wb, c0, M, dst, dbufs, idx):
        ps, pb = k.bank()
        for kc in range(DC):
            k.mm(ps[0:M, 0:SEG], w[:, kc, c0:c0 + M], hTs[:, kc, :], kc == 0, kc == DC - 1, [wb, b_hT], [pb])
        r = zi["n"] % 2; zi["n"] += 1
        zr = zraw[r]; bz = b_zraw[r]
        k.cp("act", zr[0:M, 1:SEG + 1], ps[0:M, 0:SEG], [pb], [bz])
        k.cp("dve", zr[0:M, 0:1], carry[0:M, idx:idx + 1], [b_carry], [bz])
        k.ts("dve", S[0][0:M, :], zr[0:M, 0:SEG], mu_c[0:M, idx:idx + 1], None, ALU.mult, None, [bz, b_par], [b_S[0]])
        k.stt("dve", dst, zr[0:M, 1:SEG + 1], omm_c[0:M, idx:idx + 1], S[0][0:M, :], ALU.mult, ALU.add, [bz, b_par, b_S[0]], dbufs)
        k.cp("dve", carry[0:M, idx:idx + 1], zr[0:M, SEG:SEG + 1], [bz], [b_carry])

    for sg_ in range(NSEG):
        t0 = sg_ * SEG
        for tq in range(SEG // 128):
            pre.norm_transpose(x[t0 + tq * 128: t0 + (tq + 1) * 128, :], hTs, tq * 128, b_hT)
        for (wsrc, arr, bufs, zbase) in ((wr, Rt, b_R, 0), (wk, Kt, b_K, NP), (wv, Vt, b_V, 2 * NP)):
            w, wb = pre.next_wsl()
            pre.load_w(w, wb, wsrc[:, :], CH)
            for p in range(NP):
                ztile(w, wb, p * 128, 128, arr[:, p, :], [bufs[p]], zbase + p)
        w, wb = pre.next_wsl()
        pre.load_w(w, wb, wl[:, :], 288)
        ztile(w, wb, 0, 128, zwa[:, :], [b_zl[0]], 3 * NP)
        ztile(w, wb, 128, 128, zga[:, :], [b_zl[1]], 3 * NP + 1)
        ztile(w, wb, 256, 32, zgb[:, :], [b_zl[2]], 3 * NP + 2)
        k.act(twb[0:64, :], zwa[0:64, :], AF.Tanh, [b_zl[0]], [b_lo])
        k.cp("dve", twb[64:128, :], zwa[64:128, :], [b_zl[0]], [b_lo])
        k.act(sga[:, :], zga[:, :], AF.Sigmoid, [b_zl[1]], [b_lo])
        k.act(sgb[:, :], zgb[:, :], AF.Sigmoid, [b_zl[2]], [b_lo])
        if stop == 2:
            k.finish_build(); return nc
        for p in range(NP):
            cs = slice(p * 128, (p + 1) * 128)
            ps, pb = k.bank()
            k.mm(ps[:, 0:SEG], w2a2[0:64, cs], twb[0:64, :], True, True, [bc, b_lo], [pb])
            k.act(S[1][:, :], ps[:, 0:SEG], AF.Sigmoid, [pb, b_par], [b_S[1]], bias=W0(p))
            k.ts("dve", S[1][:, :], S[1][:, :], -0.6065306597126334, None, ALU.mult, None, [b_S[1]], [b_S[1]])
            ps, pb = k.bank()
            k.mm(ps[:, 0:SEG], w2a2[64:128, cs], twb[64:128, :], True, True, [bc, b_lo], [pb])
            k.act(S[2][:, :], ps[:, 0:SEG], AF.Sigmoid, [pb, b_par], [b_S[2]], bias=A0(p))
            k.ts("dve", S[3][:, :], Kt[:, p, :], KK(p), None, ALU.mult, None, [b_K[p], b_par], [b_S[3]])
            k.tt("dve", S[4][:, :], S[3][:, :], S[3][:, :], ALU.mult, [b_S[3]], [b_S[4]])
            ps, pb = k.bank()
            k.mm(ps[:, 0:SEG], bon1[:, :], S[4][:, :], True, True, [bc, b_S[4]], [pb])
            k.act(S[4][:, :], ps[:, 0:SEG], AF.Sqrt, [pb], [b_S[4]])
            k.ts("dve", S[4][:, :], S[4][:, :], 1e-12, None, ALU.max, None, [b_S[4]], [b_S[4]])
            k.recip("dve", S[4][:, :], S[4][:, :], [b_S[4]], [b_S[4]])
            k.tt("dve", S[3][:, :], S[3][:, :], S[4][:, :], ALU.mult, [b_S[3], b_S[4]], [b_S[3]])
            k.ts("dve", S[4][:, :], S[2][:, :], 1.0, KA(p), ALU.subtract, ALU.mult, [b_S[2], b_par], [b_S[4]])
            k.stt("dve", Kt[:, p, :], S[4][:, :], 1.0, Kt[:, p, :], ALU.add, ALU.mult, [b_S[4], b_K[p]], [b_K[p]])
            k.stt("dve", RKt[:, p, :], Rt[:, p, :], RKc(p), Kt[:, p, :], ALU.mult, ALU.mult, [b_R[p], b_K[p], b_par], [b_RK[p]])
            k.tt("dve", S[2][:, :], S[3][:, :], S[2][:, :], ALU.mult, [b_S[3], b_S[2]], [b_S[2]])
            pc_, pcb = k.bank()
            for q in range(SEG // 128):
                qs = slice(q * 128, (q + 1) * 128)
                pt, ptb = k.bank()
                k.mm(pt[:, 0:128], S[1][:, qs], identf[:, :], True, True, [b_S[1], bc], [ptb])
                k.cp("act", S[5][:, qs], pt[:, 0:128], [ptb], [b_S[5]])
                k.mm(pc_[:, qs], S[5][:, qs], tri[:, :], True, True, [b_S[5], bc], [pcb])
            k.cp("act", S[4][:, :], pc_[:, 0:SEG], [pcb], [b_S[4]])
            k.act(S[5][:, :], S[4][:, :], AF.Exp, [b_S[4]], [b_S[5]])
            k.tt("dve", Rb[:, p, :], Rt[:, p, :], S[5][:, :], ALU.mult, [b_R[p], b_S[5]], [b_Rb[p]])
            k.cp("dve", pcs[:, p, :], S[5][:, :].rearrange("p (c t) -> p c t", t=64)[:, :, 63], [b_S[5]], [b_pc[p]])
            k.act(S[6][:, :], S[4][:, :], AF.Exp, [b_S[4]], [b_S[6]], scale=-1.0)
            k.tt("dve", Kb[:, p, :], Kt[:, p, :], S[6][:, :], ALU.mult, [b_K[p], b_S[6]], [b_Kb[p]])
            k.tt("dve", BTt[:, p, :], S[2][:, :], S[6][:, :], ALU.mult, [b_S[2], b_S[6]], [b_BT[p]])
            k.tt("dve", S[4][:, :], S[4][:, :], S[1][:, :], ALU.subtract, [b_S[4], b_S[1]], [b_S[4]])
            k.act(S[4][:, :], S[4][:, :], AF.Exp, [b_S[4]], [b_S[4]])
            k.stt("dve", ATt[:, p, :], S[3][:, :], -1.0, S[4][:, :], ALU.mult, ALU.mult, [b_S[3], b_S[4]], [b_AT[p]])
            for (dst, src, bs_, bd_) in ((RtO, Rb, b_Rb, b_RO), (KtO, Kb, b_Kb, b_KO), (BTO, BTt, b_BT, b_BO), (ATO, ATt, b_AT, b_AO)):
                k.dma("sp", dst[:, p, :], src[64:128, p, :], [bs_[p]], [bd_[p]], bd_[p])
            k.dma("sp", pcsO[:, p, :], pcs[64:128, p, :], [b_pc[p]], [b_pcO[p]], b_pcO[p])
        if stop == 3:
            k.finish_build(); return nc
        for c in range(NCH):
            cg = sg_ * NCH + c
            cur = cg % 2
            cols = slice(c * 64, (c + 1) * 64)
            yps, ypb = ybanks[cg % 2]
            for p in range(NP):
                pt, ptb = k.bank()
                for i, (arr, bb) in enumerate(((Vt, b_V), (BTt, b_BT), (Kb, b_Kb))):
                    k.mm(pt[0:64, i * 128:(i + 1) * 128], arr[:, p, cols], pre.identb[:, :], True, True, [bb[p], bc], [ptb])
                k.cp("act", TM[p][:, :, :], pt[0:64, 0:384].rearrange("p (a b) -> p a b", a=3), [ptb], [b_TM[p]])
            H = []
            for h in range(HO):
                p, e = h // 2, h % 2
                rows = slice(e * 64, (e + 1) * 64)
                if e == 0:
                    bt = BTt[0:64, p, cols]; at = ATt[0:64, p, cols]; rt = Rb[0:64, p, cols]; kt = Kb[0:64, p, cols]
                    deps = [b_BT[p], b_AT[p], b_Rb[p], b_Kb[p]]
                else:
                    bt = BTO[:, p, cols]; at = ATO[:, p, cols]; rt = RtO[:, p, cols]; kt = KtO[:, p, cols]
                    deps = [b_BO[p], b_AO[p], b_RO[p], b_KO[p]]
                H.append(dict(p=p, e=e, rows=rows, bt=bt, at=at, rt=rt, kt=kt, deps=deps, b_at=deps[1], b_rt=deps[2]))
            def evac_g(g_):
                dg = H[g_]; psg, pbg = dg["ps"]
                k.tt("dve", G[g_][:, :], psg[0:64, 0:320], m5[:, :], ALU.mult, [pbg, bc], [b_G[g_]])
                k.tt("dve", NTt[g_][:, :], G[g_][:, 0:64], pre.identb[0:64, 0:64], ALU.add, [b_G[g_], bc], [b_NT[g_]])
                dg["Lk"] = G[g_][:, 256:320]; dg["Pk"] = G[g_][:, 0:64]; dg["lb"] = [b_G[g_]]
            for h in range(HO):
                d = H[h]; ps, pb = k.bank()
                k.mm(ps[0:64, 0:64], d["bt"], d["at"], True, True, d["deps"], [pb])
                k.mm(ps[0:64, 64:128], d["bt"], d["rt"], True, True, d["deps"], [pb])
                k.mm(ps[0:64, 128:192], d["kt"], d["at"], True, True, d["deps"], [pb])
                k.mm(ps[0:64, 192:256], d["kt"], d["rt"], True, True, d["deps"], [pb])
                k.mm(ps[0:64, 256:320], d["at"], d["bt"], True, True, d["deps"], [pb])
                d["ps"] = (ps, pb)
                if h >= 3:
                    evac_g(h - 3)
            for g_ in range(max(0, HO - 3), HO):
                evac_g(g_)
            for lev in range(5):
                j = lev % 2
                for h in range(HO):
                    d = H[h]; ps, pb = k.bank()
                    k.mm(ps[0:64, 0:64], d["Pk"], d["Lk"], True, True, d["lb"], [pb])
                    if lev < 4:
                        k.mm(ps[0:64, 64:128], d["Lk"], d["Pk"], True, True, d["lb"], [pb])
                    d["ps"] = (ps, pb)
                    if h >= 3:
                        g_ = h - 3; dg = H[g_]; psg, pbg = dg["ps"]
                        k.cp("act", LP[g_][j][:, :], psg[0:64, 0:128], [pbg], [b_LP[g_][j]])
                        dg["Lk"] = LP[g_][j][:, 0:64]; dg["Pk"] = LP[g_][j][:, 64:128]; dg["lb"] = [b_LP[g_][j]]
                for g_ in range(max(0, HO - 3), HO):
                    dg = H[g_]; psg, pbg = dg["ps"]
                    k.cp("act", LP[g_][j][:, :], psg[0:64, 0:128], [pbg], [b_LP[g_][j]])
                    dg["Lk"] = LP[g_][j][:, 0:64]; dg["Pk"] = LP[g_][j][:, 64:128]; dg["lb"] = [b_LP[g_][j]]
                for h in range(HO):
                    d = H[h]; ps2, pb2 = k.bank()
                    k.mm(ps2[0:64, 0:64], d["Lk"], NTt[h][:, :], True, True, d["lb"] + [b_NT[h]], [pb2])
                    d["ps2"] = (ps2, pb2)
                    if h >= 3:
                        g_ = h - 3; ps3, pb3 = H[g_]["ps2"]
                        k.tt("dve", NTt[g_][:, :], NTt[g_][:, :], ps3[0:64, 0:64], ALU.add, [pb3, b_NT[g_]], [b_NT[g_]])
                for g_ in range(max(0, HO - 3), HO):
                    ps3, pb3 = H[g_]["ps2"]
                    k.tt("dve", NTt[g_][:, :], NTt[g_][:, :], ps3[0:64, 0:64], ALU.add, [pb3, b_NT[g_]], [b_NT[g_]])
            for h in range(HO):
                d = H[h]; p = d["p"]; ps, pb = k.bank()
                vte = TM[p][:, 0, d["rows"]]
                k.mm(ps[0:64, 0:64], G[h][:, 128:192], vte, True, False, [b_G[h], b_TM[p]], [pb])
                k.mm(ps[0:64, 0:64], d["at"], Tb[h][cur][:, :], False, True, [d["b_at"], b_T[h][cur]], [pb])
                d["ps"] = (ps, pb)
                if h >= 3:
                    g_ = h - 3; psg, pbg = H[g_]["ps"]
                    k.cp("act", Wsb[g_][:, :], psg[0:64, 0:64], [pbg], [b_W[g_]])
            for g_ in range(max(0, HO - 3), HO):
                psg, pbg = H[g_]["ps"]
                k.cp("act", Wsb[g_][:, :], psg[0:64, 0:64], [pbg], [b_W[g_]])
            for h in range(HO):
                d = H[h]; ps, pb = k.bank()
                k.mm(ps[0:64, 0:64], NTt[h][:, :], Wsb[h][:, :], True, True, [b_NT[h], b_W[h]], [pb])
                d["ps"] = (ps, pb)
                if h >= 3:
                    g_ = h - 3; psg, pbg = H[g_]["ps"]
                    k.cp("act", Usb[g_][:, :], psg[0:64, 0:64], [pbg], [b_U[g_]])
            for g_ in range(max(0, HO - 3), HO):
                psg, pbg = H[g_]["ps"]
                k.cp("act", Usb[g_][:, :], psg[0:64, 0:64], [pbg], [b_U[g_]])
            for h in range(HO):
                d = H[h]; p = d["p"]
                vte = TM[p][:, 0, d["rows"]]
                yo = yps[0:64, h * 64:(h + 1) * 64]
                k.mm(yo, d["rt"], Tb[h][cur][:, :], True, False, [d["b_rt"], b_T[h][cur]], [ypb])
                k.mm(yo, G[h][:, 64:128], Usb[h][:, :], False, False, [b_G[h], b_U[h]], [ypb])
                k.mm(yo, G[h][:, 192:256], vte, False, True, [b_G[h], b_TM[p]], [ypb])
            for h in range(HO):
                d = H[h]; p = d["p"]; e = d["e"]; rows = d["rows"]
                ps, pb = k.bank()
                k.mm(ps[0:64, 0:64], TM[p][:, 1, rows], Usb[h][:, :], True, False, [b_TM[p], b_U[h]], [pb])
                k.mm(ps[0:64, 0:64], TM[p][:, 2, rows], TM[p][:, 0, rows], False, True, [b_TM[p]], [pb])
                pc_ap = pcs[0:64, p, c:c + 1] if e == 0 else pcsO[:, p, c:c + 1]
                pcb_ = b_pc[p] if e == 0 else b_pcO[p]
                tj = h % 2
                k.ts("dve", Ttmp[tj][:, :], Tst[h][cur][:, :], pc_ap, None, ALU.mult, None, [b_T[h][cur], pcb_], [b_Ttmp[tj]])
                k.stt("dve", Tst[h][1 - cur][:, :], ps[0:64, 0:64], pc_ap, Ttmp[tj][:, :], ALU.mult, ALU.add, [pb, pcb_, b_Ttmp[tj]], [b_T[h][1 - cur]])
                k.cp("act", Tb[h][1 - cur][:, :], Tst[h][1 - cur][:, :], [b_T[h][1 - cur]], [b_T[h][1 - cur]])
            if stop == 4:
                k.finish_build(); return nc
            pbn, pbnb = k.bank()
            for p in range(NP):
                k.mm(pbn[0:64, 2 * p:2 * p + 2], RKt[:, p, cols], hs[:, :], True, True, [b_RK[p], bc], [pbnb])
            k.cp("act", bon[:, :], pbn[0:64, 0:HO], [pbnb], [b_bon])
            pg, pgb = k.bank()
            k.mm(pg[0:64, 0:CH], sga[:, cols], g2a[:, :], True, False, [b_lo, bc], [pgb])
            k.mm(pg[0:64, 0:CH], sgb[:, cols], g2b[:, :], False, True, [b_lo, bc], [pgb])
            if stop == 61:
                k.finish_build(); return nc
            yv = yps[0:64, 0:CH].rearrange("p (h d) -> p h d", d=64)
            k.cp("act", Y1[:, :], yps[0:64, 0:CH], [ypb], [b_Y1])
            k.red("dve", stt_[:, 0:HO], Y1[:, :].rearrange("p (h d) -> p h d", d=64), ALU.add, AX.X, [b_Y1], [b_stt])
            if stop == 615:
                k.finish_build(); return nc
            k.tt("dve", Y2[:, :], Y1[:, :], Y1[:, :], ALU.mult, [b_Y1], [b_Y2])
            k.red("dve", stt_[:, HO:2 * HO], Y2[:, :].rearrange("p (h d) -> p h d", d=64), ALU.add, AX.X, [b_Y2], [b_stt])
            if stop == 616:
                k.finish_build(); return nc
            k.ts("dve", stt_[:, 0:HO], stt_[:, 0:HO], 1.0 / 64, None, ALU.mult, None, [b_stt], [b_stt])
            k.tt("dve", stt_[:, 2 * HO:3 * HO], stt_[:, 0:HO], stt_[:, 0:HO], ALU.mult, [b_stt], [b_stt])
            k.stt("dve", stt_[:, HO:2 * HO], stt_[:, HO:2 * HO], 1.0 / 64, stt_[:, 2 * HO:3 * HO], ALU.mult, ALU.subtract, [b_stt], [b_stt])
            if stop == 617:
                k.finish_build(); return nc
            k.ts("dve", stt_[:, HO:2 * HO], stt_[:, HO:2 * HO], 64e-5, None, ALU.add, None, [b_stt], [b_stt])
            k.act(stt_[:, HO:2 * HO], stt_[:, HO:2 * HO], AF.Ln, [b_stt], [b_stt])
            k.act(stt_[:, HO:2 * HO], stt_[:, HO:2 * HO], AF.Exp, [b_stt], [b_stt], scale=-0.5)
            if stop == 62:
                k.finish_build(); return nc
            for h in range(HO):
                hsl = slice(h * 64, (h + 1) * 64)
                k.ts("dve", Y1[:, hsl], Y1[:, hsl], stt_[:, h:h + 1], stt_[:, HO + h:HO + h + 1], ALU.subtract, ALU.mult, [b_Y1, b_stt], [b_Y1])
            k.tt("dve", Y1[:, :], Y1[:, :], lg_b[:, :], ALU.mult, [b_Y1, b_par], [b_Y1])
            k.tt("dve", Y1[:, :], Y1[:, :], lb_b[:, :], ALU.add, [b_Y1, b_par], [b_Y1])
            for h in range(HO):
                p, e = h // 2, h % 2
                hsl = slice(h * 64, (h + 1) * 64)
                k.stt("dve", Y1[:, hsl], TM[p][:, 0, e * 64:(e + 1) * 64], bon[:, h:h + 1], Y1[:, hsl], ALU.mult, ALU.add, [b_TM[p], b_bon, b_Y1], [b_Y1])
            k.tt("dve", Y2[:, :], Y1[:, :], pg[0:64, 0:CH], ALU.mult, [b_Y1, pgb], [b_Y2])
            if stop == 63:
                k.finish_build(); return nc
            k.dma("sp", yb[t0 + c * 64: t0 + (c + 1) * 64, :], Y2[:, :], [b_Y2], [], b_Y2)
            if stop == 64 + cg:
                k.finish_build(); return nc
    for b_ in ybanks:
        k.unreserve(b_)
    k.finish_build()
    return nc


def build_gm(D, NT, AW, NG, k=None):
    k = k or KB(); nc, s = k.nc, k.s; B = s.buf
    DC = D // 128; NTT = NT // 128; NB = AW // 512 if AW >= 512 else 1; CW = min(512, AW)
    k.psum_banks()
    x = k.dram("x", [NT, D]); cvec = k.dram("cvec", [1, D]); wada2 = k.dram("wada2", [D, 2 * D]); bada2 = k.dram("bada2", [1, 2 * D])
    gpre = k.dram("gpre", [1, D]); ident = k.dram("ident", [128, 128])
    wu = k.dram("wu", [D, AW]); wv = k.dram("wv", [D, AW]); vgb = k.dram("vgb", [1, 2 * AW])
    ws = k.dram("ws", [NG, 128, 128]); bs = k.dram("bs", [1, NG * 128]); triu = k.dram("triu", [128, 128])
    ya = k.dram("ya", [NT, AW], kind="ExternalOutput")
    pre = Pre(k, D, wada2, bada2, cvec, gpre, ident, nwsl=3)
    identf = pre.identf; bc = pre.b_const
    hT = k.sb("hT", [128, DC, NT], BF16); b_hT = [B() for _ in range(NTT)]
    U = k.sb("U", [128, NTT, AW]); V = k.sb("V", [128, NTT, AW]); b_U = [B() for _ in range(NTT)]; b_V = [B() for _ in range(NTT)]
    wsT = k.sb("wsT", [128, NG, 128], BF16); tmpw = k.sb("tmpw", [128, 128]); b_tw = B(); tru = k.sb("tru", [128, 128])
    bs_c = k.sb("bs_c", [128, NG]); vg_b = k.sb("vg_b", [128, AW]); vb_b = k.sb("vb_b", [128, AW]); b_par = B()
    vnb = k.sb("vnb", [128, AW], BF16); b_vn = B(); st2 = k.sb("st2", [128, 4]); b_st2 = B(); yo = k.sb("yo", [128, AW]); b_yo = B()
    junk = k.sb("junk", [128, AW], BF16); b_junk = B()
    k.dma("sp", tru[:], triu[:, :], [], [bc], bc)
    for g in range(NG):
        k.dma("sp", tmpw[:], ws[g, :, :], [], [b_tw], b_tw)
        pt, pb = k.bank()
        k.mm(pt[:, 0:128], tmpw[:, :], identf[:, :], True, True, [b_tw, bc], [pb])
        k.tt("dve", wsT[:, g, :], pt[:, 0:128], tru[:, :], ALU.mult, [pb, bc], [b_par])
    pt, pb = k.bank()
    pre.col_from_dram(bs, NG * 128, pt, pb)
    k.cp("act", bs_c[:], pt[:, 0:NG], [pb], [b_par])
    for (dst, off) in ((vg_b, 0), (vb_b, AW)):
        done = 0
        while done < AW:
            w = min(512, AW - done)
            k.dma("sp", pre.rowf[0][0:1, 0:w], vgb[0:1, off + done: off + done + w], [], [pre.b_rowf[0]], pre.b_rowf[0])
            pt, pb = k.bank()
            k.mm(pt[:, 0:w], pre.ones_f[0:1, :], pre.rowf[0][0:1, 0:w], True, True, [bc, pre.b_rowf[0]], [pb])
            k.cp("act", dst[:, done:done + w], pt[:, 0:w], [pb], [b_par])
            done += w
    for t in range(NTT):
        pre.norm_transpose(x[t * 128:(t + 1) * 128, :], hT, t * 128, b_hT[t])
    for (wsrc, dst, bufs) in ((wu, U, b_U), (wv, V, b_V)):
        for n in range(AW // CW):
            w, wb = pre.next_wsl()
            pre.load_w(w, wb, wsrc[:, n * CW:(n + 1) * CW], CW)
            for t in range(NTT):
                pt, pb = k.bank()
                for kc in range(DC):
                    k.mm(pt[:, 0:CW], hT[:, kc, t * 128:(t + 1) * 128], w[:, kc, 0:CW], kc == 0, kc == DC - 1, [b_hT[t], wb], [pb])
                k.act(dst[:, t, n * CW:(n + 1) * CW], pt[:, 0:CW], AF.Gelu, [pb], [bufs[t]])
    for t in range(NTT):
        v = V[:, t, :]
        k.red("dve", st2[:, 0:1], v, ALU.add, AX.X, [b_V[t]], [b_st2])
        k.memset("dve", st2[:, 1:2], 0.0, [b_st2])
        k.act(junk[:, :], v, AF.Square, [b_V[t], b_st2], [b_junk, b_st2], accum=st2[:, 1:2])
        k.ts("dve", st2[:, 0:1], st2[:, 0:1], 1.0 / AW, None, ALU.mult, None, [b_st2], [b_st2])
        k.tt("dve", st2[:, 2:3], st2[:, 0:1], st2[:, 0:1], ALU.mult, [b_st2], [b_st2])
        k.stt("dve", st2[:, 1:2], st2[:, 1:2], 1.0 / AW, st2[:, 2:3], ALU.mult, ALU.subtract, [b_st2], [b_st2])
        k.ts("dve", st2[:, 1:2], st2[:, 1:2], 1e-5, None, ALU.add, None, [b_st2], [b_st2])
        k.act(st2[:, 1:2], st2[:, 1:2], AF.Ln, [b_st2], [b_st2])
        k.act(st2[:, 1:2], st2[:, 1:2], AF.Exp, [b_st2], [b_st2], scale=-0.5)
        k.ts("dve", v, v, st2[:, 0:1], st2[:, 1:2], ALU.subtract, ALU.mult, [b_V[t], b_st2], [b_V[t]])
        k.tt("dve", v, v, vg_b[:, :], ALU.mult, [b_V[t], b_par], [b_V[t]])
        k.tt("dve", vnb[:, :], v, vb_b[:, :], ALU.add, [b_V[t], b_par], [b_vn])
        for g4 in range(max(1, NG // 4)):
            pt, pb = k.bank()
            ng = min(4, NG)
            for q in range(ng):
                g = g4 * 4 + q
                k.mm(pt[:, q * 128:(q + 1) * 128], wsT[:, g, :], vnb[:, g * 128:(g + 1) * 128], True, True, [b_par, b_vn], [pb])
            for q in range(ng):
                g = g4 * 4 + q
                gs = slice(g * 128, (g + 1) * 128)
                k.stt("dve", yo[:, gs], pt[:, q * 128:(q + 1) * 128], bs_c[:, g:g + 1], U[:, t, gs], ALU.add, ALU.mult, [pb, b_par, b_U[t]], [b_yo])
        k.dma("sp", ya[t * 128:(t + 1) * 128, :], yo[:, :], [b_yo], [], b_yo)
    k.finish_build()
    return nc


NEG = -1.0e30


def build_mb(D, T, HO, SEG=512, k=None, xfn=None):
    k = k or KB(); nc, s = k.nc, k.s; B = s.buf
    DC = D // 128; DH = 128; BLK = 256; NBLK = T // BLK; NQT = T // 128; HC = HO * DH; NSEG = T // SEG; CW = min(512, HC)
    assert NBLK == 8
    k.psum_banks()
    x = k.dram("x", [T, D]); cvec = k.dram("cvec", [1, D]); wada2 = k.dram("wada2", [D, 2 * D]); bada2 = k.dram("bada2", [1, 2 * D])
    gpre = k.dram("gpre", [1, D]); ident = k.dram("ident", [128, 128])
    wq = k.dram("wq", [D, HC]); wk = k.dram("wk", [D, HC]); wv = k.dram("wv", [D, HC])
    slope = k.dram("slope", [1, 128]); kpos = k.dram("kpos", [128, T]); cmask = k.dram("cmask", [128, 128]); gmask = k.dram("gmask", [128, 64])
    o = k.dram("o", [T, HC], kind="ExternalOutput")
    pre = Pre(k, D, wada2, bada2, cvec, gpre, ident, nwsl=2)
    identb = pre.identb; bc = pre.b_const
    hTs = k.sb("hTs", [128, DC, SEG], BF16); b_hT = B()
    QF = k.sb("QF", [128, HO, T], BF16); KF = k.sb("KF", [128, HO, T], BF16); VT = k.sb("VT", [128, NQT, HC], BF16)
    b_Q = [B() for _ in range(HO)]; b_K = [B() for _ in range(HO)]; b_V = [B() for _ in range(NQT)]
    kp = k.sb("kp", [128, T]); cm = k.sb("cm", [128, 128]); gmc = k.sb("gmc", [128, 64]); slc = k.sb("slc", [128, 128])
    kmf = k.sb("kmf", [128, 8]); kmb = k.sb("kmb", [128, HO, 8], BF16); b_km = B()
    ali = k.sb("ali", [128, T]); b_ali = B()
    Ssb = k.sb("Ssb", [128, T]); b_S = B()
    Pb2 = [k.sb("Pb%d" % i, [128, T], BF16) for i in range(2)]; b_P2 = [B(), B()]
    PT2 = [k.sb("PT%d" % i, [128, NQT, 128], BF16) for i in range(2)]; b_PT2 = [B(), B()]
    gsb2 = [k.sb("gsb%d" % i, [128, 8]) for i in range(2)]; mx82 = [k.sb("mx8%d" % i, [128, 8]) for i in range(2)]
    selb2 = [k.sb("selb%d" % i, [128, 8]) for i in range(2)]; b_g2 = [B(), B()]
    sm2 = [k.sb("sm%d" % i, [128, 4]) for i in range(2)]; b_sm2 = [B(), B()]
    osb = [k.sb("osb%d" % i, [128, 128]) for i in range(2)]; b_o = [B(), B()]
    for (dst, src) in ((kp, kpos), (cm, cmask), (gmc, gmask)):
        k.dma("sp", dst[:], src[:, :], [], [bc], bc)
    k.dma("sp", pre.rowf[0][0:1, 0:128], slope[0:1, :], [], [pre.b_rowf[0]], pre.b_rowf[0])
    pt, pb = k.bank()
    k.mm(pt[:, 0:128], pre.ones_f[0:1, :], pre.rowf[0][0:1, 0:128], True, True, [bc, pre.b_rowf[0]], [pb])
    k.cp("act", slc[:, :], pt[:, 0:128], [pb], [bc])
    for sg_ in range(NSEG):
        t0 = sg_ * SEG
        for tq in range(SEG // 128):
            xsrc_ = xfn(sg_ * (SEG // 128) + tq) if xfn is not None else x[t0 + tq * 128: t0 + (tq + 1) * 128, :]
            pre.norm_transpose(xsrc_, hTs, tq * 128, b_hT)
        for (wsrc, dst, bufs, sc_) in ((wq, QF, b_Q, DH ** -0.5), (wk, KF, b_K, None)):
            for n in range(HC // CW):
                w, wb = pre.next_wsl()
                pre.load_w(w, wb, wsrc[:, n * CW:(n + 1) * CW], CW)
                for hh in range(CW // 128):
                    h = n * (CW // 128) + hh
                    pt, pb = k.bank()
                    for kc in range(DC):
                        k.mm(pt[:, 0:SEG], w[:, kc, hh * 128:(hh + 1) * 128], hTs[:, kc, :], kc == 0, kc == DC - 1, [wb, b_hT], [pb])
                    if sc_ is not None:
                        k.act(dst[:, h, t0:t0 + SEG], pt[:, 0:SEG], AF.Copy, [pb], [bufs[h]], scale=sc_)
                    else:
                        k.cp("dve", dst[:, h, t0:t0 + SEG], pt[:, 0:SEG], [pb], [bufs[h]])
        for n in range(HC // CW):
            w, wb = pre.next_wsl()
            pre.load_w(w, wb, wv[:, n * CW:(n + 1) * CW], CW)
            for tq in range(SEG // 128):
                tt_ = sg_ * (SEG // 128) + tq
                pt, pb = k.bank()
                for kc in range(DC):
                    k.mm(pt[:, 0:CW], hTs[:, kc, tq * 128:(tq + 1) * 128], w[:, kc, 0:CW], kc == 0, kc == DC - 1, [wb, b_hT], [pb])
                if tq % 2 == 0:
                    k.cp("act", VT[:, tt_, n * CW:(n + 1) * CW], pt[:, 0:CW], [pb], [b_V[tt_]])
                else:
                    k.cp("dve", VT[:, tt_, n * CW:(n + 1) * CW], pt[:, 0:CW], [pb], [b_V[tt_]])
    for h in range(HO):
        k.red("dve", kmf[:, :], KF[:, h, :].rearrange("p (n s) -> p n s", s=BLK), ALU.add, AX.X, [b_K[h]], [b_km])
        k.ts("dve", kmb[:, h, :], kmf[:, :], 1.0 / BLK, None, ALU.mult, None, [b_km], [b_km])
    it = 0
    for h in range(HO):
        k.ts("dve", ali[:, :], kp[:, :], slc[:, h:h + 1], None, ALU.mult, None, [bc], [b_ali])
        for qt in range(NQT):
            qb = qt // 2; nk = (qt + 1) * 128
            jj = it % 2
            Pb = Pb2[jj]; b_P = b_P2[jj]; PT = PT2[jj]; b_PT = b_PT2[jj]; gsb = gsb2[jj]; mx8 = mx82[jj]; selb = selb2[jj]; b_g = b_g2[jj]
            sm = sm2[jj]; b_sm = b_sm2[jj]
            ql = QF[:, h, qt * 128:(qt + 1) * 128]
            pg, pgb = k.bank()
            k.mm(pg[:, 0:8], ql, kmb[:, h, :], True, True, [b_Q[h], b_km], [pgb])
            k.tt("dve", gsb[:, :], pg[:, 0:8], gmc[:, qb * 8:(qb + 1) * 8], ALU.add, [pgb, bc], [b_g])
            k.max8(mx8[:, :], gsb[:, :], [b_g], [b_g])
            k.ts("dve", selb[:, :], gsb[:, :], mx8[:, 2:3], 1.0, ALU.is_ge, ALU.subtract, [b_g], [b_g])
            k.ts("dve", selb[:, :], selb[:, :], 1.0e30, None, ALU.mult, None, [b_g], [b_g])
            for kg in range((nk + 511) // 512):
                w_ = min(512, nk - kg * 512)
                ps, pb = k.bank()
                k.mm(ps[:, 0:w_], ql, KF[:, h, kg * 512: kg * 512 + w_], True, True, [b_Q[h], b_K[h]], [pb])
                for kt in range(kg * 4, kg * 4 + w_ // 128):
                    n = kt // 2
                    lo = (kt - kg * 4) * 128
                    ks = slice(kt * 128, (kt + 1) * 128)
                    if n < qb:
                        if kt % 2 == 0:
                            ks2 = slice(kt * 128, (kt + 2) * 128)
                            k.stt("dve", Ssb[:, ks2], ps[:, lo:lo + 256], selb[:, n:n + 1], ali[:, ks2], ALU.add, ALU.add, [pb, b_g, b_ali], [b_S])
                    else:
                        k.tt("dve", Ssb[:, ks], ps[:, lo:lo + 128], ali[:, ks], ALU.add, [pb, b_ali], [b_S])
                        if kt == qt:
                            k.tt("dve", Ssb[:, ks], Ssb[:, ks], cm[:, :], ALU.add, [b_S, bc], [b_S])
            k.red("dve", sm[:, 0:1], Ssb[:, 0:nk], ALU.max, AX.X, [b_S], [b_sm])
            k.ts("dve", sm[:, 0:1], sm[:, 0:1], -1.0, None, ALU.mult, None, [b_sm], [b_sm])
            k.memset("dve", sm[:, 1:2], 0.0, [b_sm])
            k.act(Pb[:, 0:nk], Ssb[:, 0:nk], AF.Exp, [b_S, b_sm], [b_P, b_sm], bias=sm[:, 0:1], accum=sm[:, 1:2])
            k.recip("dve", sm[:, 2:3], sm[:, 1:2], [b_sm], [b_sm])
            for g4 in range((qt + 4) // 4):
                nq = min(4, qt + 1 - g4 * 4)
                pt, pb = k.bank()
                for q in range(nq):
                    kt = g4 * 4 + q
                    k.mm(pt[:, q * 128:(q + 1) * 128], Pb[:, kt * 128:(kt + 1) * 128], identb[:, :], True, True, [b_P, bc], [pb])
                src = pt[:, 0:nq * 128].rearrange("p (a b) -> p a b", a=nq)
                if g4 % 2 == 0:
                    k.cp("act", PT[:, g4 * 4:g4 * 4 + nq, :], src, [pb], [b_PT])
                else:
                    k.cp("dve", PT[:, g4 * 4:g4 * 4 + nq, :], src, [pb], [b_PT])
            po, pob = k.bank()
            for kt in range(qt + 1):
                k.mm(po[:, 0:128], PT[:, kt, :], VT[:, kt, h * 128:(h + 1) * 128], kt == 0, kt == qt, [b_PT, b_V[kt]], [pob])
            j = it % 2; it += 1
            k.ts("dve", osb[j][:, :], po[:, 0:128], sm[:, 2:3], None, ALU.mult, None, [pob, b_sm], [b_o[j]])
            k.dma("sp", o[qt * 128:(qt + 1) * 128, h * 128:(h + 1) * 128], osb[j][:, :], [b_o[j]], [], b_o[j])
    k.finish_build()
    return nc


def _ag_chunks(k, loc, rows, cols, name, groups, cbuf):
    rpc = (1 << 20) // (cols * 4)
    n = rows // rpc
    gs = [k.scratch("%s_%d" % (name, i), [2 * rpc, cols]) for i in range(n)]
    for i in range(n):
        k.allgather(gs[i][:, :], loc[i * rpc:(i + 1) * rpc, :], groups, [], [], cbuf)
    return gs, rpc


def _grow(gs, rpc, r, tau):
    ci = tau // rpc; w = tau % rpc
    return gs[ci][r * rpc + w: r * rpc + w + 128, :]


def build_fused(D, F, T, HOA, HOM, AW, NG, NCORES=8, SEG=512, HCF=256, TN=512):
    k = KB(); k.fused = True
    NTo = T // 2; CH = 64 * HOA; HC = 128 * HOM
    groups = [[2 * i, 2 * i + 1] for i in range(NCORES // 2)]
    k.psum_banks()
    ya_loc = k.scratch("ya_loc", [NTo, AW]); yb_loc = k.scratch("yb_loc", [T, CH])
    x1loc = k.scratch("x1loc", [NTo, D]); o_loc = k.scratch("o_loc", [T, HC])
    xmid0 = k.scratch("xmid0", [NTo, D]); xmid1 = k.scratch("xmid1", [NTo, D])
    cb = k.s.buf()
    k.prefix = "gm_"; k.alias["gm_ya"] = ya_loc
    build_gm(D, NTo, AW, NG, k=k); k.end_phase()
    k.prefix = "rw_"; k.alias["rw_yb"] = yb_loc
    build_rw(D, T, HOA, SEG, k=k); k.end_phase()
    ybg, rp0 = _ag_chunks(k, yb_loc, T, CH, "ybg", groups, cb); k.s.barrier()
    k.prefix = "b0_"; k.alias["b0_xmid"] = xmid0; k.alias["b0_xout"] = x1loc
    ys0 = [(0, AW, lambda t: ya_loc[t * 128:(t + 1) * 128, :], None)]
    for r in range(2):
        ys0.append((AW + r * CH, CH, (lambda t, r=r: _grow(ybg, rp0, r, t * 128)), (lambda t, r=r: _grow(ybg, rp0, r, NTo + t * 128))))
    build_bd(D, F, NTo, HCF, TN, k=k, ysrc=ys0); k.end_phase()
    x1g, rp1 = _ag_chunks(k, x1loc, NTo, D, "x1g", groups, cb); k.s.barrier()
    k.prefix = "mb_"; k.alias["mb_o"] = o_loc; k.alias["mb_x"] = x1loc
    NTT = NTo // 128
    build_mb(D, T, HOM, SEG, k=k, xfn=lambda tt: _grow(x1g, rp1, tt // NTT, (tt % NTT) * 128)); k.end_phase()
    ogs, rp2 = _ag_chunks(k, o_loc, T, HC, "og", groups, cb); k.s.barrier()
    k.prefix = "b1_"; k.alias["b1_xmid"] = xmid1; k.alias["b1_xin"] = x1loc
    ys1 = []
    for r in range(2):
        ys1.append((r * HC, HC, (lambda t, r=r: _grow(ogs, rp2, r, t * 128)), (lambda t, r=r: _grow(ogs, rp2, r, NTo + t * 128))))
    build_bd(D, F, NTo, HCF, TN, k=k, ysrc=ys1); k.end_phase()
    k.s.finish()
    return k.nc


from concourse.bass_utils import run_bass_kernel_spmd

_D = 2048; _F = 5632; _S = 2048; _NB = 4; _NC = 8
_PROG = {}


def _c(a):
    return np.ascontiguousarray(a, dtype=np.float32)


def kernel(x, c, w_ada, b_ada, g_pre_mix, g_post_mix, g_pre_ffn, g_post_ffn, w_ffn_in, w_ffn_out, w_in_ab, w_out_ab,
           a_v_gain, a_v_bias, a_w_s, a_b_s, b_mu, b_w0, b_w2, b_a0, b_a2, b_g2, b_k_k, b_k_a, b_r_k, b_lnx_gain,
           b_lnx_bias, w_qkv, w_o):
    f = np.float32
    x = np.asarray(x, f); c = np.asarray(c, f); w_ada = np.asarray(w_ada, f); b_ada = np.asarray(b_ada, f)
    D = _D
    if "f" not in _PROG:
        _PROG["f"] = build_fused(_D, _F, _S, 8, 8, 1024, 8)
    nc = _PROG["f"]
    ident = np.eye(128, dtype=f)
    own = lambda hh: slice(hh * 1024, (hh + 1) * 1024)
    su = np.triu(np.ones((64, 64)), 1); iu = np.triu(np.ones((64, 64)), 0)
    mask5 = np.concatenate([su, iu, su, iu, su.T], axis=1).astype(f)
    blk = np.kron(np.eye(2), np.ones((64, 64))).astype(f)
    tri2 = (blk * np.triu(np.ones((128, 128)))).astype(f)
    hsel = np.kron(np.eye(2), np.ones((64, 1))).astype(f)
    triu = np.triu(np.ones((128, 128))).astype(f)
    kpos = np.tile(np.arange(_S, dtype=f)[None, :], (128, 1))
    cmask = np.where(np.arange(128)[None, :] <= np.arange(128)[:, None], 0.0, -1e30).astype(f)
    gmask = np.zeros((128, 64), f)
    for qb in range(8):
        for n in range(8):
            gmask[:, qb * 8 + n] = 0.0 if n < qb else -1e30
    w_in_ab0 = np.asarray(w_in_ab[0], f)
    wada0_a = _c(w_ada[0][:, 0:2 * D]); bada0_a = _c(b_ada[0][None, 0:2 * D])
    wada0_b = _c(w_ada[0][:, 2 * D:6 * D]); bada0_b = _c(b_ada[0][None, 2 * D:6 * D])
    wada1_a = _c(w_ada[1][:, 0:2 * D]); bada1_a = _c(b_ada[1][None, 0:2 * D])
    wada1_b = _c(w_ada[1][:, 2 * D:6 * D]); bada1_b = _c(b_ada[1][None, 2 * D:6 * D])
    wu = _c(w_in_ab0[:, 0:1024]); wvv = _c(w_in_ab0[:, 1024:2048])
    vgb = _c(np.concatenate([np.asarray(a_v_gain[0]), np.asarray(a_v_bias[0])])[None, :])
    ws = _c(a_w_s[0]); bs = _c(np.asarray(a_b_s[0]).reshape(1, 1024))
    gpre0 = _c(np.asarray(g_pre_mix[0])[None, :]); gpre1 = _c(np.asarray(g_pre_mix[1])[None, :])
    mu = np.asarray(b_mu[0], f); rk = np.asarray(b_r_k[0], f).reshape(1024)
    wqkv = np.asarray(w_qkv[0], f)
    shared = {}
    rwp = []; mbp = []
    for hh in range(2):
        cs = slice(hh * 512, (hh + 1) * 512)
        mu_cat = np.zeros((1, 15 * 128), f)
        mu_cat[0, 0:512] = mu[0:1024][cs]; mu_cat[0, 512:1024] = mu[1024:2048][cs]; mu_cat[0, 1024:1536] = mu[2048:3072][cs]
        mu_cat[0, 1536:1536 + 288] = mu[3072:3360]
        pvec = np.concatenate([np.asarray(b_w0[0], f)[cs], np.asarray(b_a0[0], f)[cs], np.asarray(b_k_k[0], f)[cs],
                               np.asarray(b_k_a[0], f)[cs], rk[cs]])[None, :]
        lnx = np.concatenate([np.asarray(b_lnx_gain[0], f)[cs], np.asarray(b_lnx_bias[0], f)[cs]])[None, :]
        rwp.append(dict(rw_wr=_c(w_in_ab0[:, 2048:3072][:, cs]), rw_wk=_c(w_in_ab0[:, 3072:4096][:, cs]), rw_wv=_c(w_in_ab0[:, 4096:5120][:, cs]),
                        rw_mu_cat=mu_cat, rw_pvec=_c(pvec), rw_lnx=_c(lnx), rw_w2=_c(np.asarray(b_w2[0], f)[:, cs]),
                        rw_a2=_c(np.asarray(b_a2[0], f)[:, cs]), rw_g2=_c(np.asarray(b_g2[0], f)[:, cs])))
        cs2 = slice(hh * 1024, (hh + 1) * 1024)
        sl = np.zeros((1, 128), f)
        sl[0, 0:8] = 2.0 ** (-8.0 * (np.arange(8) + hh * 8 + 1) / 16.0)
        mbp.append(dict(mb_wq=_c(wqkv[:, 0:2048][:, cs2]), mb_wk=_c(wqkv[:, 2048:4096][:, cs2]), mb_wv=_c(wqkv[:, 4096:6144][:, cs2]), mb_slope=sl))
    wl = _c(w_in_ab0[:, 5120:5408])
    com = dict(
        gm_wada2=wada0_a, gm_bada2=bada0_a, gm_gpre=gpre0, gm_ident=ident, gm_wu=wu, gm_wv=wvv, gm_vgb=vgb, gm_ws=ws, gm_bs=bs, gm_triu=triu,
        rw_wada2=wada0_a, rw_bada2=bada0_a, rw_gpre=gpre0, rw_ident=ident, rw_wl=wl, rw_mask5=mask5, rw_tri2=tri2, rw_bones=blk, rw_hsel=hsel,
        b0_wada=wada0_b, b0_bada=bada0_b, b0_wout=_c(w_out_ab[0]), b0_gpost=_c(np.asarray(g_post_mix[0])[None]), b0_gpre=_c(np.asarray(g_pre_ffn[0])[None]),
        b0_gpost2=_c(np.asarray(g_post_ffn[0])[None]), b0_wfi=_c(w_ffn_in[0]), b0_wfo=_c(w_ffn_out[0]), b0_ident=ident,
        mb_wada2=wada1_a, mb_bada2=bada1_a, mb_gpre=gpre1, mb_ident=ident, mb_kpos=kpos, mb_cmask=cmask, mb_gmask=gmask,
        b1_wada=wada1_b, b1_bada=bada1_b, b1_wout=_c(w_o[0]), b1_gpost=_c(np.asarray(g_post_mix[1])[None]), b1_gpre=_c(np.asarray(g_pre_ffn[1])[None]),
        b1_gpost2=_c(np.asarray(g_post_ffn[1])[None]), b1_wfi=_c(w_ffn_in[1]), b1_wfo=_c(w_ffn_out[1]), b1_ident=ident)
    maps = []
    for core in range(_NC):
        b, hh = core // 2, core % 2
        sel = np.zeros((128, 2), f); sel[:, hh] = 1.0
        cv = _c(c[b][None, :])
        m = dict(com)
        m.update(rwp[hh]); m.update(mbp[hh])
        m.update(gm_x=_c(x[b, own(hh)]), gm_cvec=cv, rw_x=_c(x[b]), rw_cvec=cv, b0_xin=_c(x[b, own(hh)]), b0_cvec=cv, b0_sel=sel,
                 mb_cvec=cv, b1_cvec=cv, b1_sel=sel)
        maps.append(m)
    res = run_bass_kernel_spmd(nc, maps, core_ids=list(range(_NC))).results
    out = np.zeros((_NB, _S, D), f)
    for core in range(_NC):
        b, hh = core // 2, core % 2
        out[b, own(hh)] = res[core]["b1_xout"]
    return out
```
